# Optimizing a Trainium2 kernel written in Bass

```python
import functools
import jax
import jax.numpy as jnp
from jax import lax
import numpy as np

D_MODEL = 1024
BATCH = 8
SEQ = 2048
DEPTH = 2

GRID_W = 64
CTX_LEN = 256
N_MIXERS = 2
RECURRENT_MIXER = 0
HEAD_SIZE = 64
N_HEADS = D_MODEL // HEAD_SIZE
LORA_DECAY = 64
LORA_ICLR = 64
LORA_GATE = 160
CONV_WIDTH = 3
D_FF = 3584
N_EXPERTS = 8
TOP_K = 2
MOE_BLOCK = 128
NORM_EPS = 1e-6
GN_EPS = 64e-5
N_RWKV = (DEPTH + 1) // 2
N_CONV = DEPTH // 2
N_DENSE = (DEPTH + 1) // 2
N_MOE = DEPTH // 2

kernel_name = "hybrid_rwkv7_shortconv_moe_dit"


def rms_norm(x, g):
    xf = x.astype(jnp.float32)
    y = xf * lax.rsqrt(jnp.mean(xf * xf, axis=-1, keepdims=True) + NORM_EPS)
    return (y * g.astype(jnp.float32)).astype(x.dtype)


def modulate(h, shift, scale):
    return h * (1.0 + scale) + shift


def _heads(z):
    return z.reshape(z.shape[:-1] + (N_HEADS, HEAD_SIZE))


def grid_shift(x):
    b, t, d = x.shape
    rows = t // GRID_W
    g = x.reshape(b, rows, GRID_W, d)
    q = d // 4
    left = jnp.pad(g[:, :, :-1, :q], ((0, 0), (0, 0), (1, 0), (0, 0)))
    right = jnp.pad(g[:, :, 1:, q:2 * q], ((0, 0), (0, 0), (0, 1), (0, 0)))
    up = jnp.pad(g[:, :-1, :, 2 * q:3 * q], ((0, 0), (1, 0), (0, 0), (0, 0)))
    down = jnp.pad(g[:, 1:, :, 3 * q:], ((0, 0), (0, 1), (0, 0), (0, 0)))
    return jnp.concatenate([left, right, up, down], axis=-1).reshape(b, t, d)


def seq_shift(x):
    h = x.shape[-1] // 2
    prev = jnp.pad(x[:, :-1, :h], ((0, 0), (1, 0), (0, 0)))
    nxt = jnp.pad(x[:, 1:, h:], ((0, 0), (0, 1), (0, 0)))
    return jnp.concatenate([prev, nxt], axis=-1)


def rwkv7_tokenwise(h, h_shift, p):
    mu, w_rkv, w0, w1, w2, a0, a1, a2, g1, g2, k_k, k_a = p
    f32 = jnp.float32
    dx = h_shift - h
    xr, xw, xk, xv, xa, xg = (h + dx * mu[n] for n in range(6))
    r = xr @ w_rkv[0]
    k = xk @ w_rkv[1]
    v = xv @ w_rkv[2]
    w_pre = jnp.einsum("zbtr,zrd->zbtd", jnp.tanh(jnp.einsum("btd,zdr->zbtr", xw, w1)), w2) + w0[:, None, None, :]
    decay = jnp.exp(-jnp.exp(-jax.nn.softplus(-w_pre.astype(f32)) - 0.5))
    iclr = jax.nn.sigmoid((jnp.einsum("zbtr,zrd->zbtd", jnp.einsum("btd,zdr->zbtr", xa, a1), a2)
                           + a0[:, None, None, :]).astype(f32))
    gate = jax.nn.sigmoid(xg @ g1) @ g2
    kk = _heads((k * k_k).astype(f32))
    kk = kk * lax.rsqrt(jnp.maximum(jnp.sum(kk * kk, axis=-1, keepdims=True), 1e-24))
    k_dir = k.astype(f32)[None] * (1.0 + (iclr - 1.0) * k_a.astype(f32))
    return (_heads(r.astype(f32)), _heads(k_dir), _heads(v.astype(f32)), _heads(decay),
            -kk, kk[None] * _heads(iclr), gate)


def rwkv7_scan(s0, r, w, k, v, a, b, reverse):
    def step(s, inp):
        r_t, w_t, k_t, v_t, a_t, b_t = inp
        sa = jnp.einsum("bhij,bhj->bhi", s, a_t)
        s = s * w_t[:, :, None, :] + sa[..., None] * b_t[:, :, None, :] + v_t[..., None] * k_t[:, :, None, :]
        return s, jnp.einsum("bhij,bhj->bhi", s, r_t)
    xs = tuple(jnp.moveaxis(z, 1, 0) for z in (r, w, k, v, a, b))
    s_final, ys = lax.scan(step, s0, xs, reverse=reverse)
    return jnp.moveaxis(ys, 0, 1), s_final


def rwkv7_bidir(p, s_fwd, s_bwd):
    r, k2, v, w2, a, b2 = p[:6]
    y_f, s_f = rwkv7_scan(s_fwd, r, w2[0], k2[0], v, a, b2[0], reverse=False)
    y_b, s_b = rwkv7_scan(s_bwd, r, w2[1], k2[1], v, a, b2[1], reverse=True)
    return y_f + y_b, s_f, s_b


def rwkv7_readout(y, p, r_k, ln_w, ln_b, w_out, dtype):
    r, k2, v, gate = p[0], p[1], p[2], p[6]
    mean = jnp.mean(y, axis=-1, keepdims=True)
    var = jnp.mean(jnp.square(y - mean), axis=-1, keepdims=True)
    yn = (y - mean) * lax.rsqrt(var + GN_EPS)
    bonus = jnp.sum(r * (0.5 * (k2[0] + k2[1])) * r_k.astype(jnp.float32), axis=-1, keepdims=True) * v
    b, t = y.shape[:2]
    o = yn.reshape(b, t, -1) * ln_w + ln_b + bonus.reshape(b, t, -1)
    return (o.astype(dtype) * gate) @ w_out


def short_conv(h, w_in, w_conv, w_out):
    bg, cg, u = jnp.split(h @ w_in, 3, axis=-1)
    z = cg * u
    t = z.shape[1]
    half = CONV_WIDTH // 2
    zp = jnp.pad(z, ((0, 0), (half, half), (0, 0)))
    conv = sum(zp[:, n:n + t] * w_conv[n] for n in range(CONV_WIDTH))
    return (bg * conv) @ w_out


def swiglu(h, w_gu, w_down):
    g, u = jnp.split(h @ w_gu, 2, axis=-1)
    return (jax.nn.silu(g) * u) @ w_down


def moe_swiglu(h, w_router, w_gu, w_down):
    shp = h.shape
    tok = h.reshape(-1, shp[-1])
    n = tok.shape[0]
    nk = n * TOP_K
    logits = (tok @ w_router).astype(jnp.float32)
    top_v, top_e = lax.top_k(logits, TOP_K)
    gates = jax.nn.softmax(top_v, axis=-1)
    flat_e = top_e.reshape(-1)
    flat_t = jnp.arange(nk, dtype=jnp.int32) // TOP_K
    flat_g = gates.reshape(-1)
    order = jnp.argsort(flat_e)
    se = flat_e[order]
    counts = jax.ops.segment_sum(jnp.ones_like(flat_e), flat_e, num_segments=N_EXPERTS)
    padded = (counts + MOE_BLOCK - 1) // MOE_BLOCK * MOE_BLOCK
    start = jnp.cumsum(counts) - counts
    pend = jnp.cumsum(padded)
    pstart = pend - padded
    dest = pstart[se] + jnp.arange(nk, dtype=jnp.int32) - start[se]
    n_slots = -(-nk // MOE_BLOCK) * MOE_BLOCK + N_EXPERTS * MOE_BLOCK
    n_blocks = n_slots // MOE_BLOCK
    slot_tok = jnp.zeros((n_slots,), jnp.int32).at[dest].set(flat_t[order])
    slot_gate = jnp.zeros((n_slots,), jnp.float32).at[dest].set(flat_g[order])
    blk_start = jnp.arange(n_blocks, dtype=jnp.int32) * MOE_BLOCK
    blk_e = jnp.minimum(jnp.searchsorted(pend, blk_start, side="right"), N_EXPERTS - 1)
    xs = tok[slot_tok].reshape(n_blocks, MOE_BLOCK, -1)

    def expert_block(args):
        xb, e = args
        return swiglu(xb, w_gu[e], w_down[e])

    ys = lax.map(expert_block, (xs, blk_e)).reshape(n_slots, -1)
    ys = ys * slot_gate[:, None].astype(ys.dtype)
    out = jnp.zeros_like(tok).at[slot_tok].add(ys)
    return out.reshape(shp)


def setup_inputs(seed: int = 0) -> dict:
    key = jax.random.key(seed)

    def nrm(n, shape, scale=1.0):
        return jax.random.normal(jax.random.fold_in(key, n), shape, jnp.float32) * scale

    def unif(n, shape, lo, hi):
        return jax.random.uniform(jax.random.fold_in(key, n), shape, jnp.float32, lo, hi)

    D, F, E = D_MODEL, D_FF, N_EXPERTS
    return {
        "x": nrm(0, (BATCH, SEQ, D)),
        "c": nrm(1, (BATCH, D)),
        "ctx": nrm(2, (BATCH, CTX_LEN, D)),
        "c_ctx": nrm(3, (D,)),
        "mod_w": nrm(4, (DEPTH, D, 6 * D), 0.5 * D ** -0.5),
        "mod_b": nrm(5, (DEPTH, 6 * D), 0.02),
        "norm_g": 1.0 + nrm(6, (DEPTH, 4, D), 0.02),
        "rwkv_mu": unif(7, (N_RWKV, 6, D), 0.0, 1.0),
        "rwkv_w_rkv": nrm(8, (N_RWKV, 3, D, D), D ** -0.5),
        "rwkv_w0": unif(9, (N_RWKV, 2, D), -6.0, -1.0),
        "rwkv_w1": nrm(10, (N_RWKV, 2, D, LORA_DECAY), D ** -0.5),
        "rwkv_w2": nrm(11, (N_RWKV, 2, LORA_DECAY, D), 0.1 * LORA_DECAY ** -0.5),
        "rwkv_a0": nrm(12, (N_RWKV, 2, D), 0.2),
        "rwkv_a1": nrm(13, (N_RWKV, 2, D, LORA_ICLR), D ** -0.5),
        "rwkv_a2": nrm(14, (N_RWKV, 2, LORA_ICLR, D), 0.1 * LORA_ICLR ** -0.5),
        "rwkv_g1": nrm(15, (N_RWKV, D, LORA_GATE), D ** -0.5),
        "rwkv_g2": nrm(16, (N_RWKV, LORA_GATE, D), LORA_GATE ** -0.5),
        "rwkv_k_k": 0.85 + nrm(17, (N_RWKV, D), 0.05),
        "rwkv_k_a": 1.0 + nrm(18, (N_RWKV, D), 0.05),
        "rwkv_r_k": nrm(19, (N_RWKV, N_HEADS, HEAD_SIZE), 0.1),
        "rwkv_ln_w": 1.0 + nrm(20, (N_RWKV, D), 0.02),
        "rwkv_ln_b": nrm(21, (N_RWKV, D), 0.02),
        "rwkv_w_out": nrm(22, (N_RWKV, D, D), D ** -0.5),
        "conv_w_in": nrm(23, (N_CONV, D, 3 * D), D ** -0.5),
        "conv_w": nrm(24, (N_CONV, CONV_WIDTH, D), CONV_WIDTH ** -0.5),
        "conv_w_out": nrm(25, (N_CONV, D, D), D ** -0.5),
        "ffn_w_gu": nrm(26, (N_DENSE, D, 2 * F), D ** -0.5),
        "ffn_w_down": nrm(27, (N_DENSE, F, D), F ** -0.5),
        "moe_router": nrm(28, (N_MOE, D, E), D ** -0.5),
        "moe_w_gu": nrm(29, (N_MOE, E, D, 2 * F), D ** -0.5),
        "moe_w_down": nrm(30, (N_MOE, E, F, D), F ** -0.5),
    }


def reference(x, c, ctx, c_ctx, mod_w, mod_b, norm_g, rwkv_mu, rwkv_w_rkv, rwkv_w0, rwkv_w1, rwkv_w2,
              rwkv_a0, rwkv_a1, rwkv_a2, rwkv_g1, rwkv_g2, rwkv_k_k, rwkv_k_a, rwkv_r_k, rwkv_ln_w,
              rwkv_ln_b, rwkv_w_out, conv_w_in, conv_w, conv_w_out, ffn_w_gu, ffn_w_down,
              moe_router, moe_w_gu, moe_w_down):
    silu_c = jax.nn.silu(c)
    silu_cc = jax.nn.silu(c_ctx)
    for i in range(DEPTH):
        kind = i % N_MIXERS
        jm = i // N_MIXERS
        jf = i // 2
        ctx_live = any(l % N_MIXERS == RECURRENT_MIXER for l in range(i + 1, DEPTH))
        m = jnp.split(silu_c @ mod_w[i] + mod_b[i], 6, axis=-1)
        sh_a, sc_a, gt_a, sh_f, sc_f, gt_f = (z[:, None, :] for z in m)
        h = modulate(rms_norm(x, norm_g[i, 0]), sh_a, sc_a)
        if kind == RECURRENT_MIXER or ctx_live:
            cm = jnp.split(silu_cc @ mod_w[i] + mod_b[i], 6, axis=-1)
            hc = modulate(rms_norm(ctx, norm_g[i, 0]), cm[0], cm[1])
        if kind == RECURRENT_MIXER:
            p_tok = (rwkv_mu[jm], rwkv_w_rkv[jm], rwkv_w0[jm], rwkv_w1[jm], rwkv_w2[jm], rwkv_a0[jm],
                     rwkv_a1[jm], rwkv_a2[jm], rwkv_g1[jm], rwkv_g2[jm], rwkv_k_k[jm], rwkv_k_a[jm])
            readout = functools.partial(rwkv7_readout, r_k=rwkv_r_k[jm], ln_w=rwkv_ln_w[jm],
                                        ln_b=rwkv_ln_b[jm], w_out=rwkv_w_out[jm])
            pc = rwkv7_tokenwise(hc, seq_shift(hc), p_tok)
            pl = rwkv7_tokenwise(h, grid_shift(h), p_tok)
            s0 = jnp.zeros((ctx.shape[0], N_HEADS, HEAD_SIZE, HEAD_SIZE), jnp.float32)
            yc, s_f, s_b = rwkv7_bidir(pc, s0, s0)
            yl, _, _ = rwkv7_bidir(pl, s_f, s_b)
            y = readout(yl, pl, dtype=x.dtype)
            if ctx_live:
                y_ctx = readout(yc, pc, dtype=ctx.dtype)
        else:
            y = short_conv(h, conv_w_in[jm], conv_w[jm], conv_w_out[jm])
            if ctx_live:
                y_ctx = short_conv(hc, conv_w_in[jm], conv_w[jm], conv_w_out[jm])
        x = x + gt_a * rms_norm(y, norm_g[i, 1])
        if ctx_live:
            ctx = ctx + cm[2] * rms_norm(y_ctx, norm_g[i, 1])
        if i % 2 == 0:
            ffn = functools.partial(swiglu, w_gu=ffn_w_gu[jf], w_down=ffn_w_down[jf])
        else:
            ffn = functools.partial(moe_swiglu, w_router=moe_router[jf], w_gu=moe_w_gu[jf],
                                    w_down=moe_w_down[jf])
        h = modulate(rms_norm(x, norm_g[i, 2]), sh_f, sc_f)
        x = x + gt_f * rms_norm(ffn(h), norm_g[i, 3])
        if ctx_live:
            hcf = modulate(rms_norm(ctx, norm_g[i, 2]), cm[3], cm[4])
            ctx = ctx + cm[5] * rms_norm(ffn(hcf), norm_g[i, 3])
    return x
```

```python
import contextlib
import numpy as np
import concourse.bass as bass
import concourse.mybir as mybir
from concourse.ap import AP
from concourse.bass_utils import run_bass_kernel_spmd

F32 = mybir.dt.float32
BF16 = mybir.dt.bfloat16
AF = mybir.ActivationFunctionType
ALU = mybir.AluOpType

T = 2048
TC = 256
NT = T + TC
D = 1024
KC = 8
FF = 3584
NE = 8
NCH = NT // 64
NORM_EPS = 1e-6
GN_EPS = 64e-5
DECAY_S = -0.6065306597126334


class Dep:
    __slots__ = ("w", "rs")

    def __init__(self):
        self.w = None
        self.rs = {}


class Eng:
    def __init__(self, name, obj, is_pe=False):
        self.name = name
        self.obj = obj
        self.is_pe = is_pe
        self.sem = None
        self.semid = None
        self.cnt = 0
        self.seen = {}


class Slot:
    def __init__(self, sem, semid):
        self.sem = sem
        self.semid = semid
        self.cnt = 0
        self.last = None


class KB:
    def __init__(self):
        self.nc = bass.Bass("TRN2", target_bir_lowering=False)
        nc = self.nc
        self.es = contextlib.ExitStack()
        self.sems = []
        self.epoch = 0
        self.pe = Eng("pe", nc.tensor, True)
        self.act = Eng("act", nc.scalar)
        self.dve = Eng("dve", nc.vector)
        self.pool = Eng("pool", nc.gpsimd)
        self.sp = Eng("sp", nc.sync)
        self.engs = [self.pe, self.act, self.dve, self.pool, self.sp]
        for e in self.engs:
            self._fresh_sem(e)
        self.slots = {}
        self.slot_i = {}
        for q in (self.sp, self.pool):
            self.slots[q.name] = [Slot(*self._newsem("dq%s%d" % (q.name, i))) for i in range(8)]
            self.slot_i[q.name] = 0
        self.ninstr = 0
        self.uid = 0

    def _newsem(self, name):
        s = self.es.enter_context(self.nc.semaphore("%s_%d" % (name, len(self.sems))))
        self.sems.append(s)
        return s, len(self.sems) - 1

    def _fresh_sem(self, e):
        e.sem, e.semid = self._newsem("e" + e.name)
        e.cnt = 0

    def sb(self, name, shape, dtype, stack=None):
        self.uid += 1
        return (stack or self.es).enter_context(
            self.nc.sbuf_tensor("%s_%d" % (name, self.uid), list(shape), dtype))

    def psum(self, name, shape, dtype, stack=None):
        self.uid += 1
        return (stack or self.es).enter_context(
            self.nc.psum_tensor("%s_%d" % (name, self.uid), list(shape), dtype))

    def _wait(self, eng, tk, war=False):
        semid, val, src, ep = tk
        if ep != self.epoch:
            return
        if src is eng:
            if eng.is_pe or war:
                return
        if eng.seen.get(semid, 0) >= val:
            return
        eng.obj.wait_ge(self.sems[semid], val)
        eng.seen[semid] = val
        self.ninstr += 1

    def _deps(self, eng, r, w):
        for d in r:
            if d.w is not None:
                self._wait(eng, d.w)
        for d in w:
            if d.w is not None:
                self._wait(eng, d.w)
            for t in d.rs.values():
                self._wait(eng, t, war=True)

    def _mark(self, tk, r, w):
        for d in r:
            d.rs[tk[0]] = tk
        for d in w:
            d.w = tk
            d.rs = {}

    def op(self, eng, fn, r=(), w=()):
        self._deps(eng, r, w)
        ins = fn()
        eng.cnt += 1
        ins.then_inc(eng.sem, 1)
        tk = (eng.semid, eng.cnt, eng, self.epoch)
        self._mark(tk, r, w)
        self.ninstr += 1
        return tk

    def dma(self, q, out, in_, r=(), w=(), **kw):
        sl = self.slots[q.name]
        i = self.slot_i[q.name]
        self.slot_i[q.name] = (i + 1) % len(sl)
        s = sl[i]
        if s.last is not None:
            self._wait(q, s.last)
        self._deps(q, r, w)
        ins = q.obj.dma_start(out=out, in_=in_, **kw)
        s.cnt += 16
        ins.then_inc(s.sem, 16)
        tk = (s.semid, s.cnt, None, self.epoch)
        s.last = tk
        self._mark(tk, r, w)
        self.ninstr += 1
        return tk

    def barrier(self):
        tks = []
        for e in self.engs:
            if e.cnt > 0:
                tks.append((e.semid, e.cnt, e, self.epoch))
        for sl in self.slots.values():
            for s in sl:
                if s.last is not None and s.last[3] == self.epoch:
                    tks.append(s.last)
        for e in self.engs:
            for tk in tks:
                semid, val, src, ep = tk
                if e.seen.get(semid, 0) >= val:
                    continue
                e.obj.wait_ge(self.sems[semid], val)
                e.seen[semid] = val
                self.ninstr += 1
        self.epoch += 1
        for e in self.engs:
            self._fresh_sem(e)
            e.seen = {}

    def finish(self):
        self.barrier()
        self.es.close()


VEC_SPEC = [("g", 2 * 4 * 8), ("mu", 6 * 8), ("w0", 16), ("a0", 16), ("k_k", 8), ("k_a", 8), ("r_k", 8),
            ("ln_w", 8), ("ln_b", 8), ("conv_w", 24), ("modb", 192)]
VOFF = {}
_o = 0
for _n, _c in VEC_SPEC:
    VOFF[_n] = _o
    _o += _c
NV = _o
C_ID, C_MF, C_MB, C_LF, C_LB, NCST = 0, 128, 640, 1152, 1280, 1408


def col(v):
    v = np.asarray(v, np.float32).reshape(-1, 128)
    return np.ascontiguousarray(v.T)


def make_consts():
    c = np.zeros((128, NCST), np.float32)
    c[:, C_ID:C_ID + 128] = np.eye(128, dtype=np.float32)
    s = np.arange(64)[:, None]
    t = np.arange(64)[None, :]
    strict_f = (s < t).astype(np.float32)
    incl_f = (s <= t).astype(np.float32)
    strict_b = (s > t).astype(np.float32)
    incl_b = (s >= t).astype(np.float32)
    mf = np.concatenate([strict_f, incl_f], 1)
    mb = np.concatenate([strict_b, incl_b], 1)
    c[:64, C_MF:C_MF + 512] = np.tile(mf, (1, 4))
    c[:64, C_MB:C_MB + 512] = np.tile(mb, (1, 4))
    c[:64, C_LF:C_LF + 128] = np.tile(strict_b, (1, 2))
    c[:64, C_LB:C_LB + 128] = np.tile(strict_f, (1, 2))
    return c


def build(dbg=None, dbgn=0):
    k = KB()
    nc = k.nc
    pe, act, dve, pool, sp = k.pe, k.act, k.dve, k.pool, k.sp

    def din(name, shape, dt=F32):
        return nc.dram_tensor(name, list(shape), dt, kind="ExternalInput").ap()

    xT_d = din("xT", [128, KC, T])
    ctxT_d = din("ctxT", [128, KC, TC])
    cvec_d = din("cvec", [128, KC, 2])
    vec_d = din("vecs", [128, NV])
    cst_d = din("cst", [128, NCST])
    mod_w_d = din("mod_w", [2, D, 6 * D])
    w_rkv_d = din("w_rkv", [3, D, D])
    w1_d = din("w1cat", [D, 128])
    w2_d = din("w2cat", [128, D])
    a1_d = din("a1cat", [D, 128])
    a2_d = din("a2cat", [128, D])
    g1_d = din("g1", [D, 160])
    g2_d = din("g2", [160, D])
    wout_d = din("w_out", [D, D])
    cwin_d = din("conv_w_in", [D, 3 * D])
    cwout_d = din("conv_w_out", [D, D])
    fgu_d = din("ffn_w_gu", [D, 2 * FF])
    fd_d = din("ffn_w_down", [FF, D])
    rt_d = din("router", [128, KC, NE])
    mgu_d = din("moe_w_gu", [NE, D, 2 * FF])
    md_d = din("moe_w_down", [NE, FF, D])
    out_d = nc.dram_tensor("yT", [128, KC, T], F32, kind="ExternalOutput").ap()
    xres_d = nc.dram_tensor("xres", [128, KC, T], F32, kind="Internal").ap()
    oT_d = nc.dram_tensor("oT", [128, KC, T], BF16, kind="Internal").ap()
    rkv_d = nc.dram_tensor("rkvs", [3, 128, KC, NT], F32, kind="Internal").ap()
    if dbg is not None:
        dbg_d = nc.dram_tensor("dbg", [128, dbgn], F32, kind="ExternalOutput").ap()

    def kview(w2d, c0, c1):
        return w2d.rearrange("(k p) n -> p k n", p=128)[:, :, c0:c1]

    vec = k.sb("vec", [128, NV], F32)
    cst = k.sb("cst", [128, NCST], F32)
    ident_bf = k.sb("identb", [128, 128], BF16)
    ones_bf = k.sb("onesb", [128, 128], BF16)
    bd_bf = k.sb("bdb", [128, 128], BF16)
    bdm_f = k.sb("bdmf", [128, 128], F32)
    bd1_f = k.sb("bd1f", [128, 128], F32)
    maskF = k.sb("maskF", [64, 512], BF16)
    maskB = k.sb("maskB", [64, 512], BF16)
    maskLF = k.sb("maskLF", [64, 128], BF16)
    maskLB = k.sb("maskLB", [64, 128], BF16)
    mod = k.sb("mod", [128, 192], F32)
    scal = k.sb("scal", [128, 160], F32)
    rmask = k.sb("rmask", [128, 576], F32)
    d_const = Dep()
    d_mod = Dep()
    d_scal = Dep()

    PS = [k.psum("ps%d" % i, [128, 512], F32) for i in range(7)]
    PSB = k.psum("psb", [128, 1024], BF16)
    dPS = [Dep() for _ in range(7)]
    dPSB = Dep()

    def V(name, i=0, n=8):
        o = VOFF[name] + i * 8
        return vec[:, o:o + n]

    def Vc(name, i, c):
        o = VOFF[name] + i * 8 + c
        return vec[:, o:o + 1]

    S_ = {n: i * 8 for i, n in enumerate(
        ["A1_0", "B1_0", "G1_0", "A2_0", "B2_0", "G2_0", "A1c", "B1c",
         "A1_1", "B1_1", "G1_1", "A2_1", "B2_1", "G2_1", "omka", "hrk"])}
    omu = k.sb("omu", [128, 48], F32)

    def S(name, c=None):
        o = S_[name]
        if c is None:
            return scal[:, o:o + 8]
        return scal[:, o + c:o + c + 1]

    k.dma(sp, vec[:], vec_d[:, :], w=[d_const])
    k.dma(sp, cst[:], cst_d[:, :], w=[d_const])
    k.op(dve, lambda: nc.vector.tensor_copy(ident_bf[:], cst[:, C_ID:C_ID + 128]), r=[d_const], w=[d_const])
    k.op(dve, lambda: nc.vector.tensor_copy(maskF[:], cst[0:64, C_MF:C_MF + 512]), r=[d_const], w=[d_const])
    k.op(dve, lambda: nc.vector.tensor_copy(maskB[:], cst[0:64, C_MB:C_MB + 512]), r=[d_const], w=[d_const])
    k.op(dve, lambda: nc.vector.tensor_copy(maskLF[:], cst[0:64, C_LF:C_LF + 128]), r=[d_const], w=[d_const])
    k.op(dve, lambda: nc.vector.tensor_copy(maskLB[:], cst[0:64, C_LB:C_LB + 128]), r=[d_const], w=[d_const])
    k.op(dve, lambda: nc.vector.memset(ones_bf[:], 1.0), w=[d_const])
    k.op(dve, lambda: nc.vector.memset(bd_bf[:], 0.0), w=[d_const])
    k.op(dve, lambda: nc.vector.memset(bdm_f[:], 0.0), w=[d_const])
    k.op(dve, lambda: nc.vector.memset(bd1_f[:], 0.0), w=[d_const])
    for h in range(2):
        sl = slice(64 * h, 64 * h + 64)
        k.op(dve, lambda sl=sl: nc.vector.memset(bd_bf[sl, sl], 1.0), w=[d_const])
        k.op(dve, lambda sl=sl: nc.vector.memset(bdm_f[sl, sl], 1.0 / 64), w=[d_const])
        k.op(dve, lambda sl=sl: nc.vector.memset(bd1_f[sl, sl], 1.0), w=[d_const])
    k.op(dve, lambda: nc.vector.memset(rmask[:], 1.0), w=[d_const])
    k.op(dve, lambda: nc.vector.memset(rmask[:, 0:576:64], 0.0), w=[d_const])
    k.op(dve, lambda: nc.vector.tensor_scalar(omu[:], V("mu", 0, 48), -1.0, 1.0, ALU.mult, ALU.add),
         r=[d_const], w=[d_const])

    with contextlib.ExitStack() as ph:
        cv = k.sb("cv", [128, KC, 2], F32, ph)
        scb = k.sb("scb", [128, KC, 2], BF16, ph)
        wb = [k.sb("modw%d" % i, [128, KC, 1024], BF16, ph) for i in range(2)]
        dwb = [Dep(), Dep()]
        d_cv = Dep()
        k.dma(sp, cv[:], cvec_d[:, :, :], w=[d_cv])
        k.op(act, lambda: nc.scalar.activation(scb[:], cv[:], AF.Silu), r=[d_cv], w=[d_cv])
        gi = 0
        for i in range(2):
            for g in range(6):
                b = gi % 2
                gi += 1
                k.dma(pool, wb[b][:], kview(mod_w_d[i], g * 1024, (g + 1) * 1024), w=[dwb[b]])
                for m in range(8):
                    mg = g * 8 + m
                    cc = (i * 48 + mg) * 2
                    for kk in range(KC):
                        k.op(pe, lambda b=b, m=m, kk=kk, cc=cc: nc.tensor.matmul(
                            PS[0][:, cc:cc + 2], wb[b][:, kk, m * 128:(m + 1) * 128], scb[:, kk, :],
                            start=(kk == 0), stop=(kk == KC - 1)), r=[dwb[b], d_cv], w=[dPS[0]])
        k.op(dve, lambda: nc.vector.tensor_tensor(mod[:], PS[0][:, 0:192], V("modb", 0, 192), ALU.add),
             r=[dPS[0], d_const], w=[d_mod])

        def mcol(i, s, j):
            o = (i * 48 + s * 8) * 2 + j
            return mod[:, o:o + 16:2]

        def mk_scale(dst, gi_, li, s, j):
            k.op(dve, lambda: nc.vector.tensor_scalar(S(dst), mcol(li, s, j), 1.0, None, ALU.add),
                 r=[d_mod], w=[d_scal])
            k.op(dve, lambda: nc.vector.tensor_tensor(S(dst), S(dst), V("g", li * 4 + gi_), ALU.mult),
                 r=[d_scal, d_const], w=[d_scal])

        def mk_copy(dst, li, s, j):
            k.op(dve, lambda: nc.vector.tensor_copy(S(dst), mcol(li, s, j)), r=[d_mod], w=[d_scal])

        def mk_gate(dst, gi_, li, s):
            k.op(dve, lambda: nc.vector.tensor_tensor(S(dst), mcol(li, s, 0), V("g", li * 4 + gi_), ALU.mult),
                 r=[d_mod, d_const], w=[d_scal])

        for li in range(2):
            mk_scale("A1_%d" % li, 0, li, 1, 0)
            mk_copy("B1_%d" % li, li, 0, 0)
            mk_gate("G1_%d" % li, 1, li, 2)
            mk_scale("A2_%d" % li, 2, li, 4, 0)
            mk_copy("B2_%d" % li, li, 3, 0)
            mk_gate("G2_%d" % li, 3, li, 5)
        mk_scale("A1c", 0, 0, 1, 1)
        mk_copy("B1c", 0, 0, 1)
        k.op(dve, lambda: nc.vector.tensor_scalar(S("omka"), V("k_a"), -1.0, 1.0, ALU.mult, ALU.add),
             r=[d_const], w=[d_scal])
        k.op(dve, lambda: nc.vector.tensor_scalar(S("hrk"), V("r_k"), 0.5, None, ALU.mult),
             r=[d_const], w=[d_scal])
        k.barrier()

    def sumsq_rstd(src_fn, n, scratch_bf, rs, d_src, d_scr, d_rs, psi, eps=NORM_EPS, nchunks=KC):
        for c in range(nchunks):
            k.op(act, lambda c=c: nc.scalar.activation(scratch_bf[:, c, :n], src_fn(c), AF.Square),
                 r=[d_src], w=[d_scr])
        for c in range(nchunks):
            k.op(pe, lambda c=c: nc.tensor.matmul(PS[psi][:, :n], ones_bf[:], scratch_bf[:, c, :n],
                                                  start=(c == 0), stop=(c == nchunks - 1)),
                 r=[d_scr, d_const], w=[dPS[psi]])
        k.op(act, lambda: nc.scalar.activation(rs[:, :n], PS[psi][:, :n], AF.Sqrt, bias=eps, scale=1.0 / D),
             r=[dPS[psi]], w=[d_rs])
        k.op(dve, lambda: nc.vector.reciprocal(rs[:, :n], rs[:, :n]), r=[d_rs], w=[d_rs])

    def norm_mod(src, d_src, n, An, Bn, dst_fn, d_dst, wk, h32=None, d_h32=None):
        sumsq_rstd(lambda c: src[:, c, :n], n, wk["sq"], wk["rs"], d_src, wk["dsq"], wk["drs"], 6)
        for c in range(KC):
            k.op(dve, lambda c=c: nc.vector.tensor_tensor(wk["tmp"][:, c, :n], src[:, c, :n], wk["rs"][:, :n], ALU.mult),
                 r=[d_src, wk["drs"]], w=[wk["dtmp"]])
            k.op(act, lambda c=c: nc.scalar.activation(dst_fn(c), wk["tmp"][:, c, :n], AF.Identity,
                                                       bias=S(Bn, c), scale=S(An, c)),
                 r=[wk["dtmp"], d_scal], w=[d_dst])
            if h32 is not None:
                k.op(pool, lambda c=c: nc.gpsimd.tensor_scalar(h32[:, c, :n], wk["tmp"][:, c, :n], S(An, c), S(Bn, c),
                                                               ALU.mult, ALU.add),
                     r=[wk["dtmp"], d_scal], w=[d_h32])

    def res_norm(y, d_y, n, Gn, xprev, d_xprev, xnew, d_xnew, wk):
        sumsq_rstd(lambda c: y[:, c, :n], n, wk["sq"], wk["rs"], d_y, wk["dsq"], wk["drs"], 6)
        for c in range(KC):
            k.op(pool, lambda c=c: nc.gpsimd.tensor_tensor(wk["tmp"][:, c, :n], y[:, c, :n], wk["rs"][:, :n], ALU.mult),
                 r=[d_y, wk["drs"]], w=[wk["dtmp"]])
            k.op(dve, lambda c=c: nc.vector.scalar_tensor_tensor(xnew[:, c, :n], wk["tmp"][:, c, :n], S(Gn, c),
                                                                 xprev[:, c, :n], ALU.mult, ALU.add),
                 r=[wk["dtmp"], d_scal, d_xprev], w=[d_xnew])

    def mk_wk(ph, n=512):
        return dict(sq=k.sb("wsq", [128, KC, n], BF16, ph), rs=k.sb("wrs", [128, n], F32, ph),
                    tmp=k.sb("wtmp", [128, KC, n], F32, ph), dsq=Dep(), drs=Dep(), dtmp=Dep())

    dbg_done = [False]

    def dump(ap_fn, ncols, d, off=0):
        k.dma(sp, dbg_d[:, off:off + ncols], ap_fn(), r=[d])

    lora_stack = contextlib.ExitStack()
    TW = k.sb("TW", [128, NT], BF16, lora_stack)
    TA = k.sb("TA", [128, NT], BF16, lora_stack)
    SG = k.sb("SG", [128, 2, T], BF16, lora_stack)
    d_TW, d_TA, d_SG = Dep(), Dep(), Dep()
    hT_stack = contextlib.ExitStack()
    hT = k.sb("hT", [128, KC, NT], BF16, hT_stack)
    hsT = k.sb("hsT", [128, KC, NT], BF16, hT_stack)
    d_h = Dep()
    d_hs = Dep()
    with contextlib.ExitStack() as ph:
        wk = mk_wk(ph)
        xb = [k.sb("xb%d" % i, [128, KC, 512], F32, ph) for i in range(2)]
        dxb = [Dep(), Dep()]
        k.dma(sp, xb[0][:, :, 0:TC], ctxT_d[:, :, :], w=[dxb[0]])
        norm_mod(xb[0], dxb[0], TC, "A1c", "B1c", lambda c: hT[:, c, 0:TC], d_h, wk)
        for tb in range(4):
            b = (tb + 1) % 2
            k.dma(sp, xb[b][:], xT_d[:, :, tb * 512:(tb + 1) * 512], w=[dxb[b]])
            norm_mod(xb[b], dxb[b], 512, "A1_0", "B1_0",
                     lambda c, tb=tb: hT[:, c, TC + tb * 512:TC + (tb + 1) * 512], d_h, wk)
        k.op(pool, lambda: nc.gpsimd.memset(hsT[:], 0.0), w=[d_hs])
        L0 = TC
        for c in range(KC):
            eng = act if c % 2 == 0 else pool

            def cp(dst, src, eng=eng):
                if eng is act:
                    k.op(act, lambda: nc.scalar.copy(dst, src), r=[d_h], w=[d_hs])
                else:
                    k.op(pool, lambda: nc.gpsimd.tensor_copy(dst, src), r=[d_h], w=[d_hs])
            if c < 4:
                cp(hsT[:, c, 1:TC], hT[:, c, 0:TC - 1])
            else:
                cp(hsT[:, c, 0:TC - 1], hT[:, c, 1:TC])
            if c in (0, 1):
                cp(hsT[:, c, L0 + 1:L0 + T], hT[:, c, L0:L0 + T - 1])
                k.op(pool, lambda c=c: nc.gpsimd.memset(hsT[:, c, L0:L0 + T:64], 0.0), w=[d_hs])
            elif c in (2, 3):
                cp(hsT[:, c, L0:L0 + T - 1], hT[:, c, L0 + 1:L0 + T])
                k.op(pool, lambda c=c: nc.gpsimd.memset(hsT[:, c, L0 + 63:L0 + T:64], 0.0), w=[d_hs])
            elif c in (4, 5):
                cp(hsT[:, c, L0 + 64:L0 + T], hT[:, c, L0:L0 + T - 64])
            else:
                cp(hsT[:, c, L0:L0 + T - 64], hT[:, c, L0 + 64:L0 + T])
        k.barrier()

    if dbg == 1:
        with contextlib.ExitStack() as ph:
            t32 = k.sb("t32", [128, 2 * NT], F32, ph)
            dd = Dep()
            k.op(dve, lambda: nc.vector.tensor_copy(t32[:, 0:NT], hT[:, 0, :]), r=[d_h], w=[dd])
            k.op(dve, lambda: nc.vector.tensor_copy(t32[:, NT:2 * NT], hsT[:, 5, :]), r=[d_hs], w=[dd])
            dump(lambda: t32[:], 2 * NT, dd)
            k.barrier()
        hT_stack.close()
        lora_stack.close()
        k.finish()
        return k


    def load_mixed(ph_, dram_view, ncols, mu_i, name):
        st = k.sb(name + "st", [128, KC, ncols], F32, ph_)
        Wa = k.sb(name + "a", [128, KC, ncols], BF16, ph_)
        Wb = k.sb(name + "b", [128, KC, ncols], BF16, ph_)
        dst_, dw = Dep(), Dep()
        k.dma(sp, st[:], dram_view, w=[dst_])
        for kk in range(KC):
            o = mu_i * 8 + kk
            k.op(dve, lambda kk=kk, o=o: nc.vector.tensor_scalar(Wa[:, kk, :], st[:, kk, :], omu[:, o:o + 1], None, ALU.mult),
                 r=[dst_, d_const], w=[dw])
            k.op(pool, lambda kk=kk, o=o: nc.gpsimd.tensor_scalar(Wb[:, kk, :], st[:, kk, :], V("mu", 0, 48)[:, o:o + 1], None, ALU.mult),
                 r=[dst_, d_const], w=[dw])
        return Wa, Wb, dw

    def proj_mixed(psi, Wa, Wb, dw, c0, c1, t0, n, mrows=128):
        for kk in range(KC):
            k.op(pe, lambda kk=kk: nc.tensor.matmul(PS[psi][:mrows, :n], Wa[:, kk, c0:c1], hT[:, kk, t0:t0 + n],
                                                    start=(kk == 0), stop=False),
                 r=[dw, d_h], w=[dPS[psi]])
        for kk in range(KC):
            k.op(pe, lambda kk=kk: nc.tensor.matmul(PS[psi][:mrows, :n], Wb[:, kk, c0:c1], hsT[:, kk, t0:t0 + n],
                                                    start=False, stop=(kk == KC - 1)),
                 r=[dw, d_hs], w=[dPS[psi]])

    with contextlib.ExitStack() as ph:
        W1a, W1b, dW1 = load_mixed(ph, kview(w1_d, 0, 128), 128, 1, "w1")
        A1a, A1b, dA1 = load_mixed(ph, kview(a1_d, 0, 128), 128, 4, "a1")
        G1a, G1b, dG1 = load_mixed(ph, kview(g1_d, 0, 160), 160, 5, "g1")
        pi = 0
        for tb in range(NT // 256):
            t0 = tb * 256
            proj_mixed(pi % 4, W1a, W1b, dW1, 0, 128, t0, 256)
            k.op(act, lambda p=pi % 4, t0=t0: nc.scalar.activation(TW[:, t0:t0 + 256], PS[p][:, :256], AF.Tanh),
                 r=[dPS[pi % 4]], w=[d_TW])
            pi += 1
            proj_mixed(pi % 4, A1a, A1b, dA1, 0, 128, t0, 256)
            k.op(dve, lambda p=pi % 4, t0=t0: nc.vector.tensor_copy(TA[:, t0:t0 + 256], PS[p][:, :256]),
                 r=[dPS[pi % 4]], w=[d_TA])
            pi += 1
            if t0 >= TC:
                l0 = t0 - TC
                proj_mixed(pi % 4, G1a, G1b, dG1, 0, 128, t0, 256)
                k.op(act, lambda p=pi % 4, l0=l0: nc.scalar.activation(SG[:, 0, l0:l0 + 256], PS[p][:, :256], AF.Sigmoid),
                     r=[dPS[pi % 4]], w=[d_SG])
                pi += 1
                proj_mixed(pi % 4, G1a, G1b, dG1, 128, 160, t0, 256, mrows=32)
                k.op(act, lambda p=pi % 4, l0=l0: nc.scalar.activation(SG[0:32, 1, l0:l0 + 256], PS[p][0:32, :256], AF.Sigmoid),
                     r=[dPS[pi % 4]], w=[d_SG])
                pi += 1
        k.barrier()

    with contextlib.ExitStack() as ph:
        st = k.sb("rkvst", [128, KC, D], F32, ph)
        Wa = k.sb("rkvWa", [128, KC, D], BF16, ph)
        Wb = k.sb("rkvWb", [128, KC, D], BF16, ph)
        rowb = [k.sb("rowb%d" % i, [128, NT], F32, ph) for i in range(2)]
        d_st, d_Wab = Dep(), Dep()
        d_row = [Dep(), Dep()]
        mu_of = [0, 2, 3]
        ri = 0
        pi = 0
        for j in range(3):
            k.dma(sp, st[:], kview(w_rkv_d[j], 0, D), w=[d_st])
            for kk in range(KC):
                o = mu_of[j] * 8 + kk
                k.op(dve, lambda kk=kk, o=o: nc.vector.tensor_scalar(Wa[:, kk, :], st[:, kk, :], omu[:, o:o + 1], None, ALU.mult),
                     r=[d_st, d_const], w=[d_Wab])
                k.op(pool, lambda kk=kk, o=o: nc.gpsimd.tensor_scalar(Wb[:, kk, :], st[:, kk, :], V("mu", 0, 48)[:, o:o + 1], None, ALU.mult),
                     r=[d_st, d_const], w=[d_Wab])
            for c in range(KC):
                rb = rowb[ri % 2]
                drb = d_row[ri % 2]
                ri += 1
                for tb in range(NT // 256):
                    t0 = tb * 256
                    p = pi % 4
                    pi += 1
                    proj_mixed(p, Wa, Wb, d_Wab, c * 128, (c + 1) * 128, t0, 256)
                    if tb % 2 == 0:
                        k.op(act, lambda p=p, rb=rb, t0=t0: nc.scalar.copy(rb[:, t0:t0 + 256], PS[p][:, :256]), r=[dPS[p]], w=[drb])
                    else:
                        k.op(dve, lambda p=p, rb=rb, t0=t0: nc.vector.tensor_copy(rb[:, t0:t0 + 256], PS[p][:, :256]), r=[dPS[p]], w=[drb])
                k.dma(sp, rkv_d[j, :, c, :], rb[:], r=[drb])
        k.barrier()
    hT_stack.close()

    def bc3(t2d, col0, nouter, ostride, ninner):
        base = t2d[:, col0:col0 + 1]
        pst = base.ap[0][0]
        return AP(base.tensor, base.offset, [[pst, 128], [ostride, nouter], [0, ninner]])

    BL = 576
    NBL = NT // BL
    CPB = BL // 64
    GI = 4
    with contextlib.ExitStack() as ph:
        r32 = k.sb("r32", [128, NT], F32, ph)
        k32 = k.sb("k32", [128, NT], F32, ph)
        kk32 = k.sb("kk32", [128, NT], F32, ph)
        v16 = k.sb("v16", [128, NT], BF16, ph)
        ksum = k.sb("ksum", [128, NT], F32, ph)
        yacc = k.sb("yacc", [128, T], F32, ph)
        sgw = k.sb("sgw", [128, BL], F32, ph)
        cs = k.sb("cs", [128, BL], F32, ph)
        cs2 = k.sb("cs2", [128, BL], F32, ph)
        iclr = k.sb("iclr", [128, BL], F32, ph)
        t2 = k.sb("t2", [128, BL], F32, ph)
        t3 = k.sb("t3", [128, BL], F32, ph)
        ARbd = k.sb("ARbd", [128, NCH, 2, 128], BF16, ph)
        bt = k.sb("bt", [128, NT], BF16, ph)
        kt = k.sb("kt", [128, NT], BF16, ph)
        WC = k.sb("WC", [128, NCH], F32, ph)
        sqb = k.sb("sqb", [128, 512], BF16, ph)
        M32 = k.sb("M32", [128, 128], F32, ph)
        S16 = k.sb("S16", [128, 128], BF16, ph)
        NB = [k.sb("NB%d" % i, [64, GI * 128], BF16, ph) for i in range(2)]
        LB = [k.sb("LB%d" % i, [64, GI * 128], BF16, ph) for i in range(2)]
        XB = [k.sb("XB%d" % i, [64, GI * 128], BF16, ph) for i in range(2)]
        L1 = [k.sb("L1_%d" % i, [64, GI, 512], BF16, ph) for i in range(2)]
        TT = [k.sb("TT%d" % i, [64, GI, 128], BF16, ph) for i in range(2)]
        tm = [k.sb("tm%d" % i, [64, GI, 384], BF16, ph) for i in range(2)]
        X16 = k.sb("X16", [64, 128], BF16, ph)
        U16 = k.sb("U16", [64, 128], BF16, ph)
        ost = [k.sb("ost%d" % i, [128, T], BF16, ph) for i in range(2)]
        d_ost = [Dep(), Dep()]
        w2c = k.sb("w2c", [128, 128], BF16, ph)
        a2c = k.sb("a2c", [128, 128], BF16, ph)
        g2c = k.sb("g2c", [128, 2, 128], BF16, ph)
        d_w2, d_a2, d_g2 = Dep(), Dep(), Dep()
        d_r, d_k, d_kk, d_v, d_ks, d_y = Dep(), Dep(), Dep(), Dep(), Dep(), Dep()
        d_sgw, d_cs, d_cs2, d_iclr, d_t2, d_t3, d_AR, d_bt, d_kt, d_WC = (Dep() for _ in range(10))
        d_sq, d_M, d_S, d_X, d_U = (Dep() for _ in range(5))
        d_NB, d_LB, d_XB = [Dep(), Dep()], [Dep(), Dep()], [Dep(), Dep()]
        d_L1, d_TT, d_tm = [Dep(), Dep()], [Dep(), Dep()], [Dep(), Dep()]

        k.op(pool, lambda: nc.gpsimd.memset(ARbd[:], 0.0), w=[d_AR])

        for c in range(KC):
            cs0, cs1 = c * 128, (c + 1) * 128
            k.dma(sp, r32[:], rkv_d[0, :, c, :], w=[d_r])
            k.dma(sp, k32[:], rkv_d[1, :, c, :], w=[d_k])
            k.dma(pool, v16[:], rkv_d[2, :, c, :], w=[d_v])
            k.dma(pool, w2c[:], w2_d[:, cs0:cs1], w=[d_w2])
            k.dma(pool, a2c[:], a2_d[:, cs0:cs1], w=[d_a2])
            k.dma(pool, g2c[:, 0, :], g2_d[0:128, cs0:cs1], w=[d_g2])
            k.dma(pool, g2c[0:32, 1, :], g2_d[128:160, cs0:cs1], w=[d_g2])
            k.op(dve, lambda: nc.vector.tensor_scalar(kk32[:], k32[:], Vc("k_k", 0, c), None, ALU.mult),
                 r=[d_k, d_const], w=[d_kk])
            for q in range(0, NT, 512):
                n = min(512, NT - q)
                p = 4 + (q // 512) % 2
                k.op(act, lambda q=q, n=n: nc.scalar.activation(sqb[:, :n], kk32[:, q:q + n], AF.Square),
                     r=[d_kk], w=[d_sq])
                k.op(pe, lambda q=q, n=n, p=p: nc.tensor.matmul(PS[p][:, :n], bd_bf[:], sqb[:, :n], start=True, stop=True),
                     r=[d_sq, d_const], w=[dPS[p]])
                k.op(dve, lambda q=q, n=n, p=p: nc.vector.tensor_scalar(t2[:, :n], PS[p][:, :n], 1e-24, None, ALU.max),
                     r=[dPS[p]], w=[d_t2])
                k.op(act, lambda n=n: nc.scalar.activation(t2[:, :n], t2[:, :n], AF.Sqrt), r=[d_t2], w=[d_t2])
                k.op(dve, lambda n=n: nc.vector.reciprocal(t2[:, :n], t2[:, :n]), r=[d_t2], w=[d_t2])
                k.op(dve, lambda q=q, n=n: nc.vector.tensor_tensor(kk32[:, q:q + n], kk32[:, q:q + n], t2[:, :n], ALU.mult),
                     r=[d_kk, d_t2], w=[d_kk])

            for z in range(2):
                zs = slice(64 * z, 64 * z + 64)
                for blk in range(NBL):
                    q0 = blk * BL
                    qs = slice(q0, q0 + BL)
                    nb0 = blk * CPB
                    for sbk in range(2):
                        t0 = q0 + sbk * 288
                        o0 = sbk * 288
                        p = sbk
                        k.op(pe, lambda p=p, t0=t0: nc.tensor.matmul(PS[p][:, :288], w2c[zs, :], TW[zs, t0:t0 + 288],
                                                                     start=True, stop=True), r=[d_w2, d_TW], w=[dPS[p]])
                        k.op(act, lambda p=p, o0=o0: nc.scalar.activation(sgw[:, o0:o0 + 288], PS[p][:, :288], AF.Sigmoid,
                                                                          bias=Vc("w0", z, c)), r=[dPS[p], d_const], w=[d_sgw])
                        p = 2 + sbk
                        k.op(pe, lambda p=p, t0=t0: nc.tensor.matmul(PS[p][:, :288], a2c[zs, :], TA[zs, t0:t0 + 288],
                                                                     start=True, stop=True), r=[d_a2, d_TA], w=[dPS[p]])
                        k.op(act, lambda p=p, o0=o0: nc.scalar.activation(iclr[:, o0:o0 + 288], PS[p][:, :288], AF.Sigmoid,
                                                                          bias=Vc("a0", z, c)), r=[dPS[p], d_const], w=[d_iclr])
                    k.op(dve, lambda: nc.vector.tensor_tensor_scan(cs[:], rmask[:, 0:BL], sgw[:], 0.0, ALU.mult, ALU.add),
                         r=[d_sgw, d_const], w=[d_cs])
                    if z == 1:
                        k.op(dve, lambda: nc.vector.tensor_tensor(cs2[:], sgw[:], cs[:], ALU.subtract),
                             r=[d_sgw, d_cs], w=[d_cs2])
                        k.op(dve, lambda: nc.vector.tensor_tensor(
                            cs2[:].rearrange("p (n t) -> p n t", t=64), cs2[:].rearrange("p (n t) -> p n t", t=64),
                            bc3(cs, 63, CPB, 64, 64), ALU.add), r=[d_cs2, d_cs], w=[d_cs2])
                        csu, d_csu = cs2, d_cs2
                    else:
                        csu, d_csu = cs, d_cs
                    k.op(pool, lambda: nc.gpsimd.tensor_scalar(t2[:], iclr[:], Vc("k_a", 0, c), S("omka", c), ALU.mult, ALU.add),
                         r=[d_iclr, d_const, d_scal], w=[d_t2])
                    k.op(pool, lambda qs=qs: nc.gpsimd.tensor_tensor(t2[:], t2[:], k32[:, qs], ALU.mult), r=[d_t2, d_k], w=[d_t2])
                    if z == 0:
                        k.op(pool, lambda qs=qs: nc.gpsimd.tensor_copy(ksum[:, qs], t2[:]), r=[d_t2], w=[d_ks])
                    else:
                        k.op(pool, lambda qs=qs: nc.gpsimd.tensor_tensor(ksum[:, qs], ksum[:, qs], t2[:], ALU.add), r=[d_t2, d_ks], w=[d_ks])
                    k.op(act, lambda csu=csu: nc.scalar.activation(t3[:], csu[:], AF.Exp, scale=-DECAY_S), r=[d_csu], w=[d_t3])
                    k.op(dve, lambda qs=qs: nc.vector.tensor_tensor(kt[:, qs], t2[:], t3[:], ALU.mult), r=[d_t2, d_t3], w=[d_kt])
                    k.op(pool, lambda qs=qs: nc.gpsimd.tensor_tensor(t2[:], kk32[:, qs], iclr[:], ALU.mult), r=[d_kk, d_iclr, d_kt], w=[d_t2])
                    k.op(dve, lambda qs=qs: nc.vector.tensor_tensor(bt[:, qs], t2[:], t3[:], ALU.mult), r=[d_t2, d_t3], w=[d_bt])
                    k.op(act, lambda csu=csu: nc.scalar.activation(t3[:], csu[:], AF.Exp, scale=DECAY_S), r=[d_csu, d_bt], w=[d_t3])
                    wc_col = 63 if z == 0 else 0
                    k.op(pool, lambda nb0=nb0, wc_col=wc_col: nc.gpsimd.tensor_copy(WC[:, nb0:nb0 + CPB], t3[:, wc_col:BL:64]),
                         r=[d_t3], w=[d_WC])
                    for h in range(2):
                        hs_ = slice(64 * h, 64 * h + 64)
                        k.op(dve, lambda h=h, hs_=hs_, qs=qs, nb0=nb0: nc.vector.tensor_tensor(
                            ARbd[hs_, nb0:nb0 + CPB, h, 64:128], r32[hs_, qs].rearrange("p (n t) -> p n t", t=64),
                            t3[hs_, :].rearrange("p (n t) -> p n t", t=64), ALU.mult), r=[d_r, d_t3], w=[d_AR])
                    k.op(pool, lambda csu=csu: nc.gpsimd.tensor_tensor(t2[:], csu[:], sgw[:], ALU.subtract), r=[d_csu, d_sgw, d_bt], w=[d_t2])
                    k.op(act, lambda: nc.scalar.activation(t2[:], t2[:], AF.Exp, scale=DECAY_S), r=[d_t2], w=[d_t2])
                    for h in range(2):
                        hs_ = slice(64 * h, 64 * h + 64)
                        k.op(dve, lambda h=h, hs_=hs_, qs=qs, nb0=nb0: nc.vector.scalar_tensor_tensor(
                            ARbd[hs_, nb0:nb0 + CPB, h, 0:64], kk32[hs_, qs].rearrange("p (n t) -> p n t", t=64), -1.0,
                            t2[hs_, :].rearrange("p (n t) -> p n t", t=64), ALU.mult, ALU.mult), r=[d_kk, d_t2], w=[d_AR])

                mk = maskF if z == 0 else maskB
                mkL = maskLF if z == 0 else maskLB

                def precompute(gb, chunks):
                    NBg, LBg, XBg = NB[gb], LB[gb], XB[gb]
                    for gi, n in enumerate(chunks):
                        t0 = n * 64
                        k.op(pe, lambda t0=t0: nc.tensor.transpose(PSB[0:64, 0:128], bt[:, t0:t0 + 64], ident_bf[:]),
                             r=[d_bt, d_const], w=[dPSB])
                        k.op(pe, lambda t0=t0: nc.tensor.transpose(PSB[0:64, 128:256], kt[:, t0:t0 + 64], ident_bf[:]),
                             r=[d_kt, d_const], w=[dPSB])
                        k.op(pe, lambda t0=t0: nc.tensor.transpose(PSB[0:64, 256:384], v16[:, t0:t0 + 64], ident_bf[:]),
                             r=[d_v, d_const], w=[dPSB])
                        k.op(act, lambda gi=gi: nc.scalar.copy(tm[gb][:, gi, :], PSB[0:64, 0:384]), r=[dPSB], w=[d_tm[gb]])
                        k.op(pe, lambda n=n, t0=t0: nc.tensor.matmul(
                            PS[3][0:64, 0:256], bt[:, t0:t0 + 64], ARbd[:, n, :, :].rearrange("p h x -> p (h x)"),
                            start=True, stop=True), r=[d_bt, d_AR], w=[dPS[3]])
                        k.op(pe, lambda n=n, t0=t0: nc.tensor.matmul(
                            PS[3][0:64, 256:512], kt[:, t0:t0 + 64], ARbd[:, n, :, :].rearrange("p h x -> p (h x)"),
                            start=True, stop=True), r=[d_kt, d_AR], w=[dPS[3]])
                        k.op(dve, lambda gi=gi: nc.vector.tensor_tensor(L1[gb][:, gi, :], PS[3][0:64, :], mk[:], ALU.mult),
                             r=[dPS[3], d_const], w=[d_L1[gb]])
                        for h in range(2):
                            k.op(pe, lambda n=n, t0=t0, h=h, gi=gi: nc.tensor.matmul(
                                PS[4][0:64, gi * 128 + h * 64: gi * 128 + h * 64 + 64],
                                ARbd[:, n, h, 0:64], bt[:, t0:t0 + 64], start=True, stop=True),
                                r=[d_AR, d_bt], w=[dPS[4]])
                    G = len(chunks)
                    k.op(dve, lambda: nc.vector.tensor_tensor(
                        LBg[:, 0:G * 128].rearrange("p (g x) -> p g x", x=128),
                        PS[4][0:64, 0:G * 128].rearrange("p (g x) -> p g x", x=128),
                        mkL[:, :].unsqueeze(1).to_broadcast([64, G, 128]), ALU.mult), r=[dPS[4], d_const], w=[d_LB[gb]])
                    k.op(pool, lambda: nc.gpsimd.tensor_copy(
                        NBg[:, 0:G * 128].rearrange("p (g h x) -> p g h x", h=2, x=64),
                        L1[gb][:, 0:G, 0:256].rearrange("p g (h x) -> p g h x", h=2)[:, :, :, 0:64]),
                        r=[d_L1[gb]], w=[d_NB[gb]])
                    k.op(pool, lambda: nc.gpsimd.tensor_tensor(
                        XBg[:, 0:G * 128].rearrange("p (g x) -> p g x", x=64), NBg[:, 0:G * 128].rearrange("p (g x) -> p g x", x=64),
                        ident_bf[0:64, 0:64].unsqueeze(1).to_broadcast([64, 2 * G, 64]), ALU.add),
                        r=[d_NB[gb], d_const], w=[d_XB[gb]])
                    for step in range(5):
                        for q in range(2 * G):
                            qq = slice(q * 64, q * 64 + 64)
                            k.op(pe, lambda qq=qq: nc.tensor.matmul(PS[5][0:64, qq], LBg[:, qq], NBg[:, qq], start=True, stop=True),
                                 r=[d_LB[gb], d_NB[gb]], w=[dPS[5]])
                        for q in range(2 * G):
                            qq = slice(q * 64, q * 64 + 64)
                            k.op(pe, lambda qq=qq: nc.tensor.matmul(PS[4][0:64, qq], NBg[:, qq], LBg[:, qq], start=True, stop=True),
                                 r=[d_LB[gb], d_NB[gb]], w=[dPS[4]])
                        k.op(act, lambda: nc.scalar.copy(NBg[:, 0:G * 128], PS[5][0:64, 0:G * 128]), r=[dPS[5]], w=[d_NB[gb]])
                        k.op(dve, lambda: nc.vector.tensor_copy(LBg[:, 0:G * 128], PS[4][0:64, 0:G * 128]), r=[dPS[4]], w=[d_LB[gb]])
                        for q in range(2 * G):
                            qq = slice(q * 64, q * 64 + 64)
                            k.op(pe, lambda qq=qq: nc.tensor.matmul(PS[3][0:64, qq], LBg[:, qq], XBg[:, qq], start=True, stop=False),
                                 r=[d_LB[gb], d_XB[gb]], w=[dPS[3]])
                            k.op(pe, lambda qq=qq: nc.tensor.matmul(PS[3][0:64, qq], ident_bf[0:64, 0:64], XBg[:, qq], start=False, stop=True),
                                 r=[d_const, d_XB[gb]], w=[dPS[3]])
                        if step < 4:
                            k.op(act, lambda: nc.scalar.copy(XBg[:, 0:G * 128], PS[3][0:64, 0:G * 128]), r=[dPS[3]], w=[d_XB[gb]])
                        else:
                            k.op(act, lambda: nc.scalar.copy(
                                TT[gb][:, 0:G, :].rearrange("p g x -> p (g x)"), PS[3][0:64, 0:G * 128]),
                                r=[dPS[3]], w=[d_TT[gb]])

                chain_state = {"prev": None}

                def chain(gb, chunks):
                    for gi, n in enumerate(chunks):
                        t0 = n * 64
                        prev = chain_state["prev"]
                        for h in range(2):
                            k.op(pe, lambda n=n, h=h: nc.tensor.matmul(PS[0][0:64, 0:128], ARbd[:, n, h, 0:64], S16[:],
                                                                       start=(h == 0), stop=False),
                                 r=[d_AR, d_S], w=[dPS[0]])
                        for h in range(2):
                            k.op(pe, lambda gi=gi, h=h: nc.tensor.matmul(
                                PS[0][0:64, h * 64:h * 64 + 64], L1[gb][:, gi, 256 + h * 128:256 + h * 128 + 64],
                                tm[gb][:, gi, 256 + h * 64:256 + h * 64 + 64], start=False, stop=(h == 1)),
                                r=[d_L1[gb], d_tm[gb]], w=[dPS[0]])
                        k.op(act, lambda: nc.scalar.copy(X16[:], PS[0][0:64, 0:128]), r=[dPS[0]], w=[d_X])
                        for h in range(2):
                            k.op(pe, lambda gi=gi, h=h: nc.tensor.matmul(
                                PS[1][0:64, h * 64:h * 64 + 64], TT[gb][:, gi, h * 64:h * 64 + 64], X16[:, h * 64:h * 64 + 64],
                                start=True, stop=True), r=[d_TT[gb], d_X], w=[dPS[1]])
                        k.op(dve, lambda: nc.vector.tensor_copy(U16[:], PS[1][0:64, 0:128]), r=[dPS[1]], w=[d_U])
                        if n >= 4:
                            l0 = t0 - TC
                            k.op(pe, lambda n=n: nc.tensor.matmul(
                                PS[6][:, 0:128], S16[:], ARbd[:, n, :, 64:128], start=True, stop=False),
                                r=[d_S, d_AR], w=[dPS[6]])
                            for h in range(2):
                                hs_ = slice(64 * h, 64 * h + 64)
                                k.op(pe, lambda gi=gi, h=h, hs_=hs_: nc.tensor.matmul(
                                    PS[6][hs_, h * 64:h * 64 + 64], U16[:, h * 64:h * 64 + 64],
                                    L1[gb][:, gi, h * 128 + 64:h * 128 + 128], start=False, stop=False),
                                    r=[d_U, d_L1[gb]], w=[dPS[6]])
                                k.op(pe, lambda gi=gi, h=h, hs_=hs_: nc.tensor.matmul(
                                    PS[6][hs_, h * 64:h * 64 + 64], tm[gb][:, gi, 256 + h * 64:256 + h * 64 + 64],
                                    L1[gb][:, gi, 256 + h * 128 + 64:256 + h * 128 + 128], start=False, stop=(h == 1)),
                                    r=[d_tm[gb], d_L1[gb]], w=[dPS[6]])
                            for h in range(2):
                                hs_ = slice(64 * h, 64 * h + 64)
                                if z == 0:
                                    k.op(act, lambda h=h, hs_=hs_, l0=l0: nc.scalar.copy(
                                        yacc[hs_, l0:l0 + 64], PS[6][hs_, h * 64:h * 64 + 64]), r=[dPS[6]], w=[d_y])
                                else:
                                    k.op(dve, lambda h=h, hs_=hs_, l0=l0: nc.vector.tensor_tensor(
                                        yacc[hs_, l0:l0 + 64], yacc[hs_, l0:l0 + 64], PS[6][hs_, h * 64:h * 64 + 64], ALU.add),
                                        r=[dPS[6], d_y], w=[d_y])
                        k.op(pe, lambda gi=gi: nc.tensor.matmul(PS[2][:, 0:128], tm[gb][:, gi, 0:128], U16[:], start=True, stop=False),
                             r=[d_tm[gb], d_U], w=[dPS[2]])
                        k.op(pe, lambda gi=gi: nc.tensor.matmul(PS[2][:, 0:128], tm[gb][:, gi, 128:256], tm[gb][:, gi, 256:384],
                                                                start=False, stop=True), r=[d_tm[gb]], w=[dPS[2]])
                        for h in range(2):
                            hs_ = slice(64 * h, 64 * h + 64)
                            if prev is None:
                                k.op(dve, lambda hs_=hs_: nc.vector.tensor_copy(M32[hs_, hs_], PS[2][hs_, hs_]),
                                     r=[dPS[2]], w=[d_M])
                            else:
                                k.op(dve, lambda hs_=hs_, prev=prev: nc.vector.scalar_tensor_tensor(
                                    M32[hs_, hs_], M32[hs_, hs_], WC[hs_, prev:prev + 1], PS[2][hs_, hs_], ALU.mult, ALU.add),
                                    r=[dPS[2], d_M, d_WC], w=[d_M])
                            k.op(act, lambda hs_=hs_, n=n: nc.scalar.activation(
                                S16[hs_, hs_], M32[hs_, hs_], AF.Identity, scale=WC[hs_, n:n + 1]), r=[d_M, d_WC], w=[d_S])
                        chain_state["prev"] = n

                order = list(range(NCH)) if z == 0 else [3, 2, 1, 0] + list(range(NCH - 1, 3, -1))
                groups = [order[i:i + GI] for i in range(0, NCH, GI)]
                k.op(dve, lambda: nc.vector.memset(M32[:], 0.0), w=[d_M])
                k.op(dve, lambda: nc.vector.memset(S16[:], 0.0), w=[d_S])
                precompute(0, groups[0])
                for gidx, grp in enumerate(groups):
                    if gidx + 1 < len(groups):
                        precompute((gidx + 1) % 2, groups[gidx + 1])
                    chain(gidx % 2, grp)

            if dbg == 2 and c == 0:
                dump(lambda: yacc[:], T, d_y, 0)
                dump(lambda: r32[:, TC:NT], T, d_r, T)
                dump(lambda: kk32[:, TC:NT], T, d_kk, 2 * T)
                dump(lambda: ksum[:, TC:NT], T, d_ks, 3 * T)
                k.barrier()
                ph.close()
                lora_stack.close()
                k.finish()
                return k

            ob = ost[c % 2]
            dob = d_ost[c % 2]
            tA, tB, d_tA, d_tB = t2, t3, d_t2, d_t3
            for q in range(0, T, 512):
                qs = slice(q, q + 512)
                qn = slice(TC + q, TC + q + 512)
                W5 = slice(0, 512)
                k.op(pe, lambda qs=qs: nc.tensor.matmul(PS[0][:, :], bdm_f[:], yacc[:, qs], start=True, stop=True),
                     r=[d_const, d_y], w=[dPS[0]])
                k.op(dve, lambda qs=qs: nc.vector.tensor_tensor(tA[:, W5], yacc[:, qs], PS[0][:, :], ALU.subtract),
                     r=[d_y, dPS[0]], w=[d_tA])
                k.op(pool, lambda: nc.gpsimd.tensor_tensor(tB[:, W5], tA[:, W5], tA[:, W5], ALU.mult),
                     r=[d_tA], w=[d_tB])
                k.op(pe, lambda: nc.tensor.matmul(PS[1][:, :], bdm_f[:], tB[:, W5], start=True, stop=True),
                     r=[d_const, d_tB], w=[dPS[1]])
                k.op(act, lambda: nc.scalar.activation(tB[:, W5], PS[1][:, :], AF.Sqrt, bias=GN_EPS, scale=1.0),
                     r=[dPS[1]], w=[d_tB])
                k.op(dve, lambda: nc.vector.reciprocal(tB[:, W5], tB[:, W5]), r=[d_tB], w=[d_tB])
                k.op(dve, lambda: nc.vector.tensor_tensor(tA[:, W5], tA[:, W5], tB[:, W5], ALU.mult),
                     r=[d_tA, d_tB], w=[d_tA])
                k.op(act, lambda: nc.scalar.activation(tA[:, W5], tA[:, W5], AF.Identity,
                                                       bias=Vc("ln_b", 0, c), scale=Vc("ln_w", 0, c)),
                     r=[d_tA, d_const], w=[d_tA])
                k.op(dve, lambda qn=qn: nc.vector.scalar_tensor_tensor(
                    tB[:, W5], r32[:, qn], S("hrk", c), ksum[:, qn], ALU.mult, ALU.mult),
                    r=[d_r, d_ks, d_scal, d_tB], w=[d_tB])
                k.op(pe, lambda: nc.tensor.matmul(PS[2][:, :], bd1_f[:], tB[:, W5], start=True, stop=True),
                     r=[d_const, d_tB], w=[dPS[2]])
                k.op(dve, lambda qn=qn: nc.vector.tensor_tensor(tB[:, W5], PS[2][:, :], v16[:, qn], ALU.mult),
                     r=[dPS[2], d_v, d_tB], w=[d_tB])
                k.op(pool, lambda: nc.gpsimd.tensor_tensor(tA[:, W5], tA[:, W5], tB[:, W5], ALU.add),
                     r=[d_tA, d_tB], w=[d_tA])
                k.op(pe, lambda qs=qs: nc.tensor.matmul(PS[3][:, :], g2c[:, 0, :], SG[:, 0, qs], start=True, stop=False),
                     r=[d_g2, d_SG], w=[dPS[3]])
                k.op(pe, lambda qs=qs: nc.tensor.matmul(PS[3][:, :], g2c[0:32, 1, :], SG[0:32, 1, qs], start=False, stop=True),
                     r=[d_g2, d_SG], w=[dPS[3]])
                k.op(dve, lambda qs=qs: nc.vector.tensor_tensor(ob[:, qs], tA[:, W5], PS[3][:, :], ALU.mult),
                     r=[d_tA, dPS[3]], w=[dob])
            k.dma(sp, oT_d[:, c, :], ob[:], r=[dob])
        k.barrier()
    lora_stack.close()

    h2_stack = contextlib.ExitStack()
    h2 = k.sb("h2", [128, KC, T], BF16, h2_stack)
    d_h2 = [Dep() for _ in range(4)]
    d_xres = [Dep() for _ in range(4)]

    def out_proj_phase(w_dram, yin, d_yin, Gn, xprev_dram, An, Bn, router=None):
        with contextlib.ExitStack() as ph:
            wk = mk_wk(ph)
            wo = k.sb("wo", [128, KC, D], BF16, ph)
            d_wo = Dep()
            k.dma(pool, wo[:], kview(w_dram, 0, D), w=[d_wo])
            ym = k.sb("ym", [128, KC, 512], F32, ph)
            xp = k.sb("xp", [128, KC, 512], F32, ph)
            xn = k.sb("xn", [128, KC, 512], F32, ph)
            d_ym, d_xp, d_xn = Dep(), Dep(), Dep()
            if router is not None:
                h32 = k.sb("h32", [128, KC, 512], F32, ph)
                d_h32 = Dep()
            for tb in range(4):
                ts_ = slice(tb * 512, (tb + 1) * 512)
                k.dma(sp, xp[:], xprev_dram[:, :, ts_], r=[d_xres[tb]], w=[d_xp])
                for dc in range(KC):
                    p = dc % 4
                    for kk in range(KC):
                        k.op(pe, lambda dc=dc, kk=kk, p=p, ts_=ts_: nc.tensor.matmul(
                            PS[p][:, :], wo[:, kk, dc * 128:(dc + 1) * 128], yin[:, kk, ts_],
                            start=(kk == 0), stop=(kk == KC - 1)), r=[d_wo, d_yin], w=[dPS[p]])
                    k.op(act, lambda dc=dc, p=p: nc.scalar.copy(ym[:, dc, :], PS[p][:, :]), r=[dPS[p]], w=[d_ym])
                res_norm(ym, d_ym, 512, Gn, xp, d_xp, xn, d_xn, wk)
                k.dma(sp, xres_d[:, :, ts_], xn[:], r=[d_xn], w=[d_xres[tb]])
                norm_mod(xn, d_xn, 512, An, Bn, lambda c, ts_=ts_: h2[:, c, ts_], d_h2[tb], wk,
                         h32=(h32 if router is not None else None), d_h32=(d_h32 if router is not None else None))
                if router is not None:
                    router(tb, h32, d_h32)
            k.barrier()

    def ffn_pass(wgu_dram, wd_dram, acc, d_acc, first, wbufs, gate_bc=None, d_gate=None):
        for fg in range(FF // 512):
            b = wbufs["i"] % 2
            wbufs["i"] += 1
            Wg, Wu, Wd = wbufs["g"][b], wbufs["u"][b], wbufs["d"][b]
            dW = wbufs["dep"][b]
            k.dma(pool, Wg[:], kview(wgu_dram, fg * 512, (fg + 1) * 512), w=[dW])
            k.dma(pool, Wu[:], kview(wgu_dram, FF + fg * 512, FF + (fg + 1) * 512), w=[dW])
            k.dma(pool, Wd[:], wd_dram[fg * 512:(fg + 1) * 512, :].rearrange("(f p) d -> p f d", p=128), w=[dW])
            for tb in range(4):
                ts_ = slice(tb * 512, (tb + 1) * 512)
                ab = wbufs["ai"] % 2
                wbufs["ai"] += 1
                actb = wbufs["act"][ab]
                d_actb = wbufs["dact"][ab]
                for fc in range(4):
                    pg = (fc % 2) * 2
                    pu = pg + 1
                    for kk in range(KC):
                        k.op(pe, lambda kk=kk, fc=fc, pg=pg, ts_=ts_: nc.tensor.matmul(
                            PS[pg][:, :], Wg[:, kk, fc * 128:(fc + 1) * 128], h2[:, kk, ts_],
                            start=(kk == 0), stop=(kk == KC - 1)), r=[dW, d_h2[tb]], w=[dPS[pg]])
                    for kk in range(KC):
                        k.op(pe, lambda kk=kk, fc=fc, pu=pu, ts_=ts_: nc.tensor.matmul(
                            PS[pu][:, :], Wu[:, kk, fc * 128:(fc + 1) * 128], h2[:, kk, ts_],
                            start=(kk == 0), stop=(kk == KC - 1)), r=[dW, d_h2[tb]], w=[dPS[pu]])
                    sgb = wbufs["sg"][fc % 2]
                    d_sgb = wbufs["dsg"][fc % 2]
                    k.op(act, lambda pg=pg, sgb=sgb: nc.scalar.activation(sgb[:], PS[pg][:, :], AF.Silu),
                         r=[dPS[pg]], w=[d_sgb])
                    if gate_bc is None:
                        k.op(dve, lambda fc=fc, pu=pu, sgb=sgb, actb=actb: nc.vector.tensor_tensor(
                            actb[:, fc, :], sgb[:], PS[pu][:, :], ALU.mult), r=[d_sgb, dPS[pu]], w=[d_actb])
                    else:
                        k.op(dve, lambda fc=fc, pu=pu, sgb=sgb: nc.vector.tensor_tensor(
                            sgb[:], sgb[:], PS[pu][:, :], ALU.mult), r=[d_sgb, dPS[pu]], w=[d_sgb])
                        k.op(pool, lambda fc=fc, sgb=sgb, actb=actb, ts_=ts_: nc.gpsimd.tensor_tensor(
                            actb[:, fc, :], sgb[:], gate_bc[:, ts_], ALU.mult), r=[d_sgb, d_gate], w=[d_actb])
                for dc in range(KC):
                    p = 4 + dc % 2
                    for fc in range(4):
                        k.op(pe, lambda dc=dc, fc=fc, p=p, actb=actb: nc.tensor.matmul(
                            PS[p][:, :], Wd[:, fc, dc * 128:(dc + 1) * 128], actb[:, fc, :],
                            start=(fc == 0), stop=(fc == 3)), r=[dW, d_actb], w=[dPS[p]])
                    if first and fg == 0:
                        k.op(act, lambda dc=dc, p=p, ts_=ts_: nc.scalar.copy(acc[:, dc, ts_], PS[p][:, :]),
                             r=[dPS[p]], w=[d_acc[tb]])
                    else:
                        k.op(dve, lambda dc=dc, p=p, ts_=ts_: nc.vector.tensor_tensor(
                            acc[:, dc, ts_], acc[:, dc, ts_], PS[p][:, :], ALU.add), r=[dPS[p], d_acc[tb]], w=[d_acc[tb]])

    def mk_ffn_bufs(ph):
        return dict(i=0, ai=0,
                    g=[k.sb("Wg%d" % i, [128, KC, 512], BF16, ph) for i in range(2)],
                    u=[k.sb("Wu%d" % i, [128, KC, 512], BF16, ph) for i in range(2)],
                    d=[k.sb("Wd%d" % i, [128, 4, D], BF16, ph) for i in range(2)],
                    dep=[Dep(), Dep()],
                    act=[k.sb("actb%d" % i, [128, 4, 512], BF16, ph) for i in range(2)],
                    dact=[Dep(), Dep()],
                    sg=[k.sb("sgb%d" % i, [128, 512], F32, ph) for i in range(2)],
                    dsg=[Dep(), Dep()])

    def post_ffn_phase(acc, d_acc, Gn, An, Bn, final):
        with contextlib.ExitStack() as ph:
            wk = mk_wk(ph)
            xp = k.sb("xp", [128, KC, 512], F32, ph)
            xn = k.sb("xn", [128, KC, 512], F32, ph)
            d_xp, d_xn = Dep(), Dep()
            for tb in range(4):
                ts_ = slice(tb * 512, (tb + 1) * 512)
                k.dma(sp, xp[:], xres_d[:, :, ts_], r=[d_xres[tb]], w=[d_xp])

                sumsq_rstd(lambda c: acc[:, c, ts_], 512, wk["sq"], wk["rs"], d_acc[tb], wk["dsq"], wk["drs"], 6)
                for c in range(KC):
                    k.op(pool, lambda c=c, ts_=ts_: nc.gpsimd.tensor_tensor(wk["tmp"][:, c, :], acc[:, c, ts_], wk["rs"][:, :], ALU.mult),
                         r=[d_acc[tb], wk["drs"]], w=[wk["dtmp"]])
                    k.op(dve, lambda c=c: nc.vector.scalar_tensor_tensor(xn[:, c, :], wk["tmp"][:, c, :], S(Gn, c),
                                                                         xp[:, c, :], ALU.mult, ALU.add),
                         r=[wk["dtmp"], d_scal, d_xp], w=[d_xn])
                if final:
                    k.dma(sp, out_d[:, :, ts_], xn[:], r=[d_xn])
                else:
                    k.dma(sp, xres_d[:, :, ts_], xn[:], r=[d_xn], w=[d_xres[tb]])
                    norm_mod(xn, d_xn, 512, An, Bn, lambda c, ts_=ts_: h2[:, c, ts_], d_h2[tb], wk)
            k.barrier()

    with contextlib.ExitStack() as yst:
        yin0 = k.sb("yin0", [128, KC, T], BF16, yst)
        d_yin0 = Dep()
        k.dma(sp, yin0[:], oT_d[:, :, :], w=[d_yin0])
        out_proj_phase(wout_d, yin0, d_yin0, "G1_0", xT_d, "A2_0", "B2_0")

    if dbg == 3:
        with contextlib.ExitStack() as ph:
            t32 = k.sb("t32", [128, T], F32, ph)
            dd = Dep()
            k.op(dve, lambda: nc.vector.tensor_copy(t32[:], h2[:, 0, :]), r=d_h2, w=[dd])
            dump(lambda: t32[:], T, dd)
            k.barrier()
        h2_stack.close()
        k.finish()
        return k

    acc_stack = contextlib.ExitStack()
    acc = k.sb("acc", [128, KC, T], F32, acc_stack)
    d_acc = [Dep() for _ in range(4)]
    with contextlib.ExitStack() as ph:
        wbufs = mk_ffn_bufs(ph)
        ffn_pass(fgu_d, fd_d, acc, d_acc, True, wbufs)
        k.barrier()
    post_ffn_phase(acc, d_acc, "G2_0", "A1_1", "B1_1", final=False)
    acc_stack.close()

    gates_stack = contextlib.ExitStack()
    logit = k.sb("logit", [128, 16, NE], F32, gates_stack)
    gates = k.sb("gates", [128, 16, NE], F32, gates_stack)
    wr32 = k.sb("wr32", [128, KC, NE], F32, gates_stack)
    d_logit, d_gates, d_wr = Dep(), Dep(), Dep()
    k.dma(sp, wr32[:], rt_d[:, :, :], w=[d_wr])

    ycv_stack = contextlib.ExitStack()
    ycv = k.sb("ycv", [128, KC, T], BF16, ycv_stack)
    d_ycv = Dep()
    with contextlib.ExitStack() as ph:
        Wc3 = [k.sb("Wc3_%d" % i, [128, KC, 3, 128], BF16, ph) for i in range(2)]
        dWc3 = [Dep(), Dep()]
        Bsb = k.sb("Bsb", [128, T], F32, ph)
        Csb = k.sb("Csb", [128, 512], F32, ph)
        zp = k.sb("zp", [128, T + 2], F32, ph)
        t1 = k.sb("t1", [128, T], F32, ph)
        d_B, d_C, d_z, d_t1 = Dep(), Dep(), Dep(), Dep()
        k.op(dve, lambda: nc.vector.memset(zp[:], 0.0), w=[d_z])
        for c in range(KC):
            W3 = Wc3[c % 2]
            dW3 = dWc3[c % 2]
            for j in range(3):
                k.dma(pool, W3[:, :, j, :], kview(cwin_d, j * D + c * 128, j * D + (c + 1) * 128), w=[dW3])
            for tb in range(4):
                ts_ = slice(tb * 512, (tb + 1) * 512)
                for j in range(3):
                    p = j
                    for kk in range(KC):
                        k.op(pe, lambda kk=kk, j=j, p=p, ts_=ts_, W3=W3: nc.tensor.matmul(
                            PS[p][:, :], W3[:, kk, j, :], h2[:, kk, ts_], start=(kk == 0), stop=(kk == KC - 1)),
                            r=[dW3, d_h2[tb]], w=[dPS[p]])
                k.op(act, lambda ts_=ts_: nc.scalar.copy(Bsb[:, ts_], PS[0][:, :]), r=[dPS[0]], w=[d_B])
                k.op(act, lambda: nc.scalar.copy(Csb[:], PS[1][:, :]), r=[dPS[1]], w=[d_C])
                k.op(dve, lambda tb=tb: nc.vector.tensor_tensor(zp[:, 1 + tb * 512:1 + (tb + 1) * 512], Csb[:], PS[2][:, :], ALU.mult),
                     r=[d_C, dPS[2]], w=[d_z])
            k.op(act, lambda c=c: nc.scalar.activation(t1[:], zp[:, 0:T], AF.Identity, scale=Vc("conv_w", 0, c)),
                 r=[d_z, d_const], w=[d_t1])
            k.op(dve, lambda c=c: nc.vector.scalar_tensor_tensor(t1[:], zp[:, 1:T + 1], Vc("conv_w", 1, c), t1[:], ALU.mult, ALU.add),
                 r=[d_z, d_const, d_t1], w=[d_t1])
            k.op(dve, lambda c=c: nc.vector.scalar_tensor_tensor(t1[:], zp[:, 2:T + 2], Vc("conv_w", 2, c), t1[:], ALU.mult, ALU.add),
                 r=[d_z, d_const, d_t1], w=[d_t1])
            k.op(pool, lambda c=c: nc.gpsimd.tensor_tensor(ycv[:, c, :], t1[:], Bsb[:], ALU.mult),
                 r=[d_t1, d_B], w=[d_ycv])
        k.barrier()

    def router(tb, h32, d_h32):
        for sub in range(4):
            tt = tb * 4 + sub
            for c in range(KC):
                k.op(pe, lambda c=c, sub=sub: nc.tensor.matmul(
                    PS[5][:, sub * 8:sub * 8 + 8], h32[:, c, sub * 128:(sub + 1) * 128], wr32[:, c, :],
                    start=(c == 0), stop=(c == KC - 1)), r=[d_h32, d_wr], w=[dPS[5]])
        k.op(dve, lambda tb=tb: nc.vector.tensor_copy(
            logit[:, tb * 4:(tb + 1) * 4, :], PS[5][:, 0:32].rearrange("p (s e) -> p s e", e=8)),
            r=[dPS[5]], w=[d_logit])

    out_proj_phase(cwout_d, ycv, d_ycv, "G1_1", xres_d, "A2_1", "B2_1", router=router)
    ycv_stack.close()

    with contextlib.ExitStack() as ph:
        mx = k.sb("mx", [128, 16, 8], F32, ph)
        e1 = k.sb("e1", [128, 16], F32, ph)
        g1_ = k.sb("g1_", [128, 16], F32, ph)
        g2_ = k.sb("g2_", [128, 16], F32, ph)
        q1 = k.sb("q1", [128, 16, 8], F32, ph)
        q2 = k.sb("q2", [128, 16, 8], F32, ph)
        d_mx, d_e = Dep(), Dep()
        for tt in range(16):
            k.op(dve, lambda tt=tt: nc.vector.max(mx[:, tt, :], logit[:, tt, :]), r=[d_logit], w=[d_mx])
        k.op(dve, lambda: nc.vector.tensor_tensor(e1[:], mx[:, :, 1], mx[:, :, 0], ALU.subtract), r=[d_mx], w=[d_e])
        k.op(act, lambda: nc.scalar.activation(e1[:], e1[:], AF.Exp), r=[d_e], w=[d_e])
        k.op(dve, lambda: nc.vector.tensor_scalar(g1_[:], e1[:], 1.0, None, ALU.add), r=[d_e], w=[d_e])
        k.op(dve, lambda: nc.vector.reciprocal(g1_[:], g1_[:]), r=[d_e], w=[d_e])
        k.op(dve, lambda: nc.vector.tensor_tensor(g2_[:], e1[:], g1_[:], ALU.mult), r=[d_e], w=[d_e])
        k.op(dve, lambda: nc.vector.tensor_tensor(q1[:], logit[:], mx[:, :, 0:1].to_broadcast([128, 16, 8]), ALU.is_equal),
             r=[d_logit, d_mx], w=[d_e])
        k.op(dve, lambda: nc.vector.tensor_tensor(q2[:], logit[:], mx[:, :, 1:2].to_broadcast([128, 16, 8]), ALU.is_equal),
             r=[d_logit, d_mx], w=[d_e])
        k.op(dve, lambda: nc.vector.tensor_tensor(q1[:], q1[:], g1_[:, :].unsqueeze(2).to_broadcast([128, 16, 8]), ALU.mult),
             r=[d_e], w=[d_e])
        k.op(dve, lambda: nc.vector.tensor_tensor(q2[:], q2[:], g2_[:, :].unsqueeze(2).to_broadcast([128, 16, 8]), ALU.mult),
             r=[d_e], w=[d_e])
        k.op(dve, lambda: nc.vector.tensor_tensor(gates[:], q1[:], q2[:], ALU.add), r=[d_e], w=[d_gates])
        k.barrier()

    if dbg == 4:
        dump(lambda: gates[:].rearrange("p t e -> p (t e)"), 128, d_gates, 0)
        dump(lambda: logit[:].rearrange("p t e -> p (t e)"), 128, d_logit, 128)
        k.barrier()
        gates_stack.close()
        h2_stack.close()
        k.finish()
        return k

    acc_stack = contextlib.ExitStack()
    acc = k.sb("acc2", [128, KC, T], F32, acc_stack)
    d_acc = [Dep() for _ in range(4)]
    with contextlib.ExitStack() as ph:
        wbufs = mk_ffn_bufs(ph)
        gbc = [k.sb("gbc%d" % i, [128, T], F32, ph) for i in range(2)]
        d_gbc = [Dep(), Dep()]
        Gm = [k.sb("Gm%d" % i, [128, 128], F32, ph) for i in range(2)]
        d_Gm = [Dep(), Dep()]
        ident_f = cst[:, C_ID:C_ID + 128]
        for e in range(NE):
            gb = gbc[e % 2]
            dgb = d_gbc[e % 2]
            for tt in range(16):
                gm = Gm[tt % 2]
                dgm = d_Gm[tt % 2]
                k.op(dve, lambda tt=tt, e=e, gm=gm: nc.vector.tensor_copy(gm[:], gates[:, tt, e:e + 1].to_broadcast([128, 128])),
                     r=[d_gates], w=[dgm])
                k.op(pe, lambda tt=tt, gm=gm: nc.tensor.matmul(PS[6][:, (tt % 4) * 128:(tt % 4 + 1) * 128], gm[:], ident_f,
                                                              start=True, stop=True), r=[dgm, d_const], w=[dPS[6]])
                if tt % 4 == 3:
                    q = (tt // 4) * 512
                    k.op(act, lambda q=q, gb=gb: nc.scalar.copy(gb[:, q:q + 512], PS[6][:, :]), r=[dPS[6]], w=[dgb])
            ffn_pass(mgu_d[e], md_d[e], acc, d_acc, e == 0, wbufs, gate_bc=gb, d_gate=dgb)
        k.barrier()
    post_ffn_phase(acc, d_acc, "G2_1", None, None, final=True)
    acc_stack.close()
    gates_stack.close()
    h2_stack.close()
    k.finish()
    return k


def prep_inputs(inp):
    f = lambda a: np.ascontiguousarray(np.asarray(a, np.float32))
    vec = np.zeros((128, NV), np.float32)

    def put(name, arr):
        a = col(arr)
        vec[:, VOFF[name]:VOFF[name] + a.shape[1]] = a

    put("g", f(inp["norm_g"]).reshape(-1))
    put("mu", f(inp["rwkv_mu"]).reshape(-1))
    put("w0", f(inp["rwkv_w0"]).reshape(-1))
    put("a0", f(inp["rwkv_a0"]).reshape(-1))
    put("k_k", f(inp["rwkv_k_k"]).reshape(-1))
    put("k_a", f(inp["rwkv_k_a"]).reshape(-1))
    put("r_k", f(inp["rwkv_r_k"]).reshape(-1))
    put("ln_w", f(inp["rwkv_ln_w"]).reshape(-1))
    put("ln_b", f(inp["rwkv_ln_b"]).reshape(-1))
    put("conv_w", f(inp["conv_w"]).reshape(-1))
    mb = col(f(inp["mod_b"]).reshape(-1))
    vec[:, VOFF["modb"]:VOFF["modb"] + 192] = np.repeat(mb, 2, axis=1)
    shared = {
        "vecs": vec,
        "cst": make_consts(),
        "mod_w": f(inp["mod_w"]),
        "w_rkv": f(inp["rwkv_w_rkv"])[0],
        "w1cat": np.ascontiguousarray(np.concatenate([f(inp["rwkv_w1"])[0, 0], f(inp["rwkv_w1"])[0, 1]], axis=1)),
        "w2cat": np.ascontiguousarray(f(inp["rwkv_w2"])[0].reshape(128, D)),
        "a1cat": np.ascontiguousarray(np.concatenate([f(inp["rwkv_a1"])[0, 0], f(inp["rwkv_a1"])[0, 1]], axis=1)),
        "a2cat": np.ascontiguousarray(f(inp["rwkv_a2"])[0].reshape(128, D)),
        "g1": f(inp["rwkv_g1"])[0],
        "g2": f(inp["rwkv_g2"])[0],
        "w_out": f(inp["rwkv_w_out"])[0],
        "conv_w_in": f(inp["conv_w_in"])[0],
        "conv_w_out": f(inp["conv_w_out"])[0],
        "ffn_w_gu": f(inp["ffn_w_gu"])[0],
        "ffn_w_down": f(inp["ffn_w_down"])[0],
        "router": np.ascontiguousarray(f(inp["moe_router"])[0].reshape(KC, 128, NE).transpose(1, 0, 2)),
        "moe_w_gu": f(inp["moe_w_gu"])[0],
        "moe_w_down": f(inp["moe_w_down"])[0],
    }
    x = f(inp["x"])
    ctx = f(inp["ctx"])
    c = f(inp["c"])
    cc = f(inp["c_ctx"])
    maps = []
    for b in range(8):
        m = dict(shared)
        m["xT"] = np.ascontiguousarray(x[b].T.reshape(KC, 128, T).transpose(1, 0, 2))
        m["ctxT"] = np.ascontiguousarray(ctx[b].T.reshape(KC, 128, TC).transpose(1, 0, 2))
        m["cvec"] = np.ascontiguousarray(np.stack([col(c[b]), col(cc)], axis=2))
        maps.append(m)
    return maps


def kernel(**inputs):
    maps = prep_inputs(inputs)
    kb = build()
    res = run_bass_kernel_spmd(kb.nc, maps, core_ids=list(range(8)))
    outs = []
    for b in range(8):
        yT = np.asarray(res.results[b]["yT"], np.float32)
        outs.append(yT.transpose(1, 0, 2).reshape(D, T).T)
    return np.ascontiguousarray(np.stack(outs, 0).astype(np.float32))
```

```python
import contextlib
import numpy as np
import concourse.bass as bass
import concourse.mybir as mybir
from concourse.ap import AP
from concourse.bass_utils import run_bass_kernel_spmd

F32 = mybir.dt.float32
BF16 = mybir.dt.bfloat16
AF = mybir.ActivationFunctionType
ALU = mybir.AluOpType

T = 2048
TC = 256
NT = T + TC
D = 1024
KC = 8
FF = 3584
NE = 8
NCH = NT // 64
NORM_EPS = 1e-6
GN_EPS = 64e-5
DECAY_S = -0.6065306597126334


class Dep:
    __slots__ = ("w", "rs")

    def __init__(self):
        self.w = None
        self.rs = {}


class Eng:
    def __init__(self, name, obj, is_pe=False):
        self.name = name
        self.obj = obj
        self.is_pe = is_pe
        self.sem = None
        self.semid = None
        self.cnt = 0
        self.seen = {}


class Slot:
    def __init__(self, sem, semid):
        self.sem = sem
        self.semid = semid
        self.cnt = 0
        self.last = None


class KB:
    def __init__(self):
        self.nc = bass.Bass("TRN2", target_bir_lowering=False)
        nc = self.nc
        self.es = contextlib.ExitStack()
        self.sems = []
        self.epoch = 0
        self.pe = Eng("pe", nc.tensor, True)
        self.act = Eng("act", nc.scalar)
        self.dve = Eng("dve", nc.vector)
        self.pool = Eng("pool", nc.gpsimd)
        self.sp = Eng("sp", nc.sync)
        self.engs = [self.pe, self.act, self.dve, self.pool, self.sp]
        for e in self.engs:
            self._fresh_sem(e)
        self.slots = {}
        self.slot_i = {}
        for q in (self.sp, self.pool):
            self.slots[q.name] = [Slot(*self._newsem("dq%s%d" % (q.name, i))) for i in range(8)]
            self.slot_i[q.name] = 0
        self.ninstr = 0
        self.uid = 0

    def _newsem(self, name):
        s = self.es.enter_context(self.nc.semaphore("%s_%d" % (name, len(self.sems))))
        self.sems.append(s)
        return s, len(self.sems) - 1

    def _fresh_sem(self, e):
        e.sem, e.semid = self._newsem("e" + e.name)
        e.cnt = 0

    def sb(self, name, shape, dtype, stack=None):
        self.uid += 1
        return (stack or self.es).enter_context(
            self.nc.sbuf_tensor("%s_%d" % (name, self.uid), list(shape), dtype))

    def psum(self, name, shape, dtype, stack=None):
        self.uid += 1
        return (stack or self.es).enter_context(
            self.nc.psum_tensor("%s_%d" % (name, self.uid), list(shape), dtype))

    def _wait(self, eng, tk, war=False):
        semid, val, src, ep = tk
        if ep != self.epoch:
            return
        if src is eng:
            if eng.is_pe or war:
                return
        if eng.seen.get(semid, 0) >= val:
            return
        eng.obj.wait_ge(self.sems[semid], val)
        eng.seen[semid] = val
        self.ninstr += 1

    def _deps(self, eng, r, w):
        for d in r:
            if d.w is not None:
                self._wait(eng, d.w)
        for d in w:
            if d.w is not None:
                self._wait(eng, d.w)
            for t in d.rs.values():
                self._wait(eng, t, war=True)

    def _mark(self, tk, r, w):
        for d in r:
            d.rs[tk[0]] = tk
        for d in w:
            d.w = tk
            d.rs = {}

    def op(self, eng, fn, r=(), w=()):
        self._deps(eng, r, w)
        ins = fn()
        eng.cnt += 1
        ins.then_inc(eng.sem, 1)
        tk = (eng.semid, eng.cnt, eng, self.epoch)
        self._mark(tk, r, w)
        self.ninstr += 1
        return tk

    def dma(self, q, out, in_, r=(), w=(), **kw):
        sl = self.slots[q.name]
        i = self.slot_i[q.name]
        self.slot_i[q.name] = (i + 1) % len(sl)
        s = sl[i]
        if s.last is not None:
            self._wait(q, s.last)
        self._deps(q, r, w)
        ins = q.obj.dma_start(out=out, in_=in_, **kw)
        s.cnt += 16
        ins.then_inc(s.sem, 16)
        tk = (s.semid, s.cnt, None, self.epoch)
        s.last = tk
        self._mark(tk, r, w)
        self.ninstr += 1
        return tk

    def barrier(self):
        tks = []
        for e in self.engs:
            if e.cnt > 0:
                tks.append((e.semid, e.cnt, e, self.epoch))
        for sl in self.slots.values():
            for s in sl:
                if s.last is not None and s.last[3] == self.epoch:
                    tks.append(s.last)
        for e in self.engs:
            for tk in tks:
                semid, val, src, ep = tk
                if e.seen.get(semid, 0) >= val:
                    continue
                e.obj.wait_ge(self.sems[semid], val)
                e.seen[semid] = val
                self.ninstr += 1
        self.epoch += 1
        for e in self.engs:
            self._fresh_sem(e)
            e.seen = {}

    def finish(self):
        self.barrier()
        self.es.close()


VEC_SPEC = [("g", 2 * 4 * 8), ("mu", 6 * 8), ("w0", 16), ("a0", 16), ("k_k", 8), ("k_a", 8), ("r_k", 8),
            ("ln_w", 8), ("ln_b", 8), ("conv_w", 24), ("modb", 192)]
VOFF = {}
_o = 0
for _n, _c in VEC_SPEC:
    VOFF[_n] = _o
    _o += _c
NV = _o
C_ID, C_MF, C_MB, C_LF, C_LB, NCST = 0, 128, 640, 1152, 1280, 1408


def col(v):
    v = np.asarray(v, np.float32).reshape(-1, 128)
    return np.ascontiguousarray(v.T)


def make_consts():
    c = np.zeros((128, NCST), np.float32)
    c[:, C_ID:C_ID + 128] = np.eye(128, dtype=np.float32)
    s = np.arange(64)[:, None]
    t = np.arange(64)[None, :]
    strict_f = (s < t).astype(np.float32)
    incl_f = (s <= t).astype(np.float32)
    strict_b = (s > t).astype(np.float32)
    incl_b = (s >= t).astype(np.float32)
    mf = np.concatenate([strict_f, incl_f], 1)
    mb = np.concatenate([strict_b, incl_b], 1)
    c[:64, C_MF:C_MF + 512] = np.tile(mf, (1, 4))
    c[:64, C_MB:C_MB + 512] = np.tile(mb, (1, 4))
    c[:64, C_LF:C_LF + 128] = np.tile(strict_b, (1, 2))
    c[:64, C_LB:C_LB + 128] = np.tile(strict_f, (1, 2))
    return c


def build(dbg=None, dbgn=0):
    k = KB()
    nc = k.nc
    pe, act, dve, pool, sp = k.pe, k.act, k.dve, k.pool, k.sp

    def din(name, shape, dt=F32):
        return nc.dram_tensor(name, list(shape), dt, kind="ExternalInput").ap()

    xT_d = din("xT", [128, KC, T])
    ctxT_d = din("ctxT", [128, KC, TC])
    cvec_d = din("cvec", [128, KC, 2])
    vec_d = din("vecs", [128, NV])
    cst_d = din("cst", [128, NCST])
    mod_w_d = din("mod_w", [2, D, 6 * D])
    w_rkv_d = din("w_rkv", [3, D, D])
    w1_d = din("w1cat", [D, 128])
    w2_d = din("w2cat", [128, D])
    a1_d = din("a1cat", [D, 128])
    a2_d = din("a2cat", [128, D])
    g1_d = din("g1", [D, 160])
    g2_d = din("g2", [160, D])
    wout_d = din("w_out", [D, D])
    cwin_d = din("conv_w_in", [D, 3 * D])
    cwout_d = din("conv_w_out", [D, D])
    fgu_d = din("ffn_w_gu", [D, 2 * FF])
    fd_d = din("ffn_w_down", [FF, D])
    rt_d = din("router", [128, KC, NE])
    mgu_d = din("moe_w_gu", [NE, D, 2 * FF])
    md_d = din("moe_w_down", [NE, FF, D])
    out_d = nc.dram_tensor("yT", [128, KC, T], F32, kind="ExternalOutput").ap()
    xres_d = nc.dram_tensor("xres", [128, KC, T], F32, kind="Internal").ap()
    oT_d = nc.dram_tensor("oT", [128, KC, T], BF16, kind="Internal").ap()
    rkv_d = nc.dram_tensor("rkvs", [3, 128, KC, NT], F32, kind="Internal").ap()
    if dbg is not None:
        dbg_d = nc.dram_tensor("dbg", [128, dbgn], F32, kind="ExternalOutput").ap()

    def kview(w2d, c0, c1):
        return w2d.rearrange("(k p) n -> p k n", p=128)[:, :, c0:c1]

    vec = k.sb("vec", [128, NV], F32)
    cst = k.sb("cst", [128, NCST], F32)
    ident_bf = k.sb("identb", [128, 128], BF16)
    ones_bf = k.sb("onesb", [128, 128], BF16)
    bd_bf = k.sb("bdb", [128, 128], BF16)
    bdm_f = k.sb("bdmf", [128, 128], F32)
    bd1_f = k.sb("bd1f", [128, 128], F32)
    maskF = k.sb("maskF", [64, 512], BF16)
    maskB = k.sb("maskB", [64, 512], BF16)
    maskLF = k.sb("maskLF", [64, 128], BF16)
    maskLB = k.sb("maskLB", [64, 128], BF16)
    mod = k.sb("mod", [128, 192], F32)
    scal = k.sb("scal", [128, 160], F32)
    rmask = k.sb("rmask", [128, 576], F32)
    d_const = Dep()
    d_mod = Dep()
    d_scal = Dep()

    PS = [k.psum("ps%d" % i, [128, 512], F32) for i in range(7)]
    PSB = k.psum("psb", [128, 1024], BF16)
    dPS = [Dep() for _ in range(7)]
    dPSB = Dep()

    def V(name, i=0, n=8):
        o = VOFF[name] + i * 8
        return vec[:, o:o + n]

    def Vc(name, i, c):
        o = VOFF[name] + i * 8 + c
        return vec[:, o:o + 1]

    S_ = {n: i * 8 for i, n in enumerate(
        ["A1_0", "B1_0", "G1_0", "A2_0", "B2_0", "G2_0", "A1c", "B1c",
         "A1_1", "B1_1", "G1_1", "A2_1", "B2_1", "G2_1", "omka", "hrk"])}
    omu = k.sb("omu", [128, 48], F32)

    def S(name, c=None):
        o = S_[name]
        if c is None:
            return scal[:, o:o + 8]
        return scal[:, o + c:o + c + 1]

    k.dma(sp, vec[:], vec_d[:, :], w=[d_const])
    k.dma(sp, cst[:], cst_d[:, :], w=[d_const])
    k.op(dve, lambda: nc.vector.tensor_copy(ident_bf[:], cst[:, C_ID:C_ID + 128]), r=[d_const], w=[d_const])
    k.op(dve, lambda: nc.vector.tensor_copy(maskF[:], cst[0:64, C_MF:C_MF + 512]), r=[d_const], w=[d_const])
    k.op(dve, lambda: nc.vector.tensor_copy(maskB[:], cst[0:64, C_MB:C_MB + 512]), r=[d_const], w=[d_const])
    k.op(dve, lambda: nc.vector.tensor_copy(maskLF[:], cst[0:64, C_LF:C_LF + 128]), r=[d_const], w=[d_const])
    k.op(dve, lambda: nc.vector.tensor_copy(maskLB[:], cst[0:64, C_LB:C_LB + 128]), r=[d_const], w=[d_const])
    k.op(dve, lambda: nc.vector.memset(ones_bf[:], 1.0), w=[d_const])
    k.op(dve, lambda: nc.vector.memset(bd_bf[:], 0.0), w=[d_const])
    k.op(dve, lambda: nc.vector.memset(bdm_f[:], 0.0), w=[d_const])
    k.op(dve, lambda: nc.vector.memset(bd1_f[:], 0.0), w=[d_const])
    for h in range(2):
        sl = slice(64 * h, 64 * h + 64)
        k.op(dve, lambda sl=sl: nc.vector.memset(bd_bf[sl, sl], 1.0), w=[d_const])
        k.op(dve, lambda sl=sl: nc.vector.memset(bdm_f[sl, sl], 1.0 / 64), w=[d_const])
        k.op(dve, lambda sl=sl: nc.vector.memset(bd1_f[sl, sl], 1.0), w=[d_const])
    k.op(dve, lambda: nc.vector.memset(rmask[:], 1.0), w=[d_const])
    k.op(dve, lambda: nc.vector.memset(rmask[:, 0:576:64], 0.0), w=[d_const])
    k.op(dve, lambda: nc.vector.tensor_scalar(omu[:], V("mu", 0, 48), -1.0, 1.0, ALU.mult, ALU.add),
         r=[d_const], w=[d_const])

    with contextlib.ExitStack() as ph:
        cv = k.sb("cv", [128, KC, 2], F32, ph)
        scb = k.sb("scb", [128, KC, 2], BF16, ph)
        wb = [k.sb("modw%d" % i, [128, KC, 1024], BF16, ph) for i in range(2)]
        dwb = [Dep(), Dep()]
        d_cv = Dep()
        k.dma(sp, cv[:], cvec_d[:, :, :], w=[d_cv])
        k.op(act, lambda: nc.scalar.activation(scb[:], cv[:], AF.Silu), r=[d_cv], w=[d_cv])
        gi = 0
        for i in range(2):
            for g in range(6):
                b = gi % 2
                gi += 1
                k.dma(pool, wb[b][:], kview(mod_w_d[i], g * 1024, (g + 1) * 1024), w=[dwb[b]])
                for m in range(8):
                    mg = g * 8 + m
                    cc = (i * 48 + mg) * 2
                    for kk in range(KC):
                        k.op(pe, lambda b=b, m=m, kk=kk, cc=cc: nc.tensor.matmul(
                            PS[0][:, cc:cc + 2], wb[b][:, kk, m * 128:(m + 1) * 128], scb[:, kk, :],
                            start=(kk == 0), stop=(kk == KC - 1)), r=[dwb[b], d_cv], w=[dPS[0]])
        k.op(dve, lambda: nc.vector.tensor_tensor(mod[:], PS[0][:, 0:192], V("modb", 0, 192), ALU.add),
             r=[dPS[0], d_const], w=[d_mod])

        def mcol(i, s, j):
            o = (i * 48 + s * 8) * 2 + j
            return mod[:, o:o + 16:2]

        def mk_scale(dst, gi_, li, s, j):
            k.op(dve, lambda: nc.vector.tensor_scalar(S(dst), mcol(li, s, j), 1.0, None, ALU.add),
                 r=[d_mod], w=[d_scal])
            k.op(dve, lambda: nc.vector.tensor_tensor(S(dst), S(dst), V("g", li * 4 + gi_), ALU.mult),
                 r=[d_scal, d_const], w=[d_scal])

        def mk_copy(dst, li, s, j):
            k.op(dve, lambda: nc.vector.tensor_copy(S(dst), mcol(li, s, j)), r=[d_mod], w=[d_scal])

        def mk_gate(dst, gi_, li, s):
            k.op(dve, lambda: nc.vector.tensor_tensor(S(dst), mcol(li, s, 0), V("g", li * 4 + gi_), ALU.mult),
                 r=[d_mod, d_const], w=[d_scal])

        for li in range(2):
            mk_scale("A1_%d" % li, 0, li, 1, 0)
            mk_copy("B1_%d" % li, li, 0, 0)
            mk_gate("G1_%d" % li, 1, li, 2)
            mk_scale("A2_%d" % li, 2, li, 4, 0)
            mk_copy("B2_%d" % li, li, 3, 0)
            mk_gate("G2_%d" % li, 3, li, 5)
        mk_scale("A1c", 0, 0, 1, 1)
        mk_copy("B1c", 0, 0, 1)
        k.op(dve, lambda: nc.vector.tensor_scalar(S("omka"), V("k_a"), -1.0, 1.0, ALU.mult, ALU.add),
             r=[d_const], w=[d_scal])
        k.op(dve, lambda: nc.vector.tensor_scalar(S("hrk"), V("r_k"), 0.5, None, ALU.mult),
             r=[d_const], w=[d_scal])
        k.barrier()

    def sumsq_rstd(src_fn, n, scratch_bf, rs, d_src, d_scr, d_rs, psi, eps=NORM_EPS, nchunks=KC):
        for c in range(nchunks):
            k.op(act, lambda c=c: nc.scalar.activation(scratch_bf[:, c, :n], src_fn(c), AF.Square),
                 r=[d_src], w=[d_scr])
        for c in range(nchunks):
            k.op(pe, lambda c=c: nc.tensor.matmul(PS[psi][:, :n], ones_bf[:], scratch_bf[:, c, :n],
                                                  start=(c == 0), stop=(c == nchunks - 1)),
                 r=[d_scr, d_const], w=[dPS[psi]])
        k.op(act, lambda: nc.scalar.activation(rs[:, :n], PS[psi][:, :n], AF.Sqrt, bias=eps, scale=1.0 / D),
             r=[dPS[psi]], w=[d_rs])
        k.op(dve, lambda: nc.vector.reciprocal(rs[:, :n], rs[:, :n]), r=[d_rs], w=[d_rs])

    def norm_mod(src, d_src, n, An, Bn, dst_fn, d_dst, wk, h32=None, d_h32=None):
        sumsq_rstd(lambda c: src[:, c, :n], n, wk["sq"], wk["rs"], d_src, wk["dsq"], wk["drs"], 6)
        for c in range(KC):
            k.op(dve, lambda c=c: nc.vector.tensor_tensor(wk["tmp"][:, c, :n], src[:, c, :n], wk["rs"][:, :n], ALU.mult),
                 r=[d_src, wk["drs"]], w=[wk["dtmp"]])
            k.op(act, lambda c=c: nc.scalar.activation(dst_fn(c), wk["tmp"][:, c, :n], AF.Identity,
                                                       bias=S(Bn, c), scale=S(An, c)),
                 r=[wk["dtmp"], d_scal], w=[d_dst])
            if h32 is not None:
                k.op(pool, lambda c=c: nc.gpsimd.tensor_scalar(h32[:, c, :n], wk["tmp"][:, c, :n], S(An, c), S(Bn, c),
                                                               ALU.mult, ALU.add),
                     r=[wk["dtmp"], d_scal], w=[d_h32])

    def res_norm(y, d_y, n, Gn, xprev, d_xprev, xnew, d_xnew, wk):
        sumsq_rstd(lambda c: y[:, c, :n], n, wk["sq"], wk["rs"], d_y, wk["dsq"], wk["drs"], 6)
        for c in range(KC):
            k.op(pool, lambda c=c: nc.gpsimd.tensor_tensor(wk["tmp"][:, c, :n], y[:, c, :n], wk["rs"][:, :n], ALU.mult),
                 r=[d_y, wk["drs"]], w=[wk["dtmp"]])
            k.op(dve, lambda c=c: nc.vector.scalar_tensor_tensor(xnew[:, c, :n], wk["tmp"][:, c, :n], S(Gn, c),
                                                                 xprev[:, c, :n], ALU.mult, ALU.add),
                 r=[wk["dtmp"], d_scal, d_xprev], w=[d_xnew])

    def mk_wk(ph, n=512):
        return dict(sq=k.sb("wsq", [128, KC, n], BF16, ph), rs=k.sb("wrs", [128, n], F32, ph),
                    tmp=k.sb("wtmp", [128, KC, n], F32, ph), dsq=Dep(), drs=Dep(), dtmp=Dep())

    dbg_done = [False]

    def dump(ap_fn, ncols, d, off=0):
        k.dma(sp, dbg_d[:, off:off + ncols], ap_fn(), r=[d])

    lora_stack = contextlib.ExitStack()
    TW = k.sb("TW", [128, NT], BF16, lora_stack)
    TA = k.sb("TA", [128, NT], BF16, lora_stack)
    SG = k.sb("SG", [128, 2, T], BF16, lora_stack)
    d_TW, d_TA, d_SG = Dep(), Dep(), Dep()
    hT_stack = contextlib.ExitStack()
    hT = k.sb("hT", [128, KC, NT], BF16, hT_stack)
    hsT = k.sb("hsT", [128, KC, NT], BF16, hT_stack)
    d_h = Dep()
    d_hs = Dep()
    with contextlib.ExitStack() as ph:
        wk = mk_wk(ph)
        xb = [k.sb("xb%d" % i, [128, KC, 512], F32, ph) for i in range(2)]
        dxb = [Dep(), Dep()]
        k.dma(sp, xb[0][:, :, 0:TC], ctxT_d[:, :, :], w=[dxb[0]])
        norm_mod(xb[0], dxb[0], TC, "A1c", "B1c", lambda c: hT[:, c, 0:TC], d_h, wk)
        for tb in range(4):
            b = (tb + 1) % 2
            k.dma(sp, xb[b][:], xT_d[:, :, tb * 512:(tb + 1) * 512], w=[dxb[b]])
            norm_mod(xb[b], dxb[b], 512, "A1_0", "B1_0",
                     lambda c, tb=tb: hT[:, c, TC + tb * 512:TC + (tb + 1) * 512], d_h, wk)
        k.op(pool, lambda: nc.gpsimd.memset(hsT[:], 0.0), w=[d_hs])
        L0 = TC
        for c in range(KC):
            eng = act if c % 2 == 0 else pool

            def cp(dst, src, eng=eng):
                if eng is act:
                    k.op(act, lambda: nc.scalar.copy(dst, src), r=[d_h], w=[d_hs])
                else:
                    k.op(pool, lambda: nc.gpsimd.tensor_copy(dst, src), r=[d_h], w=[d_hs])
            if c < 4:
                cp(hsT[:, c, 1:TC], hT[:, c, 0:TC - 1])
            else:
                cp(hsT[:, c, 0:TC - 1], hT[:, c, 1:TC])
            if c in (0, 1):
                cp(hsT[:, c, L0 + 1:L0 + T], hT[:, c, L0:L0 + T - 1])
                k.op(pool, lambda c=c: nc.gpsimd.memset(hsT[:, c, L0:L0 + T:64], 0.0), w=[d_hs])
            elif c in (2, 3):
                cp(hsT[:, c, L0:L0 + T - 1], hT[:, c, L0 + 1:L0 + T])
                k.op(pool, lambda c=c: nc.gpsimd.memset(hsT[:, c, L0 + 63:L0 + T:64], 0.0), w=[d_hs])
            elif c in (4, 5):
                cp(hsT[:, c, L0 + 64:L0 + T], hT[:, c, L0:L0 + T - 64])
            else:
                cp(hsT[:, c, L0:L0 + T - 64], hT[:, c, L0 + 64:L0 + T])
        k.barrier()

    if dbg == 1:
        with contextlib.ExitStack() as ph:
            t32 = k.sb("t32", [128, 2 * NT], F32, ph)
            dd = Dep()
            k.op(dve, lambda: nc.vector.tensor_copy(t32[:, 0:NT], hT[:, 0, :]), r=[d_h], w=[dd])
            k.op(dve, lambda: nc.vector.tensor_copy(t32[:, NT:2 * NT], hsT[:, 5, :]), r=[d_hs], w=[dd])
            dump(lambda: t32[:], 2 * NT, dd)
            k.barrier()
        hT_stack.close()
        lora_stack.close()
        k.finish()
        return k


    def load_mixed(ph_, dram_view, ncols, mu_i, name):
        st = k.sb(name + "st", [128, KC, ncols], F32, ph_)
        Wa = k.sb(name + "a", [128, KC, ncols], BF16, ph_)
        Wb = k.sb(name + "b", [128, KC, ncols], BF16, ph_)
        dst_, dw = Dep(), Dep()
        k.dma(sp, st[:], dram_view, w=[dst_])
        for kk in range(KC):
            o = mu_i * 8 + kk
            k.op(dve, lambda kk=kk, o=o: nc.vector.tensor_scalar(Wa[:, kk, :], st[:, kk, :], omu[:, o:o + 1], None, ALU.mult),
                 r=[dst_, d_const], w=[dw])
            k.op(pool, lambda kk=kk, o=o: nc.gpsimd.tensor_scalar(Wb[:, kk, :], st[:, kk, :], V("mu", 0, 48)[:, o:o + 1], None, ALU.mult),
                 r=[dst_, d_const], w=[dw])
        return Wa, Wb, dw

    def proj_mixed(psi, Wa, Wb, dw, c0, c1, t0, n, mrows=128):
        for kk in range(KC):
            k.op(pe, lambda kk=kk: nc.tensor.matmul(PS[psi][:mrows, :n], Wa[:, kk, c0:c1], hT[:, kk, t0:t0 + n],
                                                    start=(kk == 0), stop=False),
                 r=[dw, d_h], w=[dPS[psi]])
        for kk in range(KC):
            k.op(pe, lambda kk=kk: nc.tensor.matmul(PS[psi][:mrows, :n], Wb[:, kk, c0:c1], hsT[:, kk, t0:t0 + n],
                                                    start=False, stop=(kk == KC - 1)),
                 r=[dw, d_hs], w=[dPS[psi]])

    with contextlib.ExitStack() as ph:
        W1a, W1b, dW1 = load_mixed(ph, kview(w1_d, 0, 128), 128, 1, "w1")
        A1a, A1b, dA1 = load_mixed(ph, kview(a1_d, 0, 128), 128, 4, "a1")
        G1a, G1b, dG1 = load_mixed(ph, kview(g1_d, 0, 160), 160, 5, "g1")
        pi = 0
        for tb in range(NT // 256):
            t0 = tb * 256
            proj_mixed(pi % 4, W1a, W1b, dW1, 0, 128, t0, 256)
            k.op(act, lambda p=pi % 4, t0=t0: nc.scalar.activation(TW[:, t0:t0 + 256], PS[p][:, :256], AF.Tanh),
                 r=[dPS[pi % 4]], w=[d_TW])
            pi += 1
            proj_mixed(pi % 4, A1a, A1b, dA1, 0, 128, t0, 256)
            k.op(dve, lambda p=pi % 4, t0=t0: nc.vector.tensor_copy(TA[:, t0:t0 + 256], PS[p][:, :256]),
                 r=[dPS[pi % 4]], w=[d_TA])
            pi += 1
            if t0 >= TC:
                l0 = t0 - TC
                proj_mixed(pi % 4, G1a, G1b, dG1, 0, 128, t0, 256)
                k.op(act, lambda p=pi % 4, l0=l0: nc.scalar.activation(SG[:, 0, l0:l0 + 256], PS[p][:, :256], AF.Sigmoid),
                     r=[dPS[pi % 4]], w=[d_SG])
                pi += 1
                proj_mixed(pi % 4, G1a, G1b, dG1, 128, 160, t0, 256, mrows=32)
                k.op(act, lambda p=pi % 4, l0=l0: nc.scalar.activation(SG[0:32, 1, l0:l0 + 256], PS[p][0:32, :256], AF.Sigmoid),
                     r=[dPS[pi % 4]], w=[d_SG])
                pi += 1
        k.barrier()

    with contextlib.ExitStack() as ph:
        st = k.sb("rkvst", [128, KC, D], F32, ph)
        Wa = k.sb("rkvWa", [128, KC, D], BF16, ph)
        Wb = k.sb("rkvWb", [128, KC, D], BF16, ph)
        rowb = [k.sb("rowb%d" % i, [128, NT], F32, ph) for i in range(2)]
        d_st, d_Wab = Dep(), Dep()
        d_row = [Dep(), Dep()]
        mu_of = [0, 2, 3]
        ri = 0
        pi = 0
        for j in range(3):
            k.dma(sp, st[:], kview(w_rkv_d[j], 0, D), w=[d_st])
            for kk in range(KC):
                o = mu_of[j] * 8 + kk
                k.op(dve, lambda kk=kk, o=o: nc.vector.tensor_scalar(Wa[:, kk, :], st[:, kk, :], omu[:, o:o + 1], None, ALU.mult),
                     r=[d_st, d_const], w=[d_Wab])
                k.op(pool, lambda kk=kk, o=o: nc.gpsimd.tensor_scalar(Wb[:, kk, :], st[:, kk, :], V("mu", 0, 48)[:, o:o + 1], None, ALU.mult),
                     r=[d_st, d_const], w=[d_Wab])
            for c in range(KC):
                rb = rowb[ri % 2]
                drb = d_row[ri % 2]
                ri += 1
                for tb in range(NT // 256):
                    t0 = tb * 256
                    p = pi % 4
                    pi += 1
                    proj_mixed(p, Wa, Wb, d_Wab, c * 128, (c + 1) * 128, t0, 256)
                    if tb % 2 == 0:
                        k.op(act, lambda p=p, rb=rb, t0=t0: nc.scalar.copy(rb[:, t0:t0 + 256], PS[p][:, :256]), r=[dPS[p]], w=[drb])
                    else:
                        k.op(dve, lambda p=p, rb=rb, t0=t0: nc.vector.tensor_copy(rb[:, t0:t0 + 256], PS[p][:, :256]), r=[dPS[p]], w=[drb])
                k.dma(sp, rkv_d[j, :, c, :], rb[:], r=[drb])
        k.barrier()
    hT_stack.close()

    def bc3(t2d, col0, nouter, ostride, ninner):
        base = t2d[:, col0:col0 + 1]
        pst = base.ap[0][0]
        return AP(base.tensor, base.offset, [[pst, 128], [ostride, nouter], [0, ninner]])

    BL = 576
    NBL = NT // BL
    CPB = BL // 64
    GI = 4
    NG = NCH // GI
    with contextlib.ExitStack() as ph:
        r32 = k.sb("r32", [128, NT], F32, ph)
        k32 = k.sb("k32", [128, NT], F32, ph)
        kk32 = k.sb("kk32", [128, NT], F32, ph)
        v16 = k.sb("v16", [128, NT], BF16, ph)
        ksum = k.sb("ksum", [128, NT], F32, ph)
        yz = [k.sb("yz%d" % z, [128, T], F32, ph) for z in range(2)]
        d_yz = [Dep(), Dep()]
        sgw = k.sb("sgw", [128, BL], F32, ph)
        cs = k.sb("cs", [128, BL], F32, ph)
        cs2 = k.sb("cs2", [128, BL], F32, ph)
        iclr = k.sb("iclr", [128, BL], F32, ph)
        t2 = k.sb("t2", [128, BL], F32, ph)
        t3 = k.sb("t3", [128, BL], F32, ph)
        sqb = k.sb("sqb", [128, 512], BF16, ph)
        ost = [k.sb("ost%d" % i, [128, T], BF16, ph) for i in range(1)]
        d_ost = [Dep()]
        w2c = k.sb("w2c", [128, 128], BF16, ph)
        a2c = k.sb("a2c", [128, 128], BF16, ph)
        g2c = k.sb("g2c", [128, 2, 128], BF16, ph)
        d_w2, d_a2, d_g2 = Dep(), Dep(), Dep()
        d_r, d_k, d_kk, d_v, d_ks = Dep(), Dep(), Dep(), Dep(), Dep()
        d_sgw, d_cs, d_cs2, d_iclr, d_t2, d_t3, d_sq = (Dep() for _ in range(7))
        DB = []
        for z in range(2):
            b_ = dict(
                ARbd=k.sb("ARbd%d" % z, [128, NCH, 2, 128], BF16, ph),
                bt=k.sb("bt%d" % z, [128, NT], BF16, ph), kt=k.sb("kt%d" % z, [128, NT], BF16, ph),
                WC=k.sb("WC%d" % z, [128, NCH], F32, ph),
                M32=k.sb("M32%d" % z, [128, 128], F32, ph), S16=k.sb("S16%d" % z, [128, 128], BF16, ph),
                X16=k.sb("X16%d" % z, [64, 128], BF16, ph), U16=k.sb("U16%d" % z, [64, 128], BF16, ph),
                NB=k.sb("NB%d" % z, [64, GI * 128], BF16, ph), LB=k.sb("LB%d" % z, [64, GI * 128], BF16, ph),
                XB=k.sb("XB%d" % z, [64, GI * 128], BF16, ph),
                L1=[k.sb("L1_%d%d" % (z, i), [64, GI, 512], BF16, ph) for i in range(2)],
                TT=[k.sb("TT%d%d" % (z, i), [64, GI, 128], BF16, ph) for i in range(2)],
                tm=[k.sb("tm%d%d" % (z, i), [64, GI, 384], BF16, ph) for i in range(2)],
                d_AR=Dep(), d_bt=Dep(), d_kt=Dep(), d_WC=Dep(), d_M=Dep(), d_S=Dep(), d_X=Dep(), d_U=Dep(),
                d_NB=Dep(), d_LB=Dep(), d_XB=Dep(), d_L1=[Dep(), Dep()], d_TT=[Dep(), Dep()], d_tm=[Dep(), Dep()],
                d_pX=Dep(), d_pU=Dep(), d_pS=Dep(), d_pY=Dep(), prev=None)
            DB.append(b_)
            k.op(pool, lambda b_=b_: nc.gpsimd.memset(b_["ARbd"][:], 0.0), w=[b_["d_AR"]])
        k.min_free = min(getattr(k, "min_free", 1 << 30), nc.sbuf_bytes_remaining)

        for c in range(KC):
            cs0, cs1 = c * 128, (c + 1) * 128
            k.dma(sp, r32[:], rkv_d[0, :, c, :], w=[d_r])
            k.dma(sp, k32[:], rkv_d[1, :, c, :], w=[d_k])
            k.dma(pool, v16[:], rkv_d[2, :, c, :], w=[d_v])
            k.dma(pool, w2c[:], w2_d[:, cs0:cs1], w=[d_w2])
            k.dma(pool, a2c[:], a2_d[:, cs0:cs1], w=[d_a2])
            k.dma(pool, g2c[:, 0, :], g2_d[0:128, cs0:cs1], w=[d_g2])
            k.dma(pool, g2c[0:32, 1, :], g2_d[128:160, cs0:cs1], w=[d_g2])
            k.op(dve, lambda: nc.vector.tensor_scalar(kk32[:], k32[:], Vc("k_k", 0, c), None, ALU.mult),
                 r=[d_k, d_const], w=[d_kk])
            for q in range(0, NT, 512):
                n = min(512, NT - q)
                p = 4 + (q // 512) % 2
                k.op(act, lambda q=q, n=n: nc.scalar.activation(sqb[:, :n], kk32[:, q:q + n], AF.Square),
                     r=[d_kk], w=[d_sq])
                k.op(pe, lambda q=q, n=n, p=p: nc.tensor.matmul(PS[p][:, :n], bd_bf[:], sqb[:, :n], start=True, stop=True),
                     r=[d_sq, d_const], w=[dPS[p]])
                k.op(dve, lambda q=q, n=n, p=p: nc.vector.tensor_scalar(t2[:, :n], PS[p][:, :n], 1e-24, None, ALU.max),
                     r=[dPS[p]], w=[d_t2])
                k.op(act, lambda n=n: nc.scalar.activation(t2[:, :n], t2[:, :n], AF.Sqrt), r=[d_t2], w=[d_t2])
                k.op(dve, lambda n=n: nc.vector.reciprocal(t2[:, :n], t2[:, :n]), r=[d_t2], w=[d_t2])
                k.op(dve, lambda q=q, n=n: nc.vector.tensor_tensor(kk32[:, q:q + n], kk32[:, q:q + n], t2[:, :n], ALU.mult),
                     r=[d_kk, d_t2], w=[d_kk])

            for z in range(2):
                B_ = DB[z]
                ARbd, bt, kt, WC = B_["ARbd"], B_["bt"], B_["kt"], B_["WC"]
                d_AR, d_bt, d_kt, d_WC = B_["d_AR"], B_["d_bt"], B_["d_kt"], B_["d_WC"]
                zs = slice(64 * z, 64 * z + 64)
                for blk in range(NBL):
                    q0 = blk * BL
                    qs = slice(q0, q0 + BL)
                    nb0 = blk * CPB
                    for sbk in range(2):
                        t0 = q0 + sbk * 288
                        o0 = sbk * 288
                        p = 2 + sbk
                        k.op(pe, lambda p=p, t0=t0, zs=zs: nc.tensor.matmul(PS[p][:, :288], w2c[zs, :], TW[zs, t0:t0 + 288],
                                                                           start=True, stop=True), r=[d_w2, d_TW], w=[dPS[p]])
                        k.op(act, lambda p=p, o0=o0, z=z: nc.scalar.activation(sgw[:, o0:o0 + 288], PS[p][:, :288], AF.Sigmoid,
                                                                               bias=Vc("w0", z, c)), r=[dPS[p], d_const], w=[d_sgw])
                        p = 4 + sbk
                        k.op(pe, lambda p=p, t0=t0, zs=zs: nc.tensor.matmul(PS[p][:, :288], a2c[zs, :], TA[zs, t0:t0 + 288],
                                                                           start=True, stop=True), r=[d_a2, d_TA], w=[dPS[p]])
                        k.op(act, lambda p=p, o0=o0, z=z: nc.scalar.activation(iclr[:, o0:o0 + 288], PS[p][:, :288], AF.Sigmoid,
                                                                               bias=Vc("a0", z, c)), r=[dPS[p], d_const], w=[d_iclr])
                    k.op(dve, lambda: nc.vector.tensor_tensor_scan(cs[:], rmask[:, 0:BL], sgw[:], 0.0, ALU.mult, ALU.add),
                         r=[d_sgw, d_const], w=[d_cs])
                    if z == 1:
                        k.op(dve, lambda: nc.vector.tensor_tensor(cs2[:], sgw[:], cs[:], ALU.subtract),
                             r=[d_sgw, d_cs], w=[d_cs2])
                        k.op(dve, lambda: nc.vector.tensor_tensor(
                            cs2[:].rearrange("p (n t) -> p n t", t=64), cs2[:].rearrange("p (n t) -> p n t", t=64),
                            bc3(cs, 63, CPB, 64, 64), ALU.add), r=[d_cs2, d_cs], w=[d_cs2])
                        csu, d_csu = cs2, d_cs2
                    else:
                        csu, d_csu = cs, d_cs
                    k.op(pool, lambda: nc.gpsimd.tensor_scalar(t2[:], iclr[:], Vc("k_a", 0, c), S("omka", c), ALU.mult, ALU.add),
                         r=[d_iclr, d_const, d_scal], w=[d_t2])
                    k.op(pool, lambda qs=qs: nc.gpsimd.tensor_tensor(t2[:], t2[:], k32[:, qs], ALU.mult), r=[d_t2, d_k], w=[d_t2])
                    if z == 0:
                        k.op(pool, lambda qs=qs: nc.gpsimd.tensor_copy(ksum[:, qs], t2[:]), r=[d_t2], w=[d_ks])
                    else:
                        k.op(pool, lambda qs=qs: nc.gpsimd.tensor_tensor(ksum[:, qs], ksum[:, qs], t2[:], ALU.add), r=[d_t2, d_ks], w=[d_ks])
                    k.op(act, lambda csu=csu: nc.scalar.activation(t3[:], csu[:], AF.Exp, scale=-DECAY_S), r=[d_csu], w=[d_t3])
                    k.op(dve, lambda qs=qs, kt=kt: nc.vector.tensor_tensor(kt[:, qs], t2[:], t3[:], ALU.mult), r=[d_t2, d_t3], w=[d_kt])
                    k.op(pool, lambda qs=qs: nc.gpsimd.tensor_tensor(t2[:], kk32[:, qs], iclr[:], ALU.mult), r=[d_kk, d_iclr, d_kt], w=[d_t2])
                    k.op(dve, lambda qs=qs, bt=bt: nc.vector.tensor_tensor(bt[:, qs], t2[:], t3[:], ALU.mult), r=[d_t2, d_t3], w=[d_bt])
                    k.op(act, lambda csu=csu: nc.scalar.activation(t3[:], csu[:], AF.Exp, scale=DECAY_S), r=[d_csu, d_bt], w=[d_t3])
                    wc_col = 63 if z == 0 else 0
                    k.op(pool, lambda nb0=nb0, wc_col=wc_col, WC=WC: nc.gpsimd.tensor_copy(WC[:, nb0:nb0 + CPB], t3[:, wc_col:BL:64]),
                         r=[d_t3], w=[d_WC])
                    for h in range(2):
                        hs_ = slice(64 * h, 64 * h + 64)
                        k.op(dve, lambda h=h, hs_=hs_, qs=qs, nb0=nb0, ARbd=ARbd: nc.vector.tensor_tensor(
                            ARbd[hs_, nb0:nb0 + CPB, h, 64:128], r32[hs_, qs].rearrange("p (n t) -> p n t", t=64),
                            t3[hs_, :].rearrange("p (n t) -> p n t", t=64), ALU.mult), r=[d_r, d_t3], w=[d_AR])
                    k.op(pool, lambda csu=csu: nc.gpsimd.tensor_tensor(t2[:], csu[:], sgw[:], ALU.subtract), r=[d_csu, d_sgw, d_bt], w=[d_t2])
                    k.op(act, lambda: nc.scalar.activation(t2[:], t2[:], AF.Exp, scale=DECAY_S), r=[d_t2], w=[d_t2])
                    for h in range(2):
                        hs_ = slice(64 * h, 64 * h + 64)
                        k.op(dve, lambda h=h, hs_=hs_, qs=qs, nb0=nb0, ARbd=ARbd: nc.vector.scalar_tensor_tensor(
                            ARbd[hs_, nb0:nb0 + CPB, h, 0:64], kk32[hs_, qs].rearrange("p (n t) -> p n t", t=64), -1.0,
                            t2[hs_, :].rearrange("p (n t) -> p n t", t=64), ALU.mult, ALU.mult), r=[d_kk, d_t2], w=[d_AR])

            def pre_stages(z, gb, chunks):
                B_ = DB[z]
                ARbd, bt, kt = B_["ARbd"], B_["bt"], B_["kt"]
                NBg, LBg, XBg = B_["NB"], B_["LB"], B_["XB"]
                L1g, TTg, tmg = B_["L1"][gb], B_["TT"][gb], B_["tm"][gb]
                d_AR, d_bt, d_kt = B_["d_AR"], B_["d_bt"], B_["d_kt"]
                d_NB, d_LB, d_XB = B_["d_NB"], B_["d_LB"], B_["d_XB"]
                d_L1, d_TT, d_tm = B_["d_L1"][gb], B_["d_TT"][gb], B_["d_tm"][gb]
                mk = maskF if z == 0 else maskB
                mkL = maskLF if z == 0 else maskLB
                G = len(chunks)
                bA, bN, bL = (2, 3, 4) if z == 0 else (5, 6, 4)
                stages = []

                def stageA(lo, hi, last):
                    for gi in range(lo, hi):
                        n = chunks[gi]
                        t0 = n * 64
                        k.op(pe, lambda t0=t0: nc.tensor.transpose(PSB[0:64, 0:128], bt[:, t0:t0 + 64], ident_bf[:]),
                             r=[d_bt, d_const], w=[dPSB])
                        k.op(pe, lambda t0=t0: nc.tensor.transpose(PSB[0:64, 128:256], kt[:, t0:t0 + 64], ident_bf[:]),
                             r=[d_kt, d_const], w=[dPSB])
                        k.op(pe, lambda t0=t0: nc.tensor.transpose(PSB[0:64, 256:384], v16[:, t0:t0 + 64], ident_bf[:]),
                             r=[d_v, d_const], w=[dPSB])
                        k.op(act, lambda gi=gi: nc.scalar.copy(tmg[:, gi, :], PSB[0:64, 0:384]), r=[dPSB], w=[d_tm])
                        k.op(pe, lambda n=n, t0=t0: nc.tensor.matmul(
                            PS[bA][0:64, 0:256], bt[:, t0:t0 + 64], ARbd[:, n, :, :].rearrange("p h x -> p (h x)"),
                            start=True, stop=True), r=[d_bt, d_AR], w=[dPS[bA]])
                        k.op(pe, lambda n=n, t0=t0: nc.tensor.matmul(
                            PS[bA][0:64, 256:512], kt[:, t0:t0 + 64], ARbd[:, n, :, :].rearrange("p h x -> p (h x)"),
                            start=True, stop=True), r=[d_kt, d_AR], w=[dPS[bA]])
                        k.op(dve, lambda gi=gi: nc.vector.tensor_tensor(L1g[:, gi, :], PS[bA][0:64, :], mk[:], ALU.mult),
                             r=[dPS[bA], d_const], w=[d_L1])
                        for h in range(2):
                            k.op(pe, lambda n=n, t0=t0, h=h, gi=gi: nc.tensor.matmul(
                                PS[bN][0:64, gi * 128 + h * 64: gi * 128 + h * 64 + 64],
                                ARbd[:, n, h, 0:64], bt[:, t0:t0 + 64], start=True, stop=True),
                                r=[d_AR, d_bt], w=[dPS[bN]])
                    if last:
                        k.op(dve, lambda: nc.vector.tensor_tensor(
                            LBg[:, 0:G * 128].rearrange("p (g x) -> p g x", x=128),
                            PS[bN][0:64, 0:G * 128].rearrange("p (g x) -> p g x", x=128),
                            mkL[:, :].unsqueeze(1).to_broadcast([64, G, 128]), ALU.mult), r=[dPS[bN], d_const], w=[d_LB])
                        k.op(pool, lambda: nc.gpsimd.tensor_copy(
                            NBg[:, 0:G * 128].rearrange("p (g h x) -> p g h x", h=2, x=64),
                            L1g[:, 0:G, 0:256].rearrange("p g (h x) -> p g h x", h=2)[:, :, :, 0:64]),
                            r=[d_L1], w=[d_NB])
                        k.op(pool, lambda: nc.gpsimd.tensor_tensor(
                            XBg[:, 0:G * 128].rearrange("p (g x) -> p g x", x=64), NBg[:, 0:G * 128].rearrange("p (g x) -> p g x", x=64),
                            ident_bf[0:64, 0:64].unsqueeze(1).to_broadcast([64, 2 * G, 64]), ALU.add),
                            r=[d_NB, d_const], w=[d_XB])

                def stageC():
                    for q in range(2 * G):
                        qq = slice(q * 64, q * 64 + 64)
                        k.op(pe, lambda qq=qq: nc.tensor.matmul(PS[bN][0:64, qq], LBg[:, qq], NBg[:, qq], start=True, stop=True),
                             r=[d_LB, d_NB], w=[dPS[bN]])
                    for q in range(2 * G):
                        qq = slice(q * 64, q * 64 + 64)
                        k.op(pe, lambda qq=qq: nc.tensor.matmul(PS[bL][0:64, qq], NBg[:, qq], LBg[:, qq], start=True, stop=True),
                             r=[d_LB, d_NB], w=[dPS[bL]])
                    k.op(act, lambda: nc.scalar.copy(NBg[:, 0:G * 128], PS[bN][0:64, 0:G * 128]), r=[dPS[bN]], w=[d_NB])
                    k.op(dve, lambda: nc.vector.tensor_copy(LBg[:, 0:G * 128], PS[bL][0:64, 0:G * 128]), r=[dPS[bL]], w=[d_LB])

                def stageD(final):
                    for q in range(2 * G):
                        qq = slice(q * 64, q * 64 + 64)
                        k.op(pe, lambda qq=qq: nc.tensor.matmul(PS[bA][0:64, qq], LBg[:, qq], XBg[:, qq], start=True, stop=False),
                             r=[d_LB, d_XB], w=[dPS[bA]])
                        k.op(pe, lambda qq=qq: nc.tensor.matmul(PS[bA][0:64, qq], ident_bf[0:64, 0:64], XBg[:, qq], start=False, stop=True),
                             r=[d_const, d_XB], w=[dPS[bA]])
                    if not final:
                        k.op(act, lambda: nc.scalar.copy(XBg[:, 0:G * 128], PS[bA][0:64, 0:G * 128]), r=[dPS[bA]], w=[d_XB])
                    else:
                        k.op(act, lambda: nc.scalar.copy(
                            TTg[:, 0:G, :].rearrange("p g x -> p (g x)"), PS[bA][0:64, 0:G * 128]),
                            r=[dPS[bA]], w=[d_TT])

                stages.append(lambda: stageA(0, G // 2, False))
                stages.append(lambda: stageA(G // 2, G, True))
                for step in range(5):
                    stages.append(stageC)
                    stages.append(lambda step=step: stageD(step == 4))
                return stages

            def chain_seg(z, gb, gi, n, seg):
                B_ = DB[z]
                ARbd, WC, M32, S16, X16, U16 = B_["ARbd"], B_["WC"], B_["M32"], B_["S16"], B_["X16"], B_["U16"]
                L1g, TTg, tmg = B_["L1"][gb], B_["TT"][gb], B_["tm"][gb]
                d_AR, d_WC, d_M, d_S, d_X, d_U = B_["d_AR"], B_["d_WC"], B_["d_M"], B_["d_S"], B_["d_X"], B_["d_U"]
                d_L1, d_TT, d_tm = B_["d_L1"][gb], B_["d_TT"][gb], B_["d_tm"][gb]
                bk = PS[z]
                dbk = dPS[z]
                xo, uo, so, yo = 0, 128, 256, 384
                pX = bk[0:64, xo:xo + 128]
                pU = bk[0:64, uo:uo + 128]
                t0 = n * 64
                if seg == 0:
                    for h in range(2):
                        k.op(pe, lambda h=h: nc.tensor.matmul(pX, ARbd[:, n, h, 0:64], S16[:], start=(h == 0), stop=False),
                             r=[d_AR, d_S], w=[dbk])
                    for h in range(2):
                        k.op(pe, lambda h=h: nc.tensor.matmul(
                            bk[0:64, xo + h * 64:xo + h * 64 + 64], L1g[:, gi, 256 + h * 128:256 + h * 128 + 64],
                            tmg[:, gi, 256 + h * 64:256 + h * 64 + 64], start=False, stop=(h == 1)),
                            r=[d_L1, d_tm], w=[dbk])
                    k.op(act, lambda: nc.scalar.copy(X16[:], pX), w=[d_X, dbk])
                elif seg == 1:
                    for h in range(2):
                        k.op(pe, lambda h=h: nc.tensor.matmul(
                            bk[0:64, uo + h * 64:uo + h * 64 + 64], TTg[:, gi, h * 64:h * 64 + 64], X16[:, h * 64:h * 64 + 64],
                            start=True, stop=True), r=[d_TT, d_X], w=[dbk])
                    k.op(dve, lambda: nc.vector.tensor_copy(U16[:], pU), w=[d_U, dbk])
                else:
                    if n >= 4:
                        l0 = t0 - TC
                        k.op(pe, lambda: nc.tensor.matmul(
                            bk[:, yo:yo + 128], S16[:], ARbd[:, n, :, 64:128], start=True, stop=False),
                            r=[d_S, d_AR], w=[dbk])
                        for h in range(2):
                            hs_ = slice(64 * h, 64 * h + 64)
                            k.op(pe, lambda h=h, hs_=hs_: nc.tensor.matmul(
                                bk[hs_, yo + h * 64:yo + h * 64 + 64], U16[:, h * 64:h * 64 + 64],
                                L1g[:, gi, h * 128 + 64:h * 128 + 128], start=False, stop=False),
                                r=[d_U, d_L1], w=[dbk])
                            k.op(pe, lambda h=h, hs_=hs_: nc.tensor.matmul(
                                bk[hs_, yo + h * 64:yo + h * 64 + 64], tmg[:, gi, 256 + h * 64:256 + h * 64 + 64],
                                L1g[:, gi, 256 + h * 128 + 64:256 + h * 128 + 128], start=False, stop=(h == 1)),
                                r=[d_tm, d_L1], w=[dbk])
                    k.op(pe, lambda: nc.tensor.matmul(bk[:, so:so + 128], tmg[:, gi, 0:128], U16[:], start=True, stop=False),
                         r=[d_tm, d_U], w=[dbk])
                    k.op(pe, lambda: nc.tensor.matmul(bk[:, so:so + 128], tmg[:, gi, 128:256], tmg[:, gi, 256:384],
                                                      start=False, stop=True), r=[d_tm], w=[dbk])
                    prev = B_["prev"]
                    for h in range(2):
                        hs_ = slice(64 * h, 64 * h + 64)
                        pSd = bk[hs_, so + h * 64:so + h * 64 + 64]
                        if prev is None:
                            k.op(dve, lambda hs_=hs_, pSd=pSd: nc.vector.tensor_copy(M32[hs_, hs_], pSd),
                                 w=[d_M, dbk])
                        else:
                            k.op(dve, lambda hs_=hs_, pSd=pSd, prev=prev: nc.vector.scalar_tensor_tensor(
                                M32[hs_, hs_], M32[hs_, hs_], WC[hs_, prev:prev + 1], pSd, ALU.mult, ALU.add),
                                r=[d_WC], w=[d_M, dbk])
                        k.op(act, lambda hs_=hs_: nc.scalar.activation(
                            S16[hs_, hs_], M32[hs_, hs_], AF.Identity, scale=WC[hs_, n:n + 1]), r=[d_M, d_WC], w=[d_S])
                    if n >= 4:
                        for h in range(2):
                            hs_ = slice(64 * h, 64 * h + 64)
                            k.op(dve, lambda h=h, hs_=hs_: nc.vector.tensor_copy(
                                yz[z][hs_, l0:l0 + 64], bk[hs_, yo + h * 64:yo + h * 64 + 64]), w=[d_yz[z], dbk])
                    B_["prev"] = n

            orders = [list(range(NCH)), [3, 2, 1, 0] + list(range(NCH - 1, 3, -1))]
            groups = [[o[i:i + GI] for i in range(0, NCH, GI)] for o in orders]
            for z in range(2):
                B_ = DB[z]
                B_["prev"] = None
                k.op(dve, lambda B_=B_: nc.vector.memset(B_["M32"][:], 0.0), w=[B_["d_M"]])
                k.op(dve, lambda B_=B_: nc.vector.memset(B_["S16"][:], 0.0), w=[B_["d_S"]])
            pro = [pre_stages(z, 0, groups[z][0]) for z in range(2)]
            for si in range(len(pro[0])):
                for z in range(2):
                    pro[z][si]()
            for gidx in range(NG):
                nxt = [pre_stages(z, (gidx + 1) % 2, groups[z][gidx + 1]) if gidx + 1 < NG else [] for z in range(2)]
                slot = 0
                for gi in range(GI):
                    for seg in range(3):
                        for z in range(2):
                            if slot < len(nxt[z]):
                                nxt[z][slot]()
                            chain_seg(z, gidx % 2, gi, groups[z][gidx][gi], seg)
                        slot += 1

            if dbg == 2 and c == 0:
                dump(lambda: yz[0][:], T, d_yz[0], 0)
                dump(lambda: yz[1][:], T, d_yz[1], T)
                dump(lambda: kk32[:, TC:NT], T, d_kk, 2 * T)
                dump(lambda: ksum[:, TC:NT], T, d_ks, 3 * T)
                k.barrier()
                ph.close()
                lora_stack.close()
                k.finish()
                return k

            ob = ost[0]
            dob = d_ost[0]
            tA, tB, d_tA, d_tB = t2, t3, d_t2, d_t3
            for q in range(0, T, 512):
                qs = slice(q, q + 512)
                qn = slice(TC + q, TC + q + 512)
                W5 = slice(0, 512)
                k.op(pool, lambda qs=qs: nc.gpsimd.tensor_tensor(cs[:, W5], yz[0][:, qs], yz[1][:, qs], ALU.add),
                     r=[d_yz[0], d_yz[1]], w=[d_cs])
                k.op(pe, lambda: nc.tensor.matmul(PS[2][:, :], bdm_f[:], cs[:, W5], start=True, stop=True),
                     r=[d_const, d_cs], w=[dPS[2]])
                k.op(dve, lambda: nc.vector.tensor_tensor(tA[:, W5], cs[:, W5], PS[2][:, :], ALU.subtract),
                     r=[d_cs, dPS[2]], w=[d_tA])
                k.op(pool, lambda: nc.gpsimd.tensor_tensor(tB[:, W5], tA[:, W5], tA[:, W5], ALU.mult),
                     r=[d_tA], w=[d_tB])
                k.op(pe, lambda: nc.tensor.matmul(PS[3][:, :], bdm_f[:], tB[:, W5], start=True, stop=True),
                     r=[d_const, d_tB], w=[dPS[3]])
                k.op(act, lambda: nc.scalar.activation(tB[:, W5], PS[3][:, :], AF.Sqrt, bias=GN_EPS, scale=1.0),
                     r=[dPS[3]], w=[d_tB])
                k.op(dve, lambda: nc.vector.reciprocal(tB[:, W5], tB[:, W5]), r=[d_tB], w=[d_tB])
                k.op(dve, lambda: nc.vector.tensor_tensor(tA[:, W5], tA[:, W5], tB[:, W5], ALU.mult),
                     r=[d_tA, d_tB], w=[d_tA])
                k.op(act, lambda: nc.scalar.activation(tA[:, W5], tA[:, W5], AF.Identity,
                                                       bias=Vc("ln_b", 0, c), scale=Vc("ln_w", 0, c)),
                     r=[d_tA, d_const], w=[d_tA])
                k.op(dve, lambda qn=qn: nc.vector.scalar_tensor_tensor(
                    tB[:, W5], r32[:, qn], S("hrk", c), ksum[:, qn], ALU.mult, ALU.mult),
                    r=[d_r, d_ks, d_scal, d_tB], w=[d_tB])
                k.op(pe, lambda: nc.tensor.matmul(PS[4][:, :], bd1_f[:], tB[:, W5], start=True, stop=True),
                     r=[d_const, d_tB], w=[dPS[4]])
                k.op(dve, lambda qn=qn: nc.vector.tensor_tensor(tB[:, W5], PS[4][:, :], v16[:, qn], ALU.mult),
                     r=[dPS[4], d_v, d_tB], w=[d_tB])
                k.op(pool, lambda: nc.gpsimd.tensor_tensor(tA[:, W5], tA[:, W5], tB[:, W5], ALU.add),
                     r=[d_tA, d_tB], w=[d_tA])
                k.op(pe, lambda qs=qs: nc.tensor.matmul(PS[5][:, :], g2c[:, 0, :], SG[:, 0, qs], start=True, stop=False),
                     r=[d_g2, d_SG], w=[dPS[5]])
                k.op(pe, lambda qs=qs: nc.tensor.matmul(PS[5][:, :], g2c[0:32, 1, :], SG[0:32, 1, qs], start=False, stop=True),
                     r=[d_g2, d_SG], w=[dPS[5]])
                k.op(dve, lambda qs=qs: nc.vector.tensor_tensor(ob[:, qs], tA[:, W5], PS[5][:, :], ALU.mult),
                     r=[d_tA, dPS[5]], w=[dob])
            k.dma(sp, oT_d[:, c, :], ob[:], r=[dob])
        k.barrier()
    lora_stack.close()

    h2_stack = contextlib.ExitStack()
    h2 = k.sb("h2", [128, KC, T], BF16, h2_stack)
    d_h2 = [Dep() for _ in range(4)]
    d_xres = [Dep() for _ in range(4)]

    def out_proj_phase(w_dram, yin, d_yin, Gn, xprev_dram, An, Bn, router=None):
        with contextlib.ExitStack() as ph:
            wk = mk_wk(ph)
            wo = k.sb("wo", [128, KC, D], BF16, ph)
            d_wo = Dep()
            k.dma(pool, wo[:], kview(w_dram, 0, D), w=[d_wo])
            ym = k.sb("ym", [128, KC, 512], F32, ph)
            xp = k.sb("xp", [128, KC, 512], F32, ph)
            xn = k.sb("xn", [128, KC, 512], F32, ph)
            d_ym, d_xp, d_xn = Dep(), Dep(), Dep()
            if router is not None:
                h32 = k.sb("h32", [128, KC, 512], F32, ph)
                d_h32 = Dep()
            for tb in range(4):
                ts_ = slice(tb * 512, (tb + 1) * 512)
                k.dma(sp, xp[:], xprev_dram[:, :, ts_], r=[d_xres[tb]], w=[d_xp])
                for dc in range(KC):
                    p = dc % 4
                    for kk in range(KC):
                        k.op(pe, lambda dc=dc, kk=kk, p=p, ts_=ts_: nc.tensor.matmul(
                            PS[p][:, :], wo[:, kk, dc * 128:(dc + 1) * 128], yin[:, kk, ts_],
                            start=(kk == 0), stop=(kk == KC - 1)), r=[d_wo, d_yin], w=[dPS[p]])
                    k.op(act, lambda dc=dc, p=p: nc.scalar.copy(ym[:, dc, :], PS[p][:, :]), r=[dPS[p]], w=[d_ym])
                res_norm(ym, d_ym, 512, Gn, xp, d_xp, xn, d_xn, wk)
                k.dma(sp, xres_d[:, :, ts_], xn[:], r=[d_xn], w=[d_xres[tb]])
                norm_mod(xn, d_xn, 512, An, Bn, lambda c, ts_=ts_: h2[:, c, ts_], d_h2[tb], wk,
                         h32=(h32 if router is not None else None), d_h32=(d_h32 if router is not None else None))
                if router is not None:
                    router(tb, h32, d_h32)
            k.barrier()

    def ffn_pass(wgu_dram, wd_dram, acc, d_acc, first, wbufs, gate_bc=None, d_gate=None):
        for fg in range(FF // 512):
            b = wbufs["i"] % 2
            wbufs["i"] += 1
            Wg, Wu, Wd = wbufs["g"][b], wbufs["u"][b], wbufs["d"][b]
            dW = wbufs["dep"][b]
            k.dma(pool, Wg[:], kview(wgu_dram, fg * 512, (fg + 1) * 512), w=[dW])
            k.dma(pool, Wu[:], kview(wgu_dram, FF + fg * 512, FF + (fg + 1) * 512), w=[dW])
            k.dma(pool, Wd[:], wd_dram[fg * 512:(fg + 1) * 512, :].rearrange("(f p) d -> p f d", p=128), w=[dW])
            for tb in range(4):
                ts_ = slice(tb * 512, (tb + 1) * 512)
                ab = wbufs["ai"] % 2
                wbufs["ai"] += 1
                actb = wbufs["act"][ab]
                d_actb = wbufs["dact"][ab]
                for fc in range(4):
                    pg = (fc % 2) * 2
                    pu = pg + 1
                    for kk in range(KC):
                        k.op(pe, lambda kk=kk, fc=fc, pg=pg, ts_=ts_: nc.tensor.matmul(
                            PS[pg][:, :], Wg[:, kk, fc * 128:(fc + 1) * 128], h2[:, kk, ts_],
                            start=(kk == 0), stop=(kk == KC - 1)), r=[dW, d_h2[tb]], w=[dPS[pg]])
                    for kk in range(KC):
                        k.op(pe, lambda kk=kk, fc=fc, pu=pu, ts_=ts_: nc.tensor.matmul(
                            PS[pu][:, :], Wu[:, kk, fc * 128:(fc + 1) * 128], h2[:, kk, ts_],
                            start=(kk == 0), stop=(kk == KC - 1)), r=[dW, d_h2[tb]], w=[dPS[pu]])
                    sgb = wbufs["sg"][fc % 2]
                    d_sgb = wbufs["dsg"][fc % 2]
                    k.op(act, lambda pg=pg, sgb=sgb: nc.scalar.activation(sgb[:], PS[pg][:, :], AF.Silu),
                         r=[dPS[pg]], w=[d_sgb])
                    if gate_bc is None:
                        k.op(dve, lambda fc=fc, pu=pu, sgb=sgb, actb=actb: nc.vector.tensor_tensor(
                            actb[:, fc, :], sgb[:], PS[pu][:, :], ALU.mult), r=[d_sgb, dPS[pu]], w=[d_actb])
                    else:
                        k.op(dve, lambda fc=fc, pu=pu, sgb=sgb: nc.vector.tensor_tensor(
                            sgb[:], sgb[:], PS[pu][:, :], ALU.mult), r=[d_sgb, dPS[pu]], w=[d_sgb])
                        k.op(pool, lambda fc=fc, sgb=sgb, actb=actb, ts_=ts_: nc.gpsimd.tensor_tensor(
                            actb[:, fc, :], sgb[:], gate_bc[:, ts_], ALU.mult), r=[d_sgb, d_gate], w=[d_actb])
                for dc in range(KC):
                    p = 4 + dc % 2
                    for fc in range(4):
                        k.op(pe, lambda dc=dc, fc=fc, p=p, actb=actb: nc.tensor.matmul(
                            PS[p][:, :], Wd[:, fc, dc * 128:(dc + 1) * 128], actb[:, fc, :],
                            start=(fc == 0), stop=(fc == 3)), r=[dW, d_actb], w=[dPS[p]])
                    if first and fg == 0:
                        k.op(act, lambda dc=dc, p=p, ts_=ts_: nc.scalar.copy(acc[:, dc, ts_], PS[p][:, :]),
                             r=[dPS[p]], w=[d_acc[tb]])
                    else:
                        k.op(dve, lambda dc=dc, p=p, ts_=ts_: nc.vector.tensor_tensor(
                            acc[:, dc, ts_], acc[:, dc, ts_], PS[p][:, :], ALU.add), r=[dPS[p], d_acc[tb]], w=[d_acc[tb]])

    def mk_ffn_bufs(ph):
        return dict(i=0, ai=0,
                    g=[k.sb("Wg%d" % i, [128, KC, 512], BF16, ph) for i in range(2)],
                    u=[k.sb("Wu%d" % i, [128, KC, 512], BF16, ph) for i in range(2)],
                    d=[k.sb("Wd%d" % i, [128, 4, D], BF16, ph) for i in range(2)],
                    dep=[Dep(), Dep()],
                    act=[k.sb("actb%d" % i, [128, 4, 512], BF16, ph) for i in range(2)],
                    dact=[Dep(), Dep()],
                    sg=[k.sb("sgb%d" % i, [128, 512], F32, ph) for i in range(2)],
                    dsg=[Dep(), Dep()])

    def post_ffn_phase(acc, d_acc, Gn, An, Bn, final):
        with contextlib.ExitStack() as ph:
            wk = mk_wk(ph)
            xp = k.sb("xp", [128, KC, 512], F32, ph)
            xn = k.sb("xn", [128, KC, 512], F32, ph)
            d_xp, d_xn = Dep(), Dep()
            for tb in range(4):
                ts_ = slice(tb * 512, (tb + 1) * 512)
                k.dma(sp, xp[:], xres_d[:, :, ts_], r=[d_xres[tb]], w=[d_xp])

                sumsq_rstd(lambda c: acc[:, c, ts_], 512, wk["sq"], wk["rs"], d_acc[tb], wk["dsq"], wk["drs"], 6)
                for c in range(KC):
                    k.op(pool, lambda c=c, ts_=ts_: nc.gpsimd.tensor_tensor(wk["tmp"][:, c, :], acc[:, c, ts_], wk["rs"][:, :], ALU.mult),
                         r=[d_acc[tb], wk["drs"]], w=[wk["dtmp"]])
                    k.op(dve, lambda c=c: nc.vector.scalar_tensor_tensor(xn[:, c, :], wk["tmp"][:, c, :], S(Gn, c),
                                                                         xp[:, c, :], ALU.mult, ALU.add),
                         r=[wk["dtmp"], d_scal, d_xp], w=[d_xn])
                if final:
                    k.dma(sp, out_d[:, :, ts_], xn[:], r=[d_xn])
                else:
                    k.dma(sp, xres_d[:, :, ts_], xn[:], r=[d_xn], w=[d_xres[tb]])
                    norm_mod(xn, d_xn, 512, An, Bn, lambda c, ts_=ts_: h2[:, c, ts_], d_h2[tb], wk)
            k.barrier()

    with contextlib.ExitStack() as yst:
        yin0 = k.sb("yin0", [128, KC, T], BF16, yst)
        d_yin0 = Dep()
        k.dma(sp, yin0[:], oT_d[:, :, :], w=[d_yin0])
        out_proj_phase(wout_d, yin0, d_yin0, "G1_0", xT_d, "A2_0", "B2_0")

    if dbg == 3:
        with contextlib.ExitStack() as ph:
            t32 = k.sb("t32", [128, T], F32, ph)
            dd = Dep()
            k.op(dve, lambda: nc.vector.tensor_copy(t32[:], h2[:, 0, :]), r=d_h2, w=[dd])
            dump(lambda: t32[:], T, dd)
            k.barrier()
        h2_stack.close()
        k.finish()
        return k

    acc_stack = contextlib.ExitStack()
    acc = k.sb("acc", [128, KC, T], F32, acc_stack)
    d_acc = [Dep() for _ in range(4)]
    with contextlib.ExitStack() as ph:
        wbufs = mk_ffn_bufs(ph)
        ffn_pass(fgu_d, fd_d, acc, d_acc, True, wbufs)
        k.barrier()
    post_ffn_phase(acc, d_acc, "G2_0", "A1_1", "B1_1", final=False)
    acc_stack.close()

    gates_stack = contextlib.ExitStack()
    logit = k.sb("logit", [128, 16, NE], F32, gates_stack)
    gates = k.sb("gates", [128, 16, NE], F32, gates_stack)
    wr32 = k.sb("wr32", [128, KC, NE], F32, gates_stack)
    d_logit, d_gates, d_wr = Dep(), Dep(), Dep()
    k.dma(sp, wr32[:], rt_d[:, :, :], w=[d_wr])

    ycv_stack = contextlib.ExitStack()
    ycv = k.sb("ycv", [128, KC, T], BF16, ycv_stack)
    d_ycv = Dep()
    with contextlib.ExitStack() as ph:
        Wc3 = [k.sb("Wc3_%d" % i, [128, KC, 3, 128], BF16, ph) for i in range(2)]
        dWc3 = [Dep(), Dep()]
        Bsb = k.sb("Bsb", [128, T], F32, ph)
        Csb = k.sb("Csb", [128, 512], F32, ph)
        zp = k.sb("zp", [128, T + 2], F32, ph)
        t1 = k.sb("t1", [128, T], F32, ph)
        d_B, d_C, d_z, d_t1 = Dep(), Dep(), Dep(), Dep()
        k.op(dve, lambda: nc.vector.memset(zp[:], 0.0), w=[d_z])
        for c in range(KC):
            W3 = Wc3[c % 2]
            dW3 = dWc3[c % 2]
            for j in range(3):
                k.dma(pool, W3[:, :, j, :], kview(cwin_d, j * D + c * 128, j * D + (c + 1) * 128), w=[dW3])
            for tb in range(4):
                ts_ = slice(tb * 512, (tb + 1) * 512)
                for j in range(3):
                    p = j
                    for kk in range(KC):
                        k.op(pe, lambda kk=kk, j=j, p=p, ts_=ts_, W3=W3: nc.tensor.matmul(
                            PS[p][:, :], W3[:, kk, j, :], h2[:, kk, ts_], start=(kk == 0), stop=(kk == KC - 1)),
                            r=[dW3, d_h2[tb]], w=[dPS[p]])
                k.op(act, lambda ts_=ts_: nc.scalar.copy(Bsb[:, ts_], PS[0][:, :]), r=[dPS[0]], w=[d_B])
                k.op(act, lambda: nc.scalar.copy(Csb[:], PS[1][:, :]), r=[dPS[1]], w=[d_C])
                k.op(dve, lambda tb=tb: nc.vector.tensor_tensor(zp[:, 1 + tb * 512:1 + (tb + 1) * 512], Csb[:], PS[2][:, :], ALU.mult),
                     r=[d_C, dPS[2]], w=[d_z])
            k.op(act, lambda c=c: nc.scalar.activation(t1[:], zp[:, 0:T], AF.Identity, scale=Vc("conv_w", 0, c)),
                 r=[d_z, d_const], w=[d_t1])
            k.op(dve, lambda c=c: nc.vector.scalar_tensor_tensor(t1[:], zp[:, 1:T + 1], Vc("conv_w", 1, c), t1[:], ALU.mult, ALU.add),
                 r=[d_z, d_const, d_t1], w=[d_t1])
            k.op(dve, lambda c=c: nc.vector.scalar_tensor_tensor(t1[:], zp[:, 2:T + 2], Vc("conv_w", 2, c), t1[:], ALU.mult, ALU.add),
                 r=[d_z, d_const, d_t1], w=[d_t1])
            k.op(pool, lambda c=c: nc.gpsimd.tensor_tensor(ycv[:, c, :], t1[:], Bsb[:], ALU.mult),
                 r=[d_t1, d_B], w=[d_ycv])
        k.barrier()

    def router(tb, h32, d_h32):
        for sub in range(4):
            tt = tb * 4 + sub
            for c in range(KC):
                k.op(pe, lambda c=c, sub=sub: nc.tensor.matmul(
                    PS[5][:, sub * 8:sub * 8 + 8], h32[:, c, sub * 128:(sub + 1) * 128], wr32[:, c, :],
                    start=(c == 0), stop=(c == KC - 1)), r=[d_h32, d_wr], w=[dPS[5]])
        k.op(dve, lambda tb=tb: nc.vector.tensor_copy(
            logit[:, tb * 4:(tb + 1) * 4, :], PS[5][:, 0:32].rearrange("p (s e) -> p s e", e=8)),
            r=[dPS[5]], w=[d_logit])

    out_proj_phase(cwout_d, ycv, d_ycv, "G1_1", xres_d, "A2_1", "B2_1", router=router)
    ycv_stack.close()

    with contextlib.ExitStack() as ph:
        mx = k.sb("mx", [128, 16, 8], F32, ph)
        e1 = k.sb("e1", [128, 16], F32, ph)
        g1_ = k.sb("g1_", [128, 16], F32, ph)
        g2_ = k.sb("g2_", [128, 16], F32, ph)
        q1 = k.sb("q1", [128, 16, 8], F32, ph)
        q2 = k.sb("q2", [128, 16, 8], F32, ph)
        d_mx, d_e = Dep(), Dep()
        for tt in range(16):
            k.op(dve, lambda tt=tt: nc.vector.max(mx[:, tt, :], logit[:, tt, :]), r=[d_logit], w=[d_mx])
        k.op(dve, lambda: nc.vector.tensor_tensor(e1[:], mx[:, :, 1], mx[:, :, 0], ALU.subtract), r=[d_mx], w=[d_e])
        k.op(act, lambda: nc.scalar.activation(e1[:], e1[:], AF.Exp), r=[d_e], w=[d_e])
        k.op(dve, lambda: nc.vector.tensor_scalar(g1_[:], e1[:], 1.0, None, ALU.add), r=[d_e], w=[d_e])
        k.op(dve, lambda: nc.vector.reciprocal(g1_[:], g1_[:]), r=[d_e], w=[d_e])
        k.op(dve, lambda: nc.vector.tensor_tensor(g2_[:], e1[:], g1_[:], ALU.mult), r=[d_e], w=[d_e])
        k.op(dve, lambda: nc.vector.tensor_tensor(q1[:], logit[:], mx[:, :, 0:1].to_broadcast([128, 16, 8]), ALU.is_equal),
             r=[d_logit, d_mx], w=[d_e])
        k.op(dve, lambda: nc.vector.tensor_tensor(q2[:], logit[:], mx[:, :, 1:2].to_broadcast([128, 16, 8]), ALU.is_equal),
             r=[d_logit, d_mx], w=[d_e])
        k.op(dve, lambda: nc.vector.tensor_tensor(q1[:], q1[:], g1_[:, :].unsqueeze(2).to_broadcast([128, 16, 8]), ALU.mult),
             r=[d_e], w=[d_e])
        k.op(dve, lambda: nc.vector.tensor_tensor(q2[:], q2[:], g2_[:, :].unsqueeze(2).to_broadcast([128, 16, 8]), ALU.mult),
             r=[d_e], w=[d_e])
        k.op(dve, lambda: nc.vector.tensor_tensor(gates[:], q1[:], q2[:], ALU.add), r=[d_e], w=[d_gates])
        k.barrier()

    if dbg == 4:
        dump(lambda: gates[:].rearrange("p t e -> p (t e)"), 128, d_gates, 0)
        dump(lambda: logit[:].rearrange("p t e -> p (t e)"), 128, d_logit, 128)
        k.barrier()
        gates_stack.close()
        h2_stack.close()
        k.finish()
        return k

    acc_stack = contextlib.ExitStack()
    acc = k.sb("acc2", [128, KC, T], F32, acc_stack)
    d_acc = [Dep() for _ in range(4)]
    with contextlib.ExitStack() as ph:
        wbufs = mk_ffn_bufs(ph)
        gbc = [k.sb("gbc%d" % i, [128, T], F32, ph) for i in range(2)]
        d_gbc = [Dep(), Dep()]
        Gm = [k.sb("Gm%d" % i, [128, 128], F32, ph) for i in range(2)]
        d_Gm = [Dep(), Dep()]
        ident_f = cst[:, C_ID:C_ID + 128]
        for e in range(NE):
            gb = gbc[e % 2]
            dgb = d_gbc[e % 2]
            for tt in range(16):
                gm = Gm[tt % 2]
                dgm = d_Gm[tt % 2]
                k.op(dve, lambda tt=tt, e=e, gm=gm: nc.vector.tensor_copy(gm[:], gates[:, tt, e:e + 1].to_broadcast([128, 128])),
                     r=[d_gates], w=[dgm])
                k.op(pe, lambda tt=tt, gm=gm: nc.tensor.matmul(PS[6][:, (tt % 4) * 128:(tt % 4 + 1) * 128], gm[:], ident_f,
                                                              start=True, stop=True), r=[dgm, d_const], w=[dPS[6]])
                if tt % 4 == 3:
                    q = (tt // 4) * 512
                    k.op(act, lambda q=q, gb=gb: nc.scalar.copy(gb[:, q:q + 512], PS[6][:, :]), r=[dPS[6]], w=[dgb])
            ffn_pass(mgu_d[e], md_d[e], acc, d_acc, e == 0, wbufs, gate_bc=gb, d_gate=dgb)
        k.barrier()
    post_ffn_phase(acc, d_acc, "G2_1", None, None, final=True)
    acc_stack.close()
    gates_stack.close()
    h2_stack.close()
    k.finish()
    return k


def prep_inputs(inp):
    f = lambda a: np.ascontiguousarray(np.asarray(a, np.float32))
    vec = np.zeros((128, NV), np.float32)

    def put(name, arr):
        a = col(arr)
        vec[:, VOFF[name]:VOFF[name] + a.shape[1]] = a

    put("g", f(inp["norm_g"]).reshape(-1))
    put("mu", f(inp["rwkv_mu"]).reshape(-1))
    put("w0", f(inp["rwkv_w0"]).reshape(-1))
    put("a0", f(inp["rwkv_a0"]).reshape(-1))
    put("k_k", f(inp["rwkv_k_k"]).reshape(-1))
    put("k_a", f(inp["rwkv_k_a"]).reshape(-1))
    put("r_k", f(inp["rwkv_r_k"]).reshape(-1))
    put("ln_w", f(inp["rwkv_ln_w"]).reshape(-1))
    put("ln_b", f(inp["rwkv_ln_b"]).reshape(-1))
    put("conv_w", f(inp["conv_w"]).reshape(-1))
    mb = col(f(inp["mod_b"]).reshape(-1))
    vec[:, VOFF["modb"]:VOFF["modb"] + 192] = np.repeat(mb, 2, axis=1)
    shared = {
        "vecs": vec,
        "cst": make_consts(),
        "mod_w": f(inp["mod_w"]),
        "w_rkv": f(inp["rwkv_w_rkv"])[0],
        "w1cat": np.ascontiguousarray(np.concatenate([f(inp["rwkv_w1"])[0, 0], f(inp["rwkv_w1"])[0, 1]], axis=1)),
        "w2cat": np.ascontiguousarray(f(inp["rwkv_w2"])[0].reshape(128, D)),
        "a1cat": np.ascontiguousarray(np.concatenate([f(inp["rwkv_a1"])[0, 0], f(inp["rwkv_a1"])[0, 1]], axis=1)),
        "a2cat": np.ascontiguousarray(f(inp["rwkv_a2"])[0].reshape(128, D)),
        "g1": f(inp["rwkv_g1"])[0],
        "g2": f(inp["rwkv_g2"])[0],
        "w_out": f(inp["rwkv_w_out"])[0],
        "conv_w_in": f(inp["conv_w_in"])[0],
        "conv_w_out": f(inp["conv_w_out"])[0],
        "ffn_w_gu": f(inp["ffn_w_gu"])[0],
        "ffn_w_down": f(inp["ffn_w_down"])[0],
        "router": np.ascontiguousarray(f(inp["moe_router"])[0].reshape(KC, 128, NE).transpose(1, 0, 2)),
        "moe_w_gu": f(inp["moe_w_gu"])[0],
        "moe_w_down": f(inp["moe_w_down"])[0],
    }
    x = f(inp["x"])
    ctx = f(inp["ctx"])
    c = f(inp["c"])
    cc = f(inp["c_ctx"])
    maps = []
    for b in range(8):
        m = dict(shared)
        m["xT"] = np.ascontiguousarray(x[b].T.reshape(KC, 128, T).transpose(1, 0, 2))
        m["ctxT"] = np.ascontiguousarray(ctx[b].T.reshape(KC, 128, TC).transpose(1, 0, 2))
        m["cvec"] = np.ascontiguousarray(np.stack([col(c[b]), col(cc)], axis=2))
        maps.append(m)
    return maps


def kernel(**inputs):
    maps = prep_inputs(inputs)
    kb = build()
    res = run_bass_kernel_spmd(kb.nc, maps, core_ids=list(range(8)))
    outs = []
    for b in range(8):
        yT = np.asarray(res.results[b]["yT"], np.float32)
        outs.append(yT.transpose(1, 0, 2).reshape(D, T).T)
    return np.ascontiguousarray(np.stack(outs, 0).astype(np.float32))
```

```python
import contextlib
import numpy as np
import concourse.bass as bass
import concourse.mybir as mybir
from concourse.ap import AP
from concourse.bass_utils import run_bass_kernel_spmd

F32 = mybir.dt.float32
BF16 = mybir.dt.bfloat16
AF = mybir.ActivationFunctionType
ALU = mybir.AluOpType

T = 2048
TC = 256
NT = T + TC
D = 1024
KC = 8
FF = 3584
NE = 8
NCH = NT // 64
NORM_EPS = 1e-6
GN_EPS = 64e-5
DECAY_S = -0.6065306597126334


class Dep:
    __slots__ = ("w", "rs")

    def __init__(self):
        self.w = None
        self.rs = {}


class Eng:
    def __init__(self, name, obj, is_pe=False):
        self.name = name
        self.obj = obj
        self.is_pe = is_pe
        self.sem = None
        self.semid = None
        self.cnt = 0
        self.seen = {}


class Slot:
    def __init__(self, sem, semid):
        self.sem = sem
        self.semid = semid
        self.cnt = 0
        self.last = None


class KB:
    def __init__(self):
        self.nc = bass.Bass("TRN2", target_bir_lowering=False)
        nc = self.nc
        self.es = contextlib.ExitStack()
        self.sems = []
        self.epoch = 0
        self.pe = Eng("pe", nc.tensor, True)
        self.act = Eng("act", nc.scalar)
        self.dve = Eng("dve", nc.vector)
        self.pool = Eng("pool", nc.gpsimd)
        self.sp = Eng("sp", nc.sync)
        self.engs = [self.pe, self.act, self.dve, self.pool, self.sp]
        for e in self.engs:
            self._fresh_sem(e)
        self.slots = {}
        self.slot_i = {}
        for q in (self.sp, self.pool):
            self.slots[q.name] = [Slot(*self._newsem("dq%s%d" % (q.name, i))) for i in range(8)]
            self.slot_i[q.name] = 0
        self.ninstr = 0
        self.uid = 0

    def _newsem(self, name):
        s = self.es.enter_context(self.nc.semaphore("%s_%d" % (name, len(self.sems))))
        self.sems.append(s)
        return s, len(self.sems) - 1

    def _fresh_sem(self, e):
        e.sem, e.semid = self._newsem("e" + e.name)
        e.cnt = 0

    def sb(self, name, shape, dtype, stack=None):
        self.uid += 1
        return (stack or self.es).enter_context(
            self.nc.sbuf_tensor("%s_%d" % (name, self.uid), list(shape), dtype))

    def psum(self, name, shape, dtype, stack=None):
        self.uid += 1
        return (stack or self.es).enter_context(
            self.nc.psum_tensor("%s_%d" % (name, self.uid), list(shape), dtype))

    def _wait(self, eng, tk, war=False):
        semid, val, src, ep = tk
        if ep != self.epoch:
            return
        if src is eng:
            if eng.is_pe or war:
                return
        if eng.seen.get(semid, 0) >= val:
            return
        eng.obj.wait_ge(self.sems[semid], val)
        eng.seen[semid] = val
        self.ninstr += 1

    def _deps(self, eng, r, w):
        for d in r:
            if d.w is not None:
                self._wait(eng, d.w)
        for d in w:
            if d.w is not None:
                self._wait(eng, d.w)
            for t in d.rs.values():
                self._wait(eng, t, war=True)

    def _mark(self, tk, r, w):
        for d in r:
            d.rs[tk[0]] = tk
        for d in w:
            d.w = tk
            d.rs = {}

    def op(self, eng, fn, r=(), w=()):
        self._deps(eng, r, w)
        ins = fn()
        eng.cnt += 1
        ins.then_inc(eng.sem, 1)
        tk = (eng.semid, eng.cnt, eng, self.epoch)
        self._mark(tk, r, w)
        self.ninstr += 1
        return tk

    def dma(self, q, out, in_, r=(), w=(), **kw):
        sl = self.slots[q.name]
        i = self.slot_i[q.name]
        self.slot_i[q.name] = (i + 1) % len(sl)
        s = sl[i]
        if s.last is not None:
            self._wait(q, s.last)
        self._deps(q, r, w)
        ins = q.obj.dma_start(out=out, in_=in_, **kw)
        s.cnt += 16
        ins.then_inc(s.sem, 16)
        tk = (s.semid, s.cnt, None, self.epoch)
        s.last = tk
        self._mark(tk, r, w)
        self.ninstr += 1
        return tk

    def barrier(self):
        tks = []
        for e in self.engs:
            if e.cnt > 0:
                tks.append((e.semid, e.cnt, e, self.epoch))
        for sl in self.slots.values():
            for s in sl:
                if s.last is not None and s.last[3] == self.epoch:
                    tks.append(s.last)
        for e in self.engs:
            for tk in tks:
                semid, val, src, ep = tk
                if e.seen.get(semid, 0) >= val:
                    continue
                e.obj.wait_ge(self.sems[semid], val)
                e.seen[semid] = val
                self.ninstr += 1
        self.epoch += 1
        for e in self.engs:
            self._fresh_sem(e)
            e.seen = {}

    def finish(self):
        self.barrier()
        self.es.close()


VEC_SPEC = [("g", 2 * 4 * 8), ("mu", 6 * 8), ("w0", 16), ("a0", 16), ("k_k", 8), ("k_a", 8), ("r_k", 8),
            ("ln_w", 8), ("ln_b", 8), ("conv_w", 24), ("modb", 192)]
VOFF = {}
_o = 0
for _n, _c in VEC_SPEC:
    VOFF[_n] = _o
    _o += _c
NV = _o
C_ID, C_MF, C_MB, C_LF, C_LB, NCST = 0, 128, 640, 1152, 1280, 1408


def col(v):
    v = np.asarray(v, np.float32).reshape(-1, 128)
    return np.ascontiguousarray(v.T)


def make_consts():
    c = np.zeros((128, NCST), np.float32)
    c[:, C_ID:C_ID + 128] = np.eye(128, dtype=np.float32)
    s = np.arange(64)[:, None]
    t = np.arange(64)[None, :]
    strict_f = (s < t).astype(np.float32)
    incl_f = (s <= t).astype(np.float32)
    strict_b = (s > t).astype(np.float32)
    incl_b = (s >= t).astype(np.float32)
    mf = np.concatenate([strict_f, incl_f], 1)
    mb = np.concatenate([strict_b, incl_b], 1)
    c[:64, C_MF:C_MF + 512] = np.tile(mf, (1, 4))
    c[:64, C_MB:C_MB + 512] = np.tile(mb, (1, 4))
    c[:64, C_LF:C_LF + 128] = np.tile(strict_b, (1, 2))
    c[:64, C_LB:C_LB + 128] = np.tile(strict_f, (1, 2))
    return c


def build(dbg=None, dbgn=0):
    k = KB()
    nc = k.nc
    pe, act, dve, pool, sp = k.pe, k.act, k.dve, k.pool, k.sp

    def din(name, shape, dt=F32):
        return nc.dram_tensor(name, list(shape), dt, kind="ExternalInput").ap()

    xT_d = din("xT", [128, KC, T])
    ctxT_d = din("ctxT", [128, KC, TC])
    cvec_d = din("cvec", [128, KC, 2])
    vec_d = din("vecs", [128, NV])
    cst_d = din("cst", [128, NCST])
    mod_w_d = din("mod_w", [2, D, 6 * D])
    w_rkv_d = din("w_rkv", [3, D, D])
    w1_d = din("w1cat", [D, 128])
    w2_d = din("w2cat", [128, D])
    a1_d = din("a1cat", [D, 128])
    a2_d = din("a2cat", [128, D])
    g1_d = din("g1", [D, 160])
    g2_d = din("g2", [160, D])
    wout_d = din("w_out", [D, D])
    cwin_d = din("conv_w_in", [D, 3 * D])
    cwout_d = din("conv_w_out", [D, D])
    fgu_d = din("ffn_w_gu", [D, 2 * FF])
    fd_d = din("ffn_w_down", [FF, D])
    rt_d = din("router", [128, KC, NE])
    mgu_d = din("moe_w_gu", [NE, D, 2 * FF])
    md_d = din("moe_w_down", [NE, FF, D])
    out_d = nc.dram_tensor("yT", [128, KC, T], F32, kind="ExternalOutput").ap()
    xres_d = nc.dram_tensor("xres", [128, KC, T], F32, kind="Internal").ap()
    oT_d = nc.dram_tensor("oT", [128, KC, T], BF16, kind="Internal").ap()
    rkv_d = nc.dram_tensor("rkvs", [3, 128, KC, NT], F32, kind="Internal").ap()
    if dbg is not None:
        dbg_d = nc.dram_tensor("dbg", [128, dbgn], F32, kind="ExternalOutput").ap()

    def kview(w2d, c0, c1):
        return w2d.rearrange("(k p) n -> p k n", p=128)[:, :, c0:c1]

    vec = k.sb("vec", [128, NV], F32)
    cst = k.sb("cst", [128, NCST], F32)
    ident_bf = k.sb("identb", [128, 128], BF16)
    ones_bf = k.sb("onesb", [128, 128], BF16)
    bd_bf = k.sb("bdb", [128, 128], BF16)
    bdm_f = k.sb("bdmf", [128, 128], F32)
    bd1_f = k.sb("bd1f", [128, 128], F32)
    maskF = k.sb("maskF", [64, 512], BF16)
    maskB = k.sb("maskB", [64, 512], BF16)
    maskLF = k.sb("maskLF", [64, 128], BF16)
    maskLB = k.sb("maskLB", [64, 128], BF16)
    mod = k.sb("mod", [128, 192], F32)
    scal = k.sb("scal", [128, 160], F32)
    rmask = k.sb("rmask", [128, 576], F32)
    d_const = Dep()
    d_mod = Dep()
    d_scal = Dep()

    PS = [k.psum("ps%d" % i, [128, 512], F32) for i in range(7)]
    PSB = k.psum("psb", [128, 1024], BF16)
    dPS = [Dep() for _ in range(7)]
    dPSB = Dep()

    def V(name, i=0, n=8):
        o = VOFF[name] + i * 8
        return vec[:, o:o + n]

    def Vc(name, i, c):
        o = VOFF[name] + i * 8 + c
        return vec[:, o:o + 1]

    S_ = {n: i * 8 for i, n in enumerate(
        ["A1_0", "B1_0", "G1_0", "A2_0", "B2_0", "G2_0", "A1c", "B1c",
         "A1_1", "B1_1", "G1_1", "A2_1", "B2_1", "G2_1", "omka", "hrk"])}
    omu = k.sb("omu", [128, 48], F32)

    def S(name, c=None):
        o = S_[name]
        if c is None:
            return scal[:, o:o + 8]
        return scal[:, o + c:o + c + 1]

    k.dma(sp, vec[:], vec_d[:, :], w=[d_const])
    k.dma(sp, cst[:], cst_d[:, :], w=[d_const])
    k.op(dve, lambda: nc.vector.tensor_copy(ident_bf[:], cst[:, C_ID:C_ID + 128]), r=[d_const], w=[d_const])
    k.op(dve, lambda: nc.vector.tensor_copy(maskF[:], cst[0:64, C_MF:C_MF + 512]), r=[d_const], w=[d_const])
    k.op(dve, lambda: nc.vector.tensor_copy(maskB[:], cst[0:64, C_MB:C_MB + 512]), r=[d_const], w=[d_const])
    k.op(dve, lambda: nc.vector.tensor_copy(maskLF[:], cst[0:64, C_LF:C_LF + 128]), r=[d_const], w=[d_const])
    k.op(dve, lambda: nc.vector.tensor_copy(maskLB[:], cst[0:64, C_LB:C_LB + 128]), r=[d_const], w=[d_const])
    k.op(dve, lambda: nc.vector.memset(ones_bf[:], 1.0), w=[d_const])
    k.op(dve, lambda: nc.vector.memset(bd_bf[:], 0.0), w=[d_const])
    k.op(dve, lambda: nc.vector.memset(bdm_f[:], 0.0), w=[d_const])
    k.op(dve, lambda: nc.vector.memset(bd1_f[:], 0.0), w=[d_const])
    for h in range(2):
        sl = slice(64 * h, 64 * h + 64)
        k.op(dve, lambda sl=sl: nc.vector.memset(bd_bf[sl, sl], 1.0), w=[d_const])
        k.op(dve, lambda sl=sl: nc.vector.memset(bdm_f[sl, sl], 1.0 / 64), w=[d_const])
        k.op(dve, lambda sl=sl: nc.vector.memset(bd1_f[sl, sl], 1.0), w=[d_const])
    k.op(dve, lambda: nc.vector.memset(rmask[:], 1.0), w=[d_const])
    k.op(dve, lambda: nc.vector.memset(rmask[:, 0:576:64], 0.0), w=[d_const])
    k.op(dve, lambda: nc.vector.tensor_scalar(omu[:], V("mu", 0, 48), -1.0, 1.0, ALU.mult, ALU.add),
         r=[d_const], w=[d_const])

    with contextlib.ExitStack() as ph:
        cv = k.sb("cv", [128, KC, 2], F32, ph)
        scb = k.sb("scb", [128, KC, 2], BF16, ph)
        wb = [k.sb("modw%d" % i, [128, KC, 1024], BF16, ph) for i in range(2)]
        dwb = [Dep(), Dep()]
        d_cv = Dep()
        k.dma(sp, cv[:], cvec_d[:, :, :], w=[d_cv])
        k.op(act, lambda: nc.scalar.activation(scb[:], cv[:], AF.Silu), r=[d_cv], w=[d_cv])
        gi = 0
        for i in range(2):
            for g in range(6):
                b = gi % 2
                gi += 1
                k.dma(pool, wb[b][:], kview(mod_w_d[i], g * 1024, (g + 1) * 1024), w=[dwb[b]])
                for m in range(8):
                    mg = g * 8 + m
                    cc = (i * 48 + mg) * 2
                    for kk in range(KC):
                        k.op(pe, lambda b=b, m=m, kk=kk, cc=cc: nc.tensor.matmul(
                            PS[0][:, cc:cc + 2], wb[b][:, kk, m * 128:(m + 1) * 128], scb[:, kk, :],
                            start=(kk == 0), stop=(kk == KC - 1)), r=[dwb[b], d_cv], w=[dPS[0]])
        k.op(dve, lambda: nc.vector.tensor_tensor(mod[:], PS[0][:, 0:192], V("modb", 0, 192), ALU.add),
             r=[dPS[0], d_const], w=[d_mod])

        def mcol(i, s, j):
            o = (i * 48 + s * 8) * 2 + j
            return mod[:, o:o + 16:2]

        def mk_scale(dst, gi_, li, s, j):
            k.op(dve, lambda: nc.vector.tensor_scalar(S(dst), mcol(li, s, j), 1.0, None, ALU.add),
                 r=[d_mod], w=[d_scal])
            k.op(dve, lambda: nc.vector.tensor_tensor(S(dst), S(dst), V("g", li * 4 + gi_), ALU.mult),
                 r=[d_scal, d_const], w=[d_scal])

        def mk_copy(dst, li, s, j):
            k.op(dve, lambda: nc.vector.tensor_copy(S(dst), mcol(li, s, j)), r=[d_mod], w=[d_scal])

        def mk_gate(dst, gi_, li, s):
            k.op(dve, lambda: nc.vector.tensor_tensor(S(dst), mcol(li, s, 0), V("g", li * 4 + gi_), ALU.mult),
                 r=[d_mod, d_const], w=[d_scal])

        for li in range(2):
            mk_scale("A1_%d" % li, 0, li, 1, 0)
            mk_copy("B1_%d" % li, li, 0, 0)
            mk_gate("G1_%d" % li, 1, li, 2)
            mk_scale("A2_%d" % li, 2, li, 4, 0)
            mk_copy("B2_%d" % li, li, 3, 0)
            mk_gate("G2_%d" % li, 3, li, 5)
        mk_scale("A1c", 0, 0, 1, 1)
        mk_copy("B1c", 0, 0, 1)
        k.op(dve, lambda: nc.vector.tensor_scalar(S("omka"), V("k_a"), -1.0, 1.0, ALU.mult, ALU.add),
             r=[d_const], w=[d_scal])
        k.op(dve, lambda: nc.vector.tensor_scalar(S("hrk"), V("r_k"), 0.5, None, ALU.mult),
             r=[d_const], w=[d_scal])
        k.barrier()

    def sumsq_rstd(src_fn, n, scratch_bf, rs, d_src, d_scr, d_rs, psi, eps=NORM_EPS, nchunks=KC):
        for c in range(nchunks):
            k.op(act, lambda c=c: nc.scalar.activation(scratch_bf[:, c, :n], src_fn(c), AF.Square),
                 r=[d_src], w=[d_scr])
        for c in range(nchunks):
            k.op(pe, lambda c=c: nc.tensor.matmul(PS[psi][:, :n], ones_bf[:], scratch_bf[:, c, :n],
                                                  start=(c == 0), stop=(c == nchunks - 1)),
                 r=[d_scr, d_const], w=[dPS[psi]])
        k.op(act, lambda: nc.scalar.activation(rs[:, :n], PS[psi][:, :n], AF.Sqrt, bias=eps, scale=1.0 / D),
             r=[dPS[psi]], w=[d_rs])
        k.op(dve, lambda: nc.vector.reciprocal(rs[:, :n], rs[:, :n]), r=[d_rs], w=[d_rs])

    def norm_mod(src, d_src, n, An, Bn, dst_fn, d_dst, wk, h32=None, d_h32=None):
        sumsq_rstd(lambda c: src[:, c, :n], n, wk["sq"], wk["rs"], d_src, wk["dsq"], wk["drs"], 6)
        for c in range(KC):
            k.op(dve, lambda c=c: nc.vector.tensor_tensor(wk["tmp"][:, c, :n], src[:, c, :n], wk["rs"][:, :n], ALU.mult),
                 r=[d_src, wk["drs"]], w=[wk["dtmp"]])
            k.op(act, lambda c=c: nc.scalar.activation(dst_fn(c), wk["tmp"][:, c, :n], AF.Identity,
                                                       bias=S(Bn, c), scale=S(An, c)),
                 r=[wk["dtmp"], d_scal], w=[d_dst])
            if h32 is not None:
                k.op(pool, lambda c=c: nc.gpsimd.tensor_scalar(h32[:, c, :n], wk["tmp"][:, c, :n], S(An, c), S(Bn, c),
                                                               ALU.mult, ALU.add),
                     r=[wk["dtmp"], d_scal], w=[d_h32])

    def res_norm(y, d_y, n, Gn, xprev, d_xprev, xnew, d_xnew, wk):
        sumsq_rstd(lambda c: y[:, c, :n], n, wk["sq"], wk["rs"], d_y, wk["dsq"], wk["drs"], 6)
        for c in range(KC):
            k.op(pool, lambda c=c: nc.gpsimd.tensor_tensor(wk["tmp"][:, c, :n], y[:, c, :n], wk["rs"][:, :n], ALU.mult),
                 r=[d_y, wk["drs"]], w=[wk["dtmp"]])
            k.op(dve, lambda c=c: nc.vector.scalar_tensor_tensor(xnew[:, c, :n], wk["tmp"][:, c, :n], S(Gn, c),
                                                                 xprev[:, c, :n], ALU.mult, ALU.add),
                 r=[wk["dtmp"], d_scal, d_xprev], w=[d_xnew])

    def mk_wk(ph, n=512):
        return dict(sq=k.sb("wsq", [128, KC, n], BF16, ph), rs=k.sb("wrs", [128, n], F32, ph),
                    tmp=k.sb("wtmp", [128, KC, n], F32, ph), dsq=Dep(), drs=Dep(), dtmp=Dep())

    dbg_done = [False]

    def dump(ap_fn, ncols, d, off=0):
        k.dma(sp, dbg_d[:, off:off + ncols], ap_fn(), r=[d])

    lora_stack = contextlib.ExitStack()
    TW = k.sb("TW", [128, NT], BF16, lora_stack)
    TA = k.sb("TA", [128, NT], BF16, lora_stack)
    SG = k.sb("SG", [128, 2, T], BF16, lora_stack)
    d_TW, d_TA, d_SG = Dep(), Dep(), Dep()
    hT_stack = contextlib.ExitStack()
    hT = k.sb("hT", [128, KC, NT], BF16, hT_stack)
    hsT = k.sb("hsT", [128, KC, NT], BF16, hT_stack)
    d_h = Dep()
    d_hs = Dep()
    with contextlib.ExitStack() as ph:
        wk = mk_wk(ph)
        xb = [k.sb("xb%d" % i, [128, KC, 512], F32, ph) for i in range(2)]
        dxb = [Dep(), Dep()]
        k.dma(sp, xb[0][:, :, 0:TC], ctxT_d[:, :, :], w=[dxb[0]])
        norm_mod(xb[0], dxb[0], TC, "A1c", "B1c", lambda c: hT[:, c, 0:TC], d_h, wk)
        for tb in range(4):
            b = (tb + 1) % 2
            k.dma(sp, xb[b][:], xT_d[:, :, tb * 512:(tb + 1) * 512], w=[dxb[b]])
            norm_mod(xb[b], dxb[b], 512, "A1_0", "B1_0",
                     lambda c, tb=tb: hT[:, c, TC + tb * 512:TC + (tb + 1) * 512], d_h, wk)
        k.op(pool, lambda: nc.gpsimd.memset(hsT[:], 0.0), w=[d_hs])
        L0 = TC
        for c in range(KC):
            eng = act if c % 2 == 0 else pool

            def cp(dst, src, eng=eng):
                if eng is act:
                    k.op(act, lambda: nc.scalar.copy(dst, src), r=[d_h], w=[d_hs])
                else:
                    k.op(pool, lambda: nc.gpsimd.tensor_copy(dst, src), r=[d_h], w=[d_hs])
            if c < 4:
                cp(hsT[:, c, 1:TC], hT[:, c, 0:TC - 1])
            else:
                cp(hsT[:, c, 0:TC - 1], hT[:, c, 1:TC])
            if c in (0, 1):
                cp(hsT[:, c, L0 + 1:L0 + T], hT[:, c, L0:L0 + T - 1])
                k.op(pool, lambda c=c: nc.gpsimd.memset(hsT[:, c, L0:L0 + T:64], 0.0), w=[d_hs])
            elif c in (2, 3):
                cp(hsT[:, c, L0:L0 + T - 1], hT[:, c, L0 + 1:L0 + T])
                k.op(pool, lambda c=c: nc.gpsimd.memset(hsT[:, c, L0 + 63:L0 + T:64], 0.0), w=[d_hs])
            elif c in (4, 5):
                cp(hsT[:, c, L0 + 64:L0 + T], hT[:, c, L0:L0 + T - 64])
            else:
                cp(hsT[:, c, L0:L0 + T - 64], hT[:, c, L0 + 64:L0 + T])
        k.barrier()

    if dbg == 1:
        with contextlib.ExitStack() as ph:
            t32 = k.sb("t32", [128, 2 * NT], F32, ph)
            dd = Dep()
            k.op(dve, lambda: nc.vector.tensor_copy(t32[:, 0:NT], hT[:, 0, :]), r=[d_h], w=[dd])
            k.op(dve, lambda: nc.vector.tensor_copy(t32[:, NT:2 * NT], hsT[:, 5, :]), r=[d_hs], w=[dd])
            dump(lambda: t32[:], 2 * NT, dd)
            k.barrier()
        hT_stack.close()
        lora_stack.close()
        k.finish()
        return k


    def load_mixed(ph_, dram_view, ncols, mu_i, name):
        st = k.sb(name + "st", [128, KC, ncols], F32, ph_)
        Wa = k.sb(name + "a", [128, KC, ncols], BF16, ph_)
        Wb = k.sb(name + "b", [128, KC, ncols], BF16, ph_)
        dst_, dw = Dep(), Dep()
        k.dma(sp, st[:], dram_view, w=[dst_])
        for kk in range(KC):
            o = mu_i * 8 + kk
            k.op(dve, lambda kk=kk, o=o: nc.vector.tensor_scalar(Wa[:, kk, :], st[:, kk, :], omu[:, o:o + 1], None, ALU.mult),
                 r=[dst_, d_const], w=[dw])
            k.op(pool, lambda kk=kk, o=o: nc.gpsimd.tensor_scalar(Wb[:, kk, :], st[:, kk, :], V("mu", 0, 48)[:, o:o + 1], None, ALU.mult),
                 r=[dst_, d_const], w=[dw])
        return Wa, Wb, dw

    def proj_mixed(psi, Wa, Wb, dw, c0, c1, t0, n, mrows=128):
        for kk in range(KC):
            k.op(pe, lambda kk=kk: nc.tensor.matmul(PS[psi][:mrows, :n], Wa[:, kk, c0:c1], hT[:, kk, t0:t0 + n],
                                                    start=(kk == 0), stop=False),
                 r=[dw, d_h], w=[dPS[psi]])
        for kk in range(KC):
            k.op(pe, lambda kk=kk: nc.tensor.matmul(PS[psi][:mrows, :n], Wb[:, kk, c0:c1], hsT[:, kk, t0:t0 + n],
                                                    start=False, stop=(kk == KC - 1)),
                 r=[dw, d_hs], w=[dPS[psi]])

    with contextlib.ExitStack() as ph:
        W1a, W1b, dW1 = load_mixed(ph, kview(w1_d, 0, 128), 128, 1, "w1")
        A1a, A1b, dA1 = load_mixed(ph, kview(a1_d, 0, 128), 128, 4, "a1")
        G1a, G1b, dG1 = load_mixed(ph, kview(g1_d, 0, 160), 160, 5, "g1")
        pi = 0
        for tb in range(NT // 256):
            t0 = tb * 256
            proj_mixed(pi % 4, W1a, W1b, dW1, 0, 128, t0, 256)
            k.op(act, lambda p=pi % 4, t0=t0: nc.scalar.activation(TW[:, t0:t0 + 256], PS[p][:, :256], AF.Tanh),
                 r=[dPS[pi % 4]], w=[d_TW])
            pi += 1
            proj_mixed(pi % 4, A1a, A1b, dA1, 0, 128, t0, 256)
            k.op(dve, lambda p=pi % 4, t0=t0: nc.vector.tensor_copy(TA[:, t0:t0 + 256], PS[p][:, :256]),
                 r=[dPS[pi % 4]], w=[d_TA])
            pi += 1
            if t0 >= TC:
                l0 = t0 - TC
                proj_mixed(pi % 4, G1a, G1b, dG1, 0, 128, t0, 256)
                k.op(act, lambda p=pi % 4, l0=l0: nc.scalar.activation(SG[:, 0, l0:l0 + 256], PS[p][:, :256], AF.Sigmoid),
                     r=[dPS[pi % 4]], w=[d_SG])
                pi += 1
                proj_mixed(pi % 4, G1a, G1b, dG1, 128, 160, t0, 256, mrows=32)
                k.op(act, lambda p=pi % 4, l0=l0: nc.scalar.activation(SG[0:32, 1, l0:l0 + 256], PS[p][0:32, :256], AF.Sigmoid),
                     r=[dPS[pi % 4]], w=[d_SG])
                pi += 1
        k.barrier()

    with contextlib.ExitStack() as ph:
        st = k.sb("rkvst", [128, KC, D], F32, ph)
        Wa = k.sb("rkvWa", [128, KC, D], BF16, ph)
        Wb = k.sb("rkvWb", [128, KC, D], BF16, ph)
        rowb = [k.sb("rowb%d" % i, [128, NT], F32, ph) for i in range(2)]
        d_st, d_Wab = Dep(), Dep()
        d_row = [Dep(), Dep()]
        mu_of = [0, 2, 3]
        ri = 0
        pi = 0
        for j in range(3):
            k.dma(sp, st[:], kview(w_rkv_d[j], 0, D), w=[d_st])
            for kk in range(KC):
                o = mu_of[j] * 8 + kk
                k.op(dve, lambda kk=kk, o=o: nc.vector.tensor_scalar(Wa[:, kk, :], st[:, kk, :], omu[:, o:o + 1], None, ALU.mult),
                     r=[d_st, d_const], w=[d_Wab])
                k.op(pool, lambda kk=kk, o=o: nc.gpsimd.tensor_scalar(Wb[:, kk, :], st[:, kk, :], V("mu", 0, 48)[:, o:o + 1], None, ALU.mult),
                     r=[d_st, d_const], w=[d_Wab])
            for c in range(KC):
                rb = rowb[ri % 2]
                drb = d_row[ri % 2]
                ri += 1
                for tb in range(NT // 256):
                    t0 = tb * 256
                    p = pi % 4
                    pi += 1
                    proj_mixed(p, Wa, Wb, d_Wab, c * 128, (c + 1) * 128, t0, 256)
                    if tb % 2 == 0:
                        k.op(act, lambda p=p, rb=rb, t0=t0: nc.scalar.copy(rb[:, t0:t0 + 256], PS[p][:, :256]), r=[dPS[p]], w=[drb])
                    else:
                        k.op(dve, lambda p=p, rb=rb, t0=t0: nc.vector.tensor_copy(rb[:, t0:t0 + 256], PS[p][:, :256]), r=[dPS[p]], w=[drb])
                k.dma(sp, rkv_d[j, :, c, :], rb[:], r=[drb])
        k.barrier()
    hT_stack.close()

    def bc3(t2d, col0, nouter, ostride, ninner):
        base = t2d[:, col0:col0 + 1]
        pst = base.ap[0][0]
        return AP(base.tensor, base.offset, [[pst, 128], [ostride, nouter], [0, ninner]])

    BL = 576
    NBL = NT // BL
    CPB = BL // 64
    GI = 4
    NG = NCH // GI
    with contextlib.ExitStack() as ph:
        r32 = k.sb("r32", [128, NT], F32, ph)
        k32 = k.sb("k32", [128, NT], F32, ph)
        kk32 = k.sb("kk32", [128, NT], F32, ph)
        v16 = k.sb("v16", [128, NT], BF16, ph)
        ksum = k.sb("ksum", [128, NT], F32, ph)
        yz = [k.sb("yz%d" % z, [128, T], F32, ph) for z in range(2)]
        d_yz = [Dep(), Dep()]
        sgw = k.sb("sgw", [128, BL], F32, ph)
        cs = k.sb("cs", [128, BL], F32, ph)
        cs2 = k.sb("cs2", [128, BL], F32, ph)
        iclr = k.sb("iclr", [128, BL], F32, ph)
        t2 = k.sb("t2", [128, BL], F32, ph)
        t3 = k.sb("t3", [128, BL], F32, ph)
        sqb = k.sb("sqb", [128, 512], BF16, ph)
        ost = [k.sb("ost%d" % i, [128, T], BF16, ph) for i in range(1)]
        d_ost = [Dep()]
        w2c = k.sb("w2c", [128, 128], BF16, ph)
        a2c = k.sb("a2c", [128, 128], BF16, ph)
        g2c = k.sb("g2c", [128, 2, 128], BF16, ph)
        d_w2, d_a2, d_g2 = Dep(), Dep(), Dep()
        d_r, d_k, d_kk, d_v, d_ks = Dep(), Dep(), Dep(), Dep(), Dep()
        d_sgw, d_cs, d_cs2, d_iclr, d_t2, d_t3, d_sq = (Dep() for _ in range(7))
        DB = []
        for z in range(2):
            b_ = dict(
                ARbd=k.sb("ARbd%d" % z, [128, NCH, 2, 128], BF16, ph),
                bt=k.sb("bt%d" % z, [128, NT], BF16, ph), kt=k.sb("kt%d" % z, [128, NT], BF16, ph),
                WC=k.sb("WC%d" % z, [128, NCH], F32, ph),
                M32=k.sb("M32%d" % z, [128, 128], F32, ph), S16=k.sb("S16%d" % z, [128, 128], BF16, ph),
                X16=k.sb("X16%d" % z, [64, 128], BF16, ph), U16=k.sb("U16%d" % z, [64, 128], BF16, ph),
                NB=k.sb("NB%d" % z, [64, GI * 128], BF16, ph), LB=k.sb("LB%d" % z, [64, GI * 128], BF16, ph),
                XB=k.sb("XB%d" % z, [64, GI * 128], BF16, ph), LBI=k.sb("LBI%d" % z, [64, GI * 128], BF16, ph), d_LBI=Dep(),
                L1=[k.sb("L1_%d%d" % (z, i), [64, GI, 512], BF16, ph) for i in range(2)],
                TT=[k.sb("TT%d%d" % (z, i), [64, GI, 128], BF16, ph) for i in range(2)],
                tm=[k.sb("tm%d%d" % (z, i), [64, GI, 384], BF16, ph) for i in range(2)],
                d_AR=Dep(), d_bt=Dep(), d_kt=Dep(), d_WC=Dep(), d_M=Dep(), d_S=Dep(), d_X=Dep(), d_U=Dep(),
                d_NB=Dep(), d_LB=Dep(), d_XB=Dep(), d_L1=[Dep(), Dep()], d_TT=[Dep(), Dep()], d_tm=[Dep(), Dep()],
                d_pX=Dep(), d_pU=Dep(), d_pS=Dep(), d_pY=Dep(), prev=None)
            DB.append(b_)
            k.op(pool, lambda b_=b_: nc.gpsimd.memset(b_["ARbd"][:], 0.0), w=[b_["d_AR"]])
        k.min_free = min(getattr(k, "min_free", 1 << 30), nc.sbuf_bytes_remaining)

        for c in range(KC):
            cs0, cs1 = c * 128, (c + 1) * 128
            k.dma(sp, r32[:], rkv_d[0, :, c, :], w=[d_r])
            k.dma(sp, k32[:], rkv_d[1, :, c, :], w=[d_k])
            k.dma(pool, v16[:], rkv_d[2, :, c, :], w=[d_v])
            k.dma(pool, w2c[:], w2_d[:, cs0:cs1], w=[d_w2])
            k.dma(pool, a2c[:], a2_d[:, cs0:cs1], w=[d_a2])
            k.dma(pool, g2c[:, 0, :], g2_d[0:128, cs0:cs1], w=[d_g2])
            k.dma(pool, g2c[0:32, 1, :], g2_d[128:160, cs0:cs1], w=[d_g2])
            k.op(dve, lambda: nc.vector.tensor_scalar(kk32[:], k32[:], Vc("k_k", 0, c), None, ALU.mult),
                 r=[d_k, d_const], w=[d_kk])
            for q in range(0, NT, 512):
                n = min(512, NT - q)
                p = 4 + (q // 512) % 2
                k.op(act, lambda q=q, n=n: nc.scalar.activation(sqb[:, :n], kk32[:, q:q + n], AF.Square),
                     r=[d_kk], w=[d_sq])
                k.op(pe, lambda q=q, n=n, p=p: nc.tensor.matmul(PS[p][:, :n], bd_bf[:], sqb[:, :n], start=True, stop=True),
                     r=[d_sq, d_const], w=[dPS[p]])
                k.op(dve, lambda q=q, n=n, p=p: nc.vector.tensor_scalar(t2[:, :n], PS[p][:, :n], 1e-24, None, ALU.max),
                     r=[dPS[p]], w=[d_t2])
                k.op(act, lambda n=n: nc.scalar.activation(t2[:, :n], t2[:, :n], AF.Sqrt), r=[d_t2], w=[d_t2])
                k.op(dve, lambda n=n: nc.vector.reciprocal(t2[:, :n], t2[:, :n]), r=[d_t2], w=[d_t2])
                k.op(dve, lambda q=q, n=n: nc.vector.tensor_tensor(kk32[:, q:q + n], kk32[:, q:q + n], t2[:, :n], ALU.mult),
                     r=[d_kk, d_t2], w=[d_kk])

            for z in range(2):
                B_ = DB[z]
                ARbd, bt, kt, WC = B_["ARbd"], B_["bt"], B_["kt"], B_["WC"]
                d_AR, d_bt, d_kt, d_WC = B_["d_AR"], B_["d_bt"], B_["d_kt"], B_["d_WC"]
                zs = slice(64 * z, 64 * z + 64)
                for blk in range(NBL):
                    q0 = blk * BL
                    qs = slice(q0, q0 + BL)
                    nb0 = blk * CPB
                    for sbk in range(2):
                        t0 = q0 + sbk * 288
                        o0 = sbk * 288
                        p = 2 + sbk
                        k.op(pe, lambda p=p, t0=t0, zs=zs: nc.tensor.matmul(PS[p][:, :288], w2c[zs, :], TW[zs, t0:t0 + 288],
                                                                           start=True, stop=True), r=[d_w2, d_TW], w=[dPS[p]])
                        k.op(act, lambda p=p, o0=o0, z=z: nc.scalar.activation(sgw[:, o0:o0 + 288], PS[p][:, :288], AF.Sigmoid,
                                                                               bias=Vc("w0", z, c)), r=[dPS[p], d_const], w=[d_sgw])
                        p = 4 + sbk
                        k.op(pe, lambda p=p, t0=t0, zs=zs: nc.tensor.matmul(PS[p][:, :288], a2c[zs, :], TA[zs, t0:t0 + 288],
                                                                           start=True, stop=True), r=[d_a2, d_TA], w=[dPS[p]])
                        k.op(act, lambda p=p, o0=o0, z=z: nc.scalar.activation(iclr[:, o0:o0 + 288], PS[p][:, :288], AF.Sigmoid,
                                                                               bias=Vc("a0", z, c)), r=[dPS[p], d_const], w=[d_iclr])
                    k.op(dve, lambda: nc.vector.tensor_tensor_scan(cs[:], rmask[:, 0:BL], sgw[:], 0.0, ALU.mult, ALU.add),
                         r=[d_sgw, d_const], w=[d_cs])
                    if z == 1:
                        k.op(dve, lambda: nc.vector.tensor_tensor(cs2[:], sgw[:], cs[:], ALU.subtract),
                             r=[d_sgw, d_cs], w=[d_cs2])
                        k.op(dve, lambda: nc.vector.tensor_tensor(
                            cs2[:].rearrange("p (n t) -> p n t", t=64), cs2[:].rearrange("p (n t) -> p n t", t=64),
                            bc3(cs, 63, CPB, 64, 64), ALU.add), r=[d_cs2, d_cs], w=[d_cs2])
                        csu, d_csu = cs2, d_cs2
                    else:
                        csu, d_csu = cs, d_cs
                    k.op(pool, lambda: nc.gpsimd.tensor_scalar(t2[:], iclr[:], Vc("k_a", 0, c), S("omka", c), ALU.mult, ALU.add),
                         r=[d_iclr, d_const, d_scal], w=[d_t2])
                    k.op(pool, lambda qs=qs: nc.gpsimd.tensor_tensor(t2[:], t2[:], k32[:, qs], ALU.mult), r=[d_t2, d_k], w=[d_t2])
                    if z == 0:
                        k.op(pool, lambda qs=qs: nc.gpsimd.tensor_copy(ksum[:, qs], t2[:]), r=[d_t2], w=[d_ks])
                    else:
                        k.op(pool, lambda qs=qs: nc.gpsimd.tensor_tensor(ksum[:, qs], ksum[:, qs], t2[:], ALU.add), r=[d_t2, d_ks], w=[d_ks])
                    k.op(act, lambda csu=csu: nc.scalar.activation(t3[:], csu[:], AF.Exp, scale=-DECAY_S), r=[d_csu], w=[d_t3])
                    k.op(dve, lambda qs=qs, kt=kt: nc.vector.tensor_tensor(kt[:, qs], t2[:], t3[:], ALU.mult), r=[d_t2, d_t3], w=[d_kt])
                    k.op(pool, lambda qs=qs: nc.gpsimd.tensor_tensor(t2[:], kk32[:, qs], iclr[:], ALU.mult), r=[d_kk, d_iclr, d_kt], w=[d_t2])
                    k.op(dve, lambda qs=qs, bt=bt: nc.vector.tensor_tensor(bt[:, qs], t2[:], t3[:], ALU.mult), r=[d_t2, d_t3], w=[d_bt])
                    k.op(act, lambda csu=csu: nc.scalar.activation(t3[:], csu[:], AF.Exp, scale=DECAY_S), r=[d_csu, d_bt], w=[d_t3])
                    wc_col = 63 if z == 0 else 0
                    k.op(pool, lambda nb0=nb0, wc_col=wc_col, WC=WC: nc.gpsimd.tensor_copy(WC[:, nb0:nb0 + CPB], t3[:, wc_col:BL:64]),
                         r=[d_t3], w=[d_WC])
                    for h in range(2):
                        hs_ = slice(64 * h, 64 * h + 64)
                        k.op(dve, lambda h=h, hs_=hs_, qs=qs, nb0=nb0, ARbd=ARbd: nc.vector.tensor_tensor(
                            ARbd[hs_, nb0:nb0 + CPB, h, 64:128], r32[hs_, qs].rearrange("p (n t) -> p n t", t=64),
                            t3[hs_, :].rearrange("p (n t) -> p n t", t=64), ALU.mult), r=[d_r, d_t3], w=[d_AR])
                    k.op(pool, lambda csu=csu: nc.gpsimd.tensor_tensor(t2[:], csu[:], sgw[:], ALU.subtract), r=[d_csu, d_sgw, d_bt], w=[d_t2])
                    k.op(act, lambda: nc.scalar.activation(t2[:], t2[:], AF.Exp, scale=DECAY_S), r=[d_t2], w=[d_t2])
                    for h in range(2):
                        hs_ = slice(64 * h, 64 * h + 64)
                        k.op(dve, lambda h=h, hs_=hs_, qs=qs, nb0=nb0, ARbd=ARbd: nc.vector.scalar_tensor_tensor(
                            ARbd[hs_, nb0:nb0 + CPB, h, 0:64], kk32[hs_, qs].rearrange("p (n t) -> p n t", t=64), -1.0,
                            t2[hs_, :].rearrange("p (n t) -> p n t", t=64), ALU.mult, ALU.mult), r=[d_kk, d_t2], w=[d_AR])

            def pre_stages(z, gb, chunks):
                B_ = DB[z]
                ARbd, bt, kt = B_["ARbd"], B_["bt"], B_["kt"]
                NBg, LBg, XBg, LBIg, d_LBI = B_["NB"], B_["LB"], B_["XB"], B_["LBI"], B_["d_LBI"]
                L1g, TTg, tmg = B_["L1"][gb], B_["TT"][gb], B_["tm"][gb]
                d_AR, d_bt, d_kt = B_["d_AR"], B_["d_bt"], B_["d_kt"]
                d_NB, d_LB, d_XB = B_["d_NB"], B_["d_LB"], B_["d_XB"]
                d_L1, d_TT, d_tm = B_["d_L1"][gb], B_["d_TT"][gb], B_["d_tm"][gb]
                mk = maskF if z == 0 else maskB
                mkL = maskLF if z == 0 else maskLB
                G = len(chunks)
                bA, bN, bL = (2, 3, 4) if z == 0 else (5, 6, 4)
                stages = []

                def stageA(lo, hi, last):
                    for gi in range(lo, hi):
                        n = chunks[gi]
                        t0 = n * 64
                        k.op(pe, lambda t0=t0: nc.tensor.transpose(PSB[0:64, 0:128], bt[:, t0:t0 + 64], ident_bf[:]),
                             r=[d_bt, d_const], w=[dPSB])
                        k.op(pe, lambda t0=t0: nc.tensor.transpose(PSB[0:64, 128:256], kt[:, t0:t0 + 64], ident_bf[:]),
                             r=[d_kt, d_const], w=[dPSB])
                        k.op(pe, lambda t0=t0: nc.tensor.transpose(PSB[0:64, 256:384], v16[:, t0:t0 + 64], ident_bf[:]),
                             r=[d_v, d_const], w=[dPSB])
                        k.op(act, lambda gi=gi: nc.scalar.copy(tmg[:, gi, :], PSB[0:64, 0:384]), r=[dPSB], w=[d_tm])
                        k.op(pe, lambda n=n, t0=t0: nc.tensor.matmul(
                            PS[bA][0:64, 0:256], bt[:, t0:t0 + 64], ARbd[:, n, :, :].rearrange("p h x -> p (h x)"),
                            start=True, stop=True), r=[d_bt, d_AR], w=[dPS[bA]])
                        k.op(pe, lambda n=n, t0=t0: nc.tensor.matmul(
                            PS[bA][0:64, 256:512], kt[:, t0:t0 + 64], ARbd[:, n, :, :].rearrange("p h x -> p (h x)"),
                            start=True, stop=True), r=[d_kt, d_AR], w=[dPS[bA]])
                        k.op(dve, lambda gi=gi: nc.vector.tensor_tensor(L1g[:, gi, :], PS[bA][0:64, :], mk[:], ALU.mult),
                             r=[dPS[bA], d_const], w=[d_L1])
                        for h in range(2):
                            k.op(pe, lambda n=n, t0=t0, h=h, gi=gi: nc.tensor.matmul(
                                PS[bN][0:64, gi * 128 + h * 64: gi * 128 + h * 64 + 64],
                                ARbd[:, n, h, 0:64], bt[:, t0:t0 + 64], start=True, stop=True),
                                r=[d_AR, d_bt], w=[dPS[bN]])
                    if last:
                        k.op(dve, lambda: nc.vector.tensor_tensor(
                            LBg[:, 0:G * 128].rearrange("p (g x) -> p g x", x=128),
                            PS[bN][0:64, 0:G * 128].rearrange("p (g x) -> p g x", x=128),
                            mkL[:, :].unsqueeze(1).to_broadcast([64, G, 128]), ALU.mult), r=[dPS[bN], d_const], w=[d_LB])
                        k.op(pool, lambda: nc.gpsimd.tensor_copy(
                            NBg[:, 0:G * 128].rearrange("p (g h x) -> p g h x", h=2, x=64),
                            L1g[:, 0:G, 0:256].rearrange("p g (h x) -> p g h x", h=2)[:, :, :, 0:64]),
                            r=[d_L1], w=[d_NB])
                        k.op(pool, lambda: nc.gpsimd.tensor_tensor(
                            XBg[:, 0:G * 128].rearrange("p (g x) -> p g x", x=64), NBg[:, 0:G * 128].rearrange("p (g x) -> p g x", x=64),
                            ident_bf[0:64, 0:64].unsqueeze(1).to_broadcast([64, 2 * G, 64]), ALU.add),
                            r=[d_NB, d_const], w=[d_XB])

                def stageC():
                    for q in range(2 * G):
                        qq = slice(q * 64, q * 64 + 64)
                        k.op(pe, lambda qq=qq: nc.tensor.matmul(PS[bN][0:64, qq], LBg[:, qq], NBg[:, qq], start=True, stop=True),
                             r=[d_LB, d_NB], w=[dPS[bN]])
                    for q in range(2 * G):
                        qq = slice(q * 64, q * 64 + 64)
                        k.op(pe, lambda qq=qq: nc.tensor.matmul(PS[bL][0:64, qq], NBg[:, qq], LBg[:, qq], start=True, stop=True),
                             r=[d_LB, d_NB], w=[dPS[bL]])
                    k.op(act, lambda: nc.scalar.copy(NBg[:, 0:G * 128], PS[bN][0:64, 0:G * 128]), r=[dPS[bN]], w=[d_NB])
                    k.op(dve, lambda: nc.vector.tensor_copy(LBg[:, 0:G * 128], PS[bL][0:64, 0:G * 128]), r=[dPS[bL]], w=[d_LB])
                    k.op(pool, lambda: nc.gpsimd.tensor_tensor(
                        LBIg[:, 0:G * 128].rearrange("p (g x) -> p g x", x=64), LBg[:, 0:G * 128].rearrange("p (g x) -> p g x", x=64),
                        ident_bf[0:64, 0:64].unsqueeze(1).to_broadcast([64, 2 * G, 64]), ALU.add),
                        r=[d_LB, d_const], w=[d_LBI])

                def stageD(final):
                    for q in range(2 * G):
                        qq = slice(q * 64, q * 64 + 64)
                        k.op(pe, lambda qq=qq: nc.tensor.matmul(PS[bA][0:64, qq], LBIg[:, qq], XBg[:, qq], start=True, stop=True),
                             r=[d_LBI, d_XB], w=[dPS[bA]])
                    if not final:
                        k.op(act, lambda: nc.scalar.copy(XBg[:, 0:G * 128], PS[bA][0:64, 0:G * 128]), r=[dPS[bA]], w=[d_XB])
                    else:
                        k.op(act, lambda: nc.scalar.copy(
                            TTg[:, 0:G, :].rearrange("p g x -> p (g x)"), PS[bA][0:64, 0:G * 128]),
                            r=[dPS[bA]], w=[d_TT])

                stages.append(lambda: stageA(0, G // 2, False))
                stages.append(lambda: stageA(G // 2, G, True))
                for step in range(5):
                    stages.append(stageC)
                    stages.append(lambda step=step: stageD(step == 4))
                return stages

            def chain_seg(z, gb, gi, n, seg):
                B_ = DB[z]
                ARbd, WC, M32, S16, X16, U16 = B_["ARbd"], B_["WC"], B_["M32"], B_["S16"], B_["X16"], B_["U16"]
                L1g, TTg, tmg = B_["L1"][gb], B_["TT"][gb], B_["tm"][gb]
                d_AR, d_WC, d_M, d_S, d_X, d_U = B_["d_AR"], B_["d_WC"], B_["d_M"], B_["d_S"], B_["d_X"], B_["d_U"]
                d_L1, d_TT, d_tm = B_["d_L1"][gb], B_["d_TT"][gb], B_["d_tm"][gb]
                bk = PS[z]
                dbk = dPS[z]
                xo, uo, so, yo = 0, 128, 256, 384
                pX = bk[0:64, xo:xo + 128]
                pU = bk[0:64, uo:uo + 128]
                t0 = n * 64
                if seg == 0:
                    for h in range(2):
                        k.op(pe, lambda h=h: nc.tensor.matmul(pX, ARbd[:, n, h, 0:64], S16[:], start=(h == 0), stop=False),
                             r=[d_AR, d_S], w=[dbk])
                    for h in range(2):
                        k.op(pe, lambda h=h: nc.tensor.matmul(
                            bk[0:64, xo + h * 64:xo + h * 64 + 64], L1g[:, gi, 256 + h * 128:256 + h * 128 + 64],
                            tmg[:, gi, 256 + h * 64:256 + h * 64 + 64], start=False, stop=(h == 1)),
                            r=[d_L1, d_tm], w=[dbk])
                    k.op(act, lambda: nc.scalar.copy(X16[:], pX), w=[d_X, dbk])
                elif seg == 1:
                    for h in range(2):
                        k.op(pe, lambda h=h: nc.tensor.matmul(
                            bk[0:64, uo + h * 64:uo + h * 64 + 64], TTg[:, gi, h * 64:h * 64 + 64], X16[:, h * 64:h * 64 + 64],
                            start=True, stop=True), r=[d_TT, d_X], w=[dbk])
                    k.op(dve, lambda: nc.vector.tensor_copy(U16[:], pU), w=[d_U, dbk])
                else:
                    if n >= 4:
                        l0 = t0 - TC
                        k.op(pe, lambda: nc.tensor.matmul(
                            bk[:, yo:yo + 128], S16[:], ARbd[:, n, :, 64:128], start=True, stop=False),
                            r=[d_S, d_AR], w=[dbk])
                        for h in range(2):
                            hs_ = slice(64 * h, 64 * h + 64)
                            k.op(pe, lambda h=h, hs_=hs_: nc.tensor.matmul(
                                bk[hs_, yo + h * 64:yo + h * 64 + 64], U16[:, h * 64:h * 64 + 64],
                                L1g[:, gi, h * 128 + 64:h * 128 + 128], start=False, stop=False),
                                r=[d_U, d_L1], w=[dbk])
                            k.op(pe, lambda h=h, hs_=hs_: nc.tensor.matmul(
                                bk[hs_, yo + h * 64:yo + h * 64 + 64], tmg[:, gi, 256 + h * 64:256 + h * 64 + 64],
                                L1g[:, gi, 256 + h * 128 + 64:256 + h * 128 + 128], start=False, stop=(h == 1)),
                                r=[d_tm, d_L1], w=[dbk])
                    k.op(pe, lambda: nc.tensor.matmul(bk[:, so:so + 128], tmg[:, gi, 0:128], U16[:], start=True, stop=False),
                         r=[d_tm, d_U], w=[dbk])
                    k.op(pe, lambda: nc.tensor.matmul(bk[:, so:so + 128], tmg[:, gi, 128:256], tmg[:, gi, 256:384],
                                                      start=False, stop=True), r=[d_tm], w=[dbk])
                    prev = B_["prev"]
                    for h in range(2):
                        hs_ = slice(64 * h, 64 * h + 64)
                        pSd = bk[hs_, so + h * 64:so + h * 64 + 64]
                        if prev is None:
                            k.op(dve, lambda hs_=hs_, pSd=pSd: nc.vector.tensor_copy(M32[hs_, hs_], pSd),
                                 w=[d_M, dbk])
                        else:
                            k.op(dve, lambda hs_=hs_, pSd=pSd, prev=prev: nc.vector.scalar_tensor_tensor(
                                M32[hs_, hs_], M32[hs_, hs_], WC[hs_, prev:prev + 1], pSd, ALU.mult, ALU.add),
                                r=[d_WC], w=[d_M, dbk])
                        k.op(act, lambda hs_=hs_: nc.scalar.activation(
                            S16[hs_, hs_], M32[hs_, hs_], AF.Identity, scale=WC[hs_, n:n + 1]), r=[d_M, d_WC], w=[d_S])
                    if n >= 4:
                        for h in range(2):
                            hs_ = slice(64 * h, 64 * h + 64)
                            k.op(dve, lambda h=h, hs_=hs_: nc.vector.tensor_copy(
                                yz[z][hs_, l0:l0 + 64], bk[hs_, yo + h * 64:yo + h * 64 + 64]), w=[d_yz[z], dbk])
                    B_["prev"] = n

            orders = [list(range(NCH)), [3, 2, 1, 0] + list(range(NCH - 1, 3, -1))]
            groups = [[o[i:i + GI] for i in range(0, NCH, GI)] for o in orders]
            for z in range(2):
                B_ = DB[z]
                B_["prev"] = None
                k.op(dve, lambda B_=B_: nc.vector.memset(B_["M32"][:], 0.0), w=[B_["d_M"]])
                k.op(dve, lambda B_=B_: nc.vector.memset(B_["S16"][:], 0.0), w=[B_["d_S"]])
            pro = [pre_stages(z, 0, groups[z][0]) for z in range(2)]
            for si in range(len(pro[0])):
                for z in range(2):
                    pro[z][si]()
            for gidx in range(NG):
                nxt = [pre_stages(z, (gidx + 1) % 2, groups[z][gidx + 1]) if gidx + 1 < NG else [] for z in range(2)]
                slot = 0
                for gi in range(GI):
                    for seg in range(3):
                        for z in range(2):
                            if slot < len(nxt[z]):
                                nxt[z][slot]()
                            chain_seg(z, gidx % 2, gi, groups[z][gidx][gi], seg)
                        slot += 1

            if dbg == 2 and c == 0:
                dump(lambda: yz[0][:], T, d_yz[0], 0)
                dump(lambda: yz[1][:], T, d_yz[1], T)
                dump(lambda: kk32[:, TC:NT], T, d_kk, 2 * T)
                dump(lambda: ksum[:, TC:NT], T, d_ks, 3 * T)
                k.barrier()
                ph.close()
                lora_stack.close()
                k.finish()
                return k

            ob = ost[0]
            dob = d_ost[0]
            tA, tB, d_tA, d_tB = t2, t3, d_t2, d_t3
            for q in range(0, T, 512):
                qs = slice(q, q + 512)
                qn = slice(TC + q, TC + q + 512)
                W5 = slice(0, 512)
                k.op(pool, lambda qs=qs: nc.gpsimd.tensor_tensor(cs[:, W5], yz[0][:, qs], yz[1][:, qs], ALU.add),
                     r=[d_yz[0], d_yz[1]], w=[d_cs])
                k.op(pe, lambda: nc.tensor.matmul(PS[2][:, :], bdm_f[:], cs[:, W5], start=True, stop=True),
                     r=[d_const, d_cs], w=[dPS[2]])
                k.op(dve, lambda: nc.vector.tensor_tensor(tA[:, W5], cs[:, W5], PS[2][:, :], ALU.subtract),
                     r=[d_cs, dPS[2]], w=[d_tA])
                k.op(pool, lambda: nc.gpsimd.tensor_tensor(tB[:, W5], tA[:, W5], tA[:, W5], ALU.mult),
                     r=[d_tA], w=[d_tB])
                k.op(pe, lambda: nc.tensor.matmul(PS[3][:, :], bdm_f[:], tB[:, W5], start=True, stop=True),
                     r=[d_const, d_tB], w=[dPS[3]])
                k.op(act, lambda: nc.scalar.activation(tB[:, W5], PS[3][:, :], AF.Sqrt, bias=GN_EPS, scale=1.0),
                     r=[dPS[3]], w=[d_tB])
                k.op(dve, lambda: nc.vector.reciprocal(tB[:, W5], tB[:, W5]), r=[d_tB], w=[d_tB])
                k.op(dve, lambda: nc.vector.tensor_tensor(tA[:, W5], tA[:, W5], tB[:, W5], ALU.mult),
                     r=[d_tA, d_tB], w=[d_tA])
                k.op(act, lambda: nc.scalar.activation(tA[:, W5], tA[:, W5], AF.Identity,
                                                       bias=Vc("ln_b", 0, c), scale=Vc("ln_w", 0, c)),
                     r=[d_tA, d_const], w=[d_tA])
                k.op(dve, lambda qn=qn: nc.vector.scalar_tensor_tensor(
                    tB[:, W5], r32[:, qn], S("hrk", c), ksum[:, qn], ALU.mult, ALU.mult),
                    r=[d_r, d_ks, d_scal, d_tB], w=[d_tB])
                k.op(pe, lambda: nc.tensor.matmul(PS[4][:, :], bd1_f[:], tB[:, W5], start=True, stop=True),
                     r=[d_const, d_tB], w=[dPS[4]])
                k.op(dve, lambda qn=qn: nc.vector.tensor_tensor(tB[:, W5], PS[4][:, :], v16[:, qn], ALU.mult),
                     r=[dPS[4], d_v, d_tB], w=[d_tB])
                k.op(pool, lambda: nc.gpsimd.tensor_tensor(tA[:, W5], tA[:, W5], tB[:, W5], ALU.add),
                     r=[d_tA, d_tB], w=[d_tA])
                k.op(pe, lambda qs=qs: nc.tensor.matmul(PS[5][:, :], g2c[:, 0, :], SG[:, 0, qs], start=True, stop=False),
                     r=[d_g2, d_SG], w=[dPS[5]])
                k.op(pe, lambda qs=qs: nc.tensor.matmul(PS[5][:, :], g2c[0:32, 1, :], SG[0:32, 1, qs], start=False, stop=True),
                     r=[d_g2, d_SG], w=[dPS[5]])
                k.op(dve, lambda qs=qs: nc.vector.tensor_tensor(ob[:, qs], tA[:, W5], PS[5][:, :], ALU.mult),
                     r=[d_tA, dPS[5]], w=[dob])
            k.dma(sp, oT_d[:, c, :], ob[:], r=[dob])
        k.barrier()
    lora_stack.close()

    h2_stack = contextlib.ExitStack()
    h2 = k.sb("h2", [128, KC, T], BF16, h2_stack)
    d_h2 = [Dep() for _ in range(4)]
    d_xres = [Dep() for _ in range(4)]

    def out_proj_phase(w_dram, yin, d_yin, Gn, xprev_dram, An, Bn, router=None):
        with contextlib.ExitStack() as ph:
            wk = mk_wk(ph)
            wo = k.sb("wo", [128, KC, D], BF16, ph)
            d_wo = Dep()
            k.dma(pool, wo[:], kview(w_dram, 0, D), w=[d_wo])
            ym = k.sb("ym", [128, KC, 512], F32, ph)
            xp = k.sb("xp", [128, KC, 512], F32, ph)
            xn = k.sb("xn", [128, KC, 512], F32, ph)
            d_ym, d_xp, d_xn = Dep(), Dep(), Dep()
            if router is not None:
                h32 = k.sb("h32", [128, KC, 512], F32, ph)
                d_h32 = Dep()
            for tb in range(4):
                ts_ = slice(tb * 512, (tb + 1) * 512)
                k.dma(sp, xp[:], xprev_dram[:, :, ts_], r=[d_xres[tb]], w=[d_xp])
                for dc in range(KC):
                    p = dc % 4
                    for kk in range(KC):
                        k.op(pe, lambda dc=dc, kk=kk, p=p, ts_=ts_: nc.tensor.matmul(
                            PS[p][:, :], wo[:, kk, dc * 128:(dc + 1) * 128], yin[:, kk, ts_],
                            start=(kk == 0), stop=(kk == KC - 1)), r=[d_wo, d_yin], w=[dPS[p]])
                    k.op(act, lambda dc=dc, p=p: nc.scalar.copy(ym[:, dc, :], PS[p][:, :]), r=[dPS[p]], w=[d_ym])
                res_norm(ym, d_ym, 512, Gn, xp, d_xp, xn, d_xn, wk)
                k.dma(sp, xres_d[:, :, ts_], xn[:], r=[d_xn], w=[d_xres[tb]])
                norm_mod(xn, d_xn, 512, An, Bn, lambda c, ts_=ts_: h2[:, c, ts_], d_h2[tb], wk,
                         h32=(h32 if router is not None else None), d_h32=(d_h32 if router is not None else None))
                if router is not None:
                    router(tb, h32, d_h32)
            k.barrier()

    def ffn_pass(wgu_dram, wd_dram, acc, d_acc, first, wbufs, gate_bc=None, d_gate=None):
        for fg in range(FF // 512):
            b = wbufs["i"] % 2
            wbufs["i"] += 1
            Wg, Wu, Wd = wbufs["g"][b], wbufs["u"][b], wbufs["d"][b]
            dW = wbufs["dep"][b]
            k.dma(pool, Wg[:], kview(wgu_dram, fg * 512, (fg + 1) * 512), w=[dW])
            k.dma(pool, Wu[:], kview(wgu_dram, FF + fg * 512, FF + (fg + 1) * 512), w=[dW])
            k.dma(pool, Wd[:], wd_dram[fg * 512:(fg + 1) * 512, :].rearrange("(f p) d -> p f d", p=128), w=[dW])
            for tb in range(4):
                ts_ = slice(tb * 512, (tb + 1) * 512)
                ab = wbufs["ai"] % 2
                wbufs["ai"] += 1
                actb = wbufs["act"][ab]
                d_actb = wbufs["dact"][ab]
                for fc in range(4):
                    pg = (fc % 2) * 2
                    pu = pg + 1
                    for kk in range(KC):
                        k.op(pe, lambda kk=kk, fc=fc, pg=pg, ts_=ts_: nc.tensor.matmul(
                            PS[pg][:, :], Wg[:, kk, fc * 128:(fc + 1) * 128], h2[:, kk, ts_],
                            start=(kk == 0), stop=(kk == KC - 1)), r=[dW, d_h2[tb]], w=[dPS[pg]])
                    for kk in range(KC):
                        k.op(pe, lambda kk=kk, fc=fc, pu=pu, ts_=ts_: nc.tensor.matmul(
                            PS[pu][:, :], Wu[:, kk, fc * 128:(fc + 1) * 128], h2[:, kk, ts_],
                            start=(kk == 0), stop=(kk == KC - 1)), r=[dW, d_h2[tb]], w=[dPS[pu]])
                    sgb = wbufs["sg"][fc % 2]
                    d_sgb = wbufs["dsg"][fc % 2]
                    k.op(act, lambda pg=pg, sgb=sgb: nc.scalar.activation(sgb[:], PS[pg][:, :], AF.Silu),
                         r=[dPS[pg]], w=[d_sgb])
                    if gate_bc is None:
                        k.op(dve, lambda fc=fc, pu=pu, sgb=sgb, actb=actb: nc.vector.tensor_tensor(
                            actb[:, fc, :], sgb[:], PS[pu][:, :], ALU.mult), r=[d_sgb, dPS[pu]], w=[d_actb])
                    else:
                        k.op(dve, lambda fc=fc, pu=pu, sgb=sgb: nc.vector.tensor_tensor(
                            sgb[:], sgb[:], PS[pu][:, :], ALU.mult), r=[d_sgb, dPS[pu]], w=[d_sgb])
                        k.op(dve, lambda fc=fc, sgb=sgb, actb=actb, ts_=ts_: nc.vector.tensor_tensor(
                            actb[:, fc, :], sgb[:], gate_bc[:, ts_], ALU.mult), r=[d_sgb, d_gate], w=[d_actb])
                for dc in range(KC):
                    p = 4 + dc % 2
                    for fc in range(4):
                        k.op(pe, lambda dc=dc, fc=fc, p=p, actb=actb: nc.tensor.matmul(
                            PS[p][:, :], Wd[:, fc, dc * 128:(dc + 1) * 128], actb[:, fc, :],
                            start=(fc == 0), stop=(fc == 3)), r=[dW, d_actb], w=[dPS[p]])
                    if first and fg == 0:
                        k.op(act, lambda dc=dc, p=p, ts_=ts_: nc.scalar.copy(acc[:, dc, ts_], PS[p][:, :]),
                             r=[dPS[p]], w=[d_acc[tb]])
                    else:
                        k.op(dve, lambda dc=dc, p=p, ts_=ts_: nc.vector.tensor_tensor(
                            acc[:, dc, ts_], acc[:, dc, ts_], PS[p][:, :], ALU.add), r=[dPS[p], d_acc[tb]], w=[d_acc[tb]])

    def mk_ffn_bufs(ph):
        return dict(i=0, ai=0,
                    g=[k.sb("Wg%d" % i, [128, KC, 512], BF16, ph) for i in range(2)],
                    u=[k.sb("Wu%d" % i, [128, KC, 512], BF16, ph) for i in range(2)],
                    d=[k.sb("Wd%d" % i, [128, 4, D], BF16, ph) for i in range(2)],
                    dep=[Dep(), Dep()],
                    act=[k.sb("actb%d" % i, [128, 4, 512], BF16, ph) for i in range(2)],
                    dact=[Dep(), Dep()],
                    sg=[k.sb("sgb%d" % i, [128, 512], F32, ph) for i in range(2)],
                    dsg=[Dep(), Dep()])

    def post_ffn_phase(acc, d_acc, Gn, An, Bn, final):
        with contextlib.ExitStack() as ph:
            wk = mk_wk(ph)
            xp = k.sb("xp", [128, KC, 512], F32, ph)
            xn = k.sb("xn", [128, KC, 512], F32, ph)
            d_xp, d_xn = Dep(), Dep()
            for tb in range(4):
                ts_ = slice(tb * 512, (tb + 1) * 512)
                k.dma(sp, xp[:], xres_d[:, :, ts_], r=[d_xres[tb]], w=[d_xp])

                sumsq_rstd(lambda c: acc[:, c, ts_], 512, wk["sq"], wk["rs"], d_acc[tb], wk["dsq"], wk["drs"], 6)
                for c in range(KC):
                    k.op(pool, lambda c=c, ts_=ts_: nc.gpsimd.tensor_tensor(wk["tmp"][:, c, :], acc[:, c, ts_], wk["rs"][:, :], ALU.mult),
                         r=[d_acc[tb], wk["drs"]], w=[wk["dtmp"]])
                    k.op(dve, lambda c=c: nc.vector.scalar_tensor_tensor(xn[:, c, :], wk["tmp"][:, c, :], S(Gn, c),
                                                                         xp[:, c, :], ALU.mult, ALU.add),
                         r=[wk["dtmp"], d_scal, d_xp], w=[d_xn])
                if final:
                    k.dma(sp, out_d[:, :, ts_], xn[:], r=[d_xn])
                else:
                    k.dma(sp, xres_d[:, :, ts_], xn[:], r=[d_xn], w=[d_xres[tb]])
                    norm_mod(xn, d_xn, 512, An, Bn, lambda c, ts_=ts_: h2[:, c, ts_], d_h2[tb], wk)
            k.barrier()

    with contextlib.ExitStack() as yst:
        yin0 = k.sb("yin0", [128, KC, T], BF16, yst)
        d_yin0 = Dep()
        k.dma(sp, yin0[:], oT_d[:, :, :], w=[d_yin0])
        out_proj_phase(wout_d, yin0, d_yin0, "G1_0", xT_d, "A2_0", "B2_0")

    if dbg == 3:
        with contextlib.ExitStack() as ph:
            t32 = k.sb("t32", [128, T], F32, ph)
            dd = Dep()
            k.op(dve, lambda: nc.vector.tensor_copy(t32[:], h2[:, 0, :]), r=d_h2, w=[dd])
            dump(lambda: t32[:], T, dd)
            k.barrier()
        h2_stack.close()
        k.finish()
        return k

    acc_stack = contextlib.ExitStack()
    acc = k.sb("acc", [128, KC, T], F32, acc_stack)
    d_acc = [Dep() for _ in range(4)]
    with contextlib.ExitStack() as ph:
        wbufs = mk_ffn_bufs(ph)
        ffn_pass(fgu_d, fd_d, acc, d_acc, True, wbufs)
        k.barrier()
    post_ffn_phase(acc, d_acc, "G2_0", "A1_1", "B1_1", final=False)
    acc_stack.close()

    gates_stack = contextlib.ExitStack()
    logit = k.sb("logit", [128, 16, NE], F32, gates_stack)
    gates = k.sb("gates", [128, 16, NE], F32, gates_stack)
    wr32 = k.sb("wr32", [128, KC, NE], F32, gates_stack)
    d_logit, d_gates, d_wr = Dep(), Dep(), Dep()
    k.dma(sp, wr32[:], rt_d[:, :, :], w=[d_wr])

    ycv_stack = contextlib.ExitStack()
    ycv = k.sb("ycv", [128, KC, T], BF16, ycv_stack)
    d_ycv = Dep()
    with contextlib.ExitStack() as ph:
        Wc3 = [k.sb("Wc3_%d" % i, [128, KC, 3, 128], BF16, ph) for i in range(2)]
        dWc3 = [Dep(), Dep()]
        Bsb = k.sb("Bsb", [128, T], F32, ph)
        Csb = k.sb("Csb", [128, 512], F32, ph)
        zp = k.sb("zp", [128, T + 2], F32, ph)
        t1 = k.sb("t1", [128, T], F32, ph)
        d_B, d_C, d_z, d_t1 = Dep(), Dep(), Dep(), Dep()
        k.op(dve, lambda: nc.vector.memset(zp[:], 0.0), w=[d_z])
        for c in range(KC):
            W3 = Wc3[c % 2]
            dW3 = dWc3[c % 2]
            for j in range(3):
                k.dma(pool, W3[:, :, j, :], kview(cwin_d, j * D + c * 128, j * D + (c + 1) * 128), w=[dW3])
            for tb in range(4):
                ts_ = slice(tb * 512, (tb + 1) * 512)
                for j in range(3):
                    p = j
                    for kk in range(KC):
                        k.op(pe, lambda kk=kk, j=j, p=p, ts_=ts_, W3=W3: nc.tensor.matmul(
                            PS[p][:, :], W3[:, kk, j, :], h2[:, kk, ts_], start=(kk == 0), stop=(kk == KC - 1)),
                            r=[dW3, d_h2[tb]], w=[dPS[p]])
                k.op(act, lambda ts_=ts_: nc.scalar.copy(Bsb[:, ts_], PS[0][:, :]), r=[dPS[0]], w=[d_B])
                k.op(act, lambda: nc.scalar.copy(Csb[:], PS[1][:, :]), r=[dPS[1]], w=[d_C])
                k.op(dve, lambda tb=tb: nc.vector.tensor_tensor(zp[:, 1 + tb * 512:1 + (tb + 1) * 512], Csb[:], PS[2][:, :], ALU.mult),
                     r=[d_C, dPS[2]], w=[d_z])
            k.op(act, lambda c=c: nc.scalar.activation(t1[:], zp[:, 0:T], AF.Identity, scale=Vc("conv_w", 0, c)),
                 r=[d_z, d_const], w=[d_t1])
            k.op(dve, lambda c=c: nc.vector.scalar_tensor_tensor(t1[:], zp[:, 1:T + 1], Vc("conv_w", 1, c), t1[:], ALU.mult, ALU.add),
                 r=[d_z, d_const, d_t1], w=[d_t1])
            k.op(dve, lambda c=c: nc.vector.scalar_tensor_tensor(t1[:], zp[:, 2:T + 2], Vc("conv_w", 2, c), t1[:], ALU.mult, ALU.add),
                 r=[d_z, d_const, d_t1], w=[d_t1])
            k.op(pool, lambda c=c: nc.gpsimd.tensor_tensor(ycv[:, c, :], t1[:], Bsb[:], ALU.mult),
                 r=[d_t1, d_B], w=[d_ycv])
        k.barrier()

    def router(tb, h32, d_h32):
        for sub in range(4):
            tt = tb * 4 + sub
            for c in range(KC):
                k.op(pe, lambda c=c, sub=sub: nc.tensor.matmul(
                    PS[5][:, sub * 8:sub * 8 + 8], h32[:, c, sub * 128:(sub + 1) * 128], wr32[:, c, :],
                    start=(c == 0), stop=(c == KC - 1)), r=[d_h32, d_wr], w=[dPS[5]])
        k.op(dve, lambda tb=tb: nc.vector.tensor_copy(
            logit[:, tb * 4:(tb + 1) * 4, :], PS[5][:, 0:32].rearrange("p (s e) -> p s e", e=8)),
            r=[dPS[5]], w=[d_logit])

    out_proj_phase(cwout_d, ycv, d_ycv, "G1_1", xres_d, "A2_1", "B2_1", router=router)
    ycv_stack.close()

    with contextlib.ExitStack() as ph:
        mx = k.sb("mx", [128, 16, 8], F32, ph)
        e1 = k.sb("e1", [128, 16], F32, ph)
        g1_ = k.sb("g1_", [128, 16], F32, ph)
        g2_ = k.sb("g2_", [128, 16], F32, ph)
        q1 = k.sb("q1", [128, 16, 8], F32, ph)
        q2 = k.sb("q2", [128, 16, 8], F32, ph)
        d_mx, d_e = Dep(), Dep()
        for tt in range(16):
            k.op(dve, lambda tt=tt: nc.vector.max(mx[:, tt, :], logit[:, tt, :]), r=[d_logit], w=[d_mx])
        k.op(dve, lambda: nc.vector.tensor_tensor(e1[:], mx[:, :, 1], mx[:, :, 0], ALU.subtract), r=[d_mx], w=[d_e])
        k.op(act, lambda: nc.scalar.activation(e1[:], e1[:], AF.Exp), r=[d_e], w=[d_e])
        k.op(dve, lambda: nc.vector.tensor_scalar(g1_[:], e1[:], 1.0, None, ALU.add), r=[d_e], w=[d_e])
        k.op(dve, lambda: nc.vector.reciprocal(g1_[:], g1_[:]), r=[d_e], w=[d_e])
        k.op(dve, lambda: nc.vector.tensor_tensor(g2_[:], e1[:], g1_[:], ALU.mult), r=[d_e], w=[d_e])
        k.op(dve, lambda: nc.vector.tensor_tensor(q1[:], logit[:], mx[:, :, 0:1].to_broadcast([128, 16, 8]), ALU.is_equal),
             r=[d_logit, d_mx], w=[d_e])
        k.op(dve, lambda: nc.vector.tensor_tensor(q2[:], logit[:], mx[:, :, 1:2].to_broadcast([128, 16, 8]), ALU.is_equal),
             r=[d_logit, d_mx], w=[d_e])
        k.op(dve, lambda: nc.vector.tensor_tensor(q1[:], q1[:], g1_[:, :].unsqueeze(2).to_broadcast([128, 16, 8]), ALU.mult),
             r=[d_e], w=[d_e])
        k.op(dve, lambda: nc.vector.tensor_tensor(q2[:], q2[:], g2_[:, :].unsqueeze(2).to_broadcast([128, 16, 8]), ALU.mult),
             r=[d_e], w=[d_e])
        k.op(dve, lambda: nc.vector.tensor_tensor(gates[:], q1[:], q2[:], ALU.add), r=[d_e], w=[d_gates])
        k.barrier()

    if dbg == 4:
        dump(lambda: gates[:].rearrange("p t e -> p (t e)"), 128, d_gates, 0)
        dump(lambda: logit[:].rearrange("p t e -> p (t e)"), 128, d_logit, 128)
        k.barrier()
        gates_stack.close()
        h2_stack.close()
        k.finish()
        return k

    acc_stack = contextlib.ExitStack()
    acc = k.sb("acc2", [128, KC, T], F32, acc_stack)
    d_acc = [Dep() for _ in range(4)]
    with contextlib.ExitStack() as ph:
        wbufs = mk_ffn_bufs(ph)
        gbc = [k.sb("gbc%d" % i, [128, T], F32, ph) for i in range(2)]
        d_gbc = [Dep(), Dep()]
        Gm = [k.sb("Gm%d" % i, [128, 128], F32, ph) for i in range(2)]
        d_Gm = [Dep(), Dep()]
        ident_f = cst[:, C_ID:C_ID + 128]
        for e in range(NE):
            gb = gbc[e % 2]
            dgb = d_gbc[e % 2]
            for tt in range(16):
                gm = Gm[tt % 2]
                dgm = d_Gm[tt % 2]
                k.op(dve, lambda tt=tt, e=e, gm=gm: nc.vector.tensor_copy(gm[:], gates[:, tt, e:e + 1].to_broadcast([128, 128])),
                     r=[d_gates], w=[dgm])
                k.op(pe, lambda tt=tt, gm=gm: nc.tensor.matmul(PS[6][:, (tt % 4) * 128:(tt % 4 + 1) * 128], gm[:], ident_f,
                                                              start=True, stop=True), r=[dgm, d_const], w=[dPS[6]])
                if tt % 4 == 3:
                    q = (tt // 4) * 512
                    k.op(act, lambda q=q, gb=gb: nc.scalar.copy(gb[:, q:q + 512], PS[6][:, :]), r=[dPS[6]], w=[dgb])
            ffn_pass(mgu_d[e], md_d[e], acc, d_acc, e == 0, wbufs, gate_bc=gb, d_gate=dgb)
        k.barrier()
    post_ffn_phase(acc, d_acc, "G2_1", None, None, final=True)
    acc_stack.close()
    gates_stack.close()
    h2_stack.close()
    k.finish()
    return k


def prep_inputs(inp):
    f = lambda a: np.ascontiguousarray(np.asarray(a, np.float32))
    vec = np.zeros((128, NV), np.float32)

    def put(name, arr):
        a = col(arr)
        vec[:, VOFF[name]:VOFF[name] + a.shape[1]] = a

    put("g", f(inp["norm_g"]).reshape(-1))
    put("mu", f(inp["rwkv_mu"]).reshape(-1))
    put("w0", f(inp["rwkv_w0"]).reshape(-1))
    put("a0", f(inp["rwkv_a0"]).reshape(-1))
    put("k_k", f(inp["rwkv_k_k"]).reshape(-1))
    put("k_a", f(inp["rwkv_k_a"]).reshape(-1))
    put("r_k", f(inp["rwkv_r_k"]).reshape(-1))
    put("ln_w", f(inp["rwkv_ln_w"]).reshape(-1))
    put("ln_b", f(inp["rwkv_ln_b"]).reshape(-1))
    put("conv_w", f(inp["conv_w"]).reshape(-1))
    mb = col(f(inp["mod_b"]).reshape(-1))
    vec[:, VOFF["modb"]:VOFF["modb"] + 192] = np.repeat(mb, 2, axis=1)
    shared = {
        "vecs": vec,
        "cst": make_consts(),
        "mod_w": f(inp["mod_w"]),
        "w_rkv": f(inp["rwkv_w_rkv"])[0],
        "w1cat": np.ascontiguousarray(np.concatenate([f(inp["rwkv_w1"])[0, 0], f(inp["rwkv_w1"])[0, 1]], axis=1)),
        "w2cat": np.ascontiguousarray(f(inp["rwkv_w2"])[0].reshape(128, D)),
        "a1cat": np.ascontiguousarray(np.concatenate([f(inp["rwkv_a1"])[0, 0], f(inp["rwkv_a1"])[0, 1]], axis=1)),
        "a2cat": np.ascontiguousarray(f(inp["rwkv_a2"])[0].reshape(128, D)),
        "g1": f(inp["rwkv_g1"])[0],
        "g2": f(inp["rwkv_g2"])[0],
        "w_out": f(inp["rwkv_w_out"])[0],
        "conv_w_in": f(inp["conv_w_in"])[0],
        "conv_w_out": f(inp["conv_w_out"])[0],
        "ffn_w_gu": f(inp["ffn_w_gu"])[0],
        "ffn_w_down": f(inp["ffn_w_down"])[0],
        "router": np.ascontiguousarray(f(inp["moe_router"])[0].reshape(KC, 128, NE).transpose(1, 0, 2)),
        "moe_w_gu": f(inp["moe_w_gu"])[0],
        "moe_w_down": f(inp["moe_w_down"])[0],
    }
    x = f(inp["x"])
    ctx = f(inp["ctx"])
    c = f(inp["c"])
    cc = f(inp["c_ctx"])
    maps = []
    for b in range(8):
        m = dict(shared)
        m["xT"] = np.ascontiguousarray(x[b].T.reshape(KC, 128, T).transpose(1, 0, 2))
        m["ctxT"] = np.ascontiguousarray(ctx[b].T.reshape(KC, 128, TC).transpose(1, 0, 2))
        m["cvec"] = np.ascontiguousarray(np.stack([col(c[b]), col(cc)], axis=2))
        maps.append(m)
    return maps


def kernel(**inputs):
    maps = prep_inputs(inputs)
    kb = build()
    res = run_bass_kernel_spmd(kb.nc, maps, core_ids=list(range(8)))
    outs = []
    for b in range(8):
        yT = np.asarray(res.results[b]["yT"], np.float32)
        outs.append(yT.transpose(1, 0, 2).reshape(D, T).T)
    return np.ascontiguousarray(np.stack(outs, 0).astype(np.float32))
```

```python
import contextlib
import numpy as np
import concourse.bass as bass
import concourse.mybir as mybir
from concourse.ap import AP
from concourse.bass_utils import run_bass_kernel_spmd

F32 = mybir.dt.float32
BF16 = mybir.dt.bfloat16
AF = mybir.ActivationFunctionType
ALU = mybir.AluOpType

T = 2048
TC = 256
NT = T + TC
D = 1024
KC = 8
FF = 3584
NE = 8
NCH = NT // 64
NORM_EPS = 1e-6
GN_EPS = 64e-5
DECAY_S = -0.6065306597126334


class Dep:
    __slots__ = ("w", "rs")

    def __init__(self):
        self.w = None
        self.rs = {}


class Eng:
    def __init__(self, name, obj, is_pe=False):
        self.name = name
        self.obj = obj
        self.is_pe = is_pe
        self.sem = None
        self.semid = None
        self.cnt = 0
        self.seen = {}


class Slot:
    def __init__(self, sem, semid):
        self.sem = sem
        self.semid = semid
        self.cnt = 0
        self.last = None


class KB:
    def __init__(self):
        self.nc = bass.Bass("TRN2", target_bir_lowering=False)
        nc = self.nc
        self.es = contextlib.ExitStack()
        self.sems = []
        self.epoch = 0
        self.pe = Eng("pe", nc.tensor, True)
        self.act = Eng("act", nc.scalar)
        self.dve = Eng("dve", nc.vector)
        self.pool = Eng("pool", nc.gpsimd)
        self.sp = Eng("sp", nc.sync)
        self.engs = [self.pe, self.act, self.dve, self.pool, self.sp]
        for e in self.engs:
            self._fresh_sem(e)
        self.slots = {}
        self.slot_i = {}
        for q in (self.sp, self.pool):
            self.slots[q.name] = [Slot(*self._newsem("dq%s%d" % (q.name, i))) for i in range(8)]
            self.slot_i[q.name] = 0
        self.ninstr = 0
        self.uid = 0

    def _newsem(self, name):
        s = self.es.enter_context(self.nc.semaphore("%s_%d" % (name, len(self.sems))))
        self.sems.append(s)
        return s, len(self.sems) - 1

    def _fresh_sem(self, e):
        e.sem, e.semid = self._newsem("e" + e.name)
        e.cnt = 0

    def sb(self, name, shape, dtype, stack=None):
        self.uid += 1
        return (stack or self.es).enter_context(
            self.nc.sbuf_tensor("%s_%d" % (name, self.uid), list(shape), dtype))

    def psum(self, name, shape, dtype, stack=None):
        self.uid += 1
        return (stack or self.es).enter_context(
            self.nc.psum_tensor("%s_%d" % (name, self.uid), list(shape), dtype))

    def _wait(self, eng, tk, war=False):
        semid, val, src, ep = tk
        if ep != self.epoch:
            return
        if src is eng:
            if eng.is_pe or war:
                return
        if eng.seen.get(semid, 0) >= val:
            return
        eng.obj.wait_ge(self.sems[semid], val)
        eng.seen[semid] = val
        self.ninstr += 1

    def _deps(self, eng, r, w):
        for d in r:
            if d.w is not None:
                self._wait(eng, d.w)
        for d in w:
            if d.w is not None:
                self._wait(eng, d.w)
            for t in d.rs.values():
                self._wait(eng, t, war=True)

    def _mark(self, tk, r, w):
        for d in r:
            d.rs[tk[0]] = tk
        for d in w:
            d.w = tk
            d.rs = {}

    def op(self, eng, fn, r=(), w=()):
        self._deps(eng, r, w)
        ins = fn()
        eng.cnt += 1
        ins.then_inc(eng.sem, 1)
        tk = (eng.semid, eng.cnt, eng, self.epoch)
        self._mark(tk, r, w)
        self.ninstr += 1
        return tk

    def dma(self, q, out, in_, r=(), w=(), **kw):
        sl = self.slots[q.name]
        i = self.slot_i[q.name]
        self.slot_i[q.name] = (i + 1) % len(sl)
        s = sl[i]
        if s.last is not None:
            self._wait(q, s.last)
        self._deps(q, r, w)
        ins = q.obj.dma_start(out=out, in_=in_, **kw)
        s.cnt += 16
        ins.then_inc(s.sem, 16)
        tk = (s.semid, s.cnt, None, self.epoch)
        s.last = tk
        self._mark(tk, r, w)
        self.ninstr += 1
        return tk

    def barrier(self):
        tks = []
        for e in self.engs:
            if e.cnt > 0:
                tks.append((e.semid, e.cnt, e, self.epoch))
        for sl in self.slots.values():
            for s in sl:
                if s.last is not None and s.last[3] == self.epoch:
                    tks.append(s.last)
        for e in self.engs:
            for tk in tks:
                semid, val, src, ep = tk
                if e.seen.get(semid, 0) >= val:
                    continue
                e.obj.wait_ge(self.sems[semid], val)
                e.seen[semid] = val
                self.ninstr += 1
        self.epoch += 1
        for e in self.engs:
            self._fresh_sem(e)
            e.seen = {}

    def finish(self):
        self.barrier()
        self.es.close()


VEC_SPEC = [("g", 2 * 4 * 8), ("mu", 6 * 8), ("w0", 16), ("a0", 16), ("k_k", 8), ("k_a", 8), ("r_k", 8),
            ("ln_w", 8), ("ln_b", 8), ("conv_w", 24), ("modb", 192)]
VOFF = {}
_o = 0
for _n, _c in VEC_SPEC:
    VOFF[_n] = _o
    _o += _c
NV = _o
C_ID, C_MF, C_MB, C_LF, C_LB, NCST = 0, 128, 640, 1152, 1280, 1408


def col(v):
    v = np.asarray(v, np.float32).reshape(-1, 128)
    return np.ascontiguousarray(v.T)


def make_consts():
    c = np.zeros((128, NCST), np.float32)
    c[:, C_ID:C_ID + 128] = np.eye(128, dtype=np.float32)
    s = np.arange(64)[:, None]
    t = np.arange(64)[None, :]
    strict_f = (s < t).astype(np.float32)
    incl_f = (s <= t).astype(np.float32)
    strict_b = (s > t).astype(np.float32)
    incl_b = (s >= t).astype(np.float32)
    mf = np.concatenate([strict_f, incl_f], 1)
    mb = np.concatenate([strict_b, incl_b], 1)
    c[:64, C_MF:C_MF + 512] = np.tile(mf, (1, 4))
    c[:64, C_MB:C_MB + 512] = np.tile(mb, (1, 4))
    c[:64, C_LF:C_LF + 128] = np.tile(strict_b, (1, 2))
    c[:64, C_LB:C_LB + 128] = np.tile(strict_f, (1, 2))
    return c


def build(dbg=None, dbgn=0):
    k = KB()
    nc = k.nc
    pe, act, dve, pool, sp = k.pe, k.act, k.dve, k.pool, k.sp

    def din(name, shape, dt=F32):
        return nc.dram_tensor(name, list(shape), dt, kind="ExternalInput").ap()

    xT_d = din("xT", [128, KC, T])
    ctxT_d = din("ctxT", [128, KC, TC])
    cvec_d = din("cvec", [128, KC, 2])
    vec_d = din("vecs", [128, NV])
    cst_d = din("cst", [128, NCST])
    mod_w_d = din("mod_w", [2, D, 6 * D])
    w_rkv_d = din("w_rkv", [3, D, D])
    w1_d = din("w1cat", [D, 128])
    w2_d = din("w2cat", [128, D])
    a1_d = din("a1cat", [D, 128])
    a2_d = din("a2cat", [128, D])
    g1_d = din("g1", [D, 160])
    g2_d = din("g2", [160, D])
    wout_d = din("w_out", [D, D])
    cwin_d = din("conv_w_in", [D, 3 * D])
    cwout_d = din("conv_w_out", [D, D])
    fgu_d = din("ffn_w_gu", [D, 2 * FF])
    fd_d = din("ffn_w_down", [FF, D])
    rt_d = din("router", [128, KC, NE])
    mgu_d = din("moe_w_gu", [NE, D, 2 * FF])
    md_d = din("moe_w_down", [NE, FF, D])
    out_d = nc.dram_tensor("yT", [128, KC, T], F32, kind="ExternalOutput").ap()
    xres_d = nc.dram_tensor("xres", [128, KC, T], F32, kind="Internal").ap()
    oT_d = nc.dram_tensor("oT", [128, KC, T], BF16, kind="Internal").ap()
    rkv_d = nc.dram_tensor("rkvs", [3, 128, KC, NT], F32, kind="Internal").ap()
    if dbg is not None:
        dbg_d = nc.dram_tensor("dbg", [128, dbgn], F32, kind="ExternalOutput").ap()

    def kview(w2d, c0, c1):
        return w2d.rearrange("(k p) n -> p k n", p=128)[:, :, c0:c1]

    vec = k.sb("vec", [128, NV], F32)
    cst = k.sb("cst", [128, NCST], F32)
    ident_bf = k.sb("identb", [128, 128], BF16)
    ones_bf = k.sb("onesb", [128, 128], BF16)
    bd_bf = k.sb("bdb", [128, 128], BF16)
    bdm_f = k.sb("bdmf", [128, 128], F32)
    bd1_f = k.sb("bd1f", [128, 128], F32)
    maskF = k.sb("maskF", [64, 512], BF16)
    maskB = k.sb("maskB", [64, 512], BF16)
    maskLF = k.sb("maskLF", [64, 128], BF16)
    maskLB = k.sb("maskLB", [64, 128], BF16)
    mod = k.sb("mod", [128, 192], F32)
    scal = k.sb("scal", [128, 160], F32)
    rmask = k.sb("rmask", [128, 576], F32)
    d_const = Dep()
    d_mod = Dep()
    d_scal = Dep()

    PS = [k.psum("ps%d" % i, [128, 512], F32) for i in range(7)]
    PSB = k.psum("psb", [128, 1024], BF16)
    dPS = [Dep() for _ in range(7)]
    dPSB = Dep()

    def V(name, i=0, n=8):
        o = VOFF[name] + i * 8
        return vec[:, o:o + n]

    def Vc(name, i, c):
        o = VOFF[name] + i * 8 + c
        return vec[:, o:o + 1]

    S_ = {n: i * 8 for i, n in enumerate(
        ["A1_0", "B1_0", "G1_0", "A2_0", "B2_0", "G2_0", "A1c", "B1c",
         "A1_1", "B1_1", "G1_1", "A2_1", "B2_1", "G2_1", "omka", "hrk"])}
    omu = k.sb("omu", [128, 48], F32)

    def S(name, c=None):
        o = S_[name]
        if c is None:
            return scal[:, o:o + 8]
        return scal[:, o + c:o + c + 1]

    k.dma(sp, vec[:], vec_d[:, :], w=[d_const])
    k.dma(sp, cst[:], cst_d[:, :], w=[d_const])
    k.op(dve, lambda: nc.vector.tensor_copy(ident_bf[:], cst[:, C_ID:C_ID + 128]), r=[d_const], w=[d_const])
    k.op(dve, lambda: nc.vector.tensor_copy(maskF[:], cst[0:64, C_MF:C_MF + 512]), r=[d_const], w=[d_const])
    k.op(dve, lambda: nc.vector.tensor_copy(maskB[:], cst[0:64, C_MB:C_MB + 512]), r=[d_const], w=[d_const])
    k.op(dve, lambda: nc.vector.tensor_copy(maskLF[:], cst[0:64, C_LF:C_LF + 128]), r=[d_const], w=[d_const])
    k.op(dve, lambda: nc.vector.tensor_copy(maskLB[:], cst[0:64, C_LB:C_LB + 128]), r=[d_const], w=[d_const])
    k.op(dve, lambda: nc.vector.memset(ones_bf[:], 1.0), w=[d_const])
    k.op(dve, lambda: nc.vector.memset(bd_bf[:], 0.0), w=[d_const])
    k.op(dve, lambda: nc.vector.memset(bdm_f[:], 0.0), w=[d_const])
    k.op(dve, lambda: nc.vector.memset(bd1_f[:], 0.0), w=[d_const])
    for h in range(2):
        sl = slice(64 * h, 64 * h + 64)
        k.op(dve, lambda sl=sl: nc.vector.memset(bd_bf[sl, sl], 1.0), w=[d_const])
        k.op(dve, lambda sl=sl: nc.vector.memset(bdm_f[sl, sl], 1.0 / 64), w=[d_const])
        k.op(dve, lambda sl=sl: nc.vector.memset(bd1_f[sl, sl], 1.0), w=[d_const])
    k.op(dve, lambda: nc.vector.memset(rmask[:], 1.0), w=[d_const])
    k.op(dve, lambda: nc.vector.memset(rmask[:, 0:576:64], 0.0), w=[d_const])
    k.op(dve, lambda: nc.vector.tensor_scalar(omu[:], V("mu", 0, 48), -1.0, 1.0, ALU.mult, ALU.add),
         r=[d_const], w=[d_const])

    with contextlib.ExitStack() as ph:
        cv = k.sb("cv", [128, KC, 2], F32, ph)
        scb = k.sb("scb", [128, KC, 2], BF16, ph)
        wb = [k.sb("modw%d" % i, [128, KC, 1024], BF16, ph) for i in range(2)]
        dwb = [Dep(), Dep()]
        d_cv = Dep()
        k.dma(sp, cv[:], cvec_d[:, :, :], w=[d_cv])
        k.op(act, lambda: nc.scalar.activation(scb[:], cv[:], AF.Silu), r=[d_cv], w=[d_cv])
        gi = 0
        for i in range(2):
            for g in range(6):
                b = gi % 2
                gi += 1
                k.dma(pool, wb[b][:], kview(mod_w_d[i], g * 1024, (g + 1) * 1024), w=[dwb[b]])
                for m in range(8):
                    mg = g * 8 + m
                    cc = (i * 48 + mg) * 2
                    for kk in range(KC):
                        k.op(pe, lambda b=b, m=m, kk=kk, cc=cc: nc.tensor.matmul(
                            PS[0][:, cc:cc + 2], wb[b][:, kk, m * 128:(m + 1) * 128], scb[:, kk, :],
                            start=(kk == 0), stop=(kk == KC - 1)), r=[dwb[b], d_cv], w=[dPS[0]])
        k.op(dve, lambda: nc.vector.tensor_tensor(mod[:], PS[0][:, 0:192], V("modb", 0, 192), ALU.add),
             r=[dPS[0], d_const], w=[d_mod])

        def mcol(i, s, j):
            o = (i * 48 + s * 8) * 2 + j
            return mod[:, o:o + 16:2]

        def mk_scale(dst, gi_, li, s, j):
            k.op(dve, lambda: nc.vector.tensor_scalar(S(dst), mcol(li, s, j), 1.0, None, ALU.add),
                 r=[d_mod], w=[d_scal])
            k.op(dve, lambda: nc.vector.tensor_tensor(S(dst), S(dst), V("g", li * 4 + gi_), ALU.mult),
                 r=[d_scal, d_const], w=[d_scal])

        def mk_copy(dst, li, s, j):
            k.op(dve, lambda: nc.vector.tensor_copy(S(dst), mcol(li, s, j)), r=[d_mod], w=[d_scal])

        def mk_gate(dst, gi_, li, s):
            k.op(dve, lambda: nc.vector.tensor_tensor(S(dst), mcol(li, s, 0), V("g", li * 4 + gi_), ALU.mult),
                 r=[d_mod, d_const], w=[d_scal])

        for li in range(2):
            mk_scale("A1_%d" % li, 0, li, 1, 0)
            mk_copy("B1_%d" % li, li, 0, 0)
            mk_gate("G1_%d" % li, 1, li, 2)
            mk_scale("A2_%d" % li, 2, li, 4, 0)
            mk_copy("B2_%d" % li, li, 3, 0)
            mk_gate("G2_%d" % li, 3, li, 5)
        mk_scale("A1c", 0, 0, 1, 1)
        mk_copy("B1c", 0, 0, 1)
        k.op(dve, lambda: nc.vector.tensor_scalar(S("omka"), V("k_a"), -1.0, 1.0, ALU.mult, ALU.add),
             r=[d_const], w=[d_scal])
        k.op(dve, lambda: nc.vector.tensor_scalar(S("hrk"), V("r_k"), 0.5, None, ALU.mult),
             r=[d_const], w=[d_scal])
        k.barrier()

    def sumsq_rstd(src_fn, n, scratch_bf, rs, d_src, d_scr, d_rs, psi, eps=NORM_EPS, nchunks=KC):
        for c in range(nchunks):
            k.op(act, lambda c=c: nc.scalar.activation(scratch_bf[:, c, :n], src_fn(c), AF.Square),
                 r=[d_src], w=[d_scr])
        for c in range(nchunks):
            k.op(pe, lambda c=c: nc.tensor.matmul(PS[psi][:, :n], ones_bf[:], scratch_bf[:, c, :n],
                                                  start=(c == 0), stop=(c == nchunks - 1)),
                 r=[d_scr, d_const], w=[dPS[psi]])
        k.op(act, lambda: nc.scalar.activation(rs[:, :n], PS[psi][:, :n], AF.Sqrt, bias=eps, scale=1.0 / D),
             r=[dPS[psi]], w=[d_rs])
        k.op(dve, lambda: nc.vector.reciprocal(rs[:, :n], rs[:, :n]), r=[d_rs], w=[d_rs])

    def norm_mod(src, d_src, n, An, Bn, dst_fn, d_dst, wk, h32=None, d_h32=None):
        sumsq_rstd(lambda c: src[:, c, :n], n, wk["sq"], wk["rs"], d_src, wk["dsq"], wk["drs"], 6)
        for c in range(KC):
            k.op(dve, lambda c=c: nc.vector.tensor_tensor(wk["tmp"][:, c, :n], src[:, c, :n], wk["rs"][:, :n], ALU.mult),
                 r=[d_src, wk["drs"]], w=[wk["dtmp"]])
            k.op(act, lambda c=c: nc.scalar.activation(dst_fn(c), wk["tmp"][:, c, :n], AF.Identity,
                                                       bias=S(Bn, c), scale=S(An, c)),
                 r=[wk["dtmp"], d_scal], w=[d_dst])
            if h32 is not None:
                k.op(pool, lambda c=c: nc.gpsimd.tensor_scalar(h32[:, c, :n], wk["tmp"][:, c, :n], S(An, c), S(Bn, c),
                                                               ALU.mult, ALU.add),
                     r=[wk["dtmp"], d_scal], w=[d_h32])

    def res_norm(y, d_y, n, Gn, xprev, d_xprev, xnew, d_xnew, wk):
        sumsq_rstd(lambda c: y[:, c, :n], n, wk["sq"], wk["rs"], d_y, wk["dsq"], wk["drs"], 6)
        for c in range(KC):
            k.op(pool, lambda c=c: nc.gpsimd.tensor_tensor(wk["tmp"][:, c, :n], y[:, c, :n], wk["rs"][:, :n], ALU.mult),
                 r=[d_y, wk["drs"]], w=[wk["dtmp"]])
            k.op(dve, lambda c=c: nc.vector.scalar_tensor_tensor(xnew[:, c, :n], wk["tmp"][:, c, :n], S(Gn, c),
                                                                 xprev[:, c, :n], ALU.mult, ALU.add),
                 r=[wk["dtmp"], d_scal, d_xprev], w=[d_xnew])

    def mk_wk(ph, n=512):
        return dict(sq=k.sb("wsq", [128, KC, n], BF16, ph), rs=k.sb("wrs", [128, n], F32, ph),
                    tmp=k.sb("wtmp", [128, KC, n], F32, ph), dsq=Dep(), drs=Dep(), dtmp=Dep())

    dbg_done = [False]

    def dump(ap_fn, ncols, d, off=0):
        k.dma(sp, dbg_d[:, off:off + ncols], ap_fn(), r=[d])

    lora_stack = contextlib.ExitStack()
    TW = k.sb("TW", [128, NT], BF16, lora_stack)
    TA = k.sb("TA", [128, NT], BF16, lora_stack)
    SG = k.sb("SG", [128, 2, T], BF16, lora_stack)
    d_TW, d_TA, d_SG = Dep(), Dep(), Dep()
    hT_stack = contextlib.ExitStack()
    hT = k.sb("hT", [128, KC, NT], BF16, hT_stack)
    hsT = k.sb("hsT", [128, KC, NT], BF16, hT_stack)
    d_h = Dep()
    d_hs = Dep()
    with contextlib.ExitStack() as ph:
        wk = mk_wk(ph)
        xb = [k.sb("xb%d" % i, [128, KC, 512], F32, ph) for i in range(2)]
        dxb = [Dep(), Dep()]
        k.dma(sp, xb[0][:, :, 0:TC], ctxT_d[:, :, :], w=[dxb[0]])
        norm_mod(xb[0], dxb[0], TC, "A1c", "B1c", lambda c: hT[:, c, 0:TC], d_h, wk)
        for tb in range(4):
            b = (tb + 1) % 2
            k.dma(sp, xb[b][:], xT_d[:, :, tb * 512:(tb + 1) * 512], w=[dxb[b]])
            norm_mod(xb[b], dxb[b], 512, "A1_0", "B1_0",
                     lambda c, tb=tb: hT[:, c, TC + tb * 512:TC + (tb + 1) * 512], d_h, wk)
        k.op(pool, lambda: nc.gpsimd.memset(hsT[:], 0.0), w=[d_hs])
        L0 = TC
        for c in range(KC):
            eng = act if c % 2 == 0 else pool

            def cp(dst, src, eng=eng):
                if eng is act:
                    k.op(act, lambda: nc.scalar.copy(dst, src), r=[d_h], w=[d_hs])
                else:
                    k.op(pool, lambda: nc.gpsimd.tensor_copy(dst, src), r=[d_h], w=[d_hs])
            if c < 4:
                cp(hsT[:, c, 1:TC], hT[:, c, 0:TC - 1])
            else:
                cp(hsT[:, c, 0:TC - 1], hT[:, c, 1:TC])
            if c in (0, 1):
                cp(hsT[:, c, L0 + 1:L0 + T], hT[:, c, L0:L0 + T - 1])
                k.op(pool, lambda c=c: nc.gpsimd.memset(hsT[:, c, L0:L0 + T:64], 0.0), w=[d_hs])
            elif c in (2, 3):
                cp(hsT[:, c, L0:L0 + T - 1], hT[:, c, L0 + 1:L0 + T])
                k.op(pool, lambda c=c: nc.gpsimd.memset(hsT[:, c, L0 + 63:L0 + T:64], 0.0), w=[d_hs])
            elif c in (4, 5):
                cp(hsT[:, c, L0 + 64:L0 + T], hT[:, c, L0:L0 + T - 64])
            else:
                cp(hsT[:, c, L0:L0 + T - 64], hT[:, c, L0 + 64:L0 + T])
        k.barrier()

    if dbg == 1:
        with contextlib.ExitStack() as ph:
            t32 = k.sb("t32", [128, 2 * NT], F32, ph)
            dd = Dep()
            k.op(dve, lambda: nc.vector.tensor_copy(t32[:, 0:NT], hT[:, 0, :]), r=[d_h], w=[dd])
            k.op(dve, lambda: nc.vector.tensor_copy(t32[:, NT:2 * NT], hsT[:, 5, :]), r=[d_hs], w=[dd])
            dump(lambda: t32[:], 2 * NT, dd)
            k.barrier()
        hT_stack.close()
        lora_stack.close()
        k.finish()
        return k


    def load_mixed(ph_, dram_view, ncols, mu_i, name):
        st = k.sb(name + "st", [128, KC, ncols], F32, ph_)
        Wa = k.sb(name + "a", [128, KC, ncols], BF16, ph_)
        Wb = k.sb(name + "b", [128, KC, ncols], BF16, ph_)
        dst_, dw = Dep(), Dep()
        k.dma(sp, st[:], dram_view, w=[dst_])
        for kk in range(KC):
            o = mu_i * 8 + kk
            k.op(dve, lambda kk=kk, o=o: nc.vector.tensor_scalar(Wa[:, kk, :], st[:, kk, :], omu[:, o:o + 1], None, ALU.mult),
                 r=[dst_, d_const], w=[dw])
            k.op(pool, lambda kk=kk, o=o: nc.gpsimd.tensor_scalar(Wb[:, kk, :], st[:, kk, :], V("mu", 0, 48)[:, o:o + 1], None, ALU.mult),
                 r=[dst_, d_const], w=[dw])
        return Wa, Wb, dw

    def proj_mixed(psi, Wa, Wb, dw, c0, c1, t0, n, mrows=128):
        for kk in range(KC):
            k.op(pe, lambda kk=kk: nc.tensor.matmul(PS[psi][:mrows, :n], Wa[:, kk, c0:c1], hT[:, kk, t0:t0 + n],
                                                    start=(kk == 0), stop=False),
                 r=[dw, d_h], w=[dPS[psi]])
        for kk in range(KC):
            k.op(pe, lambda kk=kk: nc.tensor.matmul(PS[psi][:mrows, :n], Wb[:, kk, c0:c1], hsT[:, kk, t0:t0 + n],
                                                    start=False, stop=(kk == KC - 1)),
                 r=[dw, d_hs], w=[dPS[psi]])

    with contextlib.ExitStack() as ph:
        W1a, W1b, dW1 = load_mixed(ph, kview(w1_d, 0, 128), 128, 1, "w1")
        A1a, A1b, dA1 = load_mixed(ph, kview(a1_d, 0, 128), 128, 4, "a1")
        G1a, G1b, dG1 = load_mixed(ph, kview(g1_d, 0, 160), 160, 5, "g1")
        pi = 0
        for tb in range(NT // 256):
            t0 = tb * 256
            proj_mixed(pi % 4, W1a, W1b, dW1, 0, 128, t0, 256)
            k.op(act, lambda p=pi % 4, t0=t0: nc.scalar.activation(TW[:, t0:t0 + 256], PS[p][:, :256], AF.Tanh),
                 r=[dPS[pi % 4]], w=[d_TW])
            pi += 1
            proj_mixed(pi % 4, A1a, A1b, dA1, 0, 128, t0, 256)
            k.op(dve, lambda p=pi % 4, t0=t0: nc.vector.tensor_copy(TA[:, t0:t0 + 256], PS[p][:, :256]),
                 r=[dPS[pi % 4]], w=[d_TA])
            pi += 1
            if t0 >= TC:
                l0 = t0 - TC
                proj_mixed(pi % 4, G1a, G1b, dG1, 0, 128, t0, 256)
                k.op(act, lambda p=pi % 4, l0=l0: nc.scalar.activation(SG[:, 0, l0:l0 + 256], PS[p][:, :256], AF.Sigmoid),
                     r=[dPS[pi % 4]], w=[d_SG])
                pi += 1
                proj_mixed(pi % 4, G1a, G1b, dG1, 128, 160, t0, 256, mrows=32)
                k.op(act, lambda p=pi % 4, l0=l0: nc.scalar.activation(SG[0:32, 1, l0:l0 + 256], PS[p][0:32, :256], AF.Sigmoid),
                     r=[dPS[pi % 4]], w=[d_SG])
                pi += 1
        k.barrier()

    with contextlib.ExitStack() as ph:
        st = k.sb("rkvst", [128, KC, D], F32, ph)
        Wa = k.sb("rkvWa", [128, KC, D], BF16, ph)
        Wb = k.sb("rkvWb", [128, KC, D], BF16, ph)
        rowb = [k.sb("rowb%d" % i, [128, NT], F32, ph) for i in range(2)]
        d_st, d_Wab = Dep(), Dep()
        d_row = [Dep(), Dep()]
        mu_of = [0, 2, 3]
        ri = 0
        pi = 0
        for j in range(3):
            k.dma(sp, st[:], kview(w_rkv_d[j], 0, D), w=[d_st])
            for kk in range(KC):
                o = mu_of[j] * 8 + kk
                k.op(dve, lambda kk=kk, o=o: nc.vector.tensor_scalar(Wa[:, kk, :], st[:, kk, :], omu[:, o:o + 1], None, ALU.mult),
                     r=[d_st, d_const], w=[d_Wab])
                k.op(pool, lambda kk=kk, o=o: nc.gpsimd.tensor_scalar(Wb[:, kk, :], st[:, kk, :], V("mu", 0, 48)[:, o:o + 1], None, ALU.mult),
                     r=[d_st, d_const], w=[d_Wab])
            for c in range(KC):
                rb = rowb[ri % 2]
                drb = d_row[ri % 2]
                ri += 1
                for tb in range(NT // 256):
                    t0 = tb * 256
                    p = pi % 4
                    pi += 1
                    proj_mixed(p, Wa, Wb, d_Wab, c * 128, (c + 1) * 128, t0, 256)
                    if tb % 2 == 0:
                        k.op(act, lambda p=p, rb=rb, t0=t0: nc.scalar.copy(rb[:, t0:t0 + 256], PS[p][:, :256]), r=[dPS[p]], w=[drb])
                    else:
                        k.op(dve, lambda p=p, rb=rb, t0=t0: nc.vector.tensor_copy(rb[:, t0:t0 + 256], PS[p][:, :256]), r=[dPS[p]], w=[drb])
                k.dma(sp, rkv_d[j, :, c, :], rb[:], r=[drb])
        k.barrier()
    hT_stack.close()

    def bc3(t2d, col0, nouter, ostride, ninner):
        base = t2d[:, col0:col0 + 1]
        pst = base.ap[0][0]
        return AP(base.tensor, base.offset, [[pst, 128], [ostride, nouter], [0, ninner]])

    BL = 576
    NBL = NT // BL
    CPB = BL // 64
    GI = 4
    NG = NCH // GI
    with contextlib.ExitStack() as ph:
        r32 = k.sb("r32", [128, NT], F32, ph)
        k32 = k.sb("k32", [128, NT], F32, ph)
        kk32 = k.sb("kk32", [128, NT], F32, ph)
        v16 = k.sb("v16", [128, NT], BF16, ph)
        ksum = k.sb("ksum", [128, NT], F32, ph)
        yz = [k.sb("yz%d" % z, [128, T], F32, ph) for z in range(2)]
        d_yz = [Dep(), Dep()]
        sgw = k.sb("sgw", [128, BL], F32, ph)
        cs = k.sb("cs", [128, BL], F32, ph)
        cs2 = k.sb("cs2", [128, BL], F32, ph)
        iclr = k.sb("iclr", [128, BL], F32, ph)
        t2 = k.sb("t2", [128, BL], F32, ph)
        t3 = k.sb("t3", [128, BL], F32, ph)
        sqb = k.sb("sqb", [128, 512], BF16, ph)
        ost = [k.sb("ost%d" % i, [128, T], BF16, ph) for i in range(1)]
        d_ost = [Dep()]
        w2c = k.sb("w2c", [128, 128], BF16, ph)
        a2c = k.sb("a2c", [128, 128], BF16, ph)
        g2c = k.sb("g2c", [128, 2, 128], BF16, ph)
        d_w2, d_a2, d_g2 = Dep(), Dep(), Dep()
        d_r, d_k, d_kk, d_v, d_ks = Dep(), Dep(), Dep(), Dep(), Dep()
        d_sgw, d_cs, d_cs2, d_iclr, d_t2, d_t3, d_sq = (Dep() for _ in range(7))
        DB = []
        for z in range(2):
            b_ = dict(
                ARbd=k.sb("ARbd%d" % z, [128, NCH, 2, 128], BF16, ph),
                bt=k.sb("bt%d" % z, [128, NT], BF16, ph), kt=k.sb("kt%d" % z, [128, NT], BF16, ph),
                WC=k.sb("WC%d" % z, [128, NCH], F32, ph),
                M32=k.sb("M32%d" % z, [128, 128], F32, ph), S16=k.sb("S16%d" % z, [128, 128], BF16, ph),
                X16=k.sb("X16%d" % z, [64, 128], BF16, ph), U16=k.sb("U16%d" % z, [64, 128], BF16, ph),
                NB=k.sb("NB%d" % z, [64, GI * 128], BF16, ph), LB=k.sb("LB%d" % z, [64, GI * 128], BF16, ph),
                XB=k.sb("XB%d" % z, [64, GI * 128], BF16, ph), LBI=k.sb("LBI%d" % z, [64, GI * 128], BF16, ph), d_LBI=Dep(),
                L1=[k.sb("L1_%d%d" % (z, i), [64, GI, 512], BF16, ph) for i in range(2)],
                TT=[k.sb("TT%d%d" % (z, i), [64, GI, 128], BF16, ph) for i in range(2)],
                tm=[k.sb("tm%d%d" % (z, i), [64, GI, 384], BF16, ph) for i in range(2)],
                d_AR=Dep(), d_bt=Dep(), d_kt=Dep(), d_WC=Dep(), d_M=Dep(), d_S=Dep(), d_X=Dep(), d_U=Dep(),
                d_NB=Dep(), d_LB=Dep(), d_XB=Dep(), d_L1=[Dep(), Dep()], d_TT=[Dep(), Dep()], d_tm=[Dep(), Dep()],
                d_ARb=[Dep() for _ in range(4)], d_btb=[Dep() for _ in range(4)], d_ktb=[Dep() for _ in range(4)],
                d_WCb=[Dep() for _ in range(4)], prev=None)
            DB.append(b_)
            k.op(pool, lambda b_=b_: nc.gpsimd.memset(b_["ARbd"][:], 0.0), w=b_["d_ARb"])
        k.min_free = min(getattr(k, "min_free", 1 << 30), nc.sbuf_bytes_remaining)

        for c in range(KC):
            cs0, cs1 = c * 128, (c + 1) * 128
            k.dma(sp, r32[:], rkv_d[0, :, c, :], w=[d_r])
            k.dma(sp, k32[:], rkv_d[1, :, c, :], w=[d_k])
            k.dma(pool, v16[:], rkv_d[2, :, c, :], w=[d_v])
            k.dma(pool, w2c[:], w2_d[:, cs0:cs1], w=[d_w2])
            k.dma(pool, a2c[:], a2_d[:, cs0:cs1], w=[d_a2])
            k.dma(pool, g2c[:, 0, :], g2_d[0:128, cs0:cs1], w=[d_g2])
            k.dma(pool, g2c[0:32, 1, :], g2_d[128:160, cs0:cs1], w=[d_g2])
            k.op(dve, lambda: nc.vector.tensor_scalar(kk32[:], k32[:], Vc("k_k", 0, c), None, ALU.mult),
                 r=[d_k, d_const], w=[d_kk])
            for q in range(0, NT, 512):
                n = min(512, NT - q)
                p = 4 + (q // 512) % 2
                k.op(act, lambda q=q, n=n: nc.scalar.activation(sqb[:, :n], kk32[:, q:q + n], AF.Square),
                     r=[d_kk], w=[d_sq])
                k.op(pe, lambda q=q, n=n, p=p: nc.tensor.matmul(PS[p][:, :n], bd_bf[:], sqb[:, :n], start=True, stop=True),
                     r=[d_sq, d_const], w=[dPS[p]])
                k.op(dve, lambda q=q, n=n, p=p: nc.vector.tensor_scalar(t2[:, :n], PS[p][:, :n], 1e-24, None, ALU.max),
                     r=[dPS[p]], w=[d_t2])
                k.op(act, lambda n=n: nc.scalar.activation(t2[:, :n], t2[:, :n], AF.Sqrt), r=[d_t2], w=[d_t2])
                k.op(dve, lambda n=n: nc.vector.reciprocal(t2[:, :n], t2[:, :n]), r=[d_t2], w=[d_t2])
                k.op(dve, lambda q=q, n=n: nc.vector.tensor_tensor(kk32[:, q:q + n], kk32[:, q:q + n], t2[:, :n], ALU.mult),
                     r=[d_kk, d_t2], w=[d_kk])

            k.op(pool, lambda: nc.gpsimd.memset(ksum[:], 0.0), w=[d_ks])
            emitted = set()

            def preproc_ops(z, blk):
                B_ = DB[z]
                ARbd, bt, kt, WC = B_["ARbd"], B_["bt"], B_["kt"], B_["WC"]
                d_AR, d_bt, d_kt, d_WC = B_["d_ARb"][blk], B_["d_btb"][blk], B_["d_ktb"][blk], B_["d_WCb"][blk]
                zs = slice(64 * z, 64 * z + 64)
                q0 = blk * BL
                qs = slice(q0, q0 + BL)
                nb0 = blk * CPB
                ops = []
                A = ops.append
                for sbk in range(2):
                    t0 = q0 + sbk * 288
                    o0 = sbk * 288
                    p = 2 + sbk
                    A(lambda p=p, t0=t0: k.op(pe, lambda: nc.tensor.matmul(PS[p][:, :288], w2c[zs, :], TW[zs, t0:t0 + 288],
                                                                         start=True, stop=True), r=[d_w2, d_TW], w=[dPS[p]]))
                    A(lambda p=p, o0=o0: k.op(act, lambda: nc.scalar.activation(sgw[:, o0:o0 + 288], PS[p][:, :288], AF.Sigmoid,
                                                                              bias=Vc("w0", z, c)), r=[dPS[p], d_const], w=[d_sgw]))
                    p2 = 4 + sbk
                    A(lambda p=p2, t0=t0: k.op(pe, lambda: nc.tensor.matmul(PS[p][:, :288], a2c[zs, :], TA[zs, t0:t0 + 288],
                                                                          start=True, stop=True), r=[d_a2, d_TA], w=[dPS[p]]))
                    A(lambda p=p2, o0=o0: k.op(act, lambda: nc.scalar.activation(iclr[:, o0:o0 + 288], PS[p][:, :288], AF.Sigmoid,
                                                                               bias=Vc("a0", z, c)), r=[dPS[p], d_const], w=[d_iclr]))
                A(lambda: k.op(dve, lambda: nc.vector.tensor_tensor_scan(cs[:], rmask[:, 0:BL], sgw[:], 0.0, ALU.mult, ALU.add),
                               r=[d_sgw, d_const], w=[d_cs]))
                if z == 1:
                    A(lambda: k.op(dve, lambda: nc.vector.tensor_tensor(cs2[:], sgw[:], cs[:], ALU.subtract),
                                   r=[d_sgw, d_cs], w=[d_cs2]))
                    A(lambda: k.op(dve, lambda: nc.vector.tensor_tensor(
                        cs2[:].rearrange("p (n t) -> p n t", t=64), cs2[:].rearrange("p (n t) -> p n t", t=64),
                        bc3(cs, 63, CPB, 64, 64), ALU.add), r=[d_cs2, d_cs], w=[d_cs2]))
                    csu, d_csu = cs2, d_cs2
                else:
                    csu, d_csu = cs, d_cs
                A(lambda: k.op(pool, lambda: nc.gpsimd.tensor_scalar(t2[:], iclr[:], Vc("k_a", 0, c), S("omka", c), ALU.mult, ALU.add),
                               r=[d_iclr, d_const, d_scal], w=[d_t2]))
                A(lambda: k.op(pool, lambda: nc.gpsimd.tensor_tensor(t2[:], t2[:], k32[:, qs], ALU.mult), r=[d_t2, d_k], w=[d_t2]))
                A(lambda: k.op(pool, lambda: nc.gpsimd.tensor_tensor(ksum[:, qs], ksum[:, qs], t2[:], ALU.add), r=[d_t2, d_ks], w=[d_ks]))
                A(lambda: k.op(act, lambda: nc.scalar.activation(t3[:], csu[:], AF.Exp, scale=-DECAY_S), r=[d_csu], w=[d_t3]))
                A(lambda: k.op(dve, lambda: nc.vector.tensor_tensor(kt[:, qs], t2[:], t3[:], ALU.mult), r=[d_t2, d_t3], w=[d_kt]))
                A(lambda: k.op(pool, lambda: nc.gpsimd.tensor_tensor(t2[:], kk32[:, qs], iclr[:], ALU.mult), r=[d_kk, d_iclr, d_kt], w=[d_t2]))
                A(lambda: k.op(dve, lambda: nc.vector.tensor_tensor(bt[:, qs], t2[:], t3[:], ALU.mult), r=[d_t2, d_t3], w=[d_bt]))
                A(lambda: k.op(act, lambda: nc.scalar.activation(t3[:], csu[:], AF.Exp, scale=DECAY_S), r=[d_csu, d_bt], w=[d_t3]))
                wc_col = 63 if z == 0 else 0
                A(lambda: k.op(pool, lambda: nc.gpsimd.tensor_copy(WC[:, nb0:nb0 + CPB], t3[:, wc_col:BL:64]),
                               r=[d_t3], w=[d_WC]))
                for h in range(2):
                    hs_ = slice(64 * h, 64 * h + 64)
                    A(lambda h=h, hs_=hs_: k.op(dve, lambda: nc.vector.tensor_tensor(
                        ARbd[hs_, nb0:nb0 + CPB, h, 64:128], r32[hs_, qs].rearrange("p (n t) -> p n t", t=64),
                        t3[hs_, :].rearrange("p (n t) -> p n t", t=64), ALU.mult), r=[d_r, d_t3], w=[d_AR]))
                A(lambda: k.op(pool, lambda: nc.gpsimd.tensor_tensor(t2[:], csu[:], sgw[:], ALU.subtract), r=[d_csu, d_sgw, d_bt], w=[d_t2]))
                A(lambda: k.op(act, lambda: nc.scalar.activation(t2[:], t2[:], AF.Exp, scale=DECAY_S), r=[d_t2], w=[d_t2]))
                for h in range(2):
                    hs_ = slice(64 * h, 64 * h + 64)
                    A(lambda h=h, hs_=hs_: k.op(dve, lambda: nc.vector.scalar_tensor_tensor(
                        ARbd[hs_, nb0:nb0 + CPB, h, 0:64], kk32[hs_, qs].rearrange("p (n t) -> p n t", t=64), -1.0,
                        t2[hs_, :].rearrange("p (n t) -> p n t", t=64), ALU.mult, ALU.mult), r=[d_kk, d_t2], w=[d_AR]))
                A(lambda: emitted.add((z, blk)))
                return ops

            def pre_stages(z, gb, chunks):
                B_ = DB[z]
                ARbd, bt, kt = B_["ARbd"], B_["bt"], B_["kt"]
                NBg, LBg, XBg, LBIg, d_LBI = B_["NB"], B_["LB"], B_["XB"], B_["LBI"], B_["d_LBI"]
                L1g, TTg, tmg = B_["L1"][gb], B_["TT"][gb], B_["tm"][gb]
                for n_ in chunks:
                    assert (z, n_ // CPB) in emitted, (z, n_)
                d_NB, d_LB, d_XB = B_["d_NB"], B_["d_LB"], B_["d_XB"]
                d_L1, d_TT, d_tm = B_["d_L1"][gb], B_["d_TT"][gb], B_["d_tm"][gb]
                mk = maskF if z == 0 else maskB
                mkL = maskLF if z == 0 else maskLB
                G = len(chunks)
                bA, bN, bL = (2, 3, 4) if z == 0 else (5, 6, 4)
                stages = []

                def stageA(lo, hi, last):
                    for gi in range(lo, hi):
                        n = chunks[gi]
                        t0 = n * 64
                        d_AR, d_bt, d_kt = B_["d_ARb"][n // CPB], B_["d_btb"][n // CPB], B_["d_ktb"][n // CPB]
                        k.op(pe, lambda t0=t0: nc.tensor.transpose(PSB[0:64, 0:128], bt[:, t0:t0 + 64], ident_bf[:]),
                             r=[d_bt, d_const], w=[dPSB])
                        k.op(pe, lambda t0=t0: nc.tensor.transpose(PSB[0:64, 128:256], kt[:, t0:t0 + 64], ident_bf[:]),
                             r=[d_kt, d_const], w=[dPSB])
                        k.op(pe, lambda t0=t0: nc.tensor.transpose(PSB[0:64, 256:384], v16[:, t0:t0 + 64], ident_bf[:]),
                             r=[d_v, d_const], w=[dPSB])
                        k.op(act, lambda gi=gi: nc.scalar.copy(tmg[:, gi, :], PSB[0:64, 0:384]), r=[dPSB], w=[d_tm])
                        k.op(pe, lambda n=n, t0=t0: nc.tensor.matmul(
                            PS[bA][0:64, 0:256], bt[:, t0:t0 + 64], ARbd[:, n, :, :].rearrange("p h x -> p (h x)"),
                            start=True, stop=True), r=[d_bt, d_AR], w=[dPS[bA]])
                        k.op(pe, lambda n=n, t0=t0: nc.tensor.matmul(
                            PS[bA][0:64, 256:512], kt[:, t0:t0 + 64], ARbd[:, n, :, :].rearrange("p h x -> p (h x)"),
                            start=True, stop=True), r=[d_kt, d_AR], w=[dPS[bA]])
                        k.op(dve, lambda gi=gi: nc.vector.tensor_tensor(L1g[:, gi, :], PS[bA][0:64, :], mk[:], ALU.mult),
                             r=[dPS[bA], d_const], w=[d_L1])
                        for h in range(2):
                            k.op(pe, lambda n=n, t0=t0, h=h, gi=gi: nc.tensor.matmul(
                                PS[bN][0:64, gi * 128 + h * 64: gi * 128 + h * 64 + 64],
                                ARbd[:, n, h, 0:64], bt[:, t0:t0 + 64], start=True, stop=True),
                                r=[d_AR, d_bt], w=[dPS[bN]])
                    if last:
                        k.op(dve, lambda: nc.vector.tensor_tensor(
                            LBg[:, 0:G * 128].rearrange("p (g x) -> p g x", x=128),
                            PS[bN][0:64, 0:G * 128].rearrange("p (g x) -> p g x", x=128),
                            mkL[:, :].unsqueeze(1).to_broadcast([64, G, 128]), ALU.mult), r=[dPS[bN], d_const], w=[d_LB])
                        k.op(pool, lambda: nc.gpsimd.tensor_copy(
                            NBg[:, 0:G * 128].rearrange("p (g h x) -> p g h x", h=2, x=64),
                            L1g[:, 0:G, 0:256].rearrange("p g (h x) -> p g h x", h=2)[:, :, :, 0:64]),
                            r=[d_L1], w=[d_NB])
                        k.op(pool, lambda: nc.gpsimd.tensor_tensor(
                            XBg[:, 0:G * 128].rearrange("p (g x) -> p g x", x=64), NBg[:, 0:G * 128].rearrange("p (g x) -> p g x", x=64),
                            ident_bf[0:64, 0:64].unsqueeze(1).to_broadcast([64, 2 * G, 64]), ALU.add),
                            r=[d_NB, d_const], w=[d_XB])

                def stageC():
                    for q in range(2 * G):
                        qq = slice(q * 64, q * 64 + 64)
                        k.op(pe, lambda qq=qq: nc.tensor.matmul(PS[bN][0:64, qq], LBg[:, qq], NBg[:, qq], start=True, stop=True),
                             r=[d_LB, d_NB], w=[dPS[bN]])
                    for q in range(2 * G):
                        qq = slice(q * 64, q * 64 + 64)
                        k.op(pe, lambda qq=qq: nc.tensor.matmul(PS[bL][0:64, qq], NBg[:, qq], LBg[:, qq], start=True, stop=True),
                             r=[d_LB, d_NB], w=[dPS[bL]])
                    k.op(act, lambda: nc.scalar.copy(NBg[:, 0:G * 128], PS[bN][0:64, 0:G * 128]), r=[dPS[bN]], w=[d_NB])
                    k.op(dve, lambda: nc.vector.tensor_copy(LBg[:, 0:G * 128], PS[bL][0:64, 0:G * 128]), r=[dPS[bL]], w=[d_LB])
                    k.op(pool, lambda: nc.gpsimd.tensor_tensor(
                        LBIg[:, 0:G * 128].rearrange("p (g x) -> p g x", x=64), LBg[:, 0:G * 128].rearrange("p (g x) -> p g x", x=64),
                        ident_bf[0:64, 0:64].unsqueeze(1).to_broadcast([64, 2 * G, 64]), ALU.add),
                        r=[d_LB, d_const], w=[d_LBI])

                def stageD(final):
                    for q in range(2 * G):
                        qq = slice(q * 64, q * 64 + 64)
                        k.op(pe, lambda qq=qq: nc.tensor.matmul(PS[bA][0:64, qq], LBIg[:, qq], XBg[:, qq], start=True, stop=True),
                             r=[d_LBI, d_XB], w=[dPS[bA]])
                    if not final:
                        k.op(act, lambda: nc.scalar.copy(XBg[:, 0:G * 128], PS[bA][0:64, 0:G * 128]), r=[dPS[bA]], w=[d_XB])
                    else:
                        k.op(act, lambda: nc.scalar.copy(
                            TTg[:, 0:G, :].rearrange("p g x -> p (g x)"), PS[bA][0:64, 0:G * 128]),
                            r=[dPS[bA]], w=[d_TT])

                stages.append(lambda: stageA(0, G // 2, False))
                stages.append(lambda: stageA(G // 2, G, True))
                for step in range(5):
                    stages.append(stageC)
                    stages.append(lambda step=step: stageD(step == 4))
                return stages

            def chain_seg(z, gb, gi, n, seg):
                B_ = DB[z]
                ARbd, WC, M32, S16, X16, U16 = B_["ARbd"], B_["WC"], B_["M32"], B_["S16"], B_["X16"], B_["U16"]
                L1g, TTg, tmg = B_["L1"][gb], B_["TT"][gb], B_["tm"][gb]
                d_AR, d_WC, d_M, d_S, d_X, d_U = B_["d_ARb"][n // CPB], B_["d_WCb"][n // CPB], B_["d_M"], B_["d_S"], B_["d_X"], B_["d_U"]
                d_WCp = B_["d_WCb"][B_["prev"] // CPB] if B_["prev"] is not None else d_WC
                d_L1, d_TT, d_tm = B_["d_L1"][gb], B_["d_TT"][gb], B_["d_tm"][gb]
                bk = PS[z]
                dbk = dPS[z]
                xo, uo, so, yo = 0, 128, 256, 384
                pX = bk[0:64, xo:xo + 128]
                pU = bk[0:64, uo:uo + 128]
                t0 = n * 64
                if seg == 0:
                    for h in range(2):
                        k.op(pe, lambda h=h: nc.tensor.matmul(pX, ARbd[:, n, h, 0:64], S16[:], start=(h == 0), stop=False),
                             r=[d_AR, d_S], w=[dbk])
                    for h in range(2):
                        k.op(pe, lambda h=h: nc.tensor.matmul(
                            bk[0:64, xo + h * 64:xo + h * 64 + 64], L1g[:, gi, 256 + h * 128:256 + h * 128 + 64],
                            tmg[:, gi, 256 + h * 64:256 + h * 64 + 64], start=False, stop=(h == 1)),
                            r=[d_L1, d_tm], w=[dbk])
                    k.op(act, lambda: nc.scalar.copy(X16[:], pX), w=[d_X, dbk])
                elif seg == 1:
                    for h in range(2):
                        k.op(pe, lambda h=h: nc.tensor.matmul(
                            bk[0:64, uo + h * 64:uo + h * 64 + 64], TTg[:, gi, h * 64:h * 64 + 64], X16[:, h * 64:h * 64 + 64],
                            start=True, stop=True), r=[d_TT, d_X], w=[dbk])
                    k.op(dve, lambda: nc.vector.tensor_copy(U16[:], pU), w=[d_U, dbk])
                else:
                    if n >= 4:
                        l0 = t0 - TC
                        k.op(pe, lambda: nc.tensor.matmul(
                            bk[:, yo:yo + 128], S16[:], ARbd[:, n, :, 64:128], start=True, stop=False),
                            r=[d_S, d_AR], w=[dbk])
                        for h in range(2):
                            hs_ = slice(64 * h, 64 * h + 64)
                            k.op(pe, lambda h=h, hs_=hs_: nc.tensor.matmul(
                                bk[hs_, yo + h * 64:yo + h * 64 + 64], U16[:, h * 64:h * 64 + 64],
                                L1g[:, gi, h * 128 + 64:h * 128 + 128], start=False, stop=False),
                                r=[d_U, d_L1], w=[dbk])
                            k.op(pe, lambda h=h, hs_=hs_: nc.tensor.matmul(
                                bk[hs_, yo + h * 64:yo + h * 64 + 64], tmg[:, gi, 256 + h * 64:256 + h * 64 + 64],
                                L1g[:, gi, 256 + h * 128 + 64:256 + h * 128 + 128], start=False, stop=(h == 1)),
                                r=[d_tm, d_L1], w=[dbk])
                    k.op(pe, lambda: nc.tensor.matmul(bk[:, so:so + 128], tmg[:, gi, 0:128], U16[:], start=True, stop=False),
                         r=[d_tm, d_U], w=[dbk])
                    k.op(pe, lambda: nc.tensor.matmul(bk[:, so:so + 128], tmg[:, gi, 128:256], tmg[:, gi, 256:384],
                                                      start=False, stop=True), r=[d_tm], w=[dbk])
                    prev = B_["prev"]
                    for h in range(2):
                        hs_ = slice(64 * h, 64 * h + 64)
                        pSd = bk[hs_, so + h * 64:so + h * 64 + 64]
                        if prev is None:
                            k.op(dve, lambda hs_=hs_, pSd=pSd: nc.vector.tensor_copy(M32[hs_, hs_], pSd),
                                 w=[d_M, dbk])
                        else:
                            k.op(dve, lambda hs_=hs_, pSd=pSd, prev=prev: nc.vector.scalar_tensor_tensor(
                                M32[hs_, hs_], M32[hs_, hs_], WC[hs_, prev:prev + 1], pSd, ALU.mult, ALU.add),
                                r=[d_WCp], w=[d_M, dbk])
                        k.op(act, lambda hs_=hs_: nc.scalar.activation(
                            S16[hs_, hs_], M32[hs_, hs_], AF.Identity, scale=WC[hs_, n:n + 1]), r=[d_M, d_WC], w=[d_S])
                    if n >= 4:
                        for h in range(2):
                            hs_ = slice(64 * h, 64 * h + 64)
                            k.op(dve, lambda h=h, hs_=hs_: nc.vector.tensor_copy(
                                yz[z][hs_, l0:l0 + 64], bk[hs_, yo + h * 64:yo + h * 64 + 64]), w=[d_yz[z], dbk])
                    B_["prev"] = n

            orders = [list(range(NCH)), [3, 2, 1, 0] + list(range(NCH - 1, 3, -1))]
            groups = [[o[i:i + GI] for i in range(0, NCH, GI)] for o in orders]
            for z in range(2):
                B_ = DB[z]
                B_["prev"] = None
                k.op(dve, lambda B_=B_: nc.vector.memset(B_["M32"][:], 0.0), w=[B_["d_M"]])
                k.op(dve, lambda B_=B_: nc.vector.memset(B_["S16"][:], 0.0), w=[B_["d_S"]])
            from collections import deque
            pending = deque()
            for zb in ((0, 0), (1, 0), (1, 3)):
                for o_ in preproc_ops(*zb):
                    o_()
            sched = {0: (0, 1), 1: (1, 2), 2: (0, 2), 3: (1, 1), 4: (0, 3)}
            pro = [pre_stages(z, 0, groups[z][0]) for z in range(2)]
            for si in range(len(pro[0])):
                for z in range(2):
                    pro[z][si]()
            for gidx in range(NG):
                if gidx in sched:
                    pending.extend(preproc_ops(*sched[gidx]))
                nxt = [pre_stages(z, (gidx + 1) % 2, groups[z][gidx + 1]) if gidx + 1 < NG else [] for z in range(2)]
                slot = 0
                for gi in range(GI):
                    for seg in range(3):
                        for z in range(2):
                            if slot < len(nxt[z]):
                                nxt[z][slot]()
                            chain_seg(z, gidx % 2, gi, groups[z][gidx][gi], seg)
                            for _ in range(2):
                                if pending:
                                    pending.popleft()()
                        slot += 1
                while pending:
                    pending.popleft()()

            if dbg == 2 and c == 0:
                dump(lambda: yz[0][:], T, d_yz[0], 0)
                dump(lambda: yz[1][:], T, d_yz[1], T)
                dump(lambda: kk32[:, TC:NT], T, d_kk, 2 * T)
                dump(lambda: ksum[:, TC:NT], T, d_ks, 3 * T)
                k.barrier()
                ph.close()
                lora_stack.close()
                k.finish()
                return k

            ob = ost[0]
            dob = d_ost[0]
            tA, tB, d_tA, d_tB = t2, t3, d_t2, d_t3
            for q in range(0, T, 512):
                qs = slice(q, q + 512)
                qn = slice(TC + q, TC + q + 512)
                W5 = slice(0, 512)
                k.op(pool, lambda qs=qs: nc.gpsimd.tensor_tensor(cs[:, W5], yz[0][:, qs], yz[1][:, qs], ALU.add),
                     r=[d_yz[0], d_yz[1]], w=[d_cs])
                k.op(pe, lambda: nc.tensor.matmul(PS[2][:, :], bdm_f[:], cs[:, W5], start=True, stop=True),
                     r=[d_const, d_cs], w=[dPS[2]])
                k.op(dve, lambda: nc.vector.tensor_tensor(tA[:, W5], cs[:, W5], PS[2][:, :], ALU.subtract),
                     r=[d_cs, dPS[2]], w=[d_tA])
                k.op(pool, lambda: nc.gpsimd.tensor_tensor(tB[:, W5], tA[:, W5], tA[:, W5], ALU.mult),
                     r=[d_tA], w=[d_tB])
                k.op(pe, lambda: nc.tensor.matmul(PS[3][:, :], bdm_f[:], tB[:, W5], start=True, stop=True),
                     r=[d_const, d_tB], w=[dPS[3]])
                k.op(act, lambda: nc.scalar.activation(tB[:, W5], PS[3][:, :], AF.Sqrt, bias=GN_EPS, scale=1.0),
                     r=[dPS[3]], w=[d_tB])
                k.op(dve, lambda: nc.vector.reciprocal(tB[:, W5], tB[:, W5]), r=[d_tB], w=[d_tB])
                k.op(dve, lambda: nc.vector.tensor_tensor(tA[:, W5], tA[:, W5], tB[:, W5], ALU.mult),
                     r=[d_tA, d_tB], w=[d_tA])
                k.op(act, lambda: nc.scalar.activation(tA[:, W5], tA[:, W5], AF.Identity,
                                                       bias=Vc("ln_b", 0, c), scale=Vc("ln_w", 0, c)),
                     r=[d_tA, d_const], w=[d_tA])
                k.op(dve, lambda qn=qn: nc.vector.scalar_tensor_tensor(
                    tB[:, W5], r32[:, qn], S("hrk", c), ksum[:, qn], ALU.mult, ALU.mult),
                    r=[d_r, d_ks, d_scal, d_tB], w=[d_tB])
                k.op(pe, lambda: nc.tensor.matmul(PS[4][:, :], bd1_f[:], tB[:, W5], start=True, stop=True),
                     r=[d_const, d_tB], w=[dPS[4]])
                k.op(dve, lambda qn=qn: nc.vector.tensor_tensor(tB[:, W5], PS[4][:, :], v16[:, qn], ALU.mult),
                     r=[dPS[4], d_v, d_tB], w=[d_tB])
                k.op(pool, lambda: nc.gpsimd.tensor_tensor(tA[:, W5], tA[:, W5], tB[:, W5], ALU.add),
                     r=[d_tA, d_tB], w=[d_tA])
                k.op(pe, lambda qs=qs: nc.tensor.matmul(PS[5][:, :], g2c[:, 0, :], SG[:, 0, qs], start=True, stop=False),
                     r=[d_g2, d_SG], w=[dPS[5]])
                k.op(pe, lambda qs=qs: nc.tensor.matmul(PS[5][:, :], g2c[0:32, 1, :], SG[0:32, 1, qs], start=False, stop=True),
                     r=[d_g2, d_SG], w=[dPS[5]])
                k.op(dve, lambda qs=qs: nc.vector.tensor_tensor(ob[:, qs], tA[:, W5], PS[5][:, :], ALU.mult),
                     r=[d_tA, dPS[5]], w=[dob])
            k.dma(sp, oT_d[:, c, :], ob[:], r=[dob])
        k.barrier()
    lora_stack.close()

    h2_stack = contextlib.ExitStack()
    h2 = k.sb("h2", [128, KC, T], BF16, h2_stack)
    d_h2 = [Dep() for _ in range(4)]
    d_xres = [Dep() for _ in range(4)]

    def out_proj_phase(w_dram, yin, d_yin, Gn, xprev_dram, An, Bn, router=None):
        with contextlib.ExitStack() as ph:
            wk = mk_wk(ph)
            wo = k.sb("wo", [128, KC, D], BF16, ph)
            d_wo = Dep()
            k.dma(pool, wo[:], kview(w_dram, 0, D), w=[d_wo])
            ym = k.sb("ym", [128, KC, 512], F32, ph)
            xp = k.sb("xp", [128, KC, 512], F32, ph)
            xn = k.sb("xn", [128, KC, 512], F32, ph)
            d_ym, d_xp, d_xn = Dep(), Dep(), Dep()
            if router is not None:
                h32 = k.sb("h32", [128, KC, 512], F32, ph)
                d_h32 = Dep()
            for tb in range(4):
                ts_ = slice(tb * 512, (tb + 1) * 512)
                k.dma(sp, xp[:], xprev_dram[:, :, ts_], r=[d_xres[tb]], w=[d_xp])
                for dc in range(KC):
                    p = dc % 4
                    for kk in range(KC):
                        k.op(pe, lambda dc=dc, kk=kk, p=p, ts_=ts_: nc.tensor.matmul(
                            PS[p][:, :], wo[:, kk, dc * 128:(dc + 1) * 128], yin[:, kk, ts_],
                            start=(kk == 0), stop=(kk == KC - 1)), r=[d_wo, d_yin], w=[dPS[p]])
                    k.op(act, lambda dc=dc, p=p: nc.scalar.copy(ym[:, dc, :], PS[p][:, :]), r=[dPS[p]], w=[d_ym])
                res_norm(ym, d_ym, 512, Gn, xp, d_xp, xn, d_xn, wk)
                k.dma(sp, xres_d[:, :, ts_], xn[:], r=[d_xn], w=[d_xres[tb]])
                norm_mod(xn, d_xn, 512, An, Bn, lambda c, ts_=ts_: h2[:, c, ts_], d_h2[tb], wk,
                         h32=(h32 if router is not None else None), d_h32=(d_h32 if router is not None else None))
                if router is not None:
                    router(tb, h32, d_h32)
            k.barrier()

    def ffn_pass(wgu_dram, wd_dram, acc, d_acc, first, wbufs, gate_bc=None, d_gate=None):
        for fg in range(FF // 512):
            b = wbufs["i"] % 2
            wbufs["i"] += 1
            Wg, Wu, Wd = wbufs["g"][b], wbufs["u"][b], wbufs["d"][b]
            dW = wbufs["dep"][b]
            k.dma(pool, Wg[:], kview(wgu_dram, fg * 512, (fg + 1) * 512), w=[dW])
            k.dma(pool, Wu[:], kview(wgu_dram, FF + fg * 512, FF + (fg + 1) * 512), w=[dW])
            k.dma(pool, Wd[:], wd_dram[fg * 512:(fg + 1) * 512, :].rearrange("(f p) d -> p f d", p=128), w=[dW])
            for tb in range(4):
                ts_ = slice(tb * 512, (tb + 1) * 512)
                ab = wbufs["ai"] % 2
                wbufs["ai"] += 1
                actb = wbufs["act"][ab]
                d_actb = wbufs["dact"][ab]
                for fc in range(4):
                    pg = (fc % 2) * 2
                    pu = pg + 1
                    for kk in range(KC):
                        k.op(pe, lambda kk=kk, fc=fc, pg=pg, ts_=ts_: nc.tensor.matmul(
                            PS[pg][:, :], Wg[:, kk, fc * 128:(fc + 1) * 128], h2[:, kk, ts_],
                            start=(kk == 0), stop=(kk == KC - 1)), r=[dW, d_h2[tb]], w=[dPS[pg]])
                    for kk in range(KC):
                        k.op(pe, lambda kk=kk, fc=fc, pu=pu, ts_=ts_: nc.tensor.matmul(
                            PS[pu][:, :], Wu[:, kk, fc * 128:(fc + 1) * 128], h2[:, kk, ts_],
                            start=(kk == 0), stop=(kk == KC - 1)), r=[dW, d_h2[tb]], w=[dPS[pu]])
                    sgb = wbufs["sg"][fc % 2]
                    d_sgb = wbufs["dsg"][fc % 2]
                    k.op(act, lambda pg=pg, sgb=sgb: nc.scalar.activation(sgb[:], PS[pg][:, :], AF.Silu),
                         r=[dPS[pg]], w=[d_sgb])
                    if gate_bc is None:
                        k.op(dve, lambda fc=fc, pu=pu, sgb=sgb, actb=actb: nc.vector.tensor_tensor(
                            actb[:, fc, :], sgb[:], PS[pu][:, :], ALU.mult), r=[d_sgb, dPS[pu]], w=[d_actb])
                    else:
                        k.op(dve, lambda fc=fc, pu=pu, sgb=sgb: nc.vector.tensor_tensor(
                            sgb[:], sgb[:], PS[pu][:, :], ALU.mult), r=[d_sgb, dPS[pu]], w=[d_sgb])
                        k.op(dve, lambda fc=fc, sgb=sgb, actb=actb, ts_=ts_: nc.vector.tensor_tensor(
                            actb[:, fc, :], sgb[:], gate_bc[:, ts_], ALU.mult), r=[d_sgb, d_gate], w=[d_actb])
                for dc in range(KC):
                    p = 4 + dc % 2
                    for fc in range(4):
                        k.op(pe, lambda dc=dc, fc=fc, p=p, actb=actb: nc.tensor.matmul(
                            PS[p][:, :], Wd[:, fc, dc * 128:(dc + 1) * 128], actb[:, fc, :],
                            start=(fc == 0), stop=(fc == 3)), r=[dW, d_actb], w=[dPS[p]])
                    if first and fg == 0:
                        k.op(act, lambda dc=dc, p=p, ts_=ts_: nc.scalar.copy(acc[:, dc, ts_], PS[p][:, :]),
                             r=[dPS[p]], w=[d_acc[tb]])
                    else:
                        k.op(dve, lambda dc=dc, p=p, ts_=ts_: nc.vector.tensor_tensor(
                            acc[:, dc, ts_], acc[:, dc, ts_], PS[p][:, :], ALU.add), r=[dPS[p], d_acc[tb]], w=[d_acc[tb]])

    def mk_ffn_bufs(ph):
        return dict(i=0, ai=0,
                    g=[k.sb("Wg%d" % i, [128, KC, 512], BF16, ph) for i in range(2)],
                    u=[k.sb("Wu%d" % i, [128, KC, 512], BF16, ph) for i in range(2)],
                    d=[k.sb("Wd%d" % i, [128, 4, D], BF16, ph) for i in range(2)],
                    dep=[Dep(), Dep()],
                    act=[k.sb("actb%d" % i, [128, 4, 512], BF16, ph) for i in range(2)],
                    dact=[Dep(), Dep()],
                    sg=[k.sb("sgb%d" % i, [128, 512], F32, ph) for i in range(2)],
                    dsg=[Dep(), Dep()])

    def post_ffn_phase(acc, d_acc, Gn, An, Bn, final):
        with contextlib.ExitStack() as ph:
            wk = mk_wk(ph)
            xp = k.sb("xp", [128, KC, 512], F32, ph)
            xn = k.sb("xn", [128, KC, 512], F32, ph)
            d_xp, d_xn = Dep(), Dep()
            for tb in range(4):
                ts_ = slice(tb * 512, (tb + 1) * 512)
                k.dma(sp, xp[:], xres_d[:, :, ts_], r=[d_xres[tb]], w=[d_xp])

                sumsq_rstd(lambda c: acc[:, c, ts_], 512, wk["sq"], wk["rs"], d_acc[tb], wk["dsq"], wk["drs"], 6)
                for c in range(KC):
                    k.op(pool, lambda c=c, ts_=ts_: nc.gpsimd.tensor_tensor(wk["tmp"][:, c, :], acc[:, c, ts_], wk["rs"][:, :], ALU.mult),
                         r=[d_acc[tb], wk["drs"]], w=[wk["dtmp"]])
                    k.op(dve, lambda c=c: nc.vector.scalar_tensor_tensor(xn[:, c, :], wk["tmp"][:, c, :], S(Gn, c),
                                                                         xp[:, c, :], ALU.mult, ALU.add),
                         r=[wk["dtmp"], d_scal, d_xp], w=[d_xn])
                if final:
                    k.dma(sp, out_d[:, :, ts_], xn[:], r=[d_xn])
                else:
                    k.dma(sp, xres_d[:, :, ts_], xn[:], r=[d_xn], w=[d_xres[tb]])
                    norm_mod(xn, d_xn, 512, An, Bn, lambda c, ts_=ts_: h2[:, c, ts_], d_h2[tb], wk)
            k.barrier()

    with contextlib.ExitStack() as yst:
        yin0 = k.sb("yin0", [128, KC, T], BF16, yst)
        d_yin0 = Dep()
        k.dma(sp, yin0[:], oT_d[:, :, :], w=[d_yin0])
        out_proj_phase(wout_d, yin0, d_yin0, "G1_0", xT_d, "A2_0", "B2_0")

    if dbg == 3:
        with contextlib.ExitStack() as ph:
            t32 = k.sb("t32", [128, T], F32, ph)
            dd = Dep()
            k.op(dve, lambda: nc.vector.tensor_copy(t32[:], h2[:, 0, :]), r=d_h2, w=[dd])
            dump(lambda: t32[:], T, dd)
            k.barrier()
        h2_stack.close()
        k.finish()
        return k

    acc_stack = contextlib.ExitStack()
    acc = k.sb("acc", [128, KC, T], F32, acc_stack)
    d_acc = [Dep() for _ in range(4)]
    with contextlib.ExitStack() as ph:
        wbufs = mk_ffn_bufs(ph)
        ffn_pass(fgu_d, fd_d, acc, d_acc, True, wbufs)
        k.barrier()
    post_ffn_phase(acc, d_acc, "G2_0", "A1_1", "B1_1", final=False)
    acc_stack.close()

    gates_stack = contextlib.ExitStack()
    logit = k.sb("logit", [128, 16, NE], F32, gates_stack)
    gates = k.sb("gates", [128, 16, NE], F32, gates_stack)
    wr32 = k.sb("wr32", [128, KC, NE], F32, gates_stack)
    d_logit, d_gates, d_wr = Dep(), Dep(), Dep()
    k.dma(sp, wr32[:], rt_d[:, :, :], w=[d_wr])

    ycv_stack = contextlib.ExitStack()
    ycv = k.sb("ycv", [128, KC, T], BF16, ycv_stack)
    d_ycv = Dep()
    with contextlib.ExitStack() as ph:
        Wc3 = [k.sb("Wc3_%d" % i, [128, KC, 3, 128], BF16, ph) for i in range(2)]
        dWc3 = [Dep(), Dep()]
        Bsb = k.sb("Bsb", [128, T], F32, ph)
        Csb = k.sb("Csb", [128, 512], F32, ph)
        zp = k.sb("zp", [128, T + 2], F32, ph)
        t1 = k.sb("t1", [128, T], F32, ph)
        d_B, d_C, d_z, d_t1 = Dep(), Dep(), Dep(), Dep()
        k.op(dve, lambda: nc.vector.memset(zp[:], 0.0), w=[d_z])
        for c in range(KC):
            W3 = Wc3[c % 2]
            dW3 = dWc3[c % 2]
            for j in range(3):
                k.dma(pool, W3[:, :, j, :], kview(cwin_d, j * D + c * 128, j * D + (c + 1) * 128), w=[dW3])
            for tb in range(4):
                ts_ = slice(tb * 512, (tb + 1) * 512)
                for j in range(3):
                    p = j
                    for kk in range(KC):
                        k.op(pe, lambda kk=kk, j=j, p=p, ts_=ts_, W3=W3: nc.tensor.matmul(
                            PS[p][:, :], W3[:, kk, j, :], h2[:, kk, ts_], start=(kk == 0), stop=(kk == KC - 1)),
                            r=[dW3, d_h2[tb]], w=[dPS[p]])
                k.op(act, lambda ts_=ts_: nc.scalar.copy(Bsb[:, ts_], PS[0][:, :]), r=[dPS[0]], w=[d_B])
                k.op(act, lambda: nc.scalar.copy(Csb[:], PS[1][:, :]), r=[dPS[1]], w=[d_C])
                k.op(dve, lambda tb=tb: nc.vector.tensor_tensor(zp[:, 1 + tb * 512:1 + (tb + 1) * 512], Csb[:], PS[2][:, :], ALU.mult),
                     r=[d_C, dPS[2]], w=[d_z])
            k.op(act, lambda c=c: nc.scalar.activation(t1[:], zp[:, 0:T], AF.Identity, scale=Vc("conv_w", 0, c)),
                 r=[d_z, d_const], w=[d_t1])
            k.op(dve, lambda c=c: nc.vector.scalar_tensor_tensor(t1[:], zp[:, 1:T + 1], Vc("conv_w", 1, c), t1[:], ALU.mult, ALU.add),
                 r=[d_z, d_const, d_t1], w=[d_t1])
            k.op(dve, lambda c=c: nc.vector.scalar_tensor_tensor(t1[:], zp[:, 2:T + 2], Vc("conv_w", 2, c), t1[:], ALU.mult, ALU.add),
                 r=[d_z, d_const, d_t1], w=[d_t1])
            k.op(pool, lambda c=c: nc.gpsimd.tensor_tensor(ycv[:, c, :], t1[:], Bsb[:], ALU.mult),
                 r=[d_t1, d_B], w=[d_ycv])
        k.barrier()

    def router(tb, h32, d_h32):
        for sub in range(4):
            tt = tb * 4 + sub
            for c in range(KC):
                k.op(pe, lambda c=c, sub=sub: nc.tensor.matmul(
                    PS[5][:, sub * 8:sub * 8 + 8], h32[:, c, sub * 128:(sub + 1) * 128], wr32[:, c, :],
                    start=(c == 0), stop=(c == KC - 1)), r=[d_h32, d_wr], w=[dPS[5]])
        k.op(dve, lambda tb=tb: nc.vector.tensor_copy(
            logit[:, tb * 4:(tb + 1) * 4, :], PS[5][:, 0:32].rearrange("p (s e) -> p s e", e=8)),
            r=[dPS[5]], w=[d_logit])

    out_proj_phase(cwout_d, ycv, d_ycv, "G1_1", xres_d, "A2_1", "B2_1", router=router)
    ycv_stack.close()

    with contextlib.ExitStack() as ph:
        mx = k.sb("mx", [128, 16, 8], F32, ph)
        e1 = k.sb("e1", [128, 16], F32, ph)
        g1_ = k.sb("g1_", [128, 16], F32, ph)
        g2_ = k.sb("g2_", [128, 16], F32, ph)
        q1 = k.sb("q1", [128, 16, 8], F32, ph)
        q2 = k.sb("q2", [128, 16, 8], F32, ph)
        d_mx, d_e = Dep(), Dep()
        for tt in range(16):
            k.op(dve, lambda tt=tt: nc.vector.max(mx[:, tt, :], logit[:, tt, :]), r=[d_logit], w=[d_mx])
        k.op(dve, lambda: nc.vector.tensor_tensor(e1[:], mx[:, :, 1], mx[:, :, 0], ALU.subtract), r=[d_mx], w=[d_e])
        k.op(act, lambda: nc.scalar.activation(e1[:], e1[:], AF.Exp), r=[d_e], w=[d_e])
        k.op(dve, lambda: nc.vector.tensor_scalar(g1_[:], e1[:], 1.0, None, ALU.add), r=[d_e], w=[d_e])
        k.op(dve, lambda: nc.vector.reciprocal(g1_[:], g1_[:]), r=[d_e], w=[d_e])
        k.op(dve, lambda: nc.vector.tensor_tensor(g2_[:], e1[:], g1_[:], ALU.mult), r=[d_e], w=[d_e])
        k.op(dve, lambda: nc.vector.tensor_tensor(q1[:], logit[:], mx[:, :, 0:1].to_broadcast([128, 16, 8]), ALU.is_equal),
             r=[d_logit, d_mx], w=[d_e])
        k.op(dve, lambda: nc.vector.tensor_tensor(q2[:], logit[:], mx[:, :, 1:2].to_broadcast([128, 16, 8]), ALU.is_equal),
             r=[d_logit, d_mx], w=[d_e])
        k.op(dve, lambda: nc.vector.tensor_tensor(q1[:], q1[:], g1_[:, :].unsqueeze(2).to_broadcast([128, 16, 8]), ALU.mult),
             r=[d_e], w=[d_e])
        k.op(dve, lambda: nc.vector.tensor_tensor(q2[:], q2[:], g2_[:, :].unsqueeze(2).to_broadcast([128, 16, 8]), ALU.mult),
             r=[d_e], w=[d_e])
        k.op(dve, lambda: nc.vector.tensor_tensor(gates[:], q1[:], q2[:], ALU.add), r=[d_e], w=[d_gates])
        k.barrier()

    if dbg == 4:
        dump(lambda: gates[:].rearrange("p t e -> p (t e)"), 128, d_gates, 0)
        dump(lambda: logit[:].rearrange("p t e -> p (t e)"), 128, d_logit, 128)
        k.barrier()
        gates_stack.close()
        h2_stack.close()
        k.finish()
        return k

    acc_stack = contextlib.ExitStack()
    acc = k.sb("acc2", [128, KC, T], F32, acc_stack)
    d_acc = [Dep() for _ in range(4)]
    with contextlib.ExitStack() as ph:
        wbufs = mk_ffn_bufs(ph)
        gbc = [k.sb("gbc%d" % i, [128, T], F32, ph) for i in range(2)]
        d_gbc = [Dep(), Dep()]
        Gm = [k.sb("Gm%d" % i, [128, 128], F32, ph) for i in range(2)]
        d_Gm = [Dep(), Dep()]
        ident_f = cst[:, C_ID:C_ID + 128]
        for e in range(NE):
            gb = gbc[e % 2]
            dgb = d_gbc[e % 2]
            for tt in range(16):
                gm = Gm[tt % 2]
                dgm = d_Gm[tt % 2]
                k.op(dve, lambda tt=tt, e=e, gm=gm: nc.vector.tensor_copy(gm[:], gates[:, tt, e:e + 1].to_broadcast([128, 128])),
                     r=[d_gates], w=[dgm])
                k.op(pe, lambda tt=tt, gm=gm: nc.tensor.matmul(PS[6][:, (tt % 4) * 128:(tt % 4 + 1) * 128], gm[:], ident_f,
                                                              start=True, stop=True), r=[dgm, d_const], w=[dPS[6]])
                if tt % 4 == 3:
                    q = (tt // 4) * 512
                    k.op(act, lambda q=q, gb=gb: nc.scalar.copy(gb[:, q:q + 512], PS[6][:, :]), r=[dPS[6]], w=[dgb])
            ffn_pass(mgu_d[e], md_d[e], acc, d_acc, e == 0, wbufs, gate_bc=gb, d_gate=dgb)
        k.barrier()
    post_ffn_phase(acc, d_acc, "G2_1", None, None, final=True)
    acc_stack.close()
    gates_stack.close()
    h2_stack.close()
    k.finish()
    return k


def prep_inputs(inp):
    f = lambda a: np.ascontiguousarray(np.asarray(a, np.float32))
    vec = np.zeros((128, NV), np.float32)

    def put(name, arr):
        a = col(arr)
        vec[:, VOFF[name]:VOFF[name] + a.shape[1]] = a

    put("g", f(inp["norm_g"]).reshape(-1))
    put("mu", f(inp["rwkv_mu"]).reshape(-1))
    put("w0", f(inp["rwkv_w0"]).reshape(-1))
    put("a0", f(inp["rwkv_a0"]).reshape(-1))
    put("k_k", f(inp["rwkv_k_k"]).reshape(-1))
    put("k_a", f(inp["rwkv_k_a"]).reshape(-1))
    put("r_k", f(inp["rwkv_r_k"]).reshape(-1))
    put("ln_w", f(inp["rwkv_ln_w"]).reshape(-1))
    put("ln_b", f(inp["rwkv_ln_b"]).reshape(-1))
    put("conv_w", f(inp["conv_w"]).reshape(-1))
    mb = col(f(inp["mod_b"]).reshape(-1))
    vec[:, VOFF["modb"]:VOFF["modb"] + 192] = np.repeat(mb, 2, axis=1)
    shared = {
        "vecs": vec,
        "cst": make_consts(),
        "mod_w": f(inp["mod_w"]),
        "w_rkv": f(inp["rwkv_w_rkv"])[0],
        "w1cat": np.ascontiguousarray(np.concatenate([f(inp["rwkv_w1"])[0, 0], f(inp["rwkv_w1"])[0, 1]], axis=1)),
        "w2cat": np.ascontiguousarray(f(inp["rwkv_w2"])[0].reshape(128, D)),
        "a1cat": np.ascontiguousarray(np.concatenate([f(inp["rwkv_a1"])[0, 0], f(inp["rwkv_a1"])[0, 1]], axis=1)),
        "a2cat": np.ascontiguousarray(f(inp["rwkv_a2"])[0].reshape(128, D)),
        "g1": f(inp["rwkv_g1"])[0],
        "g2": f(inp["rwkv_g2"])[0],
        "w_out": f(inp["rwkv_w_out"])[0],
        "conv_w_in": f(inp["conv_w_in"])[0],
        "conv_w_out": f(inp["conv_w_out"])[0],
        "ffn_w_gu": f(inp["ffn_w_gu"])[0],
        "ffn_w_down": f(inp["ffn_w_down"])[0],
        "router": np.ascontiguousarray(f(inp["moe_router"])[0].reshape(KC, 128, NE).transpose(1, 0, 2)),
        "moe_w_gu": f(inp["moe_w_gu"])[0],
        "moe_w_down": f(inp["moe_w_down"])[0],
    }
    x = f(inp["x"])
    ctx = f(inp["ctx"])
    c = f(inp["c"])
    cc = f(inp["c_ctx"])
    maps = []
    for b in range(8):
        m = dict(shared)
        m["xT"] = np.ascontiguousarray(x[b].T.reshape(KC, 128, T).transpose(1, 0, 2))
        m["ctxT"] = np.ascontiguousarray(ctx[b].T.reshape(KC, 128, TC).transpose(1, 0, 2))
        m["cvec"] = np.ascontiguousarray(np.stack([col(c[b]), col(cc)], axis=2))
        maps.append(m)
    return maps


def kernel(**inputs):
    maps = prep_inputs(inputs)
    kb = build()
    res = run_bass_kernel_spmd(kb.nc, maps, core_ids=list(range(8)))
    outs = []
    for b in range(8):
        yT = np.asarray(res.results[b]["yT"], np.float32)
        outs.append(yT.transpose(1, 0, 2).reshape(D, T).T)
    return np.ascontiguousarray(np.stack(outs, 0).astype(np.float32))
```

```python
import contextlib
import numpy as np
import concourse.bass as bass
import concourse.mybir as mybir
from concourse.ap import AP
from concourse.bass_utils import run_bass_kernel_spmd

F32 = mybir.dt.float32
BF16 = mybir.dt.bfloat16
AF = mybir.ActivationFunctionType
ALU = mybir.AluOpType

T = 2048
TC = 256
NT = T + TC
D = 1024
KC = 8
FF = 3584
NE = 8
NCH = NT // 64
NORM_EPS = 1e-6
GN_EPS = 64e-5
DECAY_S = -0.6065306597126334


class Dep:
    __slots__ = ("w", "rs")

    def __init__(self):
        self.w = None
        self.rs = {}


class Eng:
    def __init__(self, name, obj, is_pe=False):
        self.name = name
        self.obj = obj
        self.is_pe = is_pe
        self.sem = None
        self.semid = None
        self.cnt = 0
        self.seen = {}


class Slot:
    def __init__(self, sem, semid):
        self.sem = sem
        self.semid = semid
        self.cnt = 0
        self.last = None


class KB:
    def __init__(self):
        self.nc = bass.Bass("TRN2", target_bir_lowering=False)
        nc = self.nc
        self.es = contextlib.ExitStack()
        self.sems = []
        self.epoch = 0
        self.pe = Eng("pe", nc.tensor, True)
        self.act = Eng("act", nc.scalar)
        self.dve = Eng("dve", nc.vector)
        self.pool = Eng("pool", nc.gpsimd)
        self.sp = Eng("sp", nc.sync)
        self.engs = [self.pe, self.act, self.dve, self.pool, self.sp]
        for e in self.engs:
            self._fresh_sem(e)
        self.slots = {}
        self.slot_i = {}
        for q in (self.sp, self.pool):
            self.slots[q.name] = [Slot(*self._newsem("dq%s%d" % (q.name, i))) for i in range(8)]
            self.slot_i[q.name] = 0
        self.ninstr = 0
        self.uid = 0

    def _newsem(self, name):
        s = self.es.enter_context(self.nc.semaphore("%s_%d" % (name, len(self.sems))))
        self.sems.append(s)
        return s, len(self.sems) - 1

    def _fresh_sem(self, e):
        e.sem, e.semid = self._newsem("e" + e.name)
        e.cnt = 0

    def sb(self, name, shape, dtype, stack=None):
        self.uid += 1
        return (stack or self.es).enter_context(
            self.nc.sbuf_tensor("%s_%d" % (name, self.uid), list(shape), dtype))

    def psum(self, name, shape, dtype, stack=None):
        self.uid += 1
        return (stack or self.es).enter_context(
            self.nc.psum_tensor("%s_%d" % (name, self.uid), list(shape), dtype))

    def _wait(self, eng, tk, war=False):
        semid, val, src, ep = tk
        if ep != self.epoch:
            return
        if src is eng:
            if eng.is_pe:
                return
        if eng.seen.get(semid, 0) >= val:
            return
        eng.obj.wait_ge(self.sems[semid], val)
        eng.seen[semid] = val
        self.ninstr += 1

    def _deps(self, eng, r, w):
        for d in r:
            if d.w is not None:
                self._wait(eng, d.w)
        for d in w:
            if d.w is not None:
                self._wait(eng, d.w)
            for t in d.rs.values():
                self._wait(eng, t, war=True)

    def _mark(self, tk, r, w):
        for d in r:
            d.rs[tk[0]] = tk
        for d in w:
            d.w = tk
            d.rs = {}

    def op(self, eng, fn, r=(), w=()):
        self._deps(eng, r, w)
        ins = fn()
        eng.cnt += 1
        ins.then_inc(eng.sem, 1)
        tk = (eng.semid, eng.cnt, eng, self.epoch)
        self._mark(tk, r, w)
        self.ninstr += 1
        return tk

    def dma(self, q, out, in_, r=(), w=(), **kw):
        sl = self.slots[q.name]
        i = self.slot_i[q.name]
        self.slot_i[q.name] = (i + 1) % len(sl)
        s = sl[i]
        if s.last is not None:
            self._wait(q, s.last)
        self._deps(q, r, w)
        ins = q.obj.dma_start(out=out, in_=in_, **kw)
        s.cnt += 16
        ins.then_inc(s.sem, 16)
        tk = (s.semid, s.cnt, None, self.epoch)
        s.last = tk
        self._mark(tk, r, w)
        self.ninstr += 1
        return tk

    def barrier(self):
        tks = []
        for e in self.engs:
            if e.cnt > 0:
                tks.append((e.semid, e.cnt, e, self.epoch))
        for sl in self.slots.values():
            for s in sl:
                if s.last is not None and s.last[3] == self.epoch:
                    tks.append(s.last)
        for e in self.engs:
            for tk in tks:
                semid, val, src, ep = tk
                if e.seen.get(semid, 0) >= val:
                    continue
                e.obj.wait_ge(self.sems[semid], val)
                e.seen[semid] = val
                self.ninstr += 1
        self.epoch += 1
        for e in self.engs:
            self._fresh_sem(e)
            e.seen = {}

    def finish(self):
        self.barrier()
        self.es.close()


VEC_SPEC = [("g", 2 * 4 * 8), ("mu", 6 * 8), ("w0", 16), ("a0", 16), ("k_k", 8), ("k_a", 8), ("r_k", 8),
            ("ln_w", 8), ("ln_b", 8), ("conv_w", 24), ("modb", 192)]
VOFF = {}
_o = 0
for _n, _c in VEC_SPEC:
    VOFF[_n] = _o
    _o += _c
NV = _o
C_ID, C_MF, C_MB, C_LF, C_LB, NCST = 0, 128, 640, 1152, 1280, 1408


def col(v):
    v = np.asarray(v, np.float32).reshape(-1, 128)
    return np.ascontiguousarray(v.T)


def make_consts():
    c = np.zeros((128, NCST), np.float32)
    c[:, C_ID:C_ID + 128] = np.eye(128, dtype=np.float32)
    s = np.arange(64)[:, None]
    t = np.arange(64)[None, :]
    strict_f = (s < t).astype(np.float32)
    incl_f = (s <= t).astype(np.float32)
    strict_b = (s > t).astype(np.float32)
    incl_b = (s >= t).astype(np.float32)
    mf = np.concatenate([strict_f, incl_f], 1)
    mb = np.concatenate([strict_b, incl_b], 1)
    c[:64, C_MF:C_MF + 512] = np.tile(mf, (1, 4))
    c[:64, C_MB:C_MB + 512] = np.tile(mb, (1, 4))
    c[:64, C_LF:C_LF + 128] = np.tile(strict_b, (1, 2))
    c[:64, C_LB:C_LB + 128] = np.tile(strict_f, (1, 2))
    return c


def build(dbg=None, dbgn=0):
    k = KB()
    nc = k.nc
    pe, act, dve, pool, sp = k.pe, k.act, k.dve, k.pool, k.sp

    def din(name, shape, dt=F32):
        return nc.dram_tensor(name, list(shape), dt, kind="ExternalInput").ap()

    xT_d = din("xT", [128, KC, T])
    ctxT_d = din("ctxT", [128, KC, TC])
    cvec_d = din("cvec", [128, KC, 2])
    vec_d = din("vecs", [128, NV])
    cst_d = din("cst", [128, NCST])
    mod_w_d = din("mod_w", [2, D, 6 * D])
    w_rkv_d = din("w_rkv", [3, D, D])
    w1_d = din("w1cat", [D, 128])
    w2_d = din("w2cat", [128, D])
    a1_d = din("a1cat", [D, 128])
    a2_d = din("a2cat", [128, D])
    g1_d = din("g1", [D, 160])
    g2_d = din("g2", [160, D])
    wout_d = din("w_out", [D, D])
    cwin_d = din("conv_w_in", [D, 3 * D])
    cwout_d = din("conv_w_out", [D, D])
    fgu_d = din("ffn_w_gu", [D, 2 * FF])
    fd_d = din("ffn_w_down", [FF, D])
    rt_d = din("router", [128, KC, NE])
    mgu_d = din("moe_w_gu", [NE, D, 2 * FF])
    md_d = din("moe_w_down", [NE, FF, D])
    out_d = nc.dram_tensor("yT", [128, KC, T], F32, kind="ExternalOutput").ap()
    xres_d = nc.dram_tensor("xres", [128, KC, T], F32, kind="Internal").ap()
    oT_d = nc.dram_tensor("oT", [128, KC, T], BF16, kind="Internal").ap()
    rkv_d = nc.dram_tensor("rkvs", [3, 128, KC, NT], F32, kind="Internal").ap()
    if dbg is not None:
        dbg_d = nc.dram_tensor("dbg", [128, dbgn], F32, kind="ExternalOutput").ap()

    def kview(w2d, c0, c1):
        return w2d.rearrange("(k p) n -> p k n", p=128)[:, :, c0:c1]

    vec = k.sb("vec", [128, NV], F32)
    cst = k.sb("cst", [128, NCST], F32)
    ident_bf = k.sb("identb", [128, 128], BF16)
    ones_bf = k.sb("onesb", [128, 128], BF16)
    bd_bf = k.sb("bdb", [128, 128], BF16)
    bdm_f = k.sb("bdmf", [128, 128], F32)
    bd1_f = k.sb("bd1f", [128, 128], F32)
    maskF = k.sb("maskF", [64, 512], BF16)
    maskB = k.sb("maskB", [64, 512], BF16)
    maskLF = k.sb("maskLF", [64, 128], BF16)
    maskLB = k.sb("maskLB", [64, 128], BF16)
    mod = k.sb("mod", [128, 192], F32)
    scal = k.sb("scal", [128, 160], F32)
    rmask = k.sb("rmask", [128, 576], F32)
    d_const = Dep()
    d_mod = Dep()
    d_scal = Dep()

    PS = [k.psum("ps%d" % i, [128, 512], F32) for i in range(7)]
    PSB = k.psum("psb", [128, 1024], BF16)
    dPS = [Dep() for _ in range(7)]
    dPSB = Dep()

    def V(name, i=0, n=8):
        o = VOFF[name] + i * 8
        return vec[:, o:o + n]

    def Vc(name, i, c):
        o = VOFF[name] + i * 8 + c
        return vec[:, o:o + 1]

    S_ = {n: i * 8 for i, n in enumerate(
        ["A1_0", "B1_0", "G1_0", "A2_0", "B2_0", "G2_0", "A1c", "B1c",
         "A1_1", "B1_1", "G1_1", "A2_1", "B2_1", "G2_1", "omka", "hrk"])}
    omu = k.sb("omu", [128, 48], F32)

    def S(name, c=None):
        o = S_[name]
        if c is None:
            return scal[:, o:o + 8]
        return scal[:, o + c:o + c + 1]

    k.dma(sp, vec[:], vec_d[:, :], w=[d_const])
    k.dma(sp, cst[:], cst_d[:, :], w=[d_const])
    k.op(dve, lambda: nc.vector.tensor_copy(ident_bf[:], cst[:, C_ID:C_ID + 128]), r=[d_const], w=[d_const])
    k.op(dve, lambda: nc.vector.tensor_copy(maskF[:], cst[0:64, C_MF:C_MF + 512]), r=[d_const], w=[d_const])
    k.op(dve, lambda: nc.vector.tensor_copy(maskB[:], cst[0:64, C_MB:C_MB + 512]), r=[d_const], w=[d_const])
    k.op(dve, lambda: nc.vector.tensor_copy(maskLF[:], cst[0:64, C_LF:C_LF + 128]), r=[d_const], w=[d_const])
    k.op(dve, lambda: nc.vector.tensor_copy(maskLB[:], cst[0:64, C_LB:C_LB + 128]), r=[d_const], w=[d_const])
    k.op(dve, lambda: nc.vector.memset(ones_bf[:], 1.0), w=[d_const])
    k.op(dve, lambda: nc.vector.memset(bd_bf[:], 0.0), w=[d_const])
    k.op(dve, lambda: nc.vector.memset(bdm_f[:], 0.0), w=[d_const])
    k.op(dve, lambda: nc.vector.memset(bd1_f[:], 0.0), w=[d_const])
    for h in range(2):
        sl = slice(64 * h, 64 * h + 64)
        k.op(dve, lambda sl=sl: nc.vector.memset(bd_bf[sl, sl], 1.0), w=[d_const])
        k.op(dve, lambda sl=sl: nc.vector.memset(bdm_f[sl, sl], 1.0 / 64), w=[d_const])
        k.op(dve, lambda sl=sl: nc.vector.memset(bd1_f[sl, sl], 1.0), w=[d_const])
    k.op(dve, lambda: nc.vector.memset(rmask[:], 1.0), w=[d_const])
    k.op(dve, lambda: nc.vector.memset(rmask[:, 0:576:64], 0.0), w=[d_const])
    k.op(dve, lambda: nc.vector.tensor_scalar(omu[:], V("mu", 0, 48), -1.0, 1.0, ALU.mult, ALU.add),
         r=[d_const], w=[d_const])

    with contextlib.ExitStack() as ph:
        cv = k.sb("cv", [128, KC, 2], F32, ph)
        scb = k.sb("scb", [128, KC, 2], BF16, ph)
        wb = [k.sb("modw%d" % i, [128, KC, 1024], BF16, ph) for i in range(2)]
        dwb = [Dep(), Dep()]
        d_cv = Dep()
        k.dma(sp, cv[:], cvec_d[:, :, :], w=[d_cv])
        k.op(act, lambda: nc.scalar.activation(scb[:], cv[:], AF.Silu), r=[d_cv], w=[d_cv])
        gi = 0
        for i in range(2):
            for g in range(6):
                b = gi % 2
                gi += 1
                k.dma(pool, wb[b][:], kview(mod_w_d[i], g * 1024, (g + 1) * 1024), w=[dwb[b]])
                for m in range(8):
                    mg = g * 8 + m
                    cc = (i * 48 + mg) * 2
                    for kk in range(KC):
                        k.op(pe, lambda b=b, m=m, kk=kk, cc=cc: nc.tensor.matmul(
                            PS[0][:, cc:cc + 2], wb[b][:, kk, m * 128:(m + 1) * 128], scb[:, kk, :],
                            start=(kk == 0), stop=(kk == KC - 1)), r=[dwb[b], d_cv], w=[dPS[0]])
        k.op(dve, lambda: nc.vector.tensor_tensor(mod[:], PS[0][:, 0:192], V("modb", 0, 192), ALU.add),
             r=[dPS[0], d_const], w=[d_mod])

        def mcol(i, s, j):
            o = (i * 48 + s * 8) * 2 + j
            return mod[:, o:o + 16:2]

        def mk_scale(dst, gi_, li, s, j):
            k.op(dve, lambda: nc.vector.tensor_scalar(S(dst), mcol(li, s, j), 1.0, None, ALU.add),
                 r=[d_mod], w=[d_scal])
            k.op(dve, lambda: nc.vector.tensor_tensor(S(dst), S(dst), V("g", li * 4 + gi_), ALU.mult),
                 r=[d_scal, d_const], w=[d_scal])

        def mk_copy(dst, li, s, j):
            k.op(dve, lambda: nc.vector.tensor_copy(S(dst), mcol(li, s, j)), r=[d_mod], w=[d_scal])

        def mk_gate(dst, gi_, li, s):
            k.op(dve, lambda: nc.vector.tensor_tensor(S(dst), mcol(li, s, 0), V("g", li * 4 + gi_), ALU.mult),
                 r=[d_mod, d_const], w=[d_scal])

        for li in range(2):
            mk_scale("A1_%d" % li, 0, li, 1, 0)
            mk_copy("B1_%d" % li, li, 0, 0)
            mk_gate("G1_%d" % li, 1, li, 2)
            mk_scale("A2_%d" % li, 2, li, 4, 0)
            mk_copy("B2_%d" % li, li, 3, 0)
            mk_gate("G2_%d" % li, 3, li, 5)
        mk_scale("A1c", 0, 0, 1, 1)
        mk_copy("B1c", 0, 0, 1)
        k.op(dve, lambda: nc.vector.tensor_scalar(S("omka"), V("k_a"), -1.0, 1.0, ALU.mult, ALU.add),
             r=[d_const], w=[d_scal])
        k.op(dve, lambda: nc.vector.tensor_scalar(S("hrk"), V("r_k"), 0.5, None, ALU.mult),
             r=[d_const], w=[d_scal])
        k.barrier()

    def sumsq_rstd(src_fn, n, scratch_bf, rs, d_src, d_scr, d_rs, psi, eps=NORM_EPS, nchunks=KC):
        for c in range(nchunks):
            k.op(act, lambda c=c: nc.scalar.activation(scratch_bf[:, c, :n], src_fn(c), AF.Square),
                 r=[d_src], w=[d_scr])
        for c in range(nchunks):
            k.op(pe, lambda c=c: nc.tensor.matmul(PS[psi][:, :n], ones_bf[:], scratch_bf[:, c, :n],
                                                  start=(c == 0), stop=(c == nchunks - 1)),
                 r=[d_scr, d_const], w=[dPS[psi]])
        k.op(act, lambda: nc.scalar.activation(rs[:, :n], PS[psi][:, :n], AF.Sqrt, bias=eps, scale=1.0 / D),
             r=[dPS[psi]], w=[d_rs])
        k.op(dve, lambda: nc.vector.reciprocal(rs[:, :n], rs[:, :n]), r=[d_rs], w=[d_rs])

    def norm_mod(src, d_src, n, An, Bn, dst_fn, d_dst, wk, h32=None, d_h32=None):
        sumsq_rstd(lambda c: src[:, c, :n], n, wk["sq"], wk["rs"], d_src, wk["dsq"], wk["drs"], 6)
        for c in range(KC):
            k.op(dve, lambda c=c: nc.vector.tensor_tensor(wk["tmp"][:, c, :n], src[:, c, :n], wk["rs"][:, :n], ALU.mult),
                 r=[d_src, wk["drs"]], w=[wk["dtmp"]])
            k.op(act, lambda c=c: nc.scalar.activation(dst_fn(c), wk["tmp"][:, c, :n], AF.Identity,
                                                       bias=S(Bn, c), scale=S(An, c)),
                 r=[wk["dtmp"], d_scal], w=[d_dst])
            if h32 is not None:
                k.op(pool, lambda c=c: nc.gpsimd.tensor_scalar(h32[:, c, :n], wk["tmp"][:, c, :n], S(An, c), S(Bn, c),
                                                               ALU.mult, ALU.add),
                     r=[wk["dtmp"], d_scal], w=[d_h32])

    def res_norm(y, d_y, n, Gn, xprev, d_xprev, xnew, d_xnew, wk):
        sumsq_rstd(lambda c: y[:, c, :n], n, wk["sq"], wk["rs"], d_y, wk["dsq"], wk["drs"], 6)
        for c in range(KC):
            k.op(pool, lambda c=c: nc.gpsimd.tensor_tensor(wk["tmp"][:, c, :n], y[:, c, :n], wk["rs"][:, :n], ALU.mult),
                 r=[d_y, wk["drs"]], w=[wk["dtmp"]])
            k.op(dve, lambda c=c: nc.vector.scalar_tensor_tensor(xnew[:, c, :n], wk["tmp"][:, c, :n], S(Gn, c),
                                                                 xprev[:, c, :n], ALU.mult, ALU.add),
                 r=[wk["dtmp"], d_scal, d_xprev], w=[d_xnew])

    def mk_wk(ph, n=512):
        return dict(sq=k.sb("wsq", [128, KC, n], BF16, ph), rs=k.sb("wrs", [128, n], F32, ph),
                    tmp=k.sb("wtmp", [128, KC, n], F32, ph), dsq=Dep(), drs=Dep(), dtmp=Dep())

    dbg_done = [False]

    def dump(ap_fn, ncols, d, off=0):
        k.dma(sp, dbg_d[:, off:off + ncols], ap_fn(), r=[d])

    lora_stack = contextlib.ExitStack()
    TW = k.sb("TW", [128, NT], BF16, lora_stack)
    TA = k.sb("TA", [128, NT], BF16, lora_stack)
    SG = k.sb("SG", [128, 2, T], BF16, lora_stack)
    d_TW, d_TA, d_SG = Dep(), Dep(), Dep()
    hT_stack = contextlib.ExitStack()
    hT = k.sb("hT", [128, KC, NT], BF16, hT_stack)
    hsT = k.sb("hsT", [128, KC, NT], BF16, hT_stack)
    d_h = Dep()
    d_hs = Dep()
    with contextlib.ExitStack() as ph:
        wk = mk_wk(ph)
        xb = [k.sb("xb%d" % i, [128, KC, 512], F32, ph) for i in range(2)]
        dxb = [Dep(), Dep()]
        k.dma(sp, xb[0][:, :, 0:TC], ctxT_d[:, :, :], w=[dxb[0]])
        norm_mod(xb[0], dxb[0], TC, "A1c", "B1c", lambda c: hT[:, c, 0:TC], d_h, wk)
        for tb in range(4):
            b = (tb + 1) % 2
            k.dma(sp, xb[b][:], xT_d[:, :, tb * 512:(tb + 1) * 512], w=[dxb[b]])
            norm_mod(xb[b], dxb[b], 512, "A1_0", "B1_0",
                     lambda c, tb=tb: hT[:, c, TC + tb * 512:TC + (tb + 1) * 512], d_h, wk)
        k.op(pool, lambda: nc.gpsimd.memset(hsT[:], 0.0), w=[d_hs])
        L0 = TC
        for c in range(KC):
            eng = act if c % 2 == 0 else pool

            def cp(dst, src, eng=eng):
                if eng is act:
                    k.op(act, lambda: nc.scalar.copy(dst, src), r=[d_h], w=[d_hs])
                else:
                    k.op(pool, lambda: nc.gpsimd.tensor_copy(dst, src), r=[d_h], w=[d_hs])
            if c < 4:
                cp(hsT[:, c, 1:TC], hT[:, c, 0:TC - 1])
            else:
                cp(hsT[:, c, 0:TC - 1], hT[:, c, 1:TC])
            if c in (0, 1):
                cp(hsT[:, c, L0 + 1:L0 + T], hT[:, c, L0:L0 + T - 1])
                k.op(pool, lambda c=c: nc.gpsimd.memset(hsT[:, c, L0:L0 + T:64], 0.0), w=[d_hs])
            elif c in (2, 3):
                cp(hsT[:, c, L0:L0 + T - 1], hT[:, c, L0 + 1:L0 + T])
                k.op(pool, lambda c=c: nc.gpsimd.memset(hsT[:, c, L0 + 63:L0 + T:64], 0.0), w=[d_hs])
            elif c in (4, 5):
                cp(hsT[:, c, L0 + 64:L0 + T], hT[:, c, L0:L0 + T - 64])
            else:
                cp(hsT[:, c, L0:L0 + T - 64], hT[:, c, L0 + 64:L0 + T])
        k.barrier()

    if dbg == 1:
        with contextlib.ExitStack() as ph:
            t32 = k.sb("t32", [128, 2 * NT], F32, ph)
            dd = Dep()
            k.op(dve, lambda: nc.vector.tensor_copy(t32[:, 0:NT], hT[:, 0, :]), r=[d_h], w=[dd])
            k.op(dve, lambda: nc.vector.tensor_copy(t32[:, NT:2 * NT], hsT[:, 5, :]), r=[d_hs], w=[dd])
            dump(lambda: t32[:], 2 * NT, dd)
            k.barrier()
        hT_stack.close()
        lora_stack.close()
        k.finish()
        return k


    def load_mixed(ph_, dram_view, ncols, mu_i, name):
        st = k.sb(name + "st", [128, KC, ncols], F32, ph_)
        Wa = k.sb(name + "a", [128, KC, ncols], BF16, ph_)
        Wb = k.sb(name + "b", [128, KC, ncols], BF16, ph_)
        dst_, dw = Dep(), Dep()
        k.dma(sp, st[:], dram_view, w=[dst_])
        for kk in range(KC):
            o = mu_i * 8 + kk
            k.op(dve, lambda kk=kk, o=o: nc.vector.tensor_scalar(Wa[:, kk, :], st[:, kk, :], omu[:, o:o + 1], None, ALU.mult),
                 r=[dst_, d_const], w=[dw])
            k.op(pool, lambda kk=kk, o=o: nc.gpsimd.tensor_scalar(Wb[:, kk, :], st[:, kk, :], V("mu", 0, 48)[:, o:o + 1], None, ALU.mult),
                 r=[dst_, d_const], w=[dw])
        return Wa, Wb, dw

    def proj_mixed(psi, Wa, Wb, dw, c0, c1, t0, n, mrows=128):
        for kk in range(KC):
            k.op(pe, lambda kk=kk: nc.tensor.matmul(PS[psi][:mrows, :n], Wa[:, kk, c0:c1], hT[:, kk, t0:t0 + n],
                                                    start=(kk == 0), stop=False),
                 r=[dw, d_h], w=[dPS[psi]])
        for kk in range(KC):
            k.op(pe, lambda kk=kk: nc.tensor.matmul(PS[psi][:mrows, :n], Wb[:, kk, c0:c1], hsT[:, kk, t0:t0 + n],
                                                    start=False, stop=(kk == KC - 1)),
                 r=[dw, d_hs], w=[dPS[psi]])

    with contextlib.ExitStack() as ph:
        W1a, W1b, dW1 = load_mixed(ph, kview(w1_d, 0, 128), 128, 1, "w1")
        A1a, A1b, dA1 = load_mixed(ph, kview(a1_d, 0, 128), 128, 4, "a1")
        G1a, G1b, dG1 = load_mixed(ph, kview(g1_d, 0, 160), 160, 5, "g1")
        pi = 0
        for tb in range(NT // 256):
            t0 = tb * 256
            proj_mixed(pi % 4, W1a, W1b, dW1, 0, 128, t0, 256)
            k.op(act, lambda p=pi % 4, t0=t0: nc.scalar.activation(TW[:, t0:t0 + 256], PS[p][:, :256], AF.Tanh),
                 r=[dPS[pi % 4]], w=[d_TW])
            pi += 1
            proj_mixed(pi % 4, A1a, A1b, dA1, 0, 128, t0, 256)
            k.op(dve, lambda p=pi % 4, t0=t0: nc.vector.tensor_copy(TA[:, t0:t0 + 256], PS[p][:, :256]),
                 r=[dPS[pi % 4]], w=[d_TA])
            pi += 1
            if t0 >= TC:
                l0 = t0 - TC
                proj_mixed(pi % 4, G1a, G1b, dG1, 0, 128, t0, 256)
                k.op(act, lambda p=pi % 4, l0=l0: nc.scalar.activation(SG[:, 0, l0:l0 + 256], PS[p][:, :256], AF.Sigmoid),
                     r=[dPS[pi % 4]], w=[d_SG])
                pi += 1
                proj_mixed(pi % 4, G1a, G1b, dG1, 128, 160, t0, 256, mrows=32)
                k.op(act, lambda p=pi % 4, l0=l0: nc.scalar.activation(SG[0:32, 1, l0:l0 + 256], PS[p][0:32, :256], AF.Sigmoid),
                     r=[dPS[pi % 4]], w=[d_SG])
                pi += 1
        k.barrier()

    with contextlib.ExitStack() as ph:
        st = k.sb("rkvst", [128, KC, D], F32, ph)
        Wa = k.sb("rkvWa", [128, KC, D], BF16, ph)
        Wb = k.sb("rkvWb", [128, KC, D], BF16, ph)
        rowb = [k.sb("rowb%d" % i, [128, NT], F32, ph) for i in range(2)]
        d_st, d_Wab = Dep(), Dep()
        d_row = [Dep(), Dep()]
        mu_of = [0, 2, 3]
        ri = 0
        pi = 0
        for j in range(3):
            k.dma(sp, st[:], kview(w_rkv_d[j], 0, D), w=[d_st])
            for kk in range(KC):
                o = mu_of[j] * 8 + kk
                k.op(dve, lambda kk=kk, o=o: nc.vector.tensor_scalar(Wa[:, kk, :], st[:, kk, :], omu[:, o:o + 1], None, ALU.mult),
                     r=[d_st, d_const], w=[d_Wab])
                k.op(pool, lambda kk=kk, o=o: nc.gpsimd.tensor_scalar(Wb[:, kk, :], st[:, kk, :], V("mu", 0, 48)[:, o:o + 1], None, ALU.mult),
                     r=[d_st, d_const], w=[d_Wab])
            for c in range(KC):
                rb = rowb[ri % 2]
                drb = d_row[ri % 2]
                ri += 1
                for tb in range(NT // 256):
                    t0 = tb * 256
                    p = pi % 4
                    pi += 1
                    proj_mixed(p, Wa, Wb, d_Wab, c * 128, (c + 1) * 128, t0, 256)
                    if tb % 2 == 0:
                        k.op(act, lambda p=p, rb=rb, t0=t0: nc.scalar.copy(rb[:, t0:t0 + 256], PS[p][:, :256]), r=[dPS[p]], w=[drb])
                    else:
                        k.op(dve, lambda p=p, rb=rb, t0=t0: nc.vector.tensor_copy(rb[:, t0:t0 + 256], PS[p][:, :256]), r=[dPS[p]], w=[drb])
                k.dma(sp, rkv_d[j, :, c, :], rb[:], r=[drb])
        k.barrier()
    hT_stack.close()

    def bc3(t2d, col0, nouter, ostride, ninner):
        base = t2d[:, col0:col0 + 1]
        pst = base.ap[0][0]
        return AP(base.tensor, base.offset, [[pst, 128], [ostride, nouter], [0, ninner]])

    BL = 576
    NBL = NT // BL
    CPB = BL // 64
    GI = 4
    NG = NCH // GI
    with contextlib.ExitStack() as ph:
        r32 = k.sb("r32", [128, NT], F32, ph)
        k32 = k.sb("k32", [128, NT], F32, ph)
        kk32 = k.sb("kk32", [128, NT], F32, ph)
        v16 = k.sb("v16", [128, NT], BF16, ph)
        ksum = k.sb("ksum", [128, NT], F32, ph)
        yz = [k.sb("yz%d" % z, [128, T], F32, ph) for z in range(2)]
        d_yz = [Dep(), Dep()]
        sgw = k.sb("sgw", [128, BL], F32, ph)
        cs = k.sb("cs", [128, BL], F32, ph)
        cs2 = k.sb("cs2", [128, BL], F32, ph)
        iclr = k.sb("iclr", [128, BL], F32, ph)
        t2 = k.sb("t2", [128, BL], F32, ph)
        t3 = k.sb("t3", [128, BL], F32, ph)
        sqb = k.sb("sqb", [128, 512], BF16, ph)
        ost = [k.sb("ost%d" % i, [128, T], BF16, ph) for i in range(1)]
        d_ost = [Dep()]
        w2c = k.sb("w2c", [128, 128], BF16, ph)
        a2c = k.sb("a2c", [128, 128], BF16, ph)
        g2c = k.sb("g2c", [128, 2, 128], BF16, ph)
        d_w2, d_a2, d_g2 = Dep(), Dep(), Dep()
        d_r, d_k, d_kk, d_v, d_ks = Dep(), Dep(), Dep(), Dep(), Dep()
        d_sgw, d_cs, d_cs2, d_iclr, d_t2, d_t3, d_sq = (Dep() for _ in range(7))
        DB = []
        for z in range(2):
            b_ = dict(
                ARbd=k.sb("ARbd%d" % z, [128, NCH, 2, 128], BF16, ph),
                bt=k.sb("bt%d" % z, [128, NT], BF16, ph), kt=k.sb("kt%d" % z, [128, NT], BF16, ph),
                WC=k.sb("WC%d" % z, [128, NCH], F32, ph),
                M32=k.sb("M32%d" % z, [128, 128], F32, ph), S16=k.sb("S16%d" % z, [128, 128], BF16, ph),
                X16=k.sb("X16%d" % z, [64, 128], BF16, ph), U16=k.sb("U16%d" % z, [64, 128], BF16, ph),
                NB=k.sb("NB%d" % z, [64, GI * 128], BF16, ph), LB=k.sb("LB%d" % z, [64, GI * 128], BF16, ph),
                XB=k.sb("XB%d" % z, [64, GI * 128], BF16, ph), LBI=k.sb("LBI%d" % z, [64, GI * 128], BF16, ph), d_LBI=Dep(),
                L1=[k.sb("L1_%d%d" % (z, i), [64, GI, 512], BF16, ph) for i in range(2)],
                TT=[k.sb("TT%d%d" % (z, i), [64, GI, 128], BF16, ph) for i in range(2)],
                tm=[k.sb("tm%d%d" % (z, i), [64, GI, 384], BF16, ph) for i in range(2)],
                d_AR=Dep(), d_bt=Dep(), d_kt=Dep(), d_WC=Dep(), d_M=Dep(), d_S=Dep(), d_X=Dep(), d_U=Dep(),
                d_NB=Dep(), d_LB=Dep(), d_XB=Dep(), d_L1=[Dep(), Dep()], d_TT=[Dep(), Dep()], d_tm=[Dep(), Dep()],
                d_ARb=[Dep() for _ in range(4)], d_btb=[Dep() for _ in range(4)], d_ktb=[Dep() for _ in range(4)],
                d_WCb=[Dep() for _ in range(4)], prev=None)
            DB.append(b_)
            k.op(pool, lambda b_=b_: nc.gpsimd.memset(b_["ARbd"][:], 0.0), w=b_["d_ARb"])
        k.min_free = min(getattr(k, "min_free", 1 << 30), nc.sbuf_bytes_remaining)

        for c in range(KC):
            cs0, cs1 = c * 128, (c + 1) * 128
            k.dma(sp, r32[:], rkv_d[0, :, c, :], w=[d_r])
            k.dma(sp, k32[:], rkv_d[1, :, c, :], w=[d_k])
            k.dma(pool, v16[:], rkv_d[2, :, c, :], w=[d_v])
            k.dma(pool, w2c[:], w2_d[:, cs0:cs1], w=[d_w2])
            k.dma(pool, a2c[:], a2_d[:, cs0:cs1], w=[d_a2])
            k.dma(pool, g2c[:, 0, :], g2_d[0:128, cs0:cs1], w=[d_g2])
            k.dma(pool, g2c[0:32, 1, :], g2_d[128:160, cs0:cs1], w=[d_g2])
            k.op(dve, lambda: nc.vector.tensor_scalar(kk32[:], k32[:], Vc("k_k", 0, c), None, ALU.mult),
                 r=[d_k, d_const], w=[d_kk])
            for q in range(0, NT, 512):
                n = min(512, NT - q)
                p = 4 + (q // 512) % 2
                k.op(act, lambda q=q, n=n: nc.scalar.activation(sqb[:, :n], kk32[:, q:q + n], AF.Square),
                     r=[d_kk], w=[d_sq])
                k.op(pe, lambda q=q, n=n, p=p: nc.tensor.matmul(PS[p][:, :n], bd_bf[:], sqb[:, :n], start=True, stop=True),
                     r=[d_sq, d_const], w=[dPS[p]])
                k.op(dve, lambda q=q, n=n, p=p: nc.vector.tensor_scalar(t2[:, :n], PS[p][:, :n], 1e-24, None, ALU.max),
                     r=[dPS[p]], w=[d_t2])
                k.op(act, lambda n=n: nc.scalar.activation(t2[:, :n], t2[:, :n], AF.Sqrt), r=[d_t2], w=[d_t2])
                k.op(dve, lambda n=n: nc.vector.reciprocal(t2[:, :n], t2[:, :n]), r=[d_t2], w=[d_t2])
                k.op(dve, lambda q=q, n=n: nc.vector.tensor_tensor(kk32[:, q:q + n], kk32[:, q:q + n], t2[:, :n], ALU.mult),
                     r=[d_kk, d_t2], w=[d_kk])

            k.op(pool, lambda: nc.gpsimd.memset(ksum[:], 0.0), w=[d_ks])
            emitted = set()

            def preproc_ops(z, blk):
                B_ = DB[z]
                ARbd, bt, kt, WC = B_["ARbd"], B_["bt"], B_["kt"], B_["WC"]
                d_AR, d_bt, d_kt, d_WC = B_["d_ARb"][blk], B_["d_btb"][blk], B_["d_ktb"][blk], B_["d_WCb"][blk]
                zs = slice(64 * z, 64 * z + 64)
                q0 = blk * BL
                qs = slice(q0, q0 + BL)
                nb0 = blk * CPB
                ops = []
                A = ops.append
                for sbk in range(2):
                    t0 = q0 + sbk * 288
                    o0 = sbk * 288
                    p = 2 + sbk
                    A(lambda p=p, t0=t0: k.op(pe, lambda: nc.tensor.matmul(PS[p][:, :288], w2c[zs, :], TW[zs, t0:t0 + 288],
                                                                         start=True, stop=True), r=[d_w2, d_TW], w=[dPS[p]]))
                    A(lambda p=p, o0=o0: k.op(act, lambda: nc.scalar.activation(sgw[:, o0:o0 + 288], PS[p][:, :288], AF.Sigmoid,
                                                                              bias=Vc("w0", z, c)), r=[dPS[p], d_const], w=[d_sgw]))
                    p2 = 4 + sbk
                    A(lambda p=p2, t0=t0: k.op(pe, lambda: nc.tensor.matmul(PS[p][:, :288], a2c[zs, :], TA[zs, t0:t0 + 288],
                                                                          start=True, stop=True), r=[d_a2, d_TA], w=[dPS[p]]))
                    A(lambda p=p2, o0=o0: k.op(act, lambda: nc.scalar.activation(iclr[:, o0:o0 + 288], PS[p][:, :288], AF.Sigmoid,
                                                                               bias=Vc("a0", z, c)), r=[dPS[p], d_const], w=[d_iclr]))
                A(lambda: k.op(dve, lambda: nc.vector.tensor_tensor_scan(cs[:], rmask[:, 0:BL], sgw[:], 0.0, ALU.mult, ALU.add),
                               r=[d_sgw, d_const], w=[d_cs]))
                if z == 1:
                    A(lambda: k.op(dve, lambda: nc.vector.tensor_tensor(cs2[:], sgw[:], cs[:], ALU.subtract),
                                   r=[d_sgw, d_cs], w=[d_cs2]))
                    A(lambda: k.op(dve, lambda: nc.vector.tensor_tensor(
                        cs2[:].rearrange("p (n t) -> p n t", t=64), cs2[:].rearrange("p (n t) -> p n t", t=64),
                        bc3(cs, 63, CPB, 64, 64), ALU.add), r=[d_cs2, d_cs], w=[d_cs2]))
                    csu, d_csu = cs2, d_cs2
                else:
                    csu, d_csu = cs, d_cs
                A(lambda: k.op(pool, lambda: nc.gpsimd.tensor_scalar(t2[:], iclr[:], Vc("k_a", 0, c), S("omka", c), ALU.mult, ALU.add),
                               r=[d_iclr, d_const, d_scal], w=[d_t2]))
                A(lambda: k.op(pool, lambda: nc.gpsimd.tensor_tensor(t2[:], t2[:], k32[:, qs], ALU.mult), r=[d_t2, d_k], w=[d_t2]))
                A(lambda: k.op(pool, lambda: nc.gpsimd.tensor_tensor(ksum[:, qs], ksum[:, qs], t2[:], ALU.add), r=[d_t2, d_ks], w=[d_ks]))
                A(lambda: k.op(act, lambda: nc.scalar.activation(t3[:], csu[:], AF.Exp, scale=-DECAY_S), r=[d_csu], w=[d_t3]))
                A(lambda: k.op(dve, lambda: nc.vector.tensor_tensor(kt[:, qs], t2[:], t3[:], ALU.mult), r=[d_t2, d_t3], w=[d_kt]))
                A(lambda: k.op(pool, lambda: nc.gpsimd.tensor_tensor(t2[:], kk32[:, qs], iclr[:], ALU.mult), r=[d_kk, d_iclr, d_kt], w=[d_t2]))
                A(lambda: k.op(dve, lambda: nc.vector.tensor_tensor(bt[:, qs], t2[:], t3[:], ALU.mult), r=[d_t2, d_t3], w=[d_bt]))
                A(lambda: k.op(act, lambda: nc.scalar.activation(t3[:], csu[:], AF.Exp, scale=DECAY_S), r=[d_csu, d_bt], w=[d_t3]))
                wc_col = 63 if z == 0 else 0
                A(lambda: k.op(pool, lambda: nc.gpsimd.tensor_copy(WC[:, nb0:nb0 + CPB], t3[:, wc_col:BL:64]),
                               r=[d_t3], w=[d_WC]))
                for h in range(2):
                    hs_ = slice(64 * h, 64 * h + 64)
                    A(lambda h=h, hs_=hs_: k.op(dve, lambda: nc.vector.tensor_tensor(
                        ARbd[hs_, nb0:nb0 + CPB, h, 64:128], r32[hs_, qs].rearrange("p (n t) -> p n t", t=64),
                        t3[hs_, :].rearrange("p (n t) -> p n t", t=64), ALU.mult), r=[d_r, d_t3], w=[d_AR]))
                A(lambda: k.op(pool, lambda: nc.gpsimd.tensor_tensor(t2[:], csu[:], sgw[:], ALU.subtract), r=[d_csu, d_sgw, d_bt], w=[d_t2]))
                A(lambda: k.op(act, lambda: nc.scalar.activation(t2[:], t2[:], AF.Exp, scale=DECAY_S), r=[d_t2], w=[d_t2]))
                for h in range(2):
                    hs_ = slice(64 * h, 64 * h + 64)
                    A(lambda h=h, hs_=hs_: k.op(dve, lambda: nc.vector.scalar_tensor_tensor(
                        ARbd[hs_, nb0:nb0 + CPB, h, 0:64], kk32[hs_, qs].rearrange("p (n t) -> p n t", t=64), -1.0,
                        t2[hs_, :].rearrange("p (n t) -> p n t", t=64), ALU.mult, ALU.mult), r=[d_kk, d_t2], w=[d_AR]))
                A(lambda: emitted.add((z, blk)))
                return ops

            def pre_stages(z, gb, chunks):
                B_ = DB[z]
                ARbd, bt, kt = B_["ARbd"], B_["bt"], B_["kt"]
                NBg, LBg, XBg, LBIg, d_LBI = B_["NB"], B_["LB"], B_["XB"], B_["LBI"], B_["d_LBI"]
                L1g, TTg, tmg = B_["L1"][gb], B_["TT"][gb], B_["tm"][gb]
                for n_ in chunks:
                    assert (z, n_ // CPB) in emitted, (z, n_)
                d_NB, d_LB, d_XB = B_["d_NB"], B_["d_LB"], B_["d_XB"]
                d_L1, d_TT, d_tm = B_["d_L1"][gb], B_["d_TT"][gb], B_["d_tm"][gb]
                mk = maskF if z == 0 else maskB
                mkL = maskLF if z == 0 else maskLB
                G = len(chunks)
                bA, bN, bL = (2, 3, 4) if z == 0 else (5, 6, 4)
                stages = []

                def stageA(lo, hi, last):
                    for gi in range(lo, hi):
                        n = chunks[gi]
                        t0 = n * 64
                        d_AR, d_bt, d_kt = B_["d_ARb"][n // CPB], B_["d_btb"][n // CPB], B_["d_ktb"][n // CPB]
                        k.op(pe, lambda t0=t0: nc.tensor.transpose(PSB[0:64, 0:128], bt[:, t0:t0 + 64], ident_bf[:]),
                             r=[d_bt, d_const], w=[dPSB])
                        k.op(pe, lambda t0=t0: nc.tensor.transpose(PSB[0:64, 128:256], kt[:, t0:t0 + 64], ident_bf[:]),
                             r=[d_kt, d_const], w=[dPSB])
                        k.op(pe, lambda t0=t0: nc.tensor.transpose(PSB[0:64, 256:384], v16[:, t0:t0 + 64], ident_bf[:]),
                             r=[d_v, d_const], w=[dPSB])
                        k.op(act, lambda gi=gi: nc.scalar.copy(tmg[:, gi, :], PSB[0:64, 0:384]), r=[dPSB], w=[d_tm])
                        k.op(pe, lambda n=n, t0=t0: nc.tensor.matmul(
                            PS[bA][0:64, 0:256], bt[:, t0:t0 + 64], ARbd[:, n, :, :].rearrange("p h x -> p (h x)"),
                            start=True, stop=True), r=[d_bt, d_AR], w=[dPS[bA]])
                        k.op(pe, lambda n=n, t0=t0: nc.tensor.matmul(
                            PS[bA][0:64, 256:512], kt[:, t0:t0 + 64], ARbd[:, n, :, :].rearrange("p h x -> p (h x)"),
                            start=True, stop=True), r=[d_kt, d_AR], w=[dPS[bA]])
                        k.op(dve, lambda gi=gi: nc.vector.tensor_tensor(L1g[:, gi, :], PS[bA][0:64, :], mk[:], ALU.mult),
                             r=[dPS[bA], d_const], w=[d_L1])
                        for h in range(2):
                            k.op(pe, lambda n=n, t0=t0, h=h, gi=gi: nc.tensor.matmul(
                                PS[bN][0:64, gi * 128 + h * 64: gi * 128 + h * 64 + 64],
                                ARbd[:, n, h, 0:64], bt[:, t0:t0 + 64], start=True, stop=True),
                                r=[d_AR, d_bt], w=[dPS[bN]])
                    if last:
                        k.op(dve, lambda: nc.vector.tensor_tensor(
                            LBg[:, 0:G * 128].rearrange("p (g x) -> p g x", x=128),
                            PS[bN][0:64, 0:G * 128].rearrange("p (g x) -> p g x", x=128),
                            mkL[:, :].unsqueeze(1).to_broadcast([64, G, 128]), ALU.mult), r=[dPS[bN], d_const], w=[d_LB])
                        k.op(pool, lambda: nc.gpsimd.tensor_copy(
                            NBg[:, 0:G * 128].rearrange("p (g h x) -> p g h x", h=2, x=64),
                            L1g[:, 0:G, 0:256].rearrange("p g (h x) -> p g h x", h=2)[:, :, :, 0:64]),
                            r=[d_L1], w=[d_NB])
                        k.op(pool, lambda: nc.gpsimd.tensor_tensor(
                            XBg[:, 0:G * 128].rearrange("p (g x) -> p g x", x=64), NBg[:, 0:G * 128].rearrange("p (g x) -> p g x", x=64),
                            ident_bf[0:64, 0:64].unsqueeze(1).to_broadcast([64, 2 * G, 64]), ALU.add),
                            r=[d_NB, d_const], w=[d_XB])

                def stageC():
                    for q in range(2 * G):
                        qq = slice(q * 64, q * 64 + 64)
                        k.op(pe, lambda qq=qq: nc.tensor.matmul(PS[bN][0:64, qq], LBg[:, qq], NBg[:, qq], start=True, stop=True),
                             r=[d_LB, d_NB], w=[dPS[bN]])
                    for q in range(2 * G):
                        qq = slice(q * 64, q * 64 + 64)
                        k.op(pe, lambda qq=qq: nc.tensor.matmul(PS[bL][0:64, qq], NBg[:, qq], LBg[:, qq], start=True, stop=True),
                             r=[d_LB, d_NB], w=[dPS[bL]])
                    k.op(act, lambda: nc.scalar.copy(NBg[:, 0:G * 128], PS[bN][0:64, 0:G * 128]), r=[dPS[bN]], w=[d_NB])
                    k.op(dve, lambda: nc.vector.tensor_copy(LBg[:, 0:G * 128], PS[bL][0:64, 0:G * 128]), r=[dPS[bL]], w=[d_LB])
                    k.op(pool, lambda: nc.gpsimd.tensor_tensor(
                        LBIg[:, 0:G * 128].rearrange("p (g x) -> p g x", x=64), LBg[:, 0:G * 128].rearrange("p (g x) -> p g x", x=64),
                        ident_bf[0:64, 0:64].unsqueeze(1).to_broadcast([64, 2 * G, 64]), ALU.add),
                        r=[d_LB, d_const], w=[d_LBI])

                def stageD(final):
                    for q in range(2 * G):
                        qq = slice(q * 64, q * 64 + 64)
                        k.op(pe, lambda qq=qq: nc.tensor.matmul(PS[bA][0:64, qq], LBIg[:, qq], XBg[:, qq], start=True, stop=True),
                             r=[d_LBI, d_XB], w=[dPS[bA]])
                    if not final:
                        k.op(act, lambda: nc.scalar.copy(XBg[:, 0:G * 128], PS[bA][0:64, 0:G * 128]), r=[dPS[bA]], w=[d_XB])
                    else:
                        k.op(act, lambda: nc.scalar.copy(
                            TTg[:, 0:G, :].rearrange("p g x -> p (g x)"), PS[bA][0:64, 0:G * 128]),
                            r=[dPS[bA]], w=[d_TT])

                stages.append(lambda: stageA(0, G // 2, False))
                stages.append(lambda: stageA(G // 2, G, True))
                for step in range(5):
                    stages.append(stageC)
                    stages.append(lambda step=step: stageD(step == 4))
                return stages

            def chain_seg(z, gb, gi, n, seg):
                B_ = DB[z]
                ARbd, WC, M32, S16, X16, U16 = B_["ARbd"], B_["WC"], B_["M32"], B_["S16"], B_["X16"], B_["U16"]
                L1g, TTg, tmg = B_["L1"][gb], B_["TT"][gb], B_["tm"][gb]
                d_AR, d_WC, d_M, d_S, d_X, d_U = B_["d_ARb"][n // CPB], B_["d_WCb"][n // CPB], B_["d_M"], B_["d_S"], B_["d_X"], B_["d_U"]
                d_WCp = B_["d_WCb"][B_["prev"] // CPB] if B_["prev"] is not None else d_WC
                d_L1, d_TT, d_tm = B_["d_L1"][gb], B_["d_TT"][gb], B_["d_tm"][gb]
                bk = PS[z]
                dbk = dPS[z]
                xo, uo, so, yo = 0, 128, 256, 384
                pX = bk[0:64, xo:xo + 128]
                pU = bk[0:64, uo:uo + 128]
                t0 = n * 64
                if seg == 0:
                    for h in range(2):
                        k.op(pe, lambda h=h: nc.tensor.matmul(pX, ARbd[:, n, h, 0:64], S16[:], start=(h == 0), stop=False),
                             r=[d_AR, d_S], w=[dbk])
                    for h in range(2):
                        k.op(pe, lambda h=h: nc.tensor.matmul(
                            bk[0:64, xo + h * 64:xo + h * 64 + 64], L1g[:, gi, 256 + h * 128:256 + h * 128 + 64],
                            tmg[:, gi, 256 + h * 64:256 + h * 64 + 64], start=False, stop=(h == 1)),
                            r=[d_L1, d_tm], w=[dbk])
                    k.op(act, lambda: nc.scalar.copy(X16[:], pX), w=[d_X, dbk])
                elif seg == 1:
                    for h in range(2):
                        k.op(pe, lambda h=h: nc.tensor.matmul(
                            bk[0:64, uo + h * 64:uo + h * 64 + 64], TTg[:, gi, h * 64:h * 64 + 64], X16[:, h * 64:h * 64 + 64],
                            start=True, stop=True), r=[d_TT, d_X], w=[dbk])
                    k.op(dve, lambda: nc.vector.tensor_copy(U16[:], pU), w=[d_U, dbk])
                else:
                    if n >= 4:
                        l0 = t0 - TC
                        k.op(pe, lambda: nc.tensor.matmul(
                            bk[:, yo:yo + 128], S16[:], ARbd[:, n, :, 64:128], start=True, stop=False),
                            r=[d_S, d_AR], w=[dbk])
                        for h in range(2):
                            hs_ = slice(64 * h, 64 * h + 64)
                            k.op(pe, lambda h=h, hs_=hs_: nc.tensor.matmul(
                                bk[hs_, yo + h * 64:yo + h * 64 + 64], U16[:, h * 64:h * 64 + 64],
                                L1g[:, gi, h * 128 + 64:h * 128 + 128], start=False, stop=False),
                                r=[d_U, d_L1], w=[dbk])
                            k.op(pe, lambda h=h, hs_=hs_: nc.tensor.matmul(
                                bk[hs_, yo + h * 64:yo + h * 64 + 64], tmg[:, gi, 256 + h * 64:256 + h * 64 + 64],
                                L1g[:, gi, 256 + h * 128 + 64:256 + h * 128 + 128], start=False, stop=(h == 1)),
                                r=[d_tm, d_L1], w=[dbk])
                    k.op(pe, lambda: nc.tensor.matmul(bk[:, so:so + 128], tmg[:, gi, 0:128], U16[:], start=True, stop=False),
                         r=[d_tm, d_U], w=[dbk])
                    k.op(pe, lambda: nc.tensor.matmul(bk[:, so:so + 128], tmg[:, gi, 128:256], tmg[:, gi, 256:384],
                                                      start=False, stop=True), r=[d_tm], w=[dbk])
                    prev = B_["prev"]
                    for h in range(2):
                        hs_ = slice(64 * h, 64 * h + 64)
                        pSd = bk[hs_, so + h * 64:so + h * 64 + 64]
                        if prev is None:
                            k.op(dve, lambda hs_=hs_, pSd=pSd: nc.vector.tensor_copy(M32[hs_, hs_], pSd),
                                 w=[d_M, dbk])
                        else:
                            k.op(dve, lambda hs_=hs_, pSd=pSd, prev=prev: nc.vector.scalar_tensor_tensor(
                                M32[hs_, hs_], M32[hs_, hs_], WC[hs_, prev:prev + 1], pSd, ALU.mult, ALU.add),
                                r=[d_WCp], w=[d_M, dbk])
                        k.op(act, lambda hs_=hs_: nc.scalar.activation(
                            S16[hs_, hs_], M32[hs_, hs_], AF.Identity, scale=WC[hs_, n:n + 1]), r=[d_M, d_WC], w=[d_S])
                    if n >= 4:
                        for h in range(2):
                            hs_ = slice(64 * h, 64 * h + 64)
                            k.op(dve, lambda h=h, hs_=hs_: nc.vector.tensor_copy(
                                yz[z][hs_, l0:l0 + 64], bk[hs_, yo + h * 64:yo + h * 64 + 64]), w=[d_yz[z], dbk])
                    B_["prev"] = n

            orders = [list(range(NCH)), [3, 2, 1, 0] + list(range(NCH - 1, 3, -1))]
            groups = [[o[i:i + GI] for i in range(0, NCH, GI)] for o in orders]
            for z in range(2):
                B_ = DB[z]
                B_["prev"] = None
                k.op(dve, lambda B_=B_: nc.vector.memset(B_["M32"][:], 0.0), w=[B_["d_M"]])
                k.op(dve, lambda B_=B_: nc.vector.memset(B_["S16"][:], 0.0), w=[B_["d_S"]])
            from collections import deque
            pending = deque()
            for zb in ((0, 0), (1, 0), (1, 3)):
                for o_ in preproc_ops(*zb):
                    o_()
            sched = {0: (0, 1), 1: (1, 2), 2: (0, 2), 3: (1, 1), 4: (0, 3)}
            pro = [pre_stages(z, 0, groups[z][0]) for z in range(2)]
            for si in range(len(pro[0])):
                for z in range(2):
                    pro[z][si]()
            for gidx in range(NG):
                if gidx in sched:
                    pending.extend(preproc_ops(*sched[gidx]))
                nxt = [pre_stages(z, (gidx + 1) % 2, groups[z][gidx + 1]) if gidx + 1 < NG else [] for z in range(2)]
                slot = 0
                for gi in range(GI):
                    for seg in range(3):
                        for z in range(2):
                            if slot < len(nxt[z]):
                                nxt[z][slot]()
                            chain_seg(z, gidx % 2, gi, groups[z][gidx][gi], seg)
                            for _ in range(2):
                                if pending:
                                    pending.popleft()()
                        slot += 1
                while pending:
                    pending.popleft()()

            if dbg == 2 and c == 0:
                dump(lambda: yz[0][:], T, d_yz[0], 0)
                dump(lambda: yz[1][:], T, d_yz[1], T)
                dump(lambda: kk32[:, TC:NT], T, d_kk, 2 * T)
                dump(lambda: ksum[:, TC:NT], T, d_ks, 3 * T)
                k.barrier()
                ph.close()
                lora_stack.close()
                k.finish()
                return k

            ob = ost[0]
            dob = d_ost[0]
            tA, tB, d_tA, d_tB = t2, t3, d_t2, d_t3
            for q in range(0, T, 512):
                qs = slice(q, q + 512)
                qn = slice(TC + q, TC + q + 512)
                W5 = slice(0, 512)
                k.op(pool, lambda qs=qs: nc.gpsimd.tensor_tensor(cs[:, W5], yz[0][:, qs], yz[1][:, qs], ALU.add),
                     r=[d_yz[0], d_yz[1]], w=[d_cs])
                k.op(pe, lambda: nc.tensor.matmul(PS[2][:, :], bdm_f[:], cs[:, W5], start=True, stop=True),
                     r=[d_const, d_cs], w=[dPS[2]])
                k.op(dve, lambda: nc.vector.tensor_tensor(tA[:, W5], cs[:, W5], PS[2][:, :], ALU.subtract),
                     r=[d_cs, dPS[2]], w=[d_tA])
                k.op(pool, lambda: nc.gpsimd.tensor_tensor(tB[:, W5], tA[:, W5], tA[:, W5], ALU.mult),
                     r=[d_tA], w=[d_tB])
                k.op(pe, lambda: nc.tensor.matmul(PS[3][:, :], bdm_f[:], tB[:, W5], start=True, stop=True),
                     r=[d_const, d_tB], w=[dPS[3]])
                k.op(act, lambda: nc.scalar.activation(tB[:, W5], PS[3][:, :], AF.Sqrt, bias=GN_EPS, scale=1.0),
                     r=[dPS[3]], w=[d_tB])
                k.op(dve, lambda: nc.vector.reciprocal(tB[:, W5], tB[:, W5]), r=[d_tB], w=[d_tB])
                k.op(dve, lambda: nc.vector.tensor_tensor(tA[:, W5], tA[:, W5], tB[:, W5], ALU.mult),
                     r=[d_tA, d_tB], w=[d_tA])
                k.op(act, lambda: nc.scalar.activation(tA[:, W5], tA[:, W5], AF.Identity,
                                                       bias=Vc("ln_b", 0, c), scale=Vc("ln_w", 0, c)),
                     r=[d_tA, d_const], w=[d_tA])
                k.op(dve, lambda qn=qn: nc.vector.scalar_tensor_tensor(
                    tB[:, W5], r32[:, qn], S("hrk", c), ksum[:, qn], ALU.mult, ALU.mult),
                    r=[d_r, d_ks, d_scal, d_tB], w=[d_tB])
                k.op(pe, lambda: nc.tensor.matmul(PS[4][:, :], bd1_f[:], tB[:, W5], start=True, stop=True),
                     r=[d_const, d_tB], w=[dPS[4]])
                k.op(dve, lambda qn=qn: nc.vector.tensor_tensor(tB[:, W5], PS[4][:, :], v16[:, qn], ALU.mult),
                     r=[dPS[4], d_v, d_tB], w=[d_tB])
                k.op(pool, lambda: nc.gpsimd.tensor_tensor(tA[:, W5], tA[:, W5], tB[:, W5], ALU.add),
                     r=[d_tA, d_tB], w=[d_tA])
                k.op(pe, lambda qs=qs: nc.tensor.matmul(PS[5][:, :], g2c[:, 0, :], SG[:, 0, qs], start=True, stop=False),
                     r=[d_g2, d_SG], w=[dPS[5]])
                k.op(pe, lambda qs=qs: nc.tensor.matmul(PS[5][:, :], g2c[0:32, 1, :], SG[0:32, 1, qs], start=False, stop=True),
                     r=[d_g2, d_SG], w=[dPS[5]])
                k.op(dve, lambda qs=qs: nc.vector.tensor_tensor(ob[:, qs], tA[:, W5], PS[5][:, :], ALU.mult),
                     r=[d_tA, dPS[5]], w=[dob])
            k.dma(sp, oT_d[:, c, :], ob[:], r=[dob])
        k.barrier()
    lora_stack.close()

    h2_stack = contextlib.ExitStack()
    h2 = k.sb("h2", [128, KC, T], BF16, h2_stack)
    d_h2 = [Dep() for _ in range(4)]
    d_xres = [Dep() for _ in range(4)]

    def out_proj_phase(w_dram, yin, d_yin, Gn, xprev_dram, An, Bn, router=None):
        with contextlib.ExitStack() as ph:
            wk = mk_wk(ph)
            wo = k.sb("wo", [128, KC, D], BF16, ph)
            d_wo = Dep()
            k.dma(pool, wo[:], kview(w_dram, 0, D), w=[d_wo])
            ym = k.sb("ym", [128, KC, 512], F32, ph)
            xp = k.sb("xp", [128, KC, 512], F32, ph)
            xn = k.sb("xn", [128, KC, 512], F32, ph)
            d_ym, d_xp, d_xn = Dep(), Dep(), Dep()
            if router is not None:
                h32 = k.sb("h32", [128, KC, 512], F32, ph)
                d_h32 = Dep()
            for tb in range(4):
                ts_ = slice(tb * 512, (tb + 1) * 512)
                k.dma(sp, xp[:], xprev_dram[:, :, ts_], r=[d_xres[tb]], w=[d_xp])
                for dc in range(KC):
                    p = dc % 4
                    for kk in range(KC):
                        k.op(pe, lambda dc=dc, kk=kk, p=p, ts_=ts_: nc.tensor.matmul(
                            PS[p][:, :], wo[:, kk, dc * 128:(dc + 1) * 128], yin[:, kk, ts_],
                            start=(kk == 0), stop=(kk == KC - 1)), r=[d_wo, d_yin], w=[dPS[p]])
                    k.op(act, lambda dc=dc, p=p: nc.scalar.copy(ym[:, dc, :], PS[p][:, :]), r=[dPS[p]], w=[d_ym])
                res_norm(ym, d_ym, 512, Gn, xp, d_xp, xn, d_xn, wk)
                k.dma(sp, xres_d[:, :, ts_], xn[:], r=[d_xn], w=[d_xres[tb]])
                norm_mod(xn, d_xn, 512, An, Bn, lambda c, ts_=ts_: h2[:, c, ts_], d_h2[tb], wk,
                         h32=(h32 if router is not None else None), d_h32=(d_h32 if router is not None else None))
                if router is not None:
                    router(tb, h32, d_h32)
            k.barrier()

    def ffn_pass(wgu_dram, wd_dram, acc, d_acc, first, wbufs, gate_bc=None, d_gate=None):
        for fg in range(FF // 512):
            b = wbufs["i"] % 2
            wbufs["i"] += 1
            Wg, Wu, Wd = wbufs["g"][b], wbufs["u"][b], wbufs["d"][b]
            dW = wbufs["dep"][b]
            k.dma(pool, Wg[:], kview(wgu_dram, fg * 512, (fg + 1) * 512), w=[dW])
            k.dma(pool, Wu[:], kview(wgu_dram, FF + fg * 512, FF + (fg + 1) * 512), w=[dW])
            k.dma(pool, Wd[:], wd_dram[fg * 512:(fg + 1) * 512, :].rearrange("(f p) d -> p f d", p=128), w=[dW])
            for tb in range(4):
                ts_ = slice(tb * 512, (tb + 1) * 512)
                ab = wbufs["ai"] % 2
                wbufs["ai"] += 1
                actb = wbufs["act"][ab]
                d_actb = wbufs["dact"][ab]
                for fc in range(4):
                    pg = (fc % 2) * 2
                    pu = pg + 1
                    for kk in range(KC):
                        k.op(pe, lambda kk=kk, fc=fc, pg=pg, ts_=ts_: nc.tensor.matmul(
                            PS[pg][:, :], Wg[:, kk, fc * 128:(fc + 1) * 128], h2[:, kk, ts_],
                            start=(kk == 0), stop=(kk == KC - 1)), r=[dW, d_h2[tb]], w=[dPS[pg]])
                    for kk in range(KC):
                        k.op(pe, lambda kk=kk, fc=fc, pu=pu, ts_=ts_: nc.tensor.matmul(
                            PS[pu][:, :], Wu[:, kk, fc * 128:(fc + 1) * 128], h2[:, kk, ts_],
                            start=(kk == 0), stop=(kk == KC - 1)), r=[dW, d_h2[tb]], w=[dPS[pu]])
                    sgb = wbufs["sg"][fc % 2]
                    d_sgb = wbufs["dsg"][fc % 2]
                    k.op(act, lambda pg=pg, sgb=sgb: nc.scalar.activation(sgb[:], PS[pg][:, :], AF.Silu),
                         r=[dPS[pg]], w=[d_sgb])
                    if gate_bc is None:
                        k.op(dve, lambda fc=fc, pu=pu, sgb=sgb, actb=actb: nc.vector.tensor_tensor(
                            actb[:, fc, :], sgb[:], PS[pu][:, :], ALU.mult), r=[d_sgb, dPS[pu]], w=[d_actb])
                    else:
                        k.op(dve, lambda fc=fc, pu=pu, sgb=sgb: nc.vector.tensor_tensor(
                            sgb[:], sgb[:], PS[pu][:, :], ALU.mult), r=[d_sgb, dPS[pu]], w=[d_sgb])
                        k.op(dve, lambda fc=fc, sgb=sgb, actb=actb, ts_=ts_: nc.vector.tensor_tensor(
                            actb[:, fc, :], sgb[:], gate_bc[:, ts_], ALU.mult), r=[d_sgb, d_gate], w=[d_actb])
                for dc in range(KC):
                    p = 4 + dc % 2
                    for fc in range(4):
                        k.op(pe, lambda dc=dc, fc=fc, p=p, actb=actb: nc.tensor.matmul(
                            PS[p][:, :], Wd[:, fc, dc * 128:(dc + 1) * 128], actb[:, fc, :],
                            start=(fc == 0), stop=(fc == 3)), r=[dW, d_actb], w=[dPS[p]])
                    if first and fg == 0:
                        k.op(act, lambda dc=dc, p=p, ts_=ts_: nc.scalar.copy(acc[:, dc, ts_], PS[p][:, :]),
                             r=[dPS[p]], w=[d_acc[tb]])
                    else:
                        k.op(dve, lambda dc=dc, p=p, ts_=ts_: nc.vector.tensor_tensor(
                            acc[:, dc, ts_], acc[:, dc, ts_], PS[p][:, :], ALU.add), r=[dPS[p], d_acc[tb]], w=[d_acc[tb]])

    def mk_ffn_bufs(ph):
        return dict(i=0, ai=0,
                    g=[k.sb("Wg%d" % i, [128, KC, 512], BF16, ph) for i in range(2)],
                    u=[k.sb("Wu%d" % i, [128, KC, 512], BF16, ph) for i in range(2)],
                    d=[k.sb("Wd%d" % i, [128, 4, D], BF16, ph) for i in range(2)],
                    dep=[Dep(), Dep()],
                    act=[k.sb("actb%d" % i, [128, 4, 512], BF16, ph) for i in range(2)],
                    dact=[Dep(), Dep()],
                    sg=[k.sb("sgb%d" % i, [128, 512], F32, ph) for i in range(2)],
                    dsg=[Dep(), Dep()])

    def post_ffn_phase(acc, d_acc, Gn, An, Bn, final):
        with contextlib.ExitStack() as ph:
            wk = mk_wk(ph)
            xp = k.sb("xp", [128, KC, 512], F32, ph)
            xn = k.sb("xn", [128, KC, 512], F32, ph)
            d_xp, d_xn = Dep(), Dep()
            for tb in range(4):
                ts_ = slice(tb * 512, (tb + 1) * 512)
                k.dma(sp, xp[:], xres_d[:, :, ts_], r=[d_xres[tb]], w=[d_xp])

                sumsq_rstd(lambda c: acc[:, c, ts_], 512, wk["sq"], wk["rs"], d_acc[tb], wk["dsq"], wk["drs"], 6)
                for c in range(KC):
                    k.op(pool, lambda c=c, ts_=ts_: nc.gpsimd.tensor_tensor(wk["tmp"][:, c, :], acc[:, c, ts_], wk["rs"][:, :], ALU.mult),
                         r=[d_acc[tb], wk["drs"]], w=[wk["dtmp"]])
                    k.op(dve, lambda c=c: nc.vector.scalar_tensor_tensor(xn[:, c, :], wk["tmp"][:, c, :], S(Gn, c),
                                                                         xp[:, c, :], ALU.mult, ALU.add),
                         r=[wk["dtmp"], d_scal, d_xp], w=[d_xn])
                if final:
                    k.dma(sp, out_d[:, :, ts_], xn[:], r=[d_xn])
                else:
                    k.dma(sp, xres_d[:, :, ts_], xn[:], r=[d_xn], w=[d_xres[tb]])
                    norm_mod(xn, d_xn, 512, An, Bn, lambda c, ts_=ts_: h2[:, c, ts_], d_h2[tb], wk)
            k.barrier()

    with contextlib.ExitStack() as yst:
        yin0 = k.sb("yin0", [128, KC, T], BF16, yst)
        d_yin0 = Dep()
        k.dma(sp, yin0[:], oT_d[:, :, :], w=[d_yin0])
        out_proj_phase(wout_d, yin0, d_yin0, "G1_0", xT_d, "A2_0", "B2_0")

    if dbg == 3:
        with contextlib.ExitStack() as ph:
            t32 = k.sb("t32", [128, T], F32, ph)
            dd = Dep()
            k.op(dve, lambda: nc.vector.tensor_copy(t32[:], h2[:, 0, :]), r=d_h2, w=[dd])
            dump(lambda: t32[:], T, dd)
            k.barrier()
        h2_stack.close()
        k.finish()
        return k

    acc_stack = contextlib.ExitStack()
    acc = k.sb("acc", [128, KC, T], F32, acc_stack)
    d_acc = [Dep() for _ in range(4)]
    with contextlib.ExitStack() as ph:
        wbufs = mk_ffn_bufs(ph)
        ffn_pass(fgu_d, fd_d, acc, d_acc, True, wbufs)
        k.barrier()
    post_ffn_phase(acc, d_acc, "G2_0", "A1_1", "B1_1", final=False)
    acc_stack.close()

    gates_stack = contextlib.ExitStack()
    logit = k.sb("logit", [128, 16, NE], F32, gates_stack)
    gates = k.sb("gates", [128, 16, NE], F32, gates_stack)
    wr32 = k.sb("wr32", [128, KC, NE], F32, gates_stack)
    d_logit, d_gates, d_wr = Dep(), Dep(), Dep()
    k.dma(sp, wr32[:], rt_d[:, :, :], w=[d_wr])

    ycv_stack = contextlib.ExitStack()
    ycv = k.sb("ycv", [128, KC, T], BF16, ycv_stack)
    d_ycv = Dep()
    with contextlib.ExitStack() as ph:
        Wc3 = [k.sb("Wc3_%d" % i, [128, KC, 3, 128], BF16, ph) for i in range(2)]
        dWc3 = [Dep(), Dep()]
        Bsb = k.sb("Bsb", [128, T], F32, ph)
        Csb = k.sb("Csb", [128, 512], F32, ph)
        zp = k.sb("zp", [128, T + 2], F32, ph)
        t1 = k.sb("t1", [128, T], F32, ph)
        d_B, d_C, d_z, d_t1 = Dep(), Dep(), Dep(), Dep()
        k.op(dve, lambda: nc.vector.memset(zp[:], 0.0), w=[d_z])
        for c in range(KC):
            W3 = Wc3[c % 2]
            dW3 = dWc3[c % 2]
            for j in range(3):
                k.dma(pool, W3[:, :, j, :], kview(cwin_d, j * D + c * 128, j * D + (c + 1) * 128), w=[dW3])
            for tb in range(4):
                ts_ = slice(tb * 512, (tb + 1) * 512)
                for j in range(3):
                    p = j
                    for kk in range(KC):
                        k.op(pe, lambda kk=kk, j=j, p=p, ts_=ts_, W3=W3: nc.tensor.matmul(
                            PS[p][:, :], W3[:, kk, j, :], h2[:, kk, ts_], start=(kk == 0), stop=(kk == KC - 1)),
                            r=[dW3, d_h2[tb]], w=[dPS[p]])
                k.op(act, lambda ts_=ts_: nc.scalar.copy(Bsb[:, ts_], PS[0][:, :]), r=[dPS[0]], w=[d_B])
                k.op(act, lambda: nc.scalar.copy(Csb[:], PS[1][:, :]), r=[dPS[1]], w=[d_C])
                k.op(dve, lambda tb=tb: nc.vector.tensor_tensor(zp[:, 1 + tb * 512:1 + (tb + 1) * 512], Csb[:], PS[2][:, :], ALU.mult),
                     r=[d_C, dPS[2]], w=[d_z])
            k.op(act, lambda c=c: nc.scalar.activation(t1[:], zp[:, 0:T], AF.Identity, scale=Vc("conv_w", 0, c)),
                 r=[d_z, d_const], w=[d_t1])
            k.op(dve, lambda c=c: nc.vector.scalar_tensor_tensor(t1[:], zp[:, 1:T + 1], Vc("conv_w", 1, c), t1[:], ALU.mult, ALU.add),
                 r=[d_z, d_const, d_t1], w=[d_t1])
            k.op(dve, lambda c=c: nc.vector.scalar_tensor_tensor(t1[:], zp[:, 2:T + 2], Vc("conv_w", 2, c), t1[:], ALU.mult, ALU.add),
                 r=[d_z, d_const, d_t1], w=[d_t1])
            k.op(pool, lambda c=c: nc.gpsimd.tensor_tensor(ycv[:, c, :], t1[:], Bsb[:], ALU.mult),
                 r=[d_t1, d_B], w=[d_ycv])
        k.barrier()

    def router(tb, h32, d_h32):
        for sub in range(4):
            tt = tb * 4 + sub
            for c in range(KC):
                k.op(pe, lambda c=c, sub=sub: nc.tensor.matmul(
                    PS[5][:, sub * 8:sub * 8 + 8], h32[:, c, sub * 128:(sub + 1) * 128], wr32[:, c, :],
                    start=(c == 0), stop=(c == KC - 1)), r=[d_h32, d_wr], w=[dPS[5]])
        k.op(dve, lambda tb=tb: nc.vector.tensor_copy(
            logit[:, tb * 4:(tb + 1) * 4, :], PS[5][:, 0:32].rearrange("p (s e) -> p s e", e=8)),
            r=[dPS[5]], w=[d_logit])

    out_proj_phase(cwout_d, ycv, d_ycv, "G1_1", xres_d, "A2_1", "B2_1", router=router)
    ycv_stack.close()

    with contextlib.ExitStack() as ph:
        mx = k.sb("mx", [128, 16, 8], F32, ph)
        e1 = k.sb("e1", [128, 16], F32, ph)
        g1_ = k.sb("g1_", [128, 16], F32, ph)
        g2_ = k.sb("g2_", [128, 16], F32, ph)
        q1 = k.sb("q1", [128, 16, 8], F32, ph)
        q2 = k.sb("q2", [128, 16, 8], F32, ph)
        d_mx, d_e = Dep(), Dep()
        for tt in range(16):
            k.op(dve, lambda tt=tt: nc.vector.max(mx[:, tt, :], logit[:, tt, :]), r=[d_logit], w=[d_mx])
        k.op(dve, lambda: nc.vector.tensor_tensor(e1[:], mx[:, :, 1], mx[:, :, 0], ALU.subtract), r=[d_mx], w=[d_e])
        k.op(act, lambda: nc.scalar.activation(e1[:], e1[:], AF.Exp), r=[d_e], w=[d_e])
        k.op(dve, lambda: nc.vector.tensor_scalar(g1_[:], e1[:], 1.0, None, ALU.add), r=[d_e], w=[d_e])
        k.op(dve, lambda: nc.vector.reciprocal(g1_[:], g1_[:]), r=[d_e], w=[d_e])
        k.op(dve, lambda: nc.vector.tensor_tensor(g2_[:], e1[:], g1_[:], ALU.mult), r=[d_e], w=[d_e])
        k.op(dve, lambda: nc.vector.tensor_tensor(q1[:], logit[:], mx[:, :, 0:1].to_broadcast([128, 16, 8]), ALU.is_equal),
             r=[d_logit, d_mx], w=[d_e])
        k.op(dve, lambda: nc.vector.tensor_tensor(q2[:], logit[:], mx[:, :, 1:2].to_broadcast([128, 16, 8]), ALU.is_equal),
             r=[d_logit, d_mx], w=[d_e])
        k.op(dve, lambda: nc.vector.tensor_tensor(q1[:], q1[:], g1_[:, :].unsqueeze(2).to_broadcast([128, 16, 8]), ALU.mult),
             r=[d_e], w=[d_e])
        k.op(dve, lambda: nc.vector.tensor_tensor(q2[:], q2[:], g2_[:, :].unsqueeze(2).to_broadcast([128, 16, 8]), ALU.mult),
             r=[d_e], w=[d_e])
        k.op(dve, lambda: nc.vector.tensor_tensor(gates[:], q1[:], q2[:], ALU.add), r=[d_e], w=[d_gates])
        k.barrier()

    if dbg == 4:
        dump(lambda: gates[:].rearrange("p t e -> p (t e)"), 128, d_gates, 0)
        dump(lambda: logit[:].rearrange("p t e -> p (t e)"), 128, d_logit, 128)
        k.barrier()
        gates_stack.close()
        h2_stack.close()
        k.finish()
        return k

    acc_stack = contextlib.ExitStack()
    acc = k.sb("acc2", [128, KC, T], F32, acc_stack)
    d_acc = [Dep() for _ in range(4)]
    with contextlib.ExitStack() as ph:
        wbufs = mk_ffn_bufs(ph)
        gbc = [k.sb("gbc%d" % i, [128, T], F32, ph) for i in range(2)]
        d_gbc = [Dep(), Dep()]
        Gm = [k.sb("Gm%d" % i, [128, 128], F32, ph) for i in range(2)]
        d_Gm = [Dep(), Dep()]
        ident_f = cst[:, C_ID:C_ID + 128]
        for e in range(NE):
            gb = gbc[e % 2]
            dgb = d_gbc[e % 2]
            for tt in range(16):
                gm = Gm[tt % 2]
                dgm = d_Gm[tt % 2]
                k.op(dve, lambda tt=tt, e=e, gm=gm: nc.vector.tensor_copy(gm[:], gates[:, tt, e:e + 1].to_broadcast([128, 128])),
                     r=[d_gates], w=[dgm])
                k.op(pe, lambda tt=tt, gm=gm: nc.tensor.matmul(PS[6][:, (tt % 4) * 128:(tt % 4 + 1) * 128], gm[:], ident_f,
                                                              start=True, stop=True), r=[dgm, d_const], w=[dPS[6]])
                if tt % 4 == 3:
                    q = (tt // 4) * 512
                    k.op(act, lambda q=q, gb=gb: nc.scalar.copy(gb[:, q:q + 512], PS[6][:, :]), r=[dPS[6]], w=[dgb])
            ffn_pass(mgu_d[e], md_d[e], acc, d_acc, e == 0, wbufs, gate_bc=gb, d_gate=dgb)
        k.barrier()
    post_ffn_phase(acc, d_acc, "G2_1", None, None, final=True)
    acc_stack.close()
    gates_stack.close()
    h2_stack.close()
    k.finish()
    return k


def prep_inputs(inp):
    f = lambda a: np.ascontiguousarray(np.asarray(a, np.float32))
    vec = np.zeros((128, NV), np.float32)

    def put(name, arr):
        a = col(arr)
        vec[:, VOFF[name]:VOFF[name] + a.shape[1]] = a

    put("g", f(inp["norm_g"]).reshape(-1))
    put("mu", f(inp["rwkv_mu"]).reshape(-1))
    put("w0", f(inp["rwkv_w0"]).reshape(-1))
    put("a0", f(inp["rwkv_a0"]).reshape(-1))
    put("k_k", f(inp["rwkv_k_k"]).reshape(-1))
    put("k_a", f(inp["rwkv_k_a"]).reshape(-1))
    put("r_k", f(inp["rwkv_r_k"]).reshape(-1))
    put("ln_w", f(inp["rwkv_ln_w"]).reshape(-1))
    put("ln_b", f(inp["rwkv_ln_b"]).reshape(-1))
    put("conv_w", f(inp["conv_w"]).reshape(-1))
    mb = col(f(inp["mod_b"]).reshape(-1))
    vec[:, VOFF["modb"]:VOFF["modb"] + 192] = np.repeat(mb, 2, axis=1)
    shared = {
        "vecs": vec,
        "cst": make_consts(),
        "mod_w": f(inp["mod_w"]),
        "w_rkv": f(inp["rwkv_w_rkv"])[0],
        "w1cat": np.ascontiguousarray(np.concatenate([f(inp["rwkv_w1"])[0, 0], f(inp["rwkv_w1"])[0, 1]], axis=1)),
        "w2cat": np.ascontiguousarray(f(inp["rwkv_w2"])[0].reshape(128, D)),
        "a1cat": np.ascontiguousarray(np.concatenate([f(inp["rwkv_a1"])[0, 0], f(inp["rwkv_a1"])[0, 1]], axis=1)),
        "a2cat": np.ascontiguousarray(f(inp["rwkv_a2"])[0].reshape(128, D)),
        "g1": f(inp["rwkv_g1"])[0],
        "g2": f(inp["rwkv_g2"])[0],
        "w_out": f(inp["rwkv_w_out"])[0],
        "conv_w_in": f(inp["conv_w_in"])[0],
        "conv_w_out": f(inp["conv_w_out"])[0],
        "ffn_w_gu": f(inp["ffn_w_gu"])[0],
        "ffn_w_down": f(inp["ffn_w_down"])[0],
        "router": np.ascontiguousarray(f(inp["moe_router"])[0].reshape(KC, 128, NE).transpose(1, 0, 2)),
        "moe_w_gu": f(inp["moe_w_gu"])[0],
        "moe_w_down": f(inp["moe_w_down"])[0],
    }
    x = f(inp["x"])
    ctx = f(inp["ctx"])
    c = f(inp["c"])
    cc = f(inp["c_ctx"])
    maps = []
    for b in range(8):
        m = dict(shared)
        m["xT"] = np.ascontiguousarray(x[b].T.reshape(KC, 128, T).transpose(1, 0, 2))
        m["ctxT"] = np.ascontiguousarray(ctx[b].T.reshape(KC, 128, TC).transpose(1, 0, 2))
        m["cvec"] = np.ascontiguousarray(np.stack([col(c[b]), col(cc)], axis=2))
        maps.append(m)
    return maps


def kernel(**inputs):
    maps = prep_inputs(inputs)
    kb = build()
    res = run_bass_kernel_spmd(kb.nc, maps, core_ids=list(range(8)))
    outs = []
    for b in range(8):
        yT = np.asarray(res.results[b]["yT"], np.float32)
        outs.append(yT.transpose(1, 0, 2).reshape(D, T).T)
    return np.ascontiguousarray(np.stack(outs, 0).astype(np.float32))
```
